# Optimizing a Trainium2 kernel written in Bass

```python
import jax, jax.numpy as jnp
from jax import lax
import numpy as np

D_MODEL = 2048
BATCH = 4
SEQ = 2048
DEPTH = 2

N_A = max(1, DEPTH // 2)
N_B = DEPTH - N_A

SSM_EXPAND = 2
D_INNER = SSM_EXPAND * D_MODEL
SSM_HEAD_DIM = 64
SSM_HEADS = D_INNER // SSM_HEAD_DIM
SSM_GROUPS = 8
SSM_HEADS_PER_GROUP = SSM_HEADS // SSM_GROUPS
SSM_STATE = 128
CONV_WIDTH = 4
CHUNK = 128
CONV_DIM = D_INNER + 2 * SSM_GROUPS * SSM_STATE
IN_PROJ_DIM = D_INNER + CONV_DIM + SSM_HEADS

SB_HEADS = 16
SB_HEAD_DIM = D_MODEL // SB_HEADS
Q_BLOCK = 128

N_GROUPS = 4
EXPERTS_PER_GROUP = 4
N_EXPERTS = N_GROUPS * EXPERTS_PER_GROUP
TOP_K = 2
D_EXPERT = D_MODEL // 4

EPS = 1e-5
F32 = jnp.float32

kernel_name = "yoco_mamba2_stickbreaking_hiermoe"


def rms_norm(x, g):
    xf = x.astype(F32)
    y = xf * lax.rsqrt(jnp.mean(xf * xf, axis=-1, keepdims=True) + EPS)
    return (y * g.astype(F32)).astype(x.dtype)


def causal_dwconv(x, w, b):
    out = lax.conv_general_dilated(
        x, w[:, None, :].astype(x.dtype), window_strides=(1,),
        padding=[(CONV_WIDTH - 1, 0)], dimension_numbers=("NWC", "WIO", "NWC"),
        feature_group_count=x.shape[-1])
    return out + b.astype(x.dtype)


def ssd_chunked(x, a, b, c):
    bsz, L = x.shape[0], x.shape[1]
    nc = L // CHUNK
    x = x.reshape(bsz, nc, CHUNK, SSM_GROUPS, SSM_HEADS_PER_GROUP, SSM_HEAD_DIM)
    b = b.reshape(bsz, nc, CHUNK, SSM_GROUPS, SSM_STATE)
    c = c.reshape(bsz, nc, CHUNK, SSM_GROUPS, SSM_STATE)
    a = a.reshape(bsz, nc, CHUNK, SSM_GROUPS, SSM_HEADS_PER_GROUP).transpose(0, 3, 4, 1, 2)
    a_cs = jnp.cumsum(a, axis=-1)
    idx = jnp.arange(CHUNK)
    causal = idx[:, None] >= idx[None, :]
    seg = a_cs[..., :, None] - a_cs[..., None, :]
    decay = jnp.exp(jnp.where(causal, seg, -jnp.inf))
    cb = jnp.einsum("bclgn,bcsgn->bgcls", c, b)
    m = cb[:, :, None] * decay
    y_diag = jnp.einsum("bgrcls,bcsgrp->bclgrp", m, x)
    decay_states = jnp.exp(a_cs[..., -1:] - a_cs).transpose(0, 3, 4, 1, 2)
    xd = x * decay_states[..., None]
    states = jnp.einsum("bclgn,bclgrp->bcgrpn", b, xd)
    chunk_decay = jnp.exp(a_cs[..., -1]).transpose(3, 0, 1, 2)

    def step(h, inp):
        dec, st = inp
        return h * dec[..., None, None] + st, h

    h0 = jnp.zeros(states.shape[:1] + states.shape[2:], states.dtype)
    _, prev = lax.scan(step, h0, (chunk_decay.astype(states.dtype), states.transpose(1, 0, 2, 3, 4, 5)))
    out_decay = jnp.exp(a_cs).transpose(0, 3, 4, 1, 2)[..., None]
    y_off = jnp.einsum("bclgn,cbgrpn->bclgrp", c, prev) * out_decay
    y = y_diag + y_off
    return y.reshape(bsz, L, SSM_GROUPS, SSM_HEADS_PER_GROUP, SSM_HEAD_DIM)


def gated_group_rmsnorm(y, z, w):
    bsz, L, _ = y.shape
    yf = (y.astype(F32) * jax.nn.silu(z.astype(F32))).reshape(bsz, L, SSM_GROUPS, D_INNER // SSM_GROUPS)
    yf = yf * lax.rsqrt(jnp.mean(yf * yf, axis=-1, keepdims=True) + EPS)
    return (yf.reshape(bsz, L, D_INNER) * w.astype(F32)).astype(y.dtype)


def mamba2_mixer(h, w_in, conv_w, conv_b, dt_bias, a_log, d_skip, norm_w, w_out):
    bsz, L, _ = h.shape
    zxbcdt = h @ w_in
    z = zxbcdt[..., :D_INNER]
    xbc = zxbcdt[..., D_INNER:D_INNER + CONV_DIM]
    dt = zxbcdt[..., D_INNER + CONV_DIM:]
    xbc = jax.nn.silu(causal_dwconv(xbc, conv_w, conv_b))
    gn = SSM_GROUPS * SSM_STATE
    xs = xbc[..., :D_INNER].reshape(bsz, L, SSM_GROUPS, SSM_HEADS_PER_GROUP, SSM_HEAD_DIM)
    bm = xbc[..., D_INNER:D_INNER + gn].reshape(bsz, L, SSM_GROUPS, SSM_STATE)
    cm = xbc[..., D_INNER + gn:].reshape(bsz, L, SSM_GROUPS, SSM_STATE)
    dt = jax.nn.softplus((dt + dt_bias).astype(F32)).reshape(bsz, L, SSM_GROUPS, SSM_HEADS_PER_GROUP)
    a = -jnp.exp(a_log.astype(F32)).reshape(SSM_GROUPS, SSM_HEADS_PER_GROUP)
    y = ssd_chunked(xs * dt[..., None], dt * a, bm, cm)
    y = y + xs * d_skip.reshape(SSM_GROUPS, SSM_HEADS_PER_GROUP, 1)
    y = gated_group_rmsnorm(y.reshape(bsz, L, D_INNER), z, norm_w)
    return y @ w_out


def stick_breaking_attention(q, k, v):
    S = q.shape[2]
    scale = SB_HEAD_DIM ** -0.5
    outs = []
    for i in range(S // Q_BLOCK):
        start, end = i * Q_BLOCK, (i + 1) * Q_BLOCK
        qb, kb, vb = q[:, :, start:end], k[:, :, :end], v[:, :, :end]
        z = jnp.einsum("bhqd,bhkd->bhqk", qb, kb).astype(F32) * scale
        q_pos = start + jnp.arange(Q_BLOCK)
        k_pos = jnp.arange(end)
        mask = k_pos[None, :] < q_pos[:, None]
        log_keep = jnp.where(mask, -jax.nn.softplus(z), 0.0)
        suffix = lax.cumsum(log_keep, axis=3, reverse=True) - log_keep
        attn = jnp.where(mask, jnp.exp(jax.nn.log_sigmoid(z) + suffix), 0.0)
        outs.append(jnp.einsum("bhqk,bhkd->bhqd", attn.astype(v.dtype), vb))
    return jnp.concatenate(outs, axis=2)


def hier_moe(h, w_coarse, b_coarse, w_fine, b_fine, w_gate, w_up, w_down):
    bsz, L, _ = h.shape
    t = h.reshape(bsz * L, D_MODEL)
    p_group = jax.nn.softmax((t @ w_coarse + b_coarse).astype(F32), axis=-1)
    p_g, g_idx = lax.top_k(p_group, 1)
    fine_all = jnp.einsum("td,dge->tge", t, w_fine) + b_fine
    fine = jnp.take_along_axis(fine_all, g_idx[:, :, None], axis=1)[:, 0]
    p_fine = jax.nn.softmax(fine.astype(F32), axis=-1)
    p_e, e_idx = lax.top_k(p_fine, TOP_K)
    p_e = p_e / jnp.sum(p_e, axis=-1, keepdims=True)
    gate_w = p_g * p_e
    expert_id = g_idx * EXPERTS_PER_GROUP + e_idx
    gates = jnp.sum(jax.nn.one_hot(expert_id, N_EXPERTS, dtype=F32) * gate_w[..., None], axis=1)
    hid = jax.nn.silu(jnp.einsum("td,edf->tef", t, w_gate)) * jnp.einsum("td,edf->tef", t, w_up)
    hid = hid * gates[:, :, None].astype(hid.dtype)
    y = jnp.einsum("tef,efd->td", hid, w_down)
    return y.reshape(bsz, L, D_MODEL)


def setup_inputs(seed: int = 0) -> dict:
    key = jax.random.key(seed)
    ks = jax.random.split(key, 24)
    nrm = lambda k, shape, s: jax.random.normal(k, shape, F32) * s
    dt = jnp.exp(jax.random.uniform(ks[6], (N_A, SSM_HEADS), F32, np.log(1e-3), np.log(1e-1)))
    return {
        "x": jax.random.normal(ks[0], (BATCH, SEQ, D_MODEL), F32),
        "mix_norm": 1.0 + nrm(ks[1], (DEPTH, D_MODEL), 0.02),
        "ffn_norm": 1.0 + nrm(ks[2], (DEPTH, D_MODEL), 0.02),
        "ssm_w_in": nrm(ks[3], (N_A, D_MODEL, IN_PROJ_DIM), D_MODEL ** -0.5),
        "ssm_conv_w": nrm(ks[4], (N_A, CONV_WIDTH, CONV_DIM), CONV_WIDTH ** -0.5),
        "ssm_conv_b": nrm(ks[5], (N_A, CONV_DIM), 0.01),
        "ssm_dt_bias": dt + jnp.log(-jnp.expm1(-dt)),
        "ssm_a_log": jnp.log(jax.random.uniform(ks[7], (N_A, SSM_HEADS), F32, 1.0, 16.0)),
        "ssm_d": 1.0 + nrm(ks[8], (N_A, SSM_HEADS), 0.1),
        "ssm_norm_w": 1.0 + nrm(ks[9], (N_A, D_INNER), 0.02),
        "ssm_w_out": nrm(ks[10], (N_A, D_INNER, D_MODEL), D_INNER ** -0.5),
        "kv_norm": 1.0 + nrm(ks[11], (D_MODEL,), 0.02),
        "w_k": nrm(ks[12], (D_MODEL, D_MODEL), D_MODEL ** -0.5),
        "w_v": nrm(ks[13], (D_MODEL, D_MODEL), D_MODEL ** -0.5),
        "sb_w_q": nrm(ks[14], (N_B, D_MODEL, D_MODEL), D_MODEL ** -0.5),
        "sb_w_out": nrm(ks[15], (N_B, D_MODEL, D_MODEL), D_MODEL ** -0.5),
        "moe_w_coarse": nrm(ks[16], (DEPTH, D_MODEL, N_GROUPS), D_MODEL ** -0.5),
        "moe_b_coarse": nrm(ks[17], (DEPTH, N_GROUPS), 0.01),
        "moe_w_fine": nrm(ks[18], (DEPTH, D_MODEL, N_GROUPS, EXPERTS_PER_GROUP), D_MODEL ** -0.5),
        "moe_b_fine": nrm(ks[19], (DEPTH, N_GROUPS, EXPERTS_PER_GROUP), 0.01),
        "moe_w_gate": nrm(ks[20], (DEPTH, N_EXPERTS, D_MODEL, D_EXPERT), D_MODEL ** -0.5),
        "moe_w_up": nrm(ks[21], (DEPTH, N_EXPERTS, D_MODEL, D_EXPERT), D_MODEL ** -0.5),
        "moe_w_down": nrm(ks[22], (DEPTH, N_EXPERTS, D_EXPERT, D_MODEL), D_EXPERT ** -0.5),
        "final_norm": 1.0 + nrm(ks[23], (D_MODEL,), 0.02),
    }


def reference(x, mix_norm, ffn_norm, ssm_w_in, ssm_conv_w, ssm_conv_b, ssm_dt_bias, ssm_a_log,
              ssm_d, ssm_norm_w, ssm_w_out, kv_norm, w_k, w_v, sb_w_q, sb_w_out,
              moe_w_coarse, moe_b_coarse, moe_w_fine, moe_b_fine, moe_w_gate, moe_w_up,
              moe_w_down, final_norm):
    bsz, S, _ = x.shape
    h = x
    k_sh = None
    v_sh = None
    for layer in range(DEPTH):
        hn = rms_norm(h, mix_norm[layer])
        if layer < N_A:
            mix = mamba2_mixer(hn, ssm_w_in[layer], ssm_conv_w[layer], ssm_conv_b[layer],
                               ssm_dt_bias[layer], ssm_a_log[layer], ssm_d[layer],
                               ssm_norm_w[layer], ssm_w_out[layer])
        else:
            j = layer - N_A
            q = (hn @ sb_w_q[j]).reshape(bsz, S, SB_HEADS, SB_HEAD_DIM).transpose(0, 2, 1, 3)
            o = stick_breaking_attention(q, k_sh, v_sh)
            mix = o.transpose(0, 2, 1, 3).reshape(bsz, S, D_MODEL) @ sb_w_out[j]
        h = h + mix.astype(h.dtype)
        ff = hier_moe(rms_norm(h, ffn_norm[layer]), moe_w_coarse[layer], moe_b_coarse[layer],
                      moe_w_fine[layer], moe_b_fine[layer], moe_w_gate[layer],
                      moe_w_up[layer], moe_w_down[layer])
        h = h + ff.astype(h.dtype)
        if layer == N_A - 1:
            hkv = rms_norm(h, kv_norm)
            k_sh = (hkv @ w_k).reshape(bsz, S, SB_HEADS, SB_HEAD_DIM).transpose(0, 2, 1, 3)
            v_sh = (hkv @ w_v).reshape(bsz, S, SB_HEADS, SB_HEAD_DIM).transpose(0, 2, 1, 3)
    return rms_norm(h, final_norm)
```

```python
from concourse.bass_utils import run_bass_kernel_spmd
import numpy as np
from contextlib import ExitStack
import concourse.bass as bass
import concourse.mybir as mybir

F32 = mybir.dt.float32
BF16 = mybir.dt.bfloat16
AF = mybir.ActivationFunctionType
ALU = mybir.AluOpType
AX = mybir.AxisListType


class Reg:
    __slots__ = ("name", "w", "r")

    def __init__(self, name):
        self.name = name
        self.w = None
        self.r = []


class Prog:
    ENGS = ("pe", "act", "dve", "pool", "sp")

    def __init__(self, nc, n_dma_sems=20):
        self.nc = nc
        self.es = ExitStack()
        self.ops = []
        self.eng_obj = {"pe": nc.tensor, "act": nc.scalar, "dve": nc.vector,
                        "pool": nc.gpsimd, "sp": nc.sync}
        self.n_dma_sems = n_dma_sems
        self._uid = 0
        self.allregs = []
        self.bar_from = 0
        self.tag = None
        self.scopes = False

    def sb(self, shape, dt, name=None):
        self._uid += 1
        return self.es.enter_context(self.nc.sbuf_tensor(name or f"sb{self._uid}", list(shape), dt))

    def ps(self, shape, dt, name=None):
        self._uid += 1
        return self.es.enter_context(self.nc.psum_tensor(name or f"ps{self._uid}", list(shape), dt))

    def reg(self, name=None):
        self._uid += 1
        r = Reg(name or f"r{self._uid}")
        self.allregs.append(r)
        return r

    def barrier(self):
        last = {}
        dmas = []
        for oid in range(self.bar_from, len(self.ops)):
            o = self.ops[oid]
            if o["dma"]:
                dmas.append(oid)
            else:
                last[o["eng"]] = oid
        deps = set(last.values()) | set(dmas)
        for e in self.ENGS:
            self.ops.append(dict(eng=e, fn=(lambda: None), deps=set(deps), dma=False, tag=self.tag))
        self.bar_from = len(self.ops)
        for r in self.allregs:
            r.w = None
            r.r = []

    def op(self, eng, fn, reads=(), writes=(), dma=False, nosync_same_pe=True):
        oid = len(self.ops)
        deps = set()
        for r in reads:
            if r.w is not None:
                deps.add(r.w)
        for w in writes:
            if w.w is not None:
                deps.add(w.w)
            deps.update(w.r)
        for r in reads:
            r.r.append(oid)
        for w in writes:
            w.w = oid
            w.r = []
        deps.discard(oid)
        self.ops.append(dict(eng=eng, fn=fn, deps=deps, dma=dma, tag=self.tag))
        return oid

    def emit(self):
        nc = self.nc
        ops = self.ops
        need = [False] * len(ops)
        for o in ops:
            for d in list(o["deps"]):
                od = ops[d]
                if od["eng"] == "pe" and o["eng"] == "pe" and not od["dma"] and not o["dma"]:
                    o["deps"].discard(d)
                    continue
                need[d] = True
        esem = {e: self.es.enter_context(nc.semaphore(f"s_{e}")) for e in self.ENGS}
        dsem = {}
        for q in ("sp", "pool"):
            dsem[q] = [self.es.enter_context(nc.semaphore(f"d_{q}{i}")) for i in range(self.n_dma_sems)]
        ecount = {e: 0 for e in self.ENGS}
        dcount = {q: [0] * self.n_dma_sems for q in dsem}
        drr = {q: 0 for q in dsem}
        sig = [None] * len(ops)
        waited = {}
        cur_tag = None
        for oid, o in enumerate(ops):
            eng = o["eng"]
            E = self.eng_obj[eng]
            if self.scopes and o.get("tag") != cur_tag:
                if cur_tag is not None:
                    nc.leave_named_scope(cur_tag, cur_sid, False)
                cur_tag = o.get("tag")
                if cur_tag is not None:
                    cur_sid, _ = nc.enter_named_scope(cur_tag, False)
            wl = {}
            for d in o["deps"]:
                s, v = sig[d]
                k = id(s)
                if k not in wl or wl[k][1] < v:
                    wl[k] = (s, v)
            if o["dma"]:
                slot = drr[eng]
                drr[eng] = (slot + 1) % self.n_dma_sems
                ds = dsem[eng][slot]
                prev = dcount[eng][slot]
                if prev > 0:
                    k = id(ds)
                    if k not in wl or wl[k][1] < prev:
                        wl[k] = (ds, prev)
            for k, (ws, wv) in wl.items():
                if waited.get((eng, k), 0) >= wv:
                    continue
                waited[(eng, k)] = wv
                E.wait_ge(ws, wv)
            inst = o["fn"]()
            if inst is None:
                continue
            if o["dma"]:
                dcount[eng][slot] = prev + 16
                inst.then_inc(ds, 16)
                sig[oid] = (ds, prev + 16)
            elif need[oid]:
                ecount[eng] += 1
                inst.then_inc(esem[eng], 1)
                sig[oid] = (esem[eng], ecount[eng])
        if self.scopes and cur_tag is not None:
            nc.leave_named_scope(cur_tag, cur_sid, False)
        self.sig = sig
        self.esem = esem
        return ecount

    def final_wait(self, eng, oids):
        E = self.eng_obj[eng]
        for oid in oids:
            s, v = self.sig[oid]
            E.wait_ge(s, v)


import numpy as np

D = 2048
NT = 1024
NKC = 16
EPS = 1e-5
NE = 16
DE = 512


SB_BASE = 16512
SB_TOP = 229344


class K:
    def __init__(self, nc, nw=4, look=2):
        self.nc = nc
        self.p = Prog(nc)
        self.cache = {}
        self.NW = nw
        self.LOOK = look
        self.wlist = []
        self.widx = 0
        self.wloaded = 0
        self.bank_live = [False] * 8
        self.bank_rr = 0
        self.bump = SB_BASE
        self.limit = SB_TOP
        self.phase = "perm"
        self.uid = 0

    def sb(self, name, shape, dt):
        key = (self.phase, name)
        if key not in self.cache:
            n = 1
            for d in shape[1:]:
                n *= d
            nbytes = n * (4 if dt == F32 else 2)
            nbytes = (nbytes + 31) // 32 * 32
            off = self.bump
            assert off + nbytes <= self.limit, f"SBUF overflow allocating {name} in phase {self.phase}: {off}+{nbytes} > {self.limit}"
            self.bump = off + nbytes
            self.uid += 1
            t = self.nc.alloc_sbuf_tensor_at(f"{self.phase}_{name}_{self.uid}", list(shape), dt, offset=off)
            self.names = getattr(self, "names", {})
            self.names[key] = t.name
            self.cache[key] = (t, self.p.reg(name))
        return self.cache[key]

    def begin_phase(self, name, start, limit=SB_TOP):
        self.p.barrier()
        assert not any(self.bank_live)
        self.p.tag = name
        self.phase = name
        self.bump = start
        self.limit = limit

    def setup(self):
        p = self.p
        self.banks = [(p.ps([128, 512], F32, name=f"bank{i}"), p.reg(f"bank{i}")) for i in range(8)]
        self.bankregs = {id(r) for _, r in self.banks}
        self.wring = []
        for i in range(self.NW):
            t, _ = self.sb(f"wr{i}", [128, 4096], BF16)
            r1 = p.reg(f"wr{i}")
            self.wring.append((t, [r1, r1]))

    def op(self, eng, fn, reads=(), writes=(), dma=False):
        if eng != "pe":
            bs = self.bankregs
            extra = [r for r in reads if id(r) in bs]
            if extra:
                writes = list(writes) + [r for r in extra if r not in writes]
        return self.p.op(eng, fn, reads=reads, writes=writes, dma=dma)

    def bank(self):
        for i in range(8):
            j = (self.bank_rr + i) % 8
            if not self.bank_live[j]:
                self.bank_live[j] = True
                self.bank_rr = (j + 1) % 8
                return j
        raise RuntimeError("out of PSUM banks")

    def free(self, j):
        assert self.bank_live[j]
        self.bank_live[j] = False

    def wdeclare(self, seq):
        self.wlist.extend(seq)

    def wget(self, pieces):
        key = [(str(pc[0]), pc[1], pc[2]) for pc in pieces]
        i = self.widx
        self.widx += 1
        assert [(str(pc[0]), pc[1], pc[2]) for pc in self.wlist[i]] == key, f"weight order mismatch at {i}"
        hi = min(len(self.wlist), i + 1 + self.LOOK)
        while self.wloaded < hi:
            self._wload(self.wloaded)
            self.wloaded += 1
        t, regs = self.wring[i % self.NW]
        return self._wviews(t, regs, pieces)

    def _wviews(self, t, regs, pieces):
        out = []
        off = 0
        for pi, (src, kc, nco) in enumerate(pieces):
            v = t[:, off:off + kc * nco].rearrange("p (kc f) -> p kc f", kc=kc)
            out.append((v, regs[pi]))
            off += kc * nco
        assert off <= 4096
        return out

    def _wload(self, j):
        nc = self.nc
        t, regs = self.wring[j % self.NW]
        views = self._wviews(t, regs, self.wlist[j])
        for (v, r), (src, kc, nco) in zip(views, self.wlist[j]):
            self.p.op("pool", (lambda v=v, src=src: nc.gpsimd.dma_start(
                out=v, in_=src.rearrange("(kc p) f -> p kc f", p=128))), writes=[r], dma=True)


def load_consts(k, dr, names):
    nc = k.nc
    c = getattr(k, "c", {})
    for name in names:
        shape = CONST_SHAPES[name]
        dt = BF16 if name == "ident_bf" else F32
        t, r = k.sb("c_" + name, shape, dt)
        if dt == BF16:
            k.op("pool", (lambda t=t, name=name: nc.gpsimd.dma_start(out=t[:], in_=dr[name])), writes=[r], dma=True)
        else:
            k.op("sp", (lambda t=t, name=name: nc.sync.dma_start(out=t[:], in_=dr[name])), writes=[r], dma=True)
        c[name] = (t, r)
    for nm, val in (("eps_col", EPS), ("one_col", 1.0), ("eps4_col", 4 * EPS)):
        if nm not in c:
            t, r = k.sb("c_" + nm, [128, 1], F32)
            k.op("pool", (lambda t=t, val=val: nc.gpsimd.memset(t[:], val)), writes=[r])
            c[nm] = (t, r)
    k.c = c


def host_consts():
    i = np.arange(128)
    h = {}
    h["ident_bf"] = np.eye(128, dtype=np.float32)
    h["ident_f"] = np.eye(128, dtype=np.float32)
    h["ones_f"] = np.ones((128, 128), np.float32)
    h["tri_incl"] = (i[:, None] <= i[None, :]).astype(np.float32)
    h["ustrict"] = (i[:, None] > i[None, :]).astype(np.float32)
    h["mask_le"] = (i[:, None] <= i[None, :]).astype(np.float32)
    h["mask_lt"] = (i[None, :] < i[:, None]).astype(np.float32)
    sel = np.zeros((16, 16, 128), np.float32)
    for e in range(16):
        sel[e, e, :] = 1.0
    h["sel16"] = sel.reshape(16, 16 * 128)
    h["inv128"] = np.full((128, 1), 1.0 / 128, np.float32)
    h["ones_row"] = np.ones((128, 2048), np.float32)
    return h


CONST_SHAPES = {"ident_bf": [128, 128], "ident_f": [128, 128], "ones_f": [128, 128], "tri_incl": [128, 128],
                "ustrict": [128, 128], "mask_le": [128, 128], "mask_lt": [128, 128], "sel16": [16, 2048],
                "inv128": [128, 1], "ones_row": [128, 2048]}


def rmsnorm(k, src, g, r_g, outT, r_out, ntok=NT, want_rstd=None):
    nc = k.nc
    ones, r_ones = k.c["ones_f"]
    epsc, r_eps = k.c["eps_col"]
    for th in range(ntok // 512):
        ts = slice(th * 512, (th + 1) * 512)
        view, r_h = src(th)
        b = k.bank()
        bt, rb = k.banks[b]
        for kc in range(NKC):
            sq, rsq = k.sb(f"rn_sq{kc % 2}", [128, 512], F32)
            k.op("act", (lambda sq=sq, kc=kc, view=view: nc.scalar.activation(out=sq[:], in_=view(kc), func=AF.Square)),
                 reads=[r_h], writes=[rsq])
            k.op("pe", (lambda sq=sq, kc=kc, bt=bt: nc.tensor.matmul(bt[:], lhsT=ones[:], rhs=sq[:],
                                                                     start=(kc == 0), stop=(kc == NKC - 1))),
                 reads=[rsq, r_ones], writes=[rb])
        if want_rstd is not None:
            rt, rr = want_rstd
            rview = rt[:, ts]
        else:
            rt, rr = k.sb("rn_rstd", [128, 512], F32)
            rview = rt[:]
        k.op("act", (lambda bt=bt, rview=rview: nc.scalar.activation(out=rview, in_=bt[:], func=AF.Ln, bias=epsc[:], scale=1.0 / D)),
             reads=[rb, r_eps], writes=[rr])
        k.free(b)
        k.op("act", (lambda rview=rview: nc.scalar.activation(out=rview, in_=rview, func=AF.Exp, scale=-0.5)),
             reads=[rr], writes=[rr])
        for kc in range(NKC):
            k.op("dve", (lambda kc=kc, ts=ts, rview=rview, view=view: nc.vector.scalar_tensor_tensor(
                out=outT[:, kc, ts], in0=view(kc), scalar=g[:, kc:kc + 1], in1=rview,
                op0=ALU.mult, op1=ALU.mult)), reads=[r_h, r_g, rr], writes=[r_out])


def src_resident(hT, r_h):
    return lambda th: ((lambda kc, th=th: hT[:, kc, th * 512:(th + 1) * 512]), r_h)


def src_staged(k, dram_xT):
    nc = k.nc

    def f(th):
        st, r_st = k.sb("rn_stage", [128, NKC, 512], F32)
        k.op("sp", (lambda st=st, th=th: nc.sync.dma_start(
            out=st[:], in_=dram_xT[:, th * 512:(th + 1) * 512].rearrange("(kc p) t -> p kc t", p=128))), writes=[r_st], dma=True)
        return (lambda kc, st=st: st[:, kc, :]), r_st
    return f


def wseq_moe(prm, l):
    wg, wu, wd = prm["w_gate"][l], prm["w_up"][l], prm["w_down"][l]
    seq = []
    for e in range(NE):
        for fh in range(2):
            seq.append([(wg[e][:, fh * 256:(fh + 1) * 256], NKC, 256)])
            seq.append([(wu[e][:, fh * 256:(fh + 1) * 256], NKC, 256)])
        for dq in range(2):
            seq.append([(wd[e][:, dq * 1024:(dq + 1) * 1024], 4, 1024)])
    return seq


def moe(k, hT, r_h, hnT, r_hn, prm, l):
    nc = k.nc
    c = k.c
    g, r_g = prm["ffn_g"][l]
    rstd, r_rstd = k.sb("moe_rstd", [128, NT], F32)
    rmsnorm(k, src_resident(hT, r_h), g, r_g, hnT, r_hn, want_rstd=(rstd, r_rstd))

    wr, r_wr = k.sb("moe_wr", [128, NKC, 20], F32)
    k.op("sp", lambda: nc.sync.dma_start(out=wr[:], in_=prm["w_router"][l].rearrange("(kc p) e -> p kc e", p=128)),
         writes=[r_wr], dma=True)
    k.op("dve", lambda: nc.vector.tensor_tensor(out=wr[:], in0=wr[:], in1=g[:].unsqueeze(2).to_broadcast([128, NKC, 20]),
                                               op=ALU.mult), reads=[r_wr, r_g], writes=[r_wr])
    rb_bias, r_bias = prm["b_router"][l]
    gT, r_gT = k.sb("moe_gT", [16, NT], F32)
    inv128, r_inv = c["inv128"]
    identf, r_identf = c["ident_f"]
    for tb in range(NT // 128):
        tsl = slice(tb * 128, (tb + 1) * 128)
        b = k.bank()
        bt, rb = k.banks[b]
        for kc in range(NKC):
            k.op("pe", (lambda kc=kc, tsl=tsl, bt=bt: nc.tensor.matmul(bt[:, 0:20], lhsT=hT[:, kc, tsl], rhs=wr[:, kc, :],
                                                                       start=(kc == 0), stop=(kc == NKC - 1))),
                 reads=[r_h, r_wr], writes=[rb])
        k.op("pe", (lambda tsl=tsl, bt=bt: nc.tensor.matmul(bt[:, 32:33], lhsT=rstd[:, tsl], rhs=inv128[:],
                                                            start=True, stop=True)),
             reads=[r_rstd, r_inv], writes=[rb])
        sm, r_sm = k.sb(f"moe_sm{tb % 2}", [128, 96], F32)
        rs = sm[:, 0:1]
        k.op("act", (lambda bt=bt, rs=rs: nc.scalar.copy(out=rs, in_=bt[:, 32:33])), reads=[rb], writes=[r_sm])
        lg = sm[:, 4:24]
        k.op("dve", (lambda bt=bt, lg=lg, rs=rs: nc.vector.scalar_tensor_tensor(
            out=lg, in0=bt[:, 0:20], scalar=rs, in1=rb_bias[:], op0=ALU.mult, op1=ALU.add)),
            reads=[rb, r_sm, r_bias], writes=[r_sm])
        k.free(b)
        lc = sm[:, 4:8]
        mx = sm[:, 1:2]
        k.op("dve", (lambda lc=lc, mx=mx: nc.vector.reduce_max(out=mx, in_=lc, axis=AX.X)), reads=[r_sm], writes=[r_sm])
        nmx = sm[:, 2:3]
        k.op("dve", (lambda mx=mx, nmx=nmx: nc.vector.tensor_scalar(out=nmx, in0=mx, scalar1=-1.0, scalar2=None, op0=ALU.mult)),
             reads=[r_sm], writes=[r_sm])
        ec = sm[:, 24:28]
        se = sm[:, 3:4]
        k.op("act", (lambda lc=lc, ec=ec, nmx=nmx, se=se: nc.scalar.activation(out=ec, in_=lc, func=AF.Exp, bias=nmx, scale=1.0,
                                                                               accum_out=se)),
             reads=[r_sm], writes=[r_sm])
        oh = sm[:, 28:32]
        k.op("dve", (lambda lc=lc, mx=mx, oh=oh: nc.vector.tensor_scalar(out=oh, in0=lc, scalar1=mx, scalar2=None, op0=ALU.is_ge)),
             reads=[r_sm], writes=[r_sm])
        fa = sm[:, 8:24].rearrange("p (g e) -> p g e", g=4)
        tmp = sm[:, 32:48]
        k.op("dve", (lambda fa=fa, oh=oh, tmp=tmp: nc.vector.tensor_tensor(
            out=tmp.rearrange("p (g e) -> p g e", g=4), in0=fa, in1=oh.unsqueeze(2).to_broadcast([128, 4, 4]), op=ALU.mult)),
            reads=[r_sm], writes=[r_sm])
        fn = sm[:, 48:52]
        k.op("dve", (lambda tmp=tmp, fn=fn: nc.vector.tensor_reduce(out=fn, in_=tmp.rearrange("p (g e) -> p e g", g=4),
                                                                    axis=AX.X, op=ALU.add)),
             reads=[r_sm], writes=[r_sm])
        mf = sm[:, 52:53]
        k.op("dve", (lambda fn=fn, mf=mf: nc.vector.reduce_max(out=mf, in_=fn, axis=AX.X)), reads=[r_sm], writes=[r_sm])
        nmf = sm[:, 53:54]
        k.op("dve", (lambda mf=mf, nmf=nmf: nc.vector.tensor_scalar(out=nmf, in0=mf, scalar1=-1.0, scalar2=None, op0=ALU.mult)),
             reads=[r_sm], writes=[r_sm])
        ef = sm[:, 56:60]
        k.op("act", (lambda fn=fn, ef=ef, nmf=nmf: nc.scalar.activation(out=ef, in_=fn, func=AF.Exp, bias=nmf, scale=1.0)),
             reads=[r_sm], writes=[r_sm])
        m1 = sm[:, 60:64]
        k.op("dve", (lambda fn=fn, mf=mf, m1=m1: nc.vector.tensor_scalar(out=m1, in0=fn, scalar1=mf, scalar2=None, op0=ALU.is_ge)),
             reads=[r_sm], writes=[r_sm])
        ef2 = sm[:, 64:68]
        k.op("dve", (lambda ef=ef, m1=m1, ef2=ef2: nc.vector.tensor_tensor(out=ef2, in0=ef, in1=m1, op=ALU.mult)),
             reads=[r_sm], writes=[r_sm])
        k.op("dve", (lambda ef=ef, ef2=ef2: nc.vector.tensor_tensor(out=ef2, in0=ef, in1=ef2, op=ALU.subtract)),
             reads=[r_sm], writes=[r_sm])
        e2 = sm[:, 54:55]
        k.op("dve", (lambda ef2=ef2, e2=e2: nc.vector.reduce_max(out=e2, in_=ef2, axis=AX.X)), reads=[r_sm], writes=[r_sm])
        m2 = sm[:, 68:72]
        k.op("dve", (lambda ef2=ef2, e2=e2, m2=m2: nc.vector.tensor_scalar(out=m2, in0=ef2, scalar1=e2, scalar2=None, op0=ALU.is_ge)),
             reads=[r_sm], writes=[r_sm])
        tv = sm[:, 72:76]
        k.op("dve", (lambda ef2=ef2, m2=m2, tv=tv: nc.vector.tensor_tensor(out=tv, in0=ef2, in1=m2, op=ALU.mult)),
             reads=[r_sm], writes=[r_sm])
        t1 = sm[:, 76:80]
        k.op("dve", (lambda ef=ef, m1=m1, t1=t1: nc.vector.tensor_tensor(out=t1, in0=ef, in1=m1, op=ALU.mult)),
             reads=[r_sm], writes=[r_sm])
        k.op("dve", (lambda tv=tv, t1=t1: nc.vector.tensor_tensor(out=tv, in0=tv, in1=t1, op=ALU.add)),
             reads=[r_sm], writes=[r_sm])
        den = sm[:, 55:56]
        k.op("dve", (lambda tv=tv, den=den: nc.vector.reduce_sum(out=den, in_=tv, axis=AX.X)), reads=[r_sm], writes=[r_sm])
        k.op("dve", (lambda den=den, se=se: nc.vector.tensor_tensor(out=den, in0=den, in1=se, op=ALU.mult)),
             reads=[r_sm], writes=[r_sm])
        k.op("dve", (lambda den=den: nc.vector.reciprocal(out=den, in_=den)), reads=[r_sm], writes=[r_sm])
        k.op("dve", (lambda tv=tv, den=den: nc.vector.tensor_scalar(out=tv, in0=tv, scalar1=den, scalar2=None, op0=ALU.mult)),
             reads=[r_sm], writes=[r_sm])
        gt = sm[:, 80:96]
        k.op("dve", (lambda gt=gt, oh=oh, tv=tv: nc.vector.tensor_tensor(
            out=gt.rearrange("p (g e) -> p g e", g=4), in0=oh.unsqueeze(2).to_broadcast([128, 4, 4]),
            in1=tv.unsqueeze(1).to_broadcast([128, 4, 4]), op=ALU.mult)), reads=[r_sm], writes=[r_sm])
        b2 = k.bank()
        bt2, rb2 = k.banks[b2]
        k.op("pe", (lambda gt=gt, bt2=bt2: nc.tensor.matmul(bt2[0:16, 0:128], lhsT=gt, rhs=identf[:], start=True, stop=True)),
             reads=[r_sm, r_identf], writes=[rb2])
        k.op("act", (lambda bt2=bt2, tsl=tsl: nc.scalar.copy(out=gT[:, tsl], in_=bt2[0:16, 0:128])), reads=[rb2], writes=[r_gT])
        k.free(b2)

    sel, r_sel = c["sel16"]
    wg, wu, wd = prm["w_gate"][l], prm["w_up"][l], prm["w_down"][l]
    for e in range(NE):
        gb, r_gb = k.sb(f"moe_gb{e % 2}", [128, NT], F32)
        for th in range(2):
            ts = slice(th * 512, (th + 1) * 512)
            b = k.bank()
            bt, rb = k.banks[b]
            k.op("pe", (lambda e=e, ts=ts, bt=bt: nc.tensor.matmul(bt[:], lhsT=sel[:, e * 128:(e + 1) * 128], rhs=gT[:, ts],
                                                                   start=True, stop=True)),
                 reads=[r_sel, r_gT], writes=[rb])
            k.op("act", (lambda gb=gb, ts=ts, bt=bt: nc.scalar.copy(out=gb[:, ts], in_=bt[:])), reads=[rb], writes=[r_gb])
            k.free(b)
        hid, r_hid = k.sb(f"moe_hid{e % 2}", [128, 4, NT], BF16)
        for fh in range(2):
            (wgt, r_wgt), = k.wget([(wg[e][:, fh * 256:(fh + 1) * 256], NKC, 256)])
            (wut, r_wut), = k.wget([(wu[e][:, fh * 256:(fh + 1) * 256], NKC, 256)])
            for f2 in range(2):
                fc = fh * 2 + f2
                for th in range(2):
                    ts = slice(th * 512, (th + 1) * 512)
                    bg = k.bank(); bu = k.bank()
                    btg, rbg = k.banks[bg]
                    btu, rbu = k.banks[bu]
                    for kc in range(NKC):
                        k.op("pe", (lambda kc=kc, ts=ts, btg=btg, wgt=wgt, f2=f2: nc.tensor.matmul(
                            btg[:], lhsT=wgt[:, kc, f2 * 128:(f2 + 1) * 128], rhs=hnT[:, kc, ts],
                            start=(kc == 0), stop=(kc == NKC - 1))), reads=[r_wgt, r_hn], writes=[rbg])
                    for kc in range(NKC):
                        k.op("pe", (lambda kc=kc, ts=ts, btu=btu, wut=wut, f2=f2: nc.tensor.matmul(
                            btu[:], lhsT=wut[:, kc, f2 * 128:(f2 + 1) * 128], rhs=hnT[:, kc, ts],
                            start=(kc == 0), stop=(kc == NKC - 1))), reads=[r_wut, r_hn], writes=[rbu])
                    sg, r_sg = k.sb(f"moe_sg{(fc * 2 + th) % 2}", [128, 512], F32)
                    k.op("act", (lambda sg=sg, btg=btg: nc.scalar.activation(out=sg[:], in_=btg[:], func=AF.Silu)),
                         reads=[rbg], writes=[r_sg])
                    k.free(bg)
                    k.op("dve", (lambda sg=sg, btu=btu: nc.vector.tensor_tensor(out=sg[:], in0=sg[:], in1=btu[:], op=ALU.mult)),
                         reads=[r_sg, rbu], writes=[r_sg])
                    k.free(bu)
                    k.op("dve", (lambda sg=sg, gb=gb, ts=ts, hid=hid, fc=fc: nc.vector.tensor_tensor(
                        out=hid[:, fc, ts], in0=sg[:], in1=gb[:, ts], op=ALU.mult)), reads=[r_sg, r_gb], writes=[r_hid])
        for dq in range(2):
            (wdt, r_wdt), = k.wget([(wd[e][:, dq * 1024:(dq + 1) * 1024], 4, 1024)])
            for d2 in range(8):
                dc = dq * 8 + d2
                for th in range(2):
                    ts = slice(th * 512, (th + 1) * 512)
                    b = k.bank()
                    bt, rb = k.banks[b]
                    for fc in range(4):
                        k.op("pe", (lambda fc=fc, ts=ts, bt=bt, wdt=wdt, d2=d2, hid=hid: nc.tensor.matmul(
                            bt[:], lhsT=wdt[:, fc, d2 * 128:(d2 + 1) * 128], rhs=hid[:, fc, ts],
                            start=(fc == 0), stop=(fc == 3))), reads=[r_wdt, r_hid], writes=[rb])
                    k.op("dve", (lambda dc=dc, ts=ts, bt=bt: nc.vector.tensor_tensor(out=hT[:, dc, ts], in0=hT[:, dc, ts], in1=bt[:],
                                                                                     op=ALU.add)), reads=[rb, r_h], writes=[r_h])
                    k.free(b)


def ssd_prep(k, hnT, r_hn, prm):
    nc = k.nc
    c = k.c
    wdt, r_wdt = prm["wdt"]
    dtb, r_dtb = prm["dtb"]
    A_bc, r_A = prm["A_bc"]
    onec, r_onec = c["one_col"]
    tri, r_tri = c["tri_incl"]
    ones, r_ones = c["ones_f"]
    P = {}
    for nm in ("dt", "a", "eacs", "dstate", "cdec"):
        P[nm] = k.sb("sp_" + nm, [128, 8, 64], F32)
    dt, r_dt = P["dt"]; a, r_a = P["a"]; eacs, r_eacs = P["eacs"]
    dstate, r_dst = P["dstate"]; cdec, r_cdec = P["cdec"]
    b = k.bank(); bt, rb = k.banks[b]
    for cch in range(8):
        for kc in range(NKC):
            k.op("pe", (lambda cch=cch, kc=kc, bt=bt: nc.tensor.matmul(
                bt[:, cch * 64:(cch + 1) * 64], lhsT=hnT[:, kc, cch * 128:(cch + 1) * 128], rhs=wdt[:, kc, :],
                start=(kc == 0), stop=(kc == NKC - 1))), reads=[r_hn, r_wdt], writes=[rb])
    k.op("dve", (lambda bt=bt: nc.vector.tensor_tensor(out=dt[:], in0=bt[:].rearrange("p (c h) -> p c h", c=8),
                                                      in1=dtb[:].unsqueeze(1).to_broadcast([128, 8, 64]), op=ALU.add)),
         reads=[rb, r_dtb], writes=[r_dt])
    k.free(b)
    k.op("act", lambda: nc.scalar.activation(out=dt[:], in_=dt[:], func=AF.Exp), reads=[r_dt], writes=[r_dt])
    k.op("act", lambda: nc.scalar.activation(out=dt[:], in_=dt[:], func=AF.Ln, bias=onec[:], scale=1.0),
         reads=[r_dt, r_onec], writes=[r_dt])
    k.op("dve", lambda: nc.vector.tensor_tensor(out=a[:], in0=dt[:], in1=A_bc[:].unsqueeze(1).to_broadcast([128, 8, 64]),
                                               op=ALU.mult), reads=[r_dt, r_A], writes=[r_a])
    b2 = k.bank(); bt2, rb2 = k.banks[b2]
    b3 = k.bank(); bt3, rb3 = k.banks[b3]
    for cch in range(8):
        k.op("pe", (lambda cch=cch, bt2=bt2: nc.tensor.matmul(bt2[:, cch * 64:(cch + 1) * 64], lhsT=tri[:], rhs=a[:, cch, :],
                                                              start=True, stop=True)), reads=[r_a, r_tri], writes=[rb2])
        k.op("pe", (lambda cch=cch, bt3=bt3: nc.tensor.matmul(bt3[:, cch * 64:(cch + 1) * 64], lhsT=ones[:], rhs=a[:, cch, :],
                                                              start=True, stop=True)), reads=[r_a, r_ones], writes=[rb3])
    f3 = lambda t: t[:].rearrange("p c h -> p (c h)")
    k.op("act", (lambda bt2=bt2: nc.scalar.activation(out=f3(eacs), in_=bt2[:], func=AF.Exp)), reads=[rb2], writes=[r_eacs])
    k.op("act", (lambda bt2=bt2: nc.scalar.copy(out=f3(dstate), in_=bt2[:])), reads=[rb2], writes=[r_dst])
    k.free(b2)
    k.op("act", (lambda bt3=bt3: nc.scalar.activation(out=f3(cdec), in_=bt3[:], func=AF.Exp)), reads=[rb3], writes=[r_cdec])
    k.op("dve", (lambda bt3=bt3: nc.vector.tensor_tensor(out=f3(dstate), in0=f3(dstate), in1=bt3[:], op=ALU.subtract)),
         reads=[rb3, r_dst], writes=[r_dst])
    k.free(b3)
    k.op("act", lambda: nc.scalar.activation(out=f3(dstate), in_=f3(dstate), func=AF.Exp, scale=-1.0), reads=[r_dst], writes=[r_dst])
    return P


def wseq_ssd_group(prm, g, full):
    w_in = prm["w_in"]
    seq = [[(w_in[:, 4096 + 512 * g: 4096 + 512 * g + 256], NKC, 256)],
           [(w_in[:, 4096 + 512 * g + 256: 4096 + 512 * g + 512], NKC, 256)],
           [(w_in[:, 8192 + 128 * g: 8192 + 128 * g + 128], NKC, 128), (w_in[:, 9216 + 128 * g: 9216 + 128 * g + 128], NKC, 128)]]
    if full:
        seq += [[(w_in[:, 512 * g: 512 * g + 256], NKC, 256)], [(w_in[:, 512 * g + 256: 512 * g + 512], NKC, 256)]]
    return seq


def ssd_group(k, g, mode, hnT, r_hn, P, prm, ynT_all=None, r_yn=None):
    nc = k.nc
    c = k.c
    cwh, r_cwh = prm["cwh"]
    cbh, r_cbh = prm["cbh"]
    D_bc, r_D = prm["D_bc"]
    nw, r_nw = prm["nw"]
    hal, r_hal = prm["hal"]
    S_in, r_Sin = prm["S_in"]
    flag, r_flag = prm["flag"]
    identb, r_identb = c["ident_bf"]
    identf, r_identf = c["ident_f"]
    ones, r_ones = c["ones_f"]
    tri, r_tri = c["tri_incl"]
    ustr, r_ustr = c["ustrict"]
    mle, r_mle = c["mask_le"]
    dt, r_dt = P["dt"]; a, r_a = P["a"]; eacs, r_eacs = P["eacs"]
    dstate, r_dst = P["dstate"]; cdec, r_cdec = P["cdec"]
    full = (mode == "B")
    hs = slice(8 * g, 8 * g + 8)
    seq = wseq_ssd_group(prm, g, full)

    xcT, r_xc = k.sb("sg_xcT", [128, 4, NT], BF16)
    BcT, r_Bc = k.sb("sg_BcT", [128, NT], BF16)
    CcT, r_Cc = k.sb("sg_CcT", [128, NT], BF16)
    Sg, r_S = k.sb("sg_S", [128, 512], F32)
    Sbf, r_Sbf = k.sb("sg_Sbf", [128, 512], BF16)
    h8 = lambda ap: ap.rearrange("p (h q) -> p h q", h=8)

    items = [(0, 0, 4 * g + 0, xcT[:, 0, :], r_xc), (0, 128, 4 * g + 1, xcT[:, 1, :], r_xc),
             (1, 0, 4 * g + 2, xcT[:, 2, :], r_xc), (1, 128, 4 * g + 3, xcT[:, 3, :], r_xc),
             (2, 0, 32 + g, BcT[:], r_Bc), (3, 0, 40 + g, CcT[:], r_Cc)]
    wcur = {}
    for ii, (wi, co, ci, dst, r_dstt) in enumerate(items):
        if wi == 0 and 0 not in wcur:
            wcur[0], = k.wget(seq[0])
        elif wi == 1 and 1 not in wcur:
            wcur[1], = k.wget(seq[1])
        elif wi == 2 and 2 not in wcur:
            wcur[2], wcur[3] = k.wget(seq[2])
        wt, r_wt = wcur[wi]
        pre, r_pre = k.sb("sg_pre", [128, NT + 8], F32)
        acc, r_acc = k.sb("sg_acc", [128, NT], F32)
        if full:
            k.op("pool", (lambda pre=pre, ci=ci: nc.gpsimd.tensor_copy(out=pre[:, 0:3], in_=hal[:, ci, :])),
                 reads=[r_hal], writes=[r_pre])
        else:
            k.op("pool", (lambda pre=pre: nc.gpsimd.memset(pre[:, 0:3], 0.0)), writes=[r_pre])
        for th in range(2):
            ts = slice(th * 512, (th + 1) * 512)
            b = k.bank(); bt, rb = k.banks[b]
            for kc in range(NKC):
                k.op("pe", (lambda kc=kc, ts=ts, bt=bt, wt=wt, co=co: nc.tensor.matmul(
                    bt[:], lhsT=wt[:, kc, co:co + 128], rhs=hnT[:, kc, ts], start=(kc == 0), stop=(kc == NKC - 1))),
                    reads=[r_wt, r_hn], writes=[rb])
            k.op("act", (lambda bt=bt, pre=pre, th=th: nc.scalar.copy(out=pre[:, 3 + th * 512: 3 + (th + 1) * 512], in_=bt[:])),
                 reads=[rb], writes=[r_pre])
            k.op("act", (lambda bt=bt, acc=acc, ts=ts, ci=ci: nc.scalar.activation(
                out=acc[:, ts], in_=bt[:], func=AF.Identity, bias=cbh[:, ci:ci + 1], scale=cwh[:, ci, 3:4])),
                reads=[rb, r_cwh, r_cbh], writes=[r_acc])
            k.free(b)
        for tap in range(3):
            k.op("dve", (lambda pre=pre, acc=acc, ci=ci, tap=tap: nc.vector.scalar_tensor_tensor(
                out=acc[:], in0=pre[:, tap:tap + NT], scalar=cwh[:, ci, tap:tap + 1], in1=acc[:], op0=ALU.mult, op1=ALU.add)),
                reads=[r_pre, r_acc, r_cwh], writes=[r_acc])
        if not full:
            k.op("pool", (lambda pre=pre, ci=ci: nc.gpsimd.tensor_copy(out=hal[:, ci, :], in_=pre[:, NT:NT + 3])),
                 reads=[r_pre], writes=[r_hal])
        k.op("act", (lambda acc=acc, pre=pre: nc.scalar.activation(out=pre[:, 0:NT], in_=acc[:], func=AF.Tanh)),
             reads=[r_acc], writes=[r_pre])
        k.op("dve", (lambda acc=acc, pre=pre, dst=dst: nc.vector.scalar_tensor_tensor(
            out=dst, in0=pre[:, 0:NT], scalar=1.0, in1=acc[:], op0=ALU.add, op1=ALU.mult)),
            reads=[r_pre, r_acc], writes=[r_dstt])

    if full:
        (wz0, r_wz0), = k.wget(seq[3])
        (wz1, r_wz1), = k.wget(seq[4])
        ss, r_ss = k.sb("sg_ss", [128, 8], F32)
        k.op("dve", lambda: nc.vector.memset(ss[:], 0.0), writes=[r_ss])
        k.op("dve", lambda: nc.vector.tensor_scalar(out=Sg[:], in0=S_in[:, g, :], scalar1=flag[:], scalar2=None, op0=ALU.mult),
             reads=[r_Sin, r_flag], writes=[r_S])
        k.op("act", lambda: nc.scalar.copy(out=Sbf[:], in_=Sg[:]), reads=[r_S], writes=[r_Sbf])
    else:
        k.op("pool", lambda: nc.gpsimd.memset(Sg[:], 0.0), writes=[r_S])

    for cch in range(8):
        cs = slice(cch * 128, (cch + 1) * 128)
        bx = k.bank(); btx, rbx = k.banks[bx]
        for fc in range(4):
            k.op("pe", (lambda fc=fc, cs=cs, btx=btx: nc.tensor.matmul(btx[:, fc * 128:(fc + 1) * 128], lhsT=xcT[:, fc, cs],
                                                                       rhs=identb[:], start=True, stop=True)),
                 reads=[r_xc, r_identb], writes=[rbx])
        xdt, r_xdt = k.sb(f"sg_xdt{cch % 2}", [128, 512], BF16)
        k.op("dve", (lambda btx=btx, xdt=xdt, cch=cch: nc.vector.tensor_tensor(
            out=h8(xdt[:]), in0=h8(btx[:]), in1=dt[:, cch, hs].unsqueeze(2).to_broadcast([128, 8, 64]), op=ALU.mult)),
            reads=[rbx, r_dt], writes=[r_xdt])
        if full:
            xD, r_xD = k.sb("sg_xD", [128, 512], F32)
            k.op("dve", (lambda btx=btx, xD=xD: nc.vector.tensor_tensor(
                out=h8(xD[:]), in0=h8(btx[:]), in1=D_bc[:, hs].unsqueeze(2).to_broadcast([128, 8, 64]), op=ALU.mult)),
                reads=[rbx, r_D], writes=[r_xD])
        k.free(bx)
        bB = k.bank(); btB, rbB = k.banks[bB]
        k.op("pe", (lambda cs=cs, btB=btB: nc.tensor.matmul(btB[:, 0:128], lhsT=BcT[:, cs], rhs=identb[:], start=True, stop=True)),
             reads=[r_Bc, r_identb], writes=[rbB])
        if full:
            k.op("pe", (lambda cs=cs, btB=btB: nc.tensor.matmul(btB[:, 128:256], lhsT=BcT[:, cs], rhs=CcT[:, cs], start=True, stop=True)),
                 reads=[r_Bc, r_Cc], writes=[rbB])
        Btok, r_Bt = k.sb(f"sg_Btok{cch % 2}", [128, 128], BF16)
        k.op("act", (lambda btB=btB, Btok=Btok: nc.scalar.copy(out=Btok[:], in_=btB[:, 0:128])), reads=[rbB], writes=[r_Bt])
        if full:
            cbm, r_cbm = k.sb("sg_cbm", [128, 128], F32)
            k.op("dve", (lambda btB=btB, cbm=cbm: nc.vector.tensor_tensor(out=cbm[:], in0=btB[:, 128:256], in1=mle[:], op=ALU.mult)),
                 reads=[rbB, r_mle], writes=[r_cbm])
        k.free(bB)
        if full:
            MT, r_MT = k.sb("sg_MT", [128, 8, 128], BF16)
            for hh in range(2):
                lta, r_lta = k.sb(f"sg_lta", [128, 4, 128], F32)
                k.op("dve", (lambda lta=lta, cch=cch, hh=hh: nc.vector.tensor_tensor(
                    out=lta[:], in0=ustr[:].unsqueeze(1).to_broadcast([128, 4, 128]),
                    in1=a[:, cch, 8 * g + 4 * hh:8 * g + 4 * hh + 4].unsqueeze(2).to_broadcast([128, 4, 128]), op=ALU.mult)),
                    reads=[r_ustr, r_a], writes=[r_lta])
                ba = k.bank(); bta, rba = k.banks[ba]
                for h4 in range(4):
                    k.op("pe", (lambda h4=h4, bta=bta, lta=lta: nc.tensor.matmul(
                        bta[:, h4 * 128:(h4 + 1) * 128], lhsT=lta[:, h4, :], rhs=tri[:], start=True, stop=True)),
                        reads=[r_lta, r_tri], writes=[rba])
                dec, r_dec = k.sb(f"sg_dec{hh}", [128, 512], BF16)
                k.op("act", (lambda bta=bta, dec=dec: nc.scalar.activation(out=dec[:], in_=bta[:], func=AF.Exp)),
                     reads=[rba], writes=[r_dec])
                k.free(ba)
                k.op("dve", (lambda dec=dec, MT=MT, hh=hh, cbm=cbm: nc.vector.tensor_tensor(
                    out=MT[:, hh * 4:(hh + 1) * 4, :], in0=dec[:].rearrange("p (h l) -> p h l", h=4),
                    in1=cbm[:].unsqueeze(1).to_broadcast([128, 4, 128]), op=ALU.mult)), reads=[r_dec, r_cbm], writes=[r_MT])
            by = k.bank(); bty, rby = k.banks[by]
            for h in range(8):
                k.op("pe", (lambda h=h, bty=bty, MT=MT, xdt=xdt: nc.tensor.matmul(
                    bty[:, h * 64:(h + 1) * 64], lhsT=MT[:, h, :], rhs=xdt[:, h * 64:(h + 1) * 64], start=True, stop=True)),
                    reads=[r_MT, r_xdt], writes=[rby])
            bo = k.bank(); bto, rbo = k.banks[bo]
            k.op("pe", (lambda cs=cs, bto=bto: nc.tensor.matmul(bto[:], lhsT=CcT[:, cs], rhs=Sbf[:], start=True, stop=True)),
                 reads=[r_Cc, r_Sbf], writes=[rbo])
            t1, r_t1 = k.sb("sg_t1", [128, 512], F32)
            k.op("dve", (lambda bto=bto, t1=t1, cch=cch: nc.vector.tensor_tensor(
                out=h8(t1[:]), in0=h8(bto[:]), in1=eacs[:, cch, hs].unsqueeze(2).to_broadcast([128, 8, 64]), op=ALU.mult)),
                reads=[rbo, r_eacs], writes=[r_t1])
            k.free(bo)
            k.op("pool", (lambda t1=t1, xD=xD: nc.gpsimd.tensor_tensor(out=t1[:], in0=t1[:], in1=xD[:], op=ALU.add)),
                 reads=[r_t1, r_xD], writes=[r_t1])
            ysb, r_ysb = k.sb("sg_ysb", [128, 512], F32)
            k.op("dve", (lambda bty=bty, t1=t1, ysb=ysb: nc.vector.tensor_tensor(out=ysb[:], in0=t1[:], in1=bty[:], op=ALU.add)),
                 reads=[rby, r_t1], writes=[r_ysb])
            k.free(by)
            bz = k.bank(); btz, rbz = k.banks[bz]
            for zi, (wz, r_wz) in enumerate(((wz0, r_wz0), (wz1, r_wz1))):
                for kc in range(NKC):
                    k.op("pe", (lambda kc=kc, cs=cs, btz=btz, wz=wz, zi=zi: nc.tensor.matmul(
                        btz[:, zi * 256:(zi + 1) * 256], lhsT=hnT[:, kc, cs], rhs=wz[:, kc, :],
                        start=(kc == 0), stop=(kc == NKC - 1))), reads=[r_hn, r_wz], writes=[rbz])
            zs, r_zs = k.sb("sg_zs", [128, 512], F32)
            k.op("act", (lambda btz=btz, zs=zs: nc.scalar.activation(out=zs[:], in_=btz[:], func=AF.Tanh, scale=0.5)),
                 reads=[rbz], writes=[r_zs])
            k.op("dve", (lambda btz=btz, zs=zs: nc.vector.scalar_tensor_tensor(out=zs[:], in0=zs[:], scalar=1.0, in1=btz[:],
                                                                               op0=ALU.add, op1=ALU.mult)),
                 reads=[r_zs, rbz], writes=[r_zs])
            k.free(bz)
            ygb, r_ygb = k.sb(f"sg_ygb{cch % 2}", [128, 512], BF16)
            k.op("dve", (lambda ysb=ysb, zs=zs, ygb=ygb: nc.vector.tensor_tensor(out=ygb[:], in0=ysb[:], in1=zs[:], op=ALU.mult)),
                 reads=[r_ysb, r_zs], writes=[r_ygb])
            k.op("act", (lambda cch=cch, zs=zs, ygb=ygb: nc.scalar.activation(out=zs[:], in_=ygb[:], func=AF.Square, scale=0.5,
                                                                             accum_out=ss[:, cch:cch + 1])),
                 reads=[r_ygb], writes=[r_zs, r_ss])
            btt = k.bank(); bttt, rbt = k.banks[btt]
            for fc in range(4):
                k.op("pe", (lambda fc=fc, bttt=bttt, ygb=ygb: nc.tensor.matmul(bttt[:, fc * 128:(fc + 1) * 128], lhsT=ygb[:, fc * 128:(fc + 1) * 128],
                                                                               rhs=identb[:], start=True, stop=True)),
                     reads=[r_ygb, r_identb], writes=[rbt])
            k.op("act", (lambda bttt=bttt, cs=cs: nc.scalar.copy(out=ynT_all[:, 4 * g:4 * g + 4, cs], in_=bttt[:].rearrange("p (f t) -> p f t", f=4))),
                 reads=[rbt], writes=[r_yn])
            k.free(btt)
        if (not full) or cch < 7:
            xd, r_xd = k.sb("sg_xd", [128, 512], BF16)
            k.op("pool", (lambda xd=xd, xdt=xdt, cch=cch: nc.gpsimd.tensor_tensor(
                out=h8(xd[:]), in0=h8(xdt[:]), in1=dstate[:, cch, hs].unsqueeze(2).to_broadcast([128, 8, 64]), op=ALU.mult)),
                reads=[r_xdt, r_dst], writes=[r_xd])
            bs = k.bank(); bts, rbs = k.banks[bs]
            k.op("pe", (lambda bts=bts, Btok=Btok, xd=xd: nc.tensor.matmul(bts[:], lhsT=Btok[:], rhs=xd[:], start=True, stop=True)),
                 reads=[r_Bt, r_xd], writes=[rbs])
            k.op("pool", (lambda cch=cch: nc.gpsimd.tensor_tensor(
                out=h8(Sg[:]), in0=h8(Sg[:]), in1=cdec[:, cch, hs].unsqueeze(2).to_broadcast([128, 8, 64]), op=ALU.mult)),
                reads=[r_S, r_cdec], writes=[r_S])
            k.op("dve", (lambda bts=bts: nc.vector.tensor_tensor(out=Sg[:], in0=Sg[:], in1=bts[:], op=ALU.add)), reads=[r_S, rbs], writes=[r_S])
            k.free(bs)
            if full:
                k.op("act", lambda: nc.scalar.copy(out=Sbf[:], in_=Sg[:]), reads=[r_S], writes=[r_Sbf])

    if not full:
        k.op("act", lambda: nc.scalar.copy(out=S_in[:, g, :], in_=Sg[:]), reads=[r_S], writes=[r_Sin])
        return
    eps4, r_eps4 = c["eps4_col"]
    pre_t, r_rbc = k.sb("sg_pre", [128, NT + 8], F32)
    rbc = pre_t[:, 0:NT]
    for hh in range(2):
        b = k.bank(); bt, rb = k.banks[b]
        for c4 in range(4):
            cch = hh * 4 + c4
            dg, r_dg = k.sb(f"sg_diag{c4 % 2}", [128, 128], F32)
            k.op("dve", (lambda dg=dg, cch=cch: nc.vector.tensor_scalar(out=dg[:], in0=identf[:], scalar1=ss[:, cch:cch + 1], scalar2=None,
                                                                       op0=ALU.mult)), reads=[r_identf, r_ss], writes=[r_dg])
            k.op("pe", (lambda dg=dg, bt=bt, c4=c4: nc.tensor.matmul(bt[:, c4 * 128:(c4 + 1) * 128], lhsT=ones[:], rhs=dg[:], start=True, stop=True)),
                 reads=[r_dg, r_ones], writes=[rb])
        k.op("act", (lambda bt=bt, hh=hh: nc.scalar.activation(out=rbc[:, hh * 512:(hh + 1) * 512], in_=bt[:], func=AF.Ln, bias=eps4[:],
                                                               scale=4.0 / 512)), reads=[rb, r_eps4], writes=[r_rbc])
        k.free(b)
    k.op("act", lambda: nc.scalar.activation(out=rbc, in_=rbc, func=AF.Exp, scale=-0.5), reads=[r_rbc], writes=[r_rbc])
    for fc in range(4):
        eng = "dve"
        E = nc.vector
        k.op(eng, (lambda fc=fc, E=E: E.scalar_tensor_tensor(
            out=ynT_all[:, 4 * g + fc, :], in0=ynT_all[:, 4 * g + fc, :], scalar=nw[:, 4 * g + fc:4 * g + fc + 1], in1=rbc,
            op0=ALU.mult, op1=ALU.mult)), reads=[r_yn, r_nw, r_rbc], writes=[r_yn])


def wseq_outproj(prm):
    w = prm["w_out"]
    return [[(w[0:2048, dc * 128:(dc + 1) * 128], 16, 128), (w[2048:4096, dc * 128:(dc + 1) * 128], 16, 128)] for dc in range(16)]


def out_proj(k, hT, r_h, ynT_all, r_yn, prm):
    nc = k.nc
    for dc, pieces in enumerate(wseq_outproj(prm)):
        (wo0, r_wo0), (wo1, r_wo1) = k.wget(pieces)
        for th in range(2):
            ts = slice(th * 512, (th + 1) * 512)
            b = k.bank(); bt, rb = k.banks[b]
            for fc in range(32):
                wo, r_wo = (wo0, r_wo0) if fc < 16 else (wo1, r_wo1)
                k.op("pe", (lambda fc=fc, ts=ts, bt=bt, wo=wo: nc.tensor.matmul(
                    bt[:], lhsT=wo[:, fc % 16, :], rhs=ynT_all[:, fc, ts], start=(fc == 0), stop=(fc == 31))),
                    reads=[r_wo, r_yn], writes=[rb])
            k.op("dve", (lambda dc=dc, ts=ts, bt=bt: nc.vector.tensor_tensor(out=hT[:, dc, ts], in0=hT[:, dc, ts], in1=bt[:], op=ALU.add)),
                 reads=[rb, r_h], writes=[r_h])
            k.free(b)


W_IN_COLS = 10304
KB = 1024


def dram_in(nc, name, shape, dt=F32):
    return nc.dram_tensor(name, list(shape), dt, kind="ExternalInput").ap()


def load_small(k, name, src, shape):
    nc = k.nc
    t, r = k.sb("p_" + name, shape, F32)
    k.op("sp", (lambda t=t, src=src: nc.sync.dma_start(out=t[:], in_=src)), writes=[r], dma=True)
    return t, r


L1_INPUTS = [("xT_prev", [2048, NT]), ("xT_own", [2048, NT]), ("flag", [128, 1]),
             ("mix_g0", [128, 16]), ("ffn_g0", [128, 16]), ("kv_g", [128, 16]),
             ("w_in", [2048, W_IN_COLS]), ("conv_w", [128, 48, 4]), ("conv_b", [128, 48]),
             ("dt_bias", [128, 64]), ("a_log", [128, 64]), ("d_skip", [128, 64]), ("norm_w", [128, 32]),
             ("w_out", [4096, 2048]), ("w_k", [2048, 2048]), ("w_v", [2048, 2048]),
             ("w_router0", [2048, 20]), ("b_router0", [128, 20]),
             ("w_gate0", [16, 2048, 512]), ("w_up0", [16, 2048, 512]), ("w_down0", [16, 512, 2048])]


def wseq_kv(I):
    return ([[(I["w_k"][:, cb * 256:(cb + 1) * 256], NKC, 256)] for cb in range(8)] +
            [[(I["w_v"][:, cb * 256:(cb + 1) * 256], NKC, 256)] for cb in range(8)])


def build_launch1(stop="full"):
    nc = bass.Bass("TRN2", target_bir_lowering=False)
    dr = {n: dram_in(nc, n, s) for n, s in CONST_SHAPES.items()}
    I = {n: dram_in(nc, n, s) for n, s in L1_INPUTS}
    h_out = nc.dram_tensor("h_out", [2048, NT], F32, kind="ExternalOutput").ap()
    kT_out = nc.dram_tensor("kT_out", [2048, NT], F32, kind="ExternalOutput").ap()
    v_out = nc.dram_tensor("v_out", [NT, 2048], F32, kind="ExternalOutput").ap()
    k = K(nc)
    k.setup()
    outs = []

    load_consts(k, dr, ["ident_bf", "ident_f", "ones_f", "tri_incl", "ustrict", "mask_le", "inv128"])
    hnT, r_hn = k.sb("hnT", [128, 16, NT], BF16)
    prm = {}
    mixg = load_small(k, "mix_g0", I["mix_g0"], [128, 16])
    prm["ffn_g"] = [load_small(k, "ffn_g0", I["ffn_g0"], [128, 16])]
    kvg = load_small(k, "kv_g", I["kv_g"], [128, 16])
    prm["b_router"] = [load_small(k, "b_router0", I["b_router0"], [128, 20])]
    prm["w_router"] = [I["w_router0"]]
    prm["w_gate"] = [[I["w_gate0"][e] for e in range(16)]]
    prm["w_up"] = [[I["w_up0"][e] for e in range(16)]]
    prm["w_down"] = [[I["w_down0"][e] for e in range(16)]]
    prm["w_in"] = I["w_in"]
    prm["w_out"] = I["w_out"]
    prm["flag"] = load_small(k, "flag", I["flag"], [128, 1])
    prm["dtb"] = load_small(k, "dt_bias", I["dt_bias"], [128, 64])
    prm["D_bc"] = load_small(k, "d_skip", I["d_skip"], [128, 64])
    prm["nw"] = load_small(k, "norm_w", I["norm_w"], [128, 32])
    A_bc, r_A = load_small(k, "a_log", I["a_log"], [128, 64])
    k.op("act", lambda: nc.scalar.activation(out=A_bc[:], in_=A_bc[:], func=AF.Exp), reads=[r_A], writes=[r_A])
    k.op("dve", lambda: nc.vector.tensor_scalar(out=A_bc[:], in0=A_bc[:], scalar1=-1.0, scalar2=None, op0=ALU.mult), reads=[r_A], writes=[r_A])
    prm["A_bc"] = (A_bc, r_A)
    cwh, r_cwh = k.sb("p_cwh", [128, 48, 4], F32)
    k.op("sp", lambda: nc.sync.dma_start(out=cwh[:], in_=I["conv_w"]), writes=[r_cwh], dma=True)
    k.op("dve", lambda: nc.vector.tensor_scalar(out=cwh[:], in0=cwh[:], scalar1=0.5, scalar2=None, op0=ALU.mult), reads=[r_cwh], writes=[r_cwh])
    prm["cwh"] = (cwh, r_cwh)
    cbh, r_cbh = load_small(k, "conv_b", I["conv_b"], [128, 48])
    k.op("dve", lambda: nc.vector.tensor_scalar(out=cbh[:], in0=cbh[:], scalar1=0.5, scalar2=None, op0=ALU.mult), reads=[r_cbh], writes=[r_cbh])
    prm["cbh"] = (cbh, r_cbh)
    wdt, r_wdt = k.sb("p_wdt", [128, 16, 64], BF16)
    k.op("pool", lambda: nc.gpsimd.dma_start(out=wdt[:], in_=I["w_in"][:, 10240:10304].rearrange("(kc p) f -> p kc f", p=128)),
         writes=[r_wdt], dma=True)
    prm["wdt"] = (wdt, r_wdt)
    prm["hal"] = k.sb("p_hal", [128, 48, 3], F32)
    prm["S_in"] = k.sb("p_Sin", [128, 8, 512], BF16)
    BIG = (k.bump + 63) // 64 * 64
    print("perm end", BIG - SB_BASE, "big bytes", SB_TOP - BIG)
    assert SB_TOP - BIG >= 128 * KB

    LV = ["normA", "prepA", "sA1", "sA", "normB", "sB1", "sB", "mixer", "moe", "full"]
    lv = LV.index(stop)
    nA = 0 if lv < 2 else (1 if lv == 2 else 8)
    nB = 0 if lv < 5 else (1 if lv == 5 else 8)
    for g in range(nA):
        k.wdeclare(wseq_ssd_group(prm, g, False))
    for g in range(nB):
        k.wdeclare(wseq_ssd_group(prm, g, True))
    if lv >= 7:
        k.wdeclare(wseq_outproj(prm))
    if lv >= 8:
        k.wdeclare(wseq_moe(prm, 0))
    if lv >= 9:
        k.wdeclare(wseq_kv(I))

    def early_out():
        o = k.op("pool", lambda: nc.gpsimd.dma_start(out=h_out.rearrange("(kc p) t -> p kc t", p=128), in_=hnT[:]), reads=[r_hn], dma=True)
        cnt = k.p.emit()
        print("launch1(early): ops", len(k.p.ops), "wtiles", len(k.wlist), "signals", cnt)
        k.p.final_wait("pool", [o])
        nc._knames = k.names
        return nc

    k.begin_phase("nA", BIG)
    rmsnorm(k, src_staged(k, I["xT_prev"]), mixg[0], mixg[1], hnT, r_hn)
    if lv == 0:
        return early_out()
    k.begin_phase("sA", BIG)
    P = ssd_prep(k, hnT, r_hn, prm)
    for g in range(nA):
        ssd_group(k, g, "A", hnT, r_hn, P, prm)
    if lv <= 3:
        return early_out()
    k.begin_phase("nB", BIG)
    rmsnorm(k, src_staged(k, I["xT_own"]), mixg[0], mixg[1], hnT, r_hn)
    k.begin_phase("sB", BIG)
    ynT_all, r_yn = k.sb("ynT_all", [128, 32, NT], BF16)
    assert k.bump == BIG + 64 * KB
    if lv == 4:
        return early_out()
    P = ssd_prep(k, hnT, r_hn, prm)
    for g in range(nB):
        ssd_group(k, g, "B", hnT, r_hn, P, prm, ynT_all, r_yn)
    print("sB scratch used", k.bump - BIG - 64 * KB)
    if lv <= 6:
        return early_out()
    k.begin_phase("op", BIG + 64 * KB)
    hT, r_h = k.sb("hT", [128, 16, NT], F32)
    k.op("sp", lambda: nc.sync.dma_start(out=hT[:], in_=I["xT_own"].rearrange("(kc p) t -> p kc t", p=128)), writes=[r_h], dma=True)
    out_proj(k, hT, r_h, ynT_all, r_yn, prm)
    if lv >= 8:
        k.begin_phase("moe0", BIG, BIG + 64 * KB)
        load_consts(k, dr, ["sel16"])
        moe(k, hT, r_h, hnT, r_hn, prm, 0)
        print("moe scratch used", k.bump - BIG)
    if lv >= 9:
        k.begin_phase("kv", BIG, BIG + 64 * KB)
        rmsnorm(k, src_resident(hT, r_h), kvg[0], kvg[1], hnT, r_hn)
        seq = wseq_kv(I)
        for cb in range(8):
            (wk, r_wk), = k.wget(seq[cb])
            st, r_st = k.sb(f"kv_st{cb % 2}", [128, 2, NT], F32)
            for c2 in range(2):
                for th in range(2):
                    ts = slice(th * 512, (th + 1) * 512)
                    b = k.bank(); bt, rb = k.banks[b]
                    for kc in range(NKC):
                        k.op("pe", (lambda kc=kc, ts=ts, bt=bt, wk=wk, c2=c2: nc.tensor.matmul(
                            bt[:], lhsT=wk[:, kc, c2 * 128:(c2 + 1) * 128], rhs=hnT[:, kc, ts],
                            start=(kc == 0), stop=(kc == NKC - 1))), reads=[r_wk, r_hn], writes=[rb])
                    k.op("act", (lambda bt=bt, st=st, c2=c2, ts=ts: nc.scalar.copy(out=st[:, c2, ts], in_=bt[:])), reads=[rb], writes=[r_st])
                    k.free(b)
            o = k.op("sp", (lambda st=st, cb=cb: nc.sync.dma_start(
                out=kT_out[cb * 256:(cb + 1) * 256, :].rearrange("(c p) t -> p c t", p=128), in_=st[:])), reads=[r_st], dma=True)
            outs.append(o)
        for cb in range(8):
            (wv, r_wv), = k.wget(seq[8 + cb])
            st, r_st = k.sb(f"kv_st{cb % 2}", [128, 2, NT], F32)
            stv = st[:].rearrange("p c t -> p (c t)").rearrange("p (t f) -> p t f", f=256)
            for t2 in range(4):
                b = k.bank(); bt, rb = k.banks[b]
                for ti in range(2):
                    tb = t2 * 2 + ti
                    for kc in range(NKC):
                        k.op("pe", (lambda kc=kc, tb=tb, ti=ti, bt=bt, wv=wv: nc.tensor.matmul(
                            bt[:, ti * 256:(ti + 1) * 256], lhsT=hnT[:, kc, tb * 128:(tb + 1) * 128], rhs=wv[:, kc, :],
                            start=(kc == 0), stop=(kc == NKC - 1))), reads=[r_wv, r_hn], writes=[rb])
                k.op("act", (lambda bt=bt, stv=stv, t2=t2: nc.scalar.copy(
                    out=stv[:, 2 * t2:2 * t2 + 2, :], in_=bt[:].rearrange("p (t f) -> p t f", f=256))), reads=[rb], writes=[r_st])
                k.free(b)
            o = k.op("sp", (lambda stv=stv, cb=cb: nc.sync.dma_start(
                out=v_out[:, cb * 256:(cb + 1) * 256].rearrange("(t p) f -> p t f", p=128), in_=stv)), reads=[r_st], dma=True)
            outs.append(o)
    o = k.op("sp", lambda: nc.sync.dma_start(out=h_out.rearrange("(kc p) t -> p kc t", p=128), in_=hT[:]), reads=[r_h], dma=True)
    outs.append(o)
    assert k.widx == len(k.wlist), (k.widx, len(k.wlist))
    cnt = k.p.emit()
    print("launch1: ops", len(k.p.ops), "wtiles", len(k.wlist), "signals", cnt)
    k.p.final_wait("sp", outs)
    return nc


def fm(v, n):
    return np.ascontiguousarray(np.asarray(v, np.float32).reshape(n, 128).T)


def rep(v):
    return np.ascontiguousarray(np.tile(np.asarray(v, np.float32)[None, :], (128, 1)))


def host_inputs_l1(inp, core):
    b, half = core // 2, core % 2
    x = np.asarray(inp["x"], np.float32)
    m = dict(host_consts())
    own = x[b, half * NT:(half + 1) * NT]
    prev = x[b, 0:NT] if half == 1 else np.zeros((NT, D), np.float32)
    m["xT_own"] = np.ascontiguousarray(own.T)
    m["xT_prev"] = np.ascontiguousarray(prev.T)
    m["flag"] = np.full((128, 1), float(half), np.float32)
    m["mix_g0"] = fm(inp["mix_norm"][0], 16)
    m["ffn_g0"] = fm(inp["ffn_norm"][0], 16)
    m["kv_g"] = fm(inp["kv_norm"], 16)
    m["w_in"] = np.asarray(inp["ssm_w_in"][0], np.float32)
    cw = np.asarray(inp["ssm_conv_w"][0], np.float32)
    m["conv_w"] = np.ascontiguousarray(cw.T.reshape(48, 128, 4).transpose(1, 0, 2))
    m["conv_b"] = fm(inp["ssm_conv_b"][0], 48)
    m["dt_bias"] = rep(inp["ssm_dt_bias"][0])
    m["a_log"] = rep(inp["ssm_a_log"][0])
    m["d_skip"] = rep(inp["ssm_d"][0])
    m["norm_w"] = fm(inp["ssm_norm_w"][0], 32)
    m["w_out"] = np.asarray(inp["ssm_w_out"][0], np.float32)
    m["w_k"] = np.asarray(inp["w_k"], np.float32)
    m["w_v"] = np.asarray(inp["w_v"], np.float32)
    m["w_router0"] = np.ascontiguousarray(np.concatenate(
        [np.asarray(inp["moe_w_coarse"][0], np.float32), np.asarray(inp["moe_w_fine"][0], np.float32).reshape(D, 16)], axis=1))
    m["b_router0"] = rep(np.concatenate([np.asarray(inp["moe_b_coarse"][0], np.float32),
                                         np.asarray(inp["moe_b_fine"][0], np.float32).reshape(16)]))
    m["w_gate0"] = np.asarray(inp["moe_w_gate"][0], np.float32)
    m["w_up0"] = np.asarray(inp["moe_w_up"][0], np.float32)
    m["w_down0"] = np.asarray(inp["moe_w_down"][0], np.float32)
    return m


L2_INPUTS = [("hT_in", [2048, NT]), ("kT_all", [2048, 2048]), ("v_all", [2048, 2048]),
             ("mix_g1", [128, 16]), ("ffn_g1", [128, 16]), ("final_g", [128, 16]),
             ("w_q", [2048, 2048]), ("w_o", [2048, 2048]),
             ("w_router1", [2048, 20]), ("b_router1", [128, 20]),
             ("w_gate1", [16, 2048, 512]), ("w_up1", [16, 2048, 512]), ("w_down1", [16, 512, 2048]),
             ("mask_rev", [128, 128])]


def wseq_sq(w):
    return [[(w[:, cb * 256:(cb + 1) * 256], NKC, 256)] for cb in range(8)]


def build_launch2(stop="full"):
    nc = bass.Bass("TRN2", target_bir_lowering=False)
    dr = {n: dram_in(nc, n, s) for n, s in CONST_SHAPES.items()}
    I = {n: dram_in(nc, n, s) for n, s in L2_INPUTS}
    out_d = nc.dram_tensor("outT", [2048, NT], F32, kind="ExternalOutput").ap()
    k = K(nc)
    k.setup()
    load_consts(k, dr, ["ident_bf", "ident_f", "ones_f", "inv128"])
    hn_off = k.bump
    hnT, r_hn = k.sb("hnT", [128, 16, NT], BF16)
    hn_end = k.bump
    prm = {}
    mixg = load_small(k, "mix_g1", I["mix_g1"], [128, 16])
    prm["ffn_g"] = [load_small(k, "ffn_g1", I["ffn_g1"], [128, 16])]
    fing = load_small(k, "final_g", I["final_g"], [128, 16])
    prm["b_router"] = [load_small(k, "b_router1", I["b_router1"], [128, 20])]
    prm["w_router"] = [I["w_router1"]]
    prm["w_gate"] = [[I["w_gate1"][e] for e in range(16)]]
    prm["w_up"] = [[I["w_up1"][e] for e in range(16)]]
    prm["w_down"] = [[I["w_down1"][e] for e in range(16)]]
    mrev, r_mrev = load_small(k, "mask_rev", I["mask_rev"], [128, 128])
    BIG = (k.bump + 63) // 64 * 64
    assert SB_TOP - BIG >= 128 * KB
    LV = ["q", "attn", "wo", "moe", "full"]
    lv = LV.index(stop)
    k.wdeclare(wseq_sq(I["w_q"]))
    if lv >= 2:
        k.wdeclare(wseq_sq(I["w_o"]))
    if lv >= 3:
        k.wdeclare(wseq_moe(prm, 0))

    k.begin_phase("q", BIG + 64 * KB)
    hT, r_h = k.sb("hT", [128, 16, NT], F32)
    k.op("sp", lambda: nc.sync.dma_start(out=hT[:], in_=I["hT_in"].rearrange("(kc p) t -> p kc t", p=128)), writes=[r_h], dma=True)
    k.bump = BIG
    k.limit = BIG + 64 * KB
    qT, r_q = k.sb("qT", [128, 16, NT], BF16)
    rmsnorm(k, src_resident(hT, r_h), mixg[0], mixg[1], hnT, r_hn)
    for cb, pieces in enumerate(wseq_sq(I["w_q"])):
        (wq, r_wq), = k.wget(pieces)
        for c2 in range(2):
            hd = cb * 2 + c2
            for th in range(2):
                ts = slice(th * 512, (th + 1) * 512)
                b = k.bank(); bt, rb = k.banks[b]
                for kc in range(NKC):
                    k.op("pe", (lambda kc=kc, ts=ts, bt=bt, wq=wq, c2=c2: nc.tensor.matmul(
                        bt[:], lhsT=wq[:, kc, c2 * 128:(c2 + 1) * 128], rhs=hnT[:, kc, ts], start=(kc == 0), stop=(kc == NKC - 1))),
                        reads=[r_wq, r_hn], writes=[rb])
                k.op("act", (lambda bt=bt, hd=hd, ts=ts: nc.scalar.activation(out=qT[:, hd, ts], in_=bt[:], func=AF.Copy, scale=128 ** -0.5)),
                     reads=[rb], writes=[r_q])
                k.free(b)

    def finish(src_t, r_src, bf):
        if bf:
            o = k.op("pool", lambda: nc.gpsimd.dma_start(out=out_d.rearrange("(kc p) t -> p kc t", p=128), in_=src_t[:]), reads=[r_src], dma=True)
            eng = "pool"
        else:
            o = k.op("sp", lambda: nc.sync.dma_start(out=out_d.rearrange("(kc p) t -> p kc t", p=128), in_=src_t[:]), reads=[r_src], dma=True)
            eng = "sp"
        assert k.widx == len(k.wlist), (k.widx, len(k.wlist))
        cnt = k.p.emit()
        print("launch2: ops", len(k.p.ops), "wtiles", len(k.wlist), "signals", cnt)
        k.p.final_wait(eng, [o])
        nc._knames = k.names
        return nc
    if lv == 0:
        return finish(qT, r_q, True)

    k.begin_phase("attn", BIG + 32 * KB, BIG + 64 * KB)
    es, r_es = k.sb("at_e", [128, 2048], F32)
    cs, r_cs = k.sb("at_cs", [128, 2048], F32)
    at, r_at = k.sb("at_attn", [128, 2048], BF16)
    atT, r_atT = k.sb("at_attnT", [128, 16, 128], BF16)
    kbuf = [k.sb(f"at_k{i}", [128, 2048], BF16) for i in range(2)]
    k.bump = hn_off
    k.limit = hn_end
    vbuf = [k.sb(f"at_v{i}", [128, 16, 128], BF16) for i in range(2)]
    ones_row, r_onr = k.sb("at_ones", [128, 2048], F32)
    k.op("sp", lambda: nc.sync.dma_start(out=ones_row[:], in_=dr["ones_row"]), writes=[r_onr], dma=True)
    onec, r_onec = k.c["one_col"]
    identb, r_identb = k.c["ident_bf"]
    for hd in range(16):
        kt, r_kt = kbuf[hd % 2]
        vt, r_vt = vbuf[hd % 2]
        k.op("pool", (lambda kt=kt, hd=hd: nc.gpsimd.dma_start(out=kt[:], in_=I["kT_all"][hd * 128:(hd + 1) * 128, :])), writes=[r_kt], dma=True)
        k.op("pool", (lambda vt=vt, hd=hd: nc.gpsimd.dma_start(
            out=vt[:], in_=I["v_all"][:, hd * 128:(hd + 1) * 128].rearrange("(blk p) d -> p blk d", p=128))), writes=[r_vt], dma=True)
        for i in range(8):
            qs = slice(i * 128, (i + 1) * 128)
            c0 = (7 - i) * 128
            ncol = 2048 - c0
            nblk = ncol // 128
            nbank = (ncol + 511) // 512
            zb = []
            for j in range(nbank):
                b = k.bank(); bt, rb = k.banks[b]
                w = min(512, ncol - j * 512)
                k.op("pe", (lambda bt=bt, hd=hd, qs=qs, kt=kt, j=j, w=w, c0=c0: nc.tensor.matmul(
                    bt[:, 0:w], lhsT=qT[:, hd, qs], rhs=kt[:, c0 + j * 512: c0 + j * 512 + w], start=True, stop=True)),
                    reads=[r_q, r_kt], writes=[rb])
                zb.append((b, bt, rb, w))
            for j, (b, bt, rb, w) in enumerate(zb):
                k.op("act", (lambda bt=bt, j=j, w=w: nc.scalar.activation(out=es[:, j * 512: j * 512 + w], in_=bt[:, 0:w], func=AF.Exp)),
                     reads=[rb], writes=[r_es])
            k.op("act", (lambda ncol=ncol: nc.scalar.activation(out=es[:, 0:ncol], in_=es[:, 0:ncol], func=AF.Ln, bias=onec[:], scale=1.0)),
                 reads=[r_es, r_onec], writes=[r_es])
            k.op("dve", lambda: nc.vector.tensor_tensor(out=es[:, 0:128], in0=es[:, 0:128], in1=mrev[:], op=ALU.mult),
                 reads=[r_es, r_mrev], writes=[r_es])
            k.op("dve", (lambda ncol=ncol: nc.vector.tensor_tensor_scan(out=cs[:, 0:ncol], data0=ones_row[:, 0:ncol], data1=es[:, 0:ncol],
                                                                       initial=0.0, op0=ALU.mult, op1=ALU.add)),
                 reads=[r_es, r_onr], writes=[r_cs])
            for j, (b, bt, rb, w) in enumerate(zb):
                k.op("dve", (lambda bt=bt, j=j, w=w: nc.vector.tensor_tensor(out=cs[:, j * 512: j * 512 + w], in0=cs[:, j * 512: j * 512 + w],
                                                                            in1=bt[:, 0:w], op=ALU.subtract)),
                     reads=[r_cs, rb], writes=[r_cs])
                k.free(b)
            k.op("act", (lambda ncol=ncol: nc.scalar.activation(out=at[:, 0:ncol], in_=cs[:, 0:ncol], func=AF.Exp, scale=-1.0)),
                 reads=[r_cs], writes=[r_at])
            k.op("dve", lambda: nc.vector.tensor_tensor(out=at[:, 0:128], in0=at[:, 0:128], in1=mrev[:], op=ALU.mult),
                 reads=[r_at, r_mrev], writes=[r_at])
            for j4 in range((nblk + 3) // 4):
                b = k.bank(); bt, rb = k.banks[b]
                nb = min(4, nblk - j4 * 4)
                for jj in range(nb):
                    blk = j4 * 4 + jj
                    k.op("pe", (lambda bt=bt, jj=jj, blk=blk: nc.tensor.matmul(bt[:, jj * 128:(jj + 1) * 128], lhsT=at[:, blk * 128:(blk + 1) * 128],
                                                                               rhs=identb[:], start=True, stop=True)),
                         reads=[r_at, r_identb], writes=[rb])
                k.op("act", (lambda bt=bt, j4=j4, nb=nb: nc.scalar.copy(out=atT[:, j4 * 4:j4 * 4 + nb, :],
                                                                       in_=bt[:, 0:nb * 128].rearrange("p (b q) -> p b q", b=nb))),
                     reads=[rb], writes=[r_atT])
                k.free(b)
            b = k.bank(); bt, rb = k.banks[b]
            for blk in range(nblk):
                k.op("pe", (lambda bt=bt, blk=blk, vt=vt, i=i, nblk=nblk: nc.tensor.matmul(
                    bt[:, 0:128], lhsT=vt[:, (7 - i) + blk, :], rhs=atT[:, blk, :], start=(blk == 0), stop=(blk == nblk - 1))),
                    reads=[r_vt, r_atT], writes=[rb])
            k.op("act", (lambda bt=bt, hd=hd, qs=qs: nc.scalar.copy(out=qT[:, hd, qs], in_=bt[:, 0:128])), reads=[rb], writes=[r_q])
            k.free(b)
    if lv == 1:
        return finish(qT, r_q, True)

    k.begin_phase("wo", BIG + 32 * KB, BIG + 64 * KB)
    for cb, pieces in enumerate(wseq_sq(I["w_o"])):
        (wo, r_wo), = k.wget(pieces)
        for c2 in range(2):
            dc = cb * 2 + c2
            for th in range(2):
                ts = slice(th * 512, (th + 1) * 512)
                b = k.bank(); bt, rb = k.banks[b]
                for kc in range(NKC):
                    k.op("pe", (lambda kc=kc, ts=ts, bt=bt, wo=wo, c2=c2: nc.tensor.matmul(
                        bt[:], lhsT=wo[:, kc, c2 * 128:(c2 + 1) * 128], rhs=qT[:, kc, ts], start=(kc == 0), stop=(kc == NKC - 1))),
                        reads=[r_wo, r_q], writes=[rb])
                k.op("dve", (lambda dc=dc, ts=ts, bt=bt: nc.vector.tensor_tensor(out=hT[:, dc, ts], in0=hT[:, dc, ts], in1=bt[:], op=ALU.add)),
                     reads=[rb, r_h], writes=[r_h])
                k.free(b)
    if lv == 2:
        return finish(hT, r_h, False)
    k.begin_phase("moe1", BIG, BIG + 64 * KB)
    load_consts(k, dr, ["sel16"])
    moe(k, hT, r_h, hnT, r_hn, prm, 0)
    if lv == 3:
        return finish(hT, r_h, False)
    k.begin_phase("fin", BIG, BIG + 64 * KB)
    outT, r_o = k.sb("outT", [128, 16, NT // 2], F32)
    oo = []
    nc_ = nc
    ones, r_ones = k.c["ones_f"]
    epsc, r_eps = k.c["eps_col"]
    for th in range(2):
        ts = slice(th * 512, (th + 1) * 512)
        b = k.bank(); bt, rb = k.banks[b]
        for kc in range(NKC):
            sq, rsq = k.sb(f"rn_sq{kc % 2}", [128, 512], F32)
            k.op("act", (lambda sq=sq, kc=kc, ts=ts: nc.scalar.activation(out=sq[:], in_=hT[:, kc, ts], func=AF.Square)), reads=[r_h], writes=[rsq])
            k.op("pe", (lambda sq=sq, kc=kc, bt=bt: nc.tensor.matmul(bt[:], lhsT=ones[:], rhs=sq[:], start=(kc == 0), stop=(kc == NKC - 1))),
                 reads=[rsq, r_ones], writes=[rb])
        rt, rr = k.sb("rn_rstd", [128, 512], F32)
        k.op("act", (lambda bt=bt, rt=rt: nc.scalar.activation(out=rt[:], in_=bt[:], func=AF.Ln, bias=epsc[:], scale=1.0 / D)), reads=[rb, r_eps], writes=[rr])
        k.free(b)
        k.op("act", (lambda rt=rt: nc.scalar.activation(out=rt[:], in_=rt[:], func=AF.Exp, scale=-0.5)), reads=[rr], writes=[rr])
        for kc in range(NKC):
            k.op("dve", (lambda kc=kc, ts=ts, rt=rt: nc.vector.scalar_tensor_tensor(
                out=outT[:, kc, :], in0=hT[:, kc, ts], scalar=fing[0][:, kc:kc + 1], in1=rt[:], op0=ALU.mult, op1=ALU.mult)),
                reads=[r_h, fing[1], rr], writes=[r_o])
        oo.append(k.op("sp", (lambda ts=ts: nc.sync.dma_start(out=out_d[:, ts].rearrange("(kc p) t -> p kc t", p=128), in_=outT[:])),
                       reads=[r_o], dma=True))
    assert k.widx == len(k.wlist), (k.widx, len(k.wlist))
    cnt = k.p.emit()
    print("launch2: ops", len(k.p.ops), "wtiles", len(k.wlist), "signals", cnt)
    k.p.final_wait("sp", oo)
    return nc


def host_inputs_l2(inp, core, h_out, kT_outs, v_outs):
    b, half = core // 2, core % 2
    m = dict(host_consts())
    m["hT_in"] = np.ascontiguousarray(h_out)
    kown = kT_outs[core][:, ::-1]
    vown = v_outs[core][::-1, :]
    if half == 1:
        kprev = kT_outs[core - 1][:, ::-1]
        vprev = v_outs[core - 1][::-1, :]
    else:
        kprev = np.zeros_like(kown)
        vprev = np.zeros_like(vown)
    m["kT_all"] = np.ascontiguousarray(np.concatenate([kown, kprev], axis=1))
    m["v_all"] = np.ascontiguousarray(np.concatenate([vown, vprev], axis=0))
    m["mix_g1"] = fm(inp["mix_norm"][1], 16)
    m["ffn_g1"] = fm(inp["ffn_norm"][1], 16)
    m["final_g"] = fm(inp["final_norm"], 16)
    m["w_q"] = np.asarray(inp["sb_w_q"][0], np.float32)
    m["w_o"] = np.asarray(inp["sb_w_out"][0], np.float32)
    m["w_router1"] = np.ascontiguousarray(np.concatenate(
        [np.asarray(inp["moe_w_coarse"][1], np.float32), np.asarray(inp["moe_w_fine"][1], np.float32).reshape(D, 16)], axis=1))
    m["b_router1"] = rep(np.concatenate([np.asarray(inp["moe_b_coarse"][1], np.float32),
                                         np.asarray(inp["moe_b_fine"][1], np.float32).reshape(16)]))
    m["w_gate1"] = np.asarray(inp["moe_w_gate"][1], np.float32)
    m["w_up1"] = np.asarray(inp["moe_w_up"][1], np.float32)
    m["w_down1"] = np.asarray(inp["moe_w_down"][1], np.float32)
    i = np.arange(128)
    m["mask_rev"] = (i[None, :] > 127 - i[:, None]).astype(np.float32)
    return m


F_INPUTS = L1_INPUTS + [("mix_g1", [128, 16]), ("ffn_g1", [128, 16]), ("final_g", [128, 16]),
                        ("w_q", [2048, 2048]), ("w_o", [2048, 2048]),
                        ("w_router1", [2048, 20]), ("b_router1", [128, 20]),
                        ("w_gate1", [16, 2048, 512]), ("w_up1", [16, 2048, 512]), ("w_down1", [16, 512, 2048])]


def build_fused():
    nc = bass.Bass("TRN2", target_bir_lowering=False)
    dr = {n: dram_in(nc, n, s) for n, s in CONST_SHAPES.items()}
    I = {n: dram_in(nc, n, s) for n, s in F_INPUTS}
    out_d = nc.dram_tensor("outT", [2048, NT], F32, kind="ExternalOutput").ap()
    kT_loc = nc.dram_tensor("kT_loc", [2048, NT], BF16, kind="Internal").ap()
    v_loc = nc.dram_tensor("v_loc", [NT, 2048], BF16, kind="Internal").ap()
    kT_prev = nc.dram_tensor("kT_prev", [2048, NT], BF16, kind="Internal").ap()
    v_prev = nc.dram_tensor("v_prev", [NT, 2048], BF16, kind="Internal").ap()
    xloc = nc.dram_tensor("xloc", [2048, 512], BF16, kind="Internal").ap()
    xg = nc.dram_tensor("xg", [4096, 512], BF16, kind="Internal").ap()
    k = K(nc)
    k.setup()

    load_consts(k, dr, ["ident_bf", "ident_f", "ones_f", "tri_incl", "ustrict", "mask_le"])
    hn_off = k.bump
    hnT, r_hn = k.sb("hnT", [128, 16, NT], BF16)
    hn_end = k.bump
    prm = {}
    mixg = [load_small(k, "mix_g0", I["mix_g0"], [128, 16]), load_small(k, "mix_g1", I["mix_g1"], [128, 16])]
    prm["ffn_g"] = [load_small(k, "ffn_g0", I["ffn_g0"], [128, 16]), load_small(k, "ffn_g1", I["ffn_g1"], [128, 16])]
    kvg = load_small(k, "kv_g", I["kv_g"], [128, 16])
    fing = load_small(k, "final_g", I["final_g"], [128, 16])
    prm["b_router"] = [load_small(k, "b_router0", I["b_router0"], [128, 20]), load_small(k, "b_router1", I["b_router1"], [128, 20])]
    prm["w_router"] = [I["w_router0"], I["w_router1"]]
    prm["w_gate"] = [[I["w_gate0"][e] for e in range(16)], [I["w_gate1"][e] for e in range(16)]]
    prm["w_up"] = [[I["w_up0"][e] for e in range(16)], [I["w_up1"][e] for e in range(16)]]
    prm["w_down"] = [[I["w_down0"][e] for e in range(16)], [I["w_down1"][e] for e in range(16)]]
    prm["w_in"] = I["w_in"]
    prm["w_out"] = I["w_out"]
    prm["flag"] = load_small(k, "flag", I["flag"], [128, 1])
    flag, r_flag = prm["flag"]
    prm["dtb"] = load_small(k, "dt_bias", I["dt_bias"], [128, 64])
    prm["D_bc"] = load_small(k, "d_skip", I["d_skip"], [128, 64])
    prm["nw"] = load_small(k, "norm_w", I["norm_w"], [128, 32])
    A_bc, r_A = load_small(k, "a_log", I["a_log"], [128, 64])
    k.op("act", lambda: nc.scalar.activation(out=A_bc[:], in_=A_bc[:], func=AF.Exp), reads=[r_A], writes=[r_A])
    k.op("dve", lambda: nc.vector.tensor_scalar(out=A_bc[:], in0=A_bc[:], scalar1=-1.0, scalar2=None, op0=ALU.mult), reads=[r_A], writes=[r_A])
    prm["A_bc"] = (A_bc, r_A)
    cwh, r_cwh = k.sb("p_cwh", [128, 48, 4], F32)
    k.op("sp", lambda: nc.sync.dma_start(out=cwh[:], in_=I["conv_w"]), writes=[r_cwh], dma=True)
    k.op("dve", lambda: nc.vector.tensor_scalar(out=cwh[:], in0=cwh[:], scalar1=0.5, scalar2=None, op0=ALU.mult), reads=[r_cwh], writes=[r_cwh])
    prm["cwh"] = (cwh, r_cwh)
    cbh, r_cbh = load_small(k, "conv_b", I["conv_b"], [128, 48])
    k.op("dve", lambda: nc.vector.tensor_scalar(out=cbh[:], in0=cbh[:], scalar1=0.5, scalar2=None, op0=ALU.mult), reads=[r_cbh], writes=[r_cbh])
    prm["cbh"] = (cbh, r_cbh)
    wdt, r_wdt = k.sb("p_wdt", [128, 16, 64], BF16)
    k.op("pool", lambda: nc.gpsimd.dma_start(out=wdt[:], in_=I["w_in"][:, 10240:10304].rearrange("(kc p) f -> p kc f", p=128)),
         writes=[r_wdt], dma=True)
    prm["wdt"] = (wdt, r_wdt)
    prm["hal"] = k.sb("p_hal", [128, 48, 3], F32)
    sin_off = k.bump
    prm["S_in"] = k.sb("p_Sin", [128, 8, 512], BF16)
    sin_end = k.bump
    BIG = (k.bump + 63) // 64 * 64
    print("fused: perm end", BIG - SB_BASE, "big bytes", SB_TOP - BIG)
    assert SB_TOP - BIG >= 128 * KB

    for g in range(8):
        k.wdeclare(wseq_ssd_group(prm, g, False))
    for g in range(8):
        k.wdeclare(wseq_ssd_group(prm, g, True))
    k.wdeclare(wseq_outproj(prm))
    k.wdeclare(wseq_moe(prm, 0))
    k.wdeclare(wseq_kv(I))
    k.wdeclare(wseq_sq(I["w_q"]))
    k.wdeclare(wseq_sq(I["w_o"]))
    k.wdeclare(wseq_moe(prm, 1))

    k.begin_phase("nA", BIG)
    rmsnorm(k, src_staged(k, I["xT_prev"]), mixg[0][0], mixg[0][1], hnT, r_hn)
    k.begin_phase("sA", BIG)
    P = ssd_prep(k, hnT, r_hn, prm)
    for g in range(8):
        ssd_group(k, g, "A", hnT, r_hn, P, prm)
    k.begin_phase("nB", BIG)
    rmsnorm(k, src_staged(k, I["xT_own"]), mixg[0][0], mixg[0][1], hnT, r_hn)
    k.begin_phase("sB", BIG)
    ynT_all, r_yn = k.sb("ynT_all", [128, 32, NT], BF16)
    P = ssd_prep(k, hnT, r_hn, prm)
    for g in range(8):
        ssd_group(k, g, "B", hnT, r_hn, P, prm, ynT_all, r_yn)
    k.begin_phase("op", BIG + 64 * KB)
    hT, r_h = k.sb("hT", [128, 16, NT], F32)
    k.op("sp", lambda: nc.sync.dma_start(out=hT[:], in_=I["xT_own"].rearrange("(kc p) t -> p kc t", p=128)), writes=[r_h], dma=True)
    out_proj(k, hT, r_h, ynT_all, r_yn, prm)
    k.begin_phase("moe0", BIG, BIG + 64 * KB)
    load_consts(k, dr, ["sel16", "inv128"])
    moe(k, hT, r_h, hnT, r_hn, prm, 0)

    k.begin_phase("kv", BIG, BIG + 64 * KB)
    r_kloc = k.p.reg("kT_loc"); r_vloc = k.p.reg("v_loc"); r_kp = k.p.reg("kT_prev"); r_vp = k.p.reg("v_prev")
    r_xloc = k.p.reg("xloc"); r_xg = k.p.reg("xg")
    rmsnorm(k, src_resident(hT, r_h), kvg[0], kvg[1], hnT, r_hn)
    seq = wseq_kv(I)
    for cb in range(8):
        (wk, r_wk), = k.wget(seq[cb])
        st, r_st = k.sb(f"kv_st{cb % 2}", [128, 2, NT], BF16)
        for c2 in range(2):
            for th in range(2):
                ts = slice(th * 512, (th + 1) * 512)
                b = k.bank(); bt, rb = k.banks[b]
                for kc in range(NKC):
                    k.op("pe", (lambda kc=kc, ts=ts, bt=bt, wk=wk, c2=c2: nc.tensor.matmul(
                        bt[:], lhsT=wk[:, kc, c2 * 128:(c2 + 1) * 128], rhs=hnT[:, kc, ts],
                        start=(kc == 0), stop=(kc == NKC - 1))), reads=[r_wk, r_hn], writes=[rb])
                k.op("act", (lambda bt=bt, st=st, c2=c2, ts=ts: nc.scalar.copy(out=st[:, c2, ts], in_=bt[:])), reads=[rb], writes=[r_st])
                k.free(b)
        k.op("sp", (lambda st=st, cb=cb: nc.sync.dma_start(
            out=kT_loc[cb * 256:(cb + 1) * 256, :].rearrange("(c p) t -> p c t", p=128), in_=st[:])), reads=[r_st], writes=[r_kloc], dma=True)
    for cb in range(8):
        (wv, r_wv), = k.wget(seq[8 + cb])
        st, r_st = k.sb(f"kv_st{cb % 2}", [128, 2, NT], BF16)
        stv = st[:].rearrange("p c t -> p (c t)").rearrange("p (t f) -> p t f", f=256)
        for t2 in range(4):
            b = k.bank(); bt, rb = k.banks[b]
            for ti in range(2):
                tb = t2 * 2 + ti
                for kc in range(NKC):
                    k.op("pe", (lambda kc=kc, tb=tb, ti=ti, bt=bt, wv=wv: nc.tensor.matmul(
                        bt[:, ti * 256:(ti + 1) * 256], lhsT=hnT[:, kc, tb * 128:(tb + 1) * 128], rhs=wv[:, kc, :],
                        start=(kc == 0), stop=(kc == NKC - 1))), reads=[r_wv, r_hn], writes=[rb])
            k.op("act", (lambda bt=bt, stv=stv, t2=t2: nc.scalar.copy(
                out=stv[:, 2 * t2:2 * t2 + 2, :], in_=bt[:].rearrange("p (t f) -> p t f", f=256))), reads=[rb], writes=[r_st])
            k.free(b)
        k.op("sp", (lambda stv=stv, cb=cb: nc.sync.dma_start(
            out=v_loc[:, cb * 256:(cb + 1) * 256].rearrange("(t p) f -> p t f", p=128), in_=stv)), reads=[r_st], writes=[r_vloc], dma=True)

    k.begin_phase("q", BIG, BIG + 64 * KB)
    rg = [[0, 1], [2, 3], [4, 5], [6, 7]]
    xl_v = xloc.rearrange("(t a) c -> t (a c)", a=4)
    xg_v = xg[0:2048, :].rearrange("(t a) c -> t (a c)", a=4)
    rounds = [(kT_loc[:, 0:512], xloc, xg[0:2048, :], kT_prev[:, 0:512], r_kloc, r_kp),
              (kT_loc[:, 512:1024], xloc, xg[0:2048, :], kT_prev[:, 512:1024], r_kloc, r_kp),
              (v_loc[0:512, :], xl_v, xg_v, v_prev[0:512, :], r_vloc, r_vp),
              (v_loc[512:1024, :], xl_v, xg_v, v_prev[512:1024, :], r_vloc, r_vp)]
    for (src, xin, xout, dst, r_src, r_dst) in rounds:
        k.op("sp", (lambda src=src, xin=xin: nc.sync.dma_start(out=xin, in_=src)), reads=[r_src], writes=[r_xloc], dma=True)
        k.op("pool", lambda: nc.gpsimd.collective_compute("AllGather", op=ALU.bypass, replica_groups=rg, ins=[xloc], outs=[xg]),
             reads=[r_xloc], writes=[r_xg])
        k.op("sp", (lambda xout=xout, dst=dst: nc.sync.dma_start(out=dst, in_=xout)), reads=[r_xg], writes=[r_dst], dma=True)
    qT, r_q = k.sb("qT", [128, 16, NT], BF16)
    rmsnorm(k, src_resident(hT, r_h), mixg[1][0], mixg[1][1], hnT, r_hn)
    for cb, pieces in enumerate(wseq_sq(I["w_q"])):
        (wq, r_wq), = k.wget(pieces)
        for c2 in range(2):
            hd = cb * 2 + c2
            for th in range(2):
                ts = slice(th * 512, (th + 1) * 512)
                b = k.bank(); bt, rb = k.banks[b]
                for kc in range(NKC):
                    k.op("pe", (lambda kc=kc, ts=ts, bt=bt, wq=wq, c2=c2: nc.tensor.matmul(
                        bt[:], lhsT=wq[:, kc, c2 * 128:(c2 + 1) * 128], rhs=hnT[:, kc, ts], start=(kc == 0), stop=(kc == NKC - 1))),
                        reads=[r_wq, r_hn], writes=[rb])
                k.op("act", (lambda bt=bt, hd=hd, ts=ts: nc.scalar.activation(out=qT[:, hd, ts], in_=bt[:], func=AF.Copy, scale=128 ** -0.5)),
                     reads=[rb], writes=[r_q])
                k.free(b)

    k.begin_phase("attn", BIG + 32 * KB, BIG + 64 * KB)
    sets = [dict(), dict()]
    sets[0]["es"] = k.sb("at_e0", [128, 2048], F32)
    sets[0]["cs"] = k.sb("at_cs0", [128, 2048], F32)
    sets[0]["at"] = k.sb("at_attn0", [128, 2048], BF16)
    sets[0]["atT"] = k.sb("at_attnT0", [128, 16, 128], BF16)
    kbuf = [k.sb(f"at_k{i}", [128, 2048], BF16) for i in range(2)]
    k.bump = hn_off
    k.limit = hn_end
    sets[1]["es"] = k.sb("at_e1", [128, 2048], F32)
    sets[1]["cs"] = k.sb("at_cs1", [128, 2048], F32)
    sets[1]["at"] = k.sb("at_attn1", [128, 2048], BF16)
    sets[1]["atT"] = k.sb("at_attnT1", [128, 16, 128], BF16)
    vbuf = [k.sb(f"at_v{i}", [128, 16, 128], BF16) for i in range(2)]
    k.bump = sin_off
    k.limit = sin_end
    load_consts(k, dr, ["mask_lt"])
    sets[0]["negT"] = k.sb("at_negT0", [128, 1], F32)
    sets[1]["negT"] = k.sb("at_negT1", [128, 1], F32)
    onec, r_onec = k.c["one_col"]
    identb, r_identb = k.c["ident_bf"]
    mlt, r_mlt = k.c["mask_lt"]

    def load_kv(hd):
        kt, r_kt = kbuf[hd % 2]
        vt, r_vt = vbuf[hd % 2]
        k.op("sp", (lambda: nc.sync.dma_start(out=kt[:, 0:NT], in_=kT_prev[hd * 128:(hd + 1) * 128, :])), writes=[r_kt], dma=True)
        k.op("sp", (lambda: nc.sync.dma_start(out=kt[:, NT:2 * NT], in_=kT_loc[hd * 128:(hd + 1) * 128, :])), writes=[r_kt], dma=True)
        k.op("sp", (lambda: nc.sync.dma_start(
            out=vt[:, 0:8, :], in_=v_prev[:, hd * 128:(hd + 1) * 128].rearrange("(blk p) d -> p blk d", p=128))), writes=[r_vt], dma=True)
        k.op("sp", (lambda: nc.sync.dma_start(
            out=vt[:, 8:16, :], in_=v_loc[:, hd * 128:(hd + 1) * 128].rearrange("(blk p) d -> p blk d", p=128))), writes=[r_vt], dma=True)
        k.op("pool", (lambda: nc.gpsimd.tensor_scalar(out=vt[:, 0:8, :], in0=vt[:, 0:8, :], scalar1=flag[:], scalar2=None, op0=ALU.mult)),
             reads=[r_vt, r_flag], writes=[r_vt])

    def stage1(n, hd, i):
        S = sets[n % 2]
        es, r_es = S["es"]; cs, r_cs = S["cs"]; ngT, r_ngT = S["negT"]
        kt, r_kt = kbuf[hd % 2]
        qs = slice(i * 128, (i + 1) * 128)
        nblk = 9 + i
        ncol = nblk * 128
        nbank = (ncol + 511) // 512
        zb = []
        for j in range(nbank):
            b = k.bank(); bt, rb = k.banks[b]
            w = min(512, ncol - j * 512)
            k.op("pe", (lambda bt=bt, j=j, w=w: nc.tensor.matmul(
                bt[:, 0:w], lhsT=qT[:, hd, qs], rhs=kt[:, j * 512: j * 512 + w], start=True, stop=True)),
                reads=[r_q, r_kt], writes=[rb])
            zb.append((b, bt, rb, w))
        for j, (b, bt, rb, w) in enumerate(zb):
            k.op("act", (lambda bt=bt, j=j, w=w: nc.scalar.activation(out=cs[:, j * 512: j * 512 + w], in_=bt[:, 0:w], func=AF.Exp)),
                 reads=[rb], writes=[r_cs])
        k.op("act", (lambda: nc.scalar.activation(out=es[:, 0:ncol], in_=cs[:, 0:ncol], func=AF.Ln, bias=onec[:], scale=1.0)),
             reads=[r_cs, r_onec], writes=[r_es])
        k.op("dve", (lambda: nc.vector.tensor_tensor(out=es[:, ncol - 128:ncol], in0=es[:, ncol - 128:ncol], in1=mlt[:], op=ALU.mult)),
             reads=[r_es, r_mlt], writes=[r_es])
        k.op("dve", (lambda: nc.vector.tensor_tensor_scan(out=cs[:, 0:ncol], data0=onec[:].to_broadcast([128, ncol]), data1=es[:, 0:ncol],
                                                          initial=0.0, op0=ALU.mult, op1=ALU.add)),
             reads=[r_es, r_onec], writes=[r_cs])
        k.op("dve", (lambda: nc.vector.tensor_scalar(out=ngT[:], in0=cs[:, ncol - 1:ncol], scalar1=-1.0, scalar2=None, op0=ALU.mult)),
             reads=[r_cs], writes=[r_ngT])
        for j, (b, bt, rb, w) in enumerate(zb):
            lo = j * 512
            if j == 0:
                k.op("dve", (lambda bt=bt: nc.vector.tensor_copy(out=es[:, 0:1], in_=bt[:, 0:1])), reads=[rb], writes=[r_es])
                k.op("dve", (lambda bt=bt, w=w: nc.vector.tensor_tensor(out=es[:, 1:w], in0=cs[:, 0:w - 1], in1=bt[:, 1:w], op=ALU.add)),
                     reads=[r_cs, rb], writes=[r_es])
            else:
                k.op("dve", (lambda bt=bt, w=w, lo=lo: nc.vector.tensor_tensor(out=es[:, lo:lo + w], in0=cs[:, lo - 1:lo + w - 1], in1=bt[:, 0:w], op=ALU.add)),
                     reads=[r_cs, rb], writes=[r_es])
            k.free(b)

    def stage2(n, hd, i):
        S = sets[n % 2]
        es, r_es = S["es"]; at, r_at = S["at"]; atT, r_atT = S["atT"]; ngT, r_ngT = S["negT"]
        nblk = 9 + i
        ncol = nblk * 128
        k.op("act", (lambda: nc.scalar.activation(out=at[:, 0:ncol], in_=es[:, 0:ncol], func=AF.Exp, bias=ngT[:], scale=1.0)),
             reads=[r_es, r_ngT], writes=[r_at])
        k.op("dve", (lambda: nc.vector.tensor_tensor(out=at[:, ncol - 128:ncol], in0=at[:, ncol - 128:ncol], in1=mlt[:], op=ALU.mult)),
             reads=[r_at, r_mlt], writes=[r_at])
        for j4 in range((nblk + 3) // 4):
            b = k.bank(); bt, rb = k.banks[b]
            nb = min(4, nblk - j4 * 4)
            for jj in range(nb):
                blk = j4 * 4 + jj
                k.op("pe", (lambda bt=bt, jj=jj, blk=blk: nc.tensor.matmul(bt[:, jj * 128:(jj + 1) * 128], lhsT=at[:, blk * 128:(blk + 1) * 128],
                                                                           rhs=identb[:], start=True, stop=True)),
                     reads=[r_at, r_identb], writes=[rb])
            eng = "act" if j4 % 2 == 0 else "dve"
            if eng == "act":
                k.op("act", (lambda bt=bt, j4=j4, nb=nb: nc.scalar.copy(out=atT[:, j4 * 4:j4 * 4 + nb, :],
                                                                       in_=bt[:, 0:nb * 128].rearrange("p (b q) -> p b q", b=nb))),
                     reads=[rb], writes=[r_atT])
            else:
                k.op("dve", (lambda bt=bt, j4=j4, nb=nb: nc.vector.tensor_copy(out=atT[:, j4 * 4:j4 * 4 + nb, :],
                                                                              in_=bt[:, 0:nb * 128].rearrange("p (b q) -> p b q", b=nb))),
                     reads=[rb], writes=[r_atT])
            k.free(b)

    def stage3(n, hd, i):
        S = sets[n % 2]
        atT, r_atT = S["atT"]
        vt, r_vt = vbuf[hd % 2]
        qs = slice(i * 128, (i + 1) * 128)
        nblk = 9 + i
        b = k.bank(); bt, rb = k.banks[b]
        for blk in range(nblk):
            k.op("pe", (lambda bt=bt, blk=blk: nc.tensor.matmul(
                bt[:, 0:128], lhsT=vt[:, blk, :], rhs=atT[:, blk, :], start=(blk == 0), stop=(blk == nblk - 1))),
                reads=[r_vt, r_atT], writes=[rb])
        k.op("act", (lambda bt=bt: nc.scalar.copy(out=qT[:, hd, qs], in_=bt[:, 0:128])), reads=[rb], writes=[r_q])
        k.free(b)

    its = [(hd, i) for hd in range(16) for i in range(8)]
    load_kv(0)
    stage1(0, *its[0])
    for n, (hd, i) in enumerate(its):
        if i == 2 and hd + 1 < 16:
            load_kv(hd + 1)
        if n + 1 < len(its):
            stage1(n + 1, *its[n + 1])
        stage2(n, hd, i)
        stage3(n, hd, i)

    k.begin_phase("wo", BIG + 32 * KB, BIG + 64 * KB)
    for cb, pieces in enumerate(wseq_sq(I["w_o"])):
        (wo, r_wo), = k.wget(pieces)
        for c2 in range(2):
            dc = cb * 2 + c2
            for th in range(2):
                ts = slice(th * 512, (th + 1) * 512)
                b = k.bank(); bt, rb = k.banks[b]
                for kc in range(NKC):
                    k.op("pe", (lambda kc=kc, ts=ts, bt=bt, wo=wo, c2=c2: nc.tensor.matmul(
                        bt[:], lhsT=wo[:, kc, c2 * 128:(c2 + 1) * 128], rhs=qT[:, kc, ts], start=(kc == 0), stop=(kc == NKC - 1))),
                        reads=[r_wo, r_q], writes=[rb])
                k.op("dve", (lambda dc=dc, ts=ts, bt=bt: nc.vector.tensor_tensor(out=hT[:, dc, ts], in0=hT[:, dc, ts], in1=bt[:], op=ALU.add)),
                     reads=[rb, r_h], writes=[r_h])
                k.free(b)
    k.begin_phase("moe1", BIG, BIG + 64 * KB)
    load_consts(k, dr, ["sel16", "inv128"])
    moe(k, hT, r_h, hnT, r_hn, prm, 1)
    k.begin_phase("fin", BIG, BIG + 64 * KB)
    outT, r_o = k.sb("outT", [128, 16, NT // 2], F32)
    oo = []
    ones, r_ones = k.c["ones_f"]
    epsc, r_eps = k.c["eps_col"]
    for th in range(2):
        ts = slice(th * 512, (th + 1) * 512)
        b = k.bank(); bt, rb = k.banks[b]
        for kc in range(NKC):
            sq, rsq = k.sb(f"rn_sq{kc % 2}", [128, 512], F32)
            k.op("act", (lambda sq=sq, kc=kc, ts=ts: nc.scalar.activation(out=sq[:], in_=hT[:, kc, ts], func=AF.Square)), reads=[r_h], writes=[rsq])
            k.op("pe", (lambda sq=sq, kc=kc, bt=bt: nc.tensor.matmul(bt[:], lhsT=ones[:], rhs=sq[:], start=(kc == 0), stop=(kc == NKC - 1))),
                 reads=[rsq, r_ones], writes=[rb])
        rt, rr = k.sb("rn_rstd", [128, 512], F32)
        k.op("act", (lambda bt=bt, rt=rt: nc.scalar.activation(out=rt[:], in_=bt[:], func=AF.Ln, bias=epsc[:], scale=1.0 / D)), reads=[rb, r_eps], writes=[rr])
        k.free(b)
        k.op("act", (lambda rt=rt: nc.scalar.activation(out=rt[:], in_=rt[:], func=AF.Exp, scale=-0.5)), reads=[rr], writes=[rr])
        for kc in range(NKC):
            k.op("dve", (lambda kc=kc, ts=ts, rt=rt: nc.vector.scalar_tensor_tensor(
                out=outT[:, kc, :], in0=hT[:, kc, ts], scalar=fing[0][:, kc:kc + 1], in1=rt[:], op0=ALU.mult, op1=ALU.mult)),
                reads=[r_h, fing[1], rr], writes=[r_o])
        oo.append(k.op("sp", (lambda ts=ts: nc.sync.dma_start(out=out_d[:, ts].rearrange("(kc p) t -> p kc t", p=128), in_=outT[:])),
                       reads=[r_o], dma=True))
    assert k.widx == len(k.wlist), (k.widx, len(k.wlist))
    cnt = k.p.emit()
    print("fused: ops", len(k.p.ops), "wtiles", len(k.wlist), "signals", cnt)
    k.p.final_wait("sp", oo)
    return nc


def host_inputs_fused(inp, core):
    m = host_inputs_l1(inp, core)
    m["mix_g1"] = fm(inp["mix_norm"][1], 16)
    m["ffn_g1"] = fm(inp["ffn_norm"][1], 16)
    m["final_g"] = fm(inp["final_norm"], 16)
    m["w_q"] = np.asarray(inp["sb_w_q"][0], np.float32)
    m["w_o"] = np.asarray(inp["sb_w_out"][0], np.float32)
    m["w_router1"] = np.ascontiguousarray(np.concatenate(
        [np.asarray(inp["moe_w_coarse"][1], np.float32), np.asarray(inp["moe_w_fine"][1], np.float32).reshape(D, 16)], axis=1))
    m["b_router1"] = rep(np.concatenate([np.asarray(inp["moe_b_coarse"][1], np.float32),
                                         np.asarray(inp["moe_b_fine"][1], np.float32).reshape(16)]))
    m["w_gate1"] = np.asarray(inp["moe_w_gate"][1], np.float32)
    m["w_up1"] = np.asarray(inp["moe_w_up"][1], np.float32)
    m["w_down1"] = np.asarray(inp["moe_w_down"][1], np.float32)
    return m


_PROGS = {}


def kernel(**inputs):
    inp = {k_: np.asarray(v) for k_, v in inputs.items()}
    ncores = 8
    names = set(CONST_SHAPES) | {n for n, _ in F_INPUTS}
    if "fused" not in _PROGS:
        _PROGS["fused"] = build_fused()
    nc = _PROGS["fused"]
    ims = []
    for c in range(ncores):
        m = host_inputs_fused(inp, c)
        ims.append({n: v for n, v in m.items() if n in names})
    res = run_bass_kernel_spmd(nc, ims, core_ids=list(range(ncores)))
    out = np.zeros((4, 2048, 2048), np.float32)
    for c in range(ncores):
        b, half = c // 2, c % 2
        out[b, half * NT:(half + 1) * NT, :] = np.asarray(res.results[c]["outT"], np.float32).T
    return out
```

```python
from concourse.bass_utils import run_bass_kernel_spmd
import numpy as np
from contextlib import ExitStack
import concourse.bass as bass
import concourse.mybir as mybir

F32 = mybir.dt.float32
BF16 = mybir.dt.bfloat16
AF = mybir.ActivationFunctionType
ALU = mybir.AluOpType
AX = mybir.AxisListType


class Reg:
    __slots__ = ("name", "w", "r")

    def __init__(self, name):
        self.name = name
        self.w = None
        self.r = []


class Prog:
    ENGS = ("pe", "act", "dve", "pool", "sp")

    def __init__(self, nc, n_dma_sems=20):
        self.nc = nc
        self.es = ExitStack()
        self.ops = []
        self.eng_obj = {"pe": nc.tensor, "act": nc.scalar, "dve": nc.vector,
                        "pool": nc.gpsimd, "sp": nc.sync}
        self.n_dma_sems = n_dma_sems
        self._uid = 0
        self.allregs = []
        self.bar_from = 0
        self.tag = None
        self.scopes = False

    def sb(self, shape, dt, name=None):
        self._uid += 1
        return self.es.enter_context(self.nc.sbuf_tensor(name or f"sb{self._uid}", list(shape), dt))

    def ps(self, shape, dt, name=None):
        self._uid += 1
        return self.es.enter_context(self.nc.psum_tensor(name or f"ps{self._uid}", list(shape), dt))

    def reg(self, name=None):
        self._uid += 1
        r = Reg(name or f"r{self._uid}")
        self.allregs.append(r)
        return r

    def barrier(self):
        last = {}
        dmas = []
        for oid in range(self.bar_from, len(self.ops)):
            o = self.ops[oid]
            if o["dma"]:
                dmas.append(oid)
            else:
                last[o["eng"]] = oid
        deps = set(last.values()) | set(dmas)
        for e in self.ENGS:
            self.ops.append(dict(eng=e, fn=(lambda: None), deps=set(deps), dma=False, tag=self.tag))
        self.bar_from = len(self.ops)
        for r in self.allregs:
            r.w = None
            r.r = []

    def op(self, eng, fn, reads=(), writes=(), dma=False, nosync_same_pe=True):
        oid = len(self.ops)
        deps = set()
        for r in reads:
            if r.w is not None:
                deps.add(r.w)
        for w in writes:
            if w.w is not None:
                deps.add(w.w)
            deps.update(w.r)
        for r in reads:
            r.r.append(oid)
        for w in writes:
            w.w = oid
            w.r = []
        deps.discard(oid)
        self.ops.append(dict(eng=eng, fn=fn, deps=deps, dma=dma, tag=self.tag))
        return oid

    def emit(self):
        nc = self.nc
        ops = self.ops
        need = [False] * len(ops)
        for o in ops:
            for d in list(o["deps"]):
                od = ops[d]
                if od["eng"] == "pe" and o["eng"] == "pe" and not od["dma"] and not o["dma"]:
                    o["deps"].discard(d)
                    continue
                need[d] = True
        esem = {e: self.es.enter_context(nc.semaphore(f"s_{e}")) for e in self.ENGS}
        dsem = {}
        for q in ("sp", "pool"):
            dsem[q] = [self.es.enter_context(nc.semaphore(f"d_{q}{i}")) for i in range(self.n_dma_sems)]
        ecount = {e: 0 for e in self.ENGS}
        dcount = {q: [0] * self.n_dma_sems for q in dsem}
        drr = {q: 0 for q in dsem}
        sig = [None] * len(ops)
        waited = {}
        cur_tag = None
        for oid, o in enumerate(ops):
            eng = o["eng"]
            E = self.eng_obj[eng]
            if self.scopes and o.get("tag") != cur_tag:
                if cur_tag is not None:
                    nc.leave_named_scope(cur_tag, cur_sid, False)
                cur_tag = o.get("tag")
                if cur_tag is not None:
                    cur_sid, _ = nc.enter_named_scope(cur_tag, False)
            wl = {}
            for d in o["deps"]:
                s, v = sig[d]
                k = id(s)
                if k not in wl or wl[k][1] < v:
                    wl[k] = (s, v)
            if o["dma"]:
                slot = drr[eng]
                drr[eng] = (slot + 1) % self.n_dma_sems
                ds = dsem[eng][slot]
                prev = dcount[eng][slot]
                if prev > 0:
                    k = id(ds)
                    if k not in wl or wl[k][1] < prev:
                        wl[k] = (ds, prev)
            for k, (ws, wv) in wl.items():
                if waited.get((eng, k), 0) >= wv:
                    continue
                waited[(eng, k)] = wv
                E.wait_ge(ws, wv)
            inst = o["fn"]()
            if inst is None:
                continue
            if o["dma"]:
                dcount[eng][slot] = prev + 16
                inst.then_inc(ds, 16)
                sig[oid] = (ds, prev + 16)
            elif need[oid]:
                ecount[eng] += 1
                inst.then_inc(esem[eng], 1)
                sig[oid] = (esem[eng], ecount[eng])
        if self.scopes and cur_tag is not None:
            nc.leave_named_scope(cur_tag, cur_sid, False)
        self.sig = sig
        self.esem = esem
        return ecount

    def final_wait(self, eng, oids):
        E = self.eng_obj[eng]
        for oid in oids:
            s, v = self.sig[oid]
            E.wait_ge(s, v)


import numpy as np

D = 2048
NT = 1024
NKC = 16
EPS = 1e-5
NE = 16
DE = 512


SB_BASE = 16512
SB_TOP = 229344


class K:
    def __init__(self, nc, nw=4, look=2):
        self.nc = nc
        self.p = Prog(nc)
        self.cache = {}
        self.NW = nw
        self.LOOK = look
        self.wlist = []
        self.widx = 0
        self.wloaded = 0
        self.bank_live = [False] * 8
        self.bank_rr = 0
        self.bump = SB_BASE
        self.limit = SB_TOP
        self.phase = "perm"
        self.uid = 0

    def sb(self, name, shape, dt):
        key = (self.phase, name)
        if key not in self.cache:
            n = 1
            for d in shape[1:]:
                n *= d
            nbytes = n * (4 if dt == F32 else 2)
            nbytes = (nbytes + 31) // 32 * 32
            off = self.bump
            assert off + nbytes <= self.limit, f"SBUF overflow allocating {name} in phase {self.phase}: {off}+{nbytes} > {self.limit}"
            self.bump = off + nbytes
            self.uid += 1
            t = self.nc.alloc_sbuf_tensor_at(f"{self.phase}_{name}_{self.uid}", list(shape), dt, offset=off)
            self.names = getattr(self, "names", {})
            self.names[key] = t.name
            self.cache[key] = (t, self.p.reg(name))
        return self.cache[key]

    def begin_phase(self, name, start, limit=SB_TOP):
        self.p.barrier()
        assert not any(self.bank_live)
        self.p.tag = name
        self.phase = name
        self.bump = start
        self.limit = limit

    def setup(self):
        p = self.p
        self.banks = [(p.ps([128, 512], F32, name=f"bank{i}"), p.reg(f"bank{i}")) for i in range(8)]
        self.bankregs = {id(r) for _, r in self.banks}
        self.wring = []
        for i in range(self.NW):
            t, _ = self.sb(f"wr{i}", [128, 4096], BF16)
            r1 = p.reg(f"wr{i}")
            self.wring.append((t, [r1, r1]))

    def op(self, eng, fn, reads=(), writes=(), dma=False):
        if eng != "pe":
            bs = self.bankregs
            extra = [r for r in reads if id(r) in bs]
            if extra:
                writes = list(writes) + [r for r in extra if r not in writes]
        return self.p.op(eng, fn, reads=reads, writes=writes, dma=dma)

    def bank(self):
        for i in range(8):
            j = (self.bank_rr + i) % 8
            if not self.bank_live[j]:
                self.bank_live[j] = True
                self.bank_rr = (j + 1) % 8
                return j
        raise RuntimeError("out of PSUM banks")

    def free(self, j):
        assert self.bank_live[j]
        self.bank_live[j] = False

    def wdeclare(self, seq):
        self.wlist.extend(seq)

    def wget(self, pieces):
        key = [(str(pc[0]), pc[1], pc[2]) for pc in pieces]
        i = self.widx
        self.widx += 1
        assert [(str(pc[0]), pc[1], pc[2]) for pc in self.wlist[i]] == key, f"weight order mismatch at {i}"
        hi = min(len(self.wlist), i + 1 + self.LOOK)
        while self.wloaded < hi:
            self._wload(self.wloaded)
            self.wloaded += 1
        t, regs = self.wring[i % self.NW]
        return self._wviews(t, regs, pieces)

    def _wviews(self, t, regs, pieces):
        out = []
        off = 0
        for pi, (src, kc, nco) in enumerate(pieces):
            v = t[:, off:off + kc * nco].rearrange("p (kc f) -> p kc f", kc=kc)
            out.append((v, regs[pi]))
            off += kc * nco
        assert off <= 4096
        return out

    def _wload(self, j):
        nc = self.nc
        t, regs = self.wring[j % self.NW]
        views = self._wviews(t, regs, self.wlist[j])
        for (v, r), (src, kc, nco) in zip(views, self.wlist[j]):
            self.p.op("pool", (lambda v=v, src=src: nc.gpsimd.dma_start(
                out=v, in_=src.rearrange("(kc p) f -> p kc f", p=128))), writes=[r], dma=True)


def load_consts(k, dr, names):
    nc = k.nc
    c = getattr(k, "c", {})
    for name in names:
        shape = CONST_SHAPES[name]
        dt = BF16 if name == "ident_bf" else F32
        t, r = k.sb("c_" + name, shape, dt)
        if dt == BF16:
            k.op("pool", (lambda t=t, name=name: nc.gpsimd.dma_start(out=t[:], in_=dr[name])), writes=[r], dma=True)
        else:
            k.op("sp", (lambda t=t, name=name: nc.sync.dma_start(out=t[:], in_=dr[name])), writes=[r], dma=True)
        c[name] = (t, r)
    for nm, val in (("eps_col", EPS), ("one_col", 1.0), ("eps4_col", 4 * EPS)):
        if nm not in c:
            t, r = k.sb("c_" + nm, [128, 1], F32)
            k.op("pool", (lambda t=t, val=val: nc.gpsimd.memset(t[:], val)), writes=[r])
            c[nm] = (t, r)
    k.c = c


def host_consts():
    i = np.arange(128)
    h = {}
    h["ident_bf"] = np.eye(128, dtype=np.float32)
    h["ident_f"] = np.eye(128, dtype=np.float32)
    h["ones_f"] = np.ones((128, 128), np.float32)
    h["tri_incl"] = (i[:, None] <= i[None, :]).astype(np.float32)
    h["ustrict"] = (i[:, None] > i[None, :]).astype(np.float32)
    h["mask_le"] = (i[:, None] <= i[None, :]).astype(np.float32)
    h["mask_lt"] = (i[None, :] < i[:, None]).astype(np.float32)
    sel = np.zeros((16, 16, 128), np.float32)
    for e in range(16):
        sel[e, e, :] = 1.0
    h["sel16"] = sel.reshape(16, 16 * 128)
    h["inv128"] = np.full((128, 1), 1.0 / 128, np.float32)
    h["ones_row"] = np.ones((128, 2048), np.float32)
    return h


CONST_SHAPES = {"ident_bf": [128, 128], "ident_f": [128, 128], "ones_f": [128, 128], "tri_incl": [128, 128],
                "ustrict": [128, 128], "mask_le": [128, 128], "mask_lt": [128, 128], "sel16": [16, 2048],
                "inv128": [128, 1], "ones_row": [128, 2048]}


def rmsnorm(k, src, g, r_g, outT, r_out, ntok=NT, want_rstd=None):
    nc = k.nc
    ones, r_ones = k.c["ones_f"]
    epsc, r_eps = k.c["eps_col"]
    for th in range(ntok // 512):
        ts = slice(th * 512, (th + 1) * 512)
        view, r_h = src(th)
        b = k.bank()
        bt, rb = k.banks[b]
        for kc in range(NKC):
            sq, rsq = k.sb(f"rn_sq{kc % 2}", [128, 512], F32)
            k.op("act", (lambda sq=sq, kc=kc, view=view: nc.scalar.activation(out=sq[:], in_=view(kc), func=AF.Square)),
                 reads=[r_h], writes=[rsq])
            k.op("pe", (lambda sq=sq, kc=kc, bt=bt: nc.tensor.matmul(bt[:], lhsT=ones[:], rhs=sq[:],
                                                                     start=(kc == 0), stop=(kc == NKC - 1))),
                 reads=[rsq, r_ones], writes=[rb])
        if want_rstd is not None:
            rt, rr = want_rstd
            rview = rt[:, ts]
        else:
            rt, rr = k.sb("rn_rstd", [128, 512], F32)
            rview = rt[:]
        k.op("act", (lambda bt=bt, rview=rview: nc.scalar.activation(out=rview, in_=bt[:], func=AF.Ln, bias=epsc[:], scale=1.0 / D)),
             reads=[rb, r_eps], writes=[rr])
        k.free(b)
        k.op("act", (lambda rview=rview: nc.scalar.activation(out=rview, in_=rview, func=AF.Exp, scale=-0.5)),
             reads=[rr], writes=[rr])
        for kc in range(NKC):
            k.op("dve", (lambda kc=kc, ts=ts, rview=rview, view=view: nc.vector.scalar_tensor_tensor(
                out=outT[:, kc, ts], in0=view(kc), scalar=g[:, kc:kc + 1], in1=rview,
                op0=ALU.mult, op1=ALU.mult)), reads=[r_h, r_g, rr], writes=[r_out])


def src_resident(hT, r_h):
    return lambda th: ((lambda kc, th=th: hT[:, kc, th * 512:(th + 1) * 512]), r_h)


def src_staged(k, dram_xT):
    nc = k.nc

    def f(th):
        st, r_st = k.sb("rn_stage", [128, NKC, 512], F32)
        k.op("sp", (lambda st=st, th=th: nc.sync.dma_start(
            out=st[:], in_=dram_xT[:, th * 512:(th + 1) * 512].rearrange("(kc p) t -> p kc t", p=128))), writes=[r_st], dma=True)
        return (lambda kc, st=st: st[:, kc, :]), r_st
    return f


def wseq_moe(prm, l):
    wg, wu, wd = prm["w_gate"][l], prm["w_up"][l], prm["w_down"][l]
    seq = []
    for e in range(NE):
        for fh in range(2):
            seq.append([(wg[e][:, fh * 256:(fh + 1) * 256], NKC, 256)])
            seq.append([(wu[e][:, fh * 256:(fh + 1) * 256], NKC, 256)])
        for dq in range(2):
            seq.append([(wd[e][:, dq * 1024:(dq + 1) * 1024], 4, 1024)])
    return seq


def moe(k, hT, r_h, hnT, r_hn, prm, l):
    nc = k.nc
    c = k.c
    g, r_g = prm["ffn_g"][l]
    rstd, r_rstd = k.sb("moe_rstd", [128, NT], F32)
    rmsnorm(k, src_resident(hT, r_h), g, r_g, hnT, r_hn, want_rstd=(rstd, r_rstd))

    wr, r_wr = k.sb("moe_wr", [128, NKC, 20], F32)
    k.op("sp", lambda: nc.sync.dma_start(out=wr[:], in_=prm["w_router"][l].rearrange("(kc p) e -> p kc e", p=128)),
         writes=[r_wr], dma=True)
    k.op("dve", lambda: nc.vector.tensor_tensor(out=wr[:], in0=wr[:], in1=g[:].unsqueeze(2).to_broadcast([128, NKC, 20]),
                                               op=ALU.mult), reads=[r_wr, r_g], writes=[r_wr])
    rb_bias, r_bias = prm["b_router"][l]
    gT, r_gT = k.sb("moe_gT", [16, NT], F32)
    inv128, r_inv = c["inv128"]
    identf, r_identf = c["ident_f"]
    for tb in range(NT // 128):
        tsl = slice(tb * 128, (tb + 1) * 128)
        b = k.bank()
        bt, rb = k.banks[b]
        for kc in range(NKC):
            k.op("pe", (lambda kc=kc, tsl=tsl, bt=bt: nc.tensor.matmul(bt[:, 0:20], lhsT=hT[:, kc, tsl], rhs=wr[:, kc, :],
                                                                       start=(kc == 0), stop=(kc == NKC - 1))),
                 reads=[r_h, r_wr], writes=[rb])
        k.op("pe", (lambda tsl=tsl, bt=bt: nc.tensor.matmul(bt[:, 32:33], lhsT=rstd[:, tsl], rhs=inv128[:],
                                                            start=True, stop=True)),
             reads=[r_rstd, r_inv], writes=[rb])
        sm, r_sm = k.sb(f"moe_sm{tb % 2}", [128, 96], F32)
        rs = sm[:, 0:1]
        k.op("act", (lambda bt=bt, rs=rs: nc.scalar.copy(out=rs, in_=bt[:, 32:33])), reads=[rb], writes=[r_sm])
        lg = sm[:, 4:24]
        k.op("dve", (lambda bt=bt, lg=lg, rs=rs: nc.vector.scalar_tensor_tensor(
            out=lg, in0=bt[:, 0:20], scalar=rs, in1=rb_bias[:], op0=ALU.mult, op1=ALU.add)),
            reads=[rb, r_sm, r_bias], writes=[r_sm])
        k.free(b)
        lc = sm[:, 4:8]
        mx = sm[:, 1:2]
        k.op("dve", (lambda lc=lc, mx=mx: nc.vector.reduce_max(out=mx, in_=lc, axis=AX.X)), reads=[r_sm], writes=[r_sm])
        nmx = sm[:, 2:3]
        k.op("dve", (lambda mx=mx, nmx=nmx: nc.vector.tensor_scalar(out=nmx, in0=mx, scalar1=-1.0, scalar2=None, op0=ALU.mult)),
             reads=[r_sm], writes=[r_sm])
        ec = sm[:, 24:28]
        se = sm[:, 3:4]
        k.op("act", (lambda lc=lc, ec=ec, nmx=nmx, se=se: nc.scalar.activation(out=ec, in_=lc, func=AF.Exp, bias=nmx, scale=1.0,
                                                                               accum_out=se)),
             reads=[r_sm], writes=[r_sm])
        oh = sm[:, 28:32]
        k.op("dve", (lambda lc=lc, mx=mx, oh=oh: nc.vector.tensor_scalar(out=oh, in0=lc, scalar1=mx, scalar2=None, op0=ALU.is_ge)),
             reads=[r_sm], writes=[r_sm])
        fa = sm[:, 8:24].rearrange("p (g e) -> p g e", g=4)
        tmp = sm[:, 32:48]
        k.op("dve", (lambda fa=fa, oh=oh, tmp=tmp: nc.vector.tensor_tensor(
            out=tmp.rearrange("p (g e) -> p g e", g=4), in0=fa, in1=oh.unsqueeze(2).to_broadcast([128, 4, 4]), op=ALU.mult)),
            reads=[r_sm], writes=[r_sm])
        fn = sm[:, 48:52]
        k.op("dve", (lambda tmp=tmp, fn=fn: nc.vector.tensor_reduce(out=fn, in_=tmp.rearrange("p (g e) -> p e g", g=4),
                                                                    axis=AX.X, op=ALU.add)),
             reads=[r_sm], writes=[r_sm])
        mf = sm[:, 52:53]
        k.op("dve", (lambda fn=fn, mf=mf: nc.vector.reduce_max(out=mf, in_=fn, axis=AX.X)), reads=[r_sm], writes=[r_sm])
        nmf = sm[:, 53:54]
        k.op("dve", (lambda mf=mf, nmf=nmf: nc.vector.tensor_scalar(out=nmf, in0=mf, scalar1=-1.0, scalar2=None, op0=ALU.mult)),
             reads=[r_sm], writes=[r_sm])
        ef = sm[:, 56:60]
        k.op("act", (lambda fn=fn, ef=ef, nmf=nmf: nc.scalar.activation(out=ef, in_=fn, func=AF.Exp, bias=nmf, scale=1.0)),
             reads=[r_sm], writes=[r_sm])
        m1 = sm[:, 60:64]
        k.op("dve", (lambda fn=fn, mf=mf, m1=m1: nc.vector.tensor_scalar(out=m1, in0=fn, scalar1=mf, scalar2=None, op0=ALU.is_ge)),
             reads=[r_sm], writes=[r_sm])
        ef2 = sm[:, 64:68]
        k.op("dve", (lambda ef=ef, m1=m1, ef2=ef2: nc.vector.tensor_tensor(out=ef2, in0=ef, in1=m1, op=ALU.mult)),
             reads=[r_sm], writes=[r_sm])
        k.op("dve", (lambda ef=ef, ef2=ef2: nc.vector.tensor_tensor(out=ef2, in0=ef, in1=ef2, op=ALU.subtract)),
             reads=[r_sm], writes=[r_sm])
        e2 = sm[:, 54:55]
        k.op("dve", (lambda ef2=ef2, e2=e2: nc.vector.reduce_max(out=e2, in_=ef2, axis=AX.X)), reads=[r_sm], writes=[r_sm])
        m2 = sm[:, 68:72]
        k.op("dve", (lambda ef2=ef2, e2=e2, m2=m2: nc.vector.tensor_scalar(out=m2, in0=ef2, scalar1=e2, scalar2=None, op0=ALU.is_ge)),
             reads=[r_sm], writes=[r_sm])
        tv = sm[:, 72:76]
        k.op("dve", (lambda ef2=ef2, m2=m2, tv=tv: nc.vector.tensor_tensor(out=tv, in0=ef2, in1=m2, op=ALU.mult)),
             reads=[r_sm], writes=[r_sm])
        t1 = sm[:, 76:80]
        k.op("dve", (lambda ef=ef, m1=m1, t1=t1: nc.vector.tensor_tensor(out=t1, in0=ef, in1=m1, op=ALU.mult)),
             reads=[r_sm], writes=[r_sm])
        k.op("dve", (lambda tv=tv, t1=t1: nc.vector.tensor_tensor(out=tv, in0=tv, in1=t1, op=ALU.add)),
             reads=[r_sm], writes=[r_sm])
        den = sm[:, 55:56]
        k.op("dve", (lambda tv=tv, den=den: nc.vector.reduce_sum(out=den, in_=tv, axis=AX.X)), reads=[r_sm], writes=[r_sm])
        k.op("dve", (lambda den=den, se=se: nc.vector.tensor_tensor(out=den, in0=den, in1=se, op=ALU.mult)),
             reads=[r_sm], writes=[r_sm])
        k.op("dve", (lambda den=den: nc.vector.reciprocal(out=den, in_=den)), reads=[r_sm], writes=[r_sm])
        k.op("dve", (lambda tv=tv, den=den: nc.vector.tensor_scalar(out=tv, in0=tv, scalar1=den, scalar2=None, op0=ALU.mult)),
             reads=[r_sm], writes=[r_sm])
        gt = sm[:, 80:96]
        k.op("dve", (lambda gt=gt, oh=oh, tv=tv: nc.vector.tensor_tensor(
            out=gt.rearrange("p (g e) -> p g e", g=4), in0=oh.unsqueeze(2).to_broadcast([128, 4, 4]),
            in1=tv.unsqueeze(1).to_broadcast([128, 4, 4]), op=ALU.mult)), reads=[r_sm], writes=[r_sm])
        b2 = k.bank()
        bt2, rb2 = k.banks[b2]
        k.op("pe", (lambda gt=gt, bt2=bt2: nc.tensor.matmul(bt2[0:16, 0:128], lhsT=gt, rhs=identf[:], start=True, stop=True)),
             reads=[r_sm, r_identf], writes=[rb2])
        k.op("act", (lambda bt2=bt2, tsl=tsl: nc.scalar.copy(out=gT[:, tsl], in_=bt2[0:16, 0:128])), reads=[rb2], writes=[r_gT])
        k.free(b2)

    sel, r_sel = c["sel16"]
    wg, wu, wd = prm["w_gate"][l], prm["w_up"][l], prm["w_down"][l]
    for e in range(NE):
        gb, r_gb = k.sb(f"moe_gb{e % 2}", [128, NT], F32)
        for th in range(2):
            ts = slice(th * 512, (th + 1) * 512)
            b = k.bank()
            bt, rb = k.banks[b]
            k.op("pe", (lambda e=e, ts=ts, bt=bt: nc.tensor.matmul(bt[:], lhsT=sel[:, e * 128:(e + 1) * 128], rhs=gT[:, ts],
                                                                   start=True, stop=True)),
                 reads=[r_sel, r_gT], writes=[rb])
            k.op("act", (lambda gb=gb, ts=ts, bt=bt: nc.scalar.copy(out=gb[:, ts], in_=bt[:])), reads=[rb], writes=[r_gb])
            k.free(b)
        hid, r_hid = k.sb(f"moe_hid{e % 2}", [128, 4, NT], BF16)
        for fh in range(2):
            (wgt, r_wgt), = k.wget([(wg[e][:, fh * 256:(fh + 1) * 256], NKC, 256)])
            (wut, r_wut), = k.wget([(wu[e][:, fh * 256:(fh + 1) * 256], NKC, 256)])
            for f2 in range(2):
                fc = fh * 2 + f2
                for th in range(2):
                    ts = slice(th * 512, (th + 1) * 512)
                    bg = k.bank(); bu = k.bank()
                    btg, rbg = k.banks[bg]
                    btu, rbu = k.banks[bu]
                    for kc in range(NKC):
                        k.op("pe", (lambda kc=kc, ts=ts, btg=btg, wgt=wgt, f2=f2: nc.tensor.matmul(
                            btg[:], lhsT=wgt[:, kc, f2 * 128:(f2 + 1) * 128], rhs=hnT[:, kc, ts],
                            start=(kc == 0), stop=(kc == NKC - 1))), reads=[r_wgt, r_hn], writes=[rbg])
                    for kc in range(NKC):
                        k.op("pe", (lambda kc=kc, ts=ts, btu=btu, wut=wut, f2=f2: nc.tensor.matmul(
                            btu[:], lhsT=wut[:, kc, f2 * 128:(f2 + 1) * 128], rhs=hnT[:, kc, ts],
                            start=(kc == 0), stop=(kc == NKC - 1))), reads=[r_wut, r_hn], writes=[rbu])
                    sg, r_sg = k.sb(f"moe_sg{(fc * 2 + th) % 2}", [128, 512], F32)
                    k.op("act", (lambda sg=sg, btg=btg: nc.scalar.activation(out=sg[:], in_=btg[:], func=AF.Silu)),
                         reads=[rbg], writes=[r_sg])
                    k.free(bg)
                    k.op("dve", (lambda sg=sg, btu=btu: nc.vector.tensor_tensor(out=sg[:], in0=sg[:], in1=btu[:], op=ALU.mult)),
                         reads=[r_sg, rbu], writes=[r_sg])
                    k.free(bu)
                    k.op("dve", (lambda sg=sg, gb=gb, ts=ts, hid=hid, fc=fc: nc.vector.tensor_tensor(
                        out=hid[:, fc, ts], in0=sg[:], in1=gb[:, ts], op=ALU.mult)), reads=[r_sg, r_gb], writes=[r_hid])
        for dq in range(2):
            (wdt, r_wdt), = k.wget([(wd[e][:, dq * 1024:(dq + 1) * 1024], 4, 1024)])
            for d2 in range(8):
                dc = dq * 8 + d2
                for th in range(2):
                    ts = slice(th * 512, (th + 1) * 512)
                    b = k.bank()
                    bt, rb = k.banks[b]
                    for fc in range(4):
                        k.op("pe", (lambda fc=fc, ts=ts, bt=bt, wdt=wdt, d2=d2, hid=hid: nc.tensor.matmul(
                            bt[:], lhsT=wdt[:, fc, d2 * 128:(d2 + 1) * 128], rhs=hid[:, fc, ts],
                            start=(fc == 0), stop=(fc == 3))), reads=[r_wdt, r_hid], writes=[rb])
                    k.op("dve", (lambda dc=dc, ts=ts, bt=bt: nc.vector.tensor_tensor(out=hT[:, dc, ts], in0=hT[:, dc, ts], in1=bt[:],
                                                                                     op=ALU.add)), reads=[rb, r_h], writes=[r_h])
                    k.free(b)


def ssd_prep(k, hnT, r_hn, prm):
    nc = k.nc
    c = k.c
    wdt, r_wdt = prm["wdt"]
    dtb, r_dtb = prm["dtb"]
    A_bc, r_A = prm["A_bc"]
    onec, r_onec = c["one_col"]
    tri, r_tri = c["tri_incl"]
    ones, r_ones = c["ones_f"]
    P = {}
    for nm in ("dt", "a", "eacs", "dstate", "cdec"):
        P[nm] = k.sb("sp_" + nm, [128, 8, 64], F32)
    dt, r_dt = P["dt"]; a, r_a = P["a"]; eacs, r_eacs = P["eacs"]
    dstate, r_dst = P["dstate"]; cdec, r_cdec = P["cdec"]
    b = k.bank(); bt, rb = k.banks[b]
    for cch in range(8):
        for kc in range(NKC):
            k.op("pe", (lambda cch=cch, kc=kc, bt=bt: nc.tensor.matmul(
                bt[:, cch * 64:(cch + 1) * 64], lhsT=hnT[:, kc, cch * 128:(cch + 1) * 128], rhs=wdt[:, kc, :],
                start=(kc == 0), stop=(kc == NKC - 1))), reads=[r_hn, r_wdt], writes=[rb])
    k.op("dve", (lambda bt=bt: nc.vector.tensor_tensor(out=dt[:], in0=bt[:].rearrange("p (c h) -> p c h", c=8),
                                                      in1=dtb[:].unsqueeze(1).to_broadcast([128, 8, 64]), op=ALU.add)),
         reads=[rb, r_dtb], writes=[r_dt])
    k.free(b)
    k.op("act", lambda: nc.scalar.activation(out=dt[:], in_=dt[:], func=AF.Exp), reads=[r_dt], writes=[r_dt])
    k.op("act", lambda: nc.scalar.activation(out=dt[:], in_=dt[:], func=AF.Ln, bias=onec[:], scale=1.0),
         reads=[r_dt, r_onec], writes=[r_dt])
    k.op("dve", lambda: nc.vector.tensor_tensor(out=a[:], in0=dt[:], in1=A_bc[:].unsqueeze(1).to_broadcast([128, 8, 64]),
                                               op=ALU.mult), reads=[r_dt, r_A], writes=[r_a])
    b2 = k.bank(); bt2, rb2 = k.banks[b2]
    b3 = k.bank(); bt3, rb3 = k.banks[b3]
    for cch in range(8):
        k.op("pe", (lambda cch=cch, bt2=bt2: nc.tensor.matmul(bt2[:, cch * 64:(cch + 1) * 64], lhsT=tri[:], rhs=a[:, cch, :],
                                                              start=True, stop=True)), reads=[r_a, r_tri], writes=[rb2])
        k.op("pe", (lambda cch=cch, bt3=bt3: nc.tensor.matmul(bt3[:, cch * 64:(cch + 1) * 64], lhsT=ones[:], rhs=a[:, cch, :],
                                                              start=True, stop=True)), reads=[r_a, r_ones], writes=[rb3])
    f3 = lambda t: t[:].rearrange("p c h -> p (c h)")
    k.op("act", (lambda bt2=bt2: nc.scalar.activation(out=f3(eacs), in_=bt2[:], func=AF.Exp)), reads=[rb2], writes=[r_eacs])
    k.op("act", (lambda bt2=bt2: nc.scalar.copy(out=f3(dstate), in_=bt2[:])), reads=[rb2], writes=[r_dst])
    k.free(b2)
    k.op("act", (lambda bt3=bt3: nc.scalar.activation(out=f3(cdec), in_=bt3[:], func=AF.Exp)), reads=[rb3], writes=[r_cdec])
    k.op("dve", (lambda bt3=bt3: nc.vector.tensor_tensor(out=f3(dstate), in0=f3(dstate), in1=bt3[:], op=ALU.subtract)),
         reads=[rb3, r_dst], writes=[r_dst])
    k.free(b3)
    k.op("act", lambda: nc.scalar.activation(out=f3(dstate), in_=f3(dstate), func=AF.Exp, scale=-1.0), reads=[r_dst], writes=[r_dst])
    return P


def wseq_ssd_group(prm, g, full):
    w_in = prm["w_in"]
    seq = [[(w_in[:, 4096 + 512 * g: 4096 + 512 * g + 256], NKC, 256)],
           [(w_in[:, 4096 + 512 * g + 256: 4096 + 512 * g + 512], NKC, 256)],
           [(w_in[:, 8192 + 128 * g: 8192 + 128 * g + 128], NKC, 128), (w_in[:, 9216 + 128 * g: 9216 + 128 * g + 128], NKC, 128)]]
    if full:
        seq += [[(w_in[:, 512 * g: 512 * g + 256], NKC, 256)], [(w_in[:, 512 * g + 256: 512 * g + 512], NKC, 256)]]
    return seq


def ssd_group(k, g, mode, hnT, r_hn, P, prm, ynT_all=None, r_yn=None):
    nc = k.nc
    c = k.c
    cwh, r_cwh = prm["cwh"]
    cbh, r_cbh = prm["cbh"]
    D_bc, r_D = prm["D_bc"]
    nw, r_nw = prm["nw"]
    hal, r_hal = prm["hal"]
    S_in, r_Sin = prm["S_in"]
    flag, r_flag = prm["flag"]
    identb, r_identb = c["ident_bf"]
    identf, r_identf = c["ident_f"]
    ones, r_ones = c["ones_f"]
    tri, r_tri = c["tri_incl"]
    ustr, r_ustr = c["ustrict"]
    mle, r_mle = c["mask_le"]
    dt, r_dt = P["dt"]; a, r_a = P["a"]; eacs, r_eacs = P["eacs"]
    dstate, r_dst = P["dstate"]; cdec, r_cdec = P["cdec"]
    full = (mode == "B")
    hs = slice(8 * g, 8 * g + 8)
    seq = wseq_ssd_group(prm, g, full)

    xcT, r_xc = k.sb("sg_xcT", [128, 4, NT], BF16)
    BcT, r_Bc = k.sb("sg_BcT", [128, NT], BF16)
    CcT, r_Cc = k.sb("sg_CcT", [128, NT], BF16)
    Sg, r_S = k.sb("sg_S", [128, 512], F32)
    Sbf, r_Sbf = k.sb("sg_Sbf", [128, 512], BF16)
    h8 = lambda ap: ap.rearrange("p (h q) -> p h q", h=8)

    items = [(0, 0, 4 * g + 0, xcT[:, 0, :], r_xc), (0, 128, 4 * g + 1, xcT[:, 1, :], r_xc),
             (1, 0, 4 * g + 2, xcT[:, 2, :], r_xc), (1, 128, 4 * g + 3, xcT[:, 3, :], r_xc),
             (2, 0, 32 + g, BcT[:], r_Bc), (3, 0, 40 + g, CcT[:], r_Cc)]
    wcur = {}
    for ii, (wi, co, ci, dst, r_dstt) in enumerate(items):
        if wi == 0 and 0 not in wcur:
            wcur[0], = k.wget(seq[0])
        elif wi == 1 and 1 not in wcur:
            wcur[1], = k.wget(seq[1])
        elif wi == 2 and 2 not in wcur:
            wcur[2], wcur[3] = k.wget(seq[2])
        wt, r_wt = wcur[wi]
        pre, r_pre = k.sb("sg_pre", [128, NT + 8], F32)
        acc, r_acc = k.sb("sg_acc", [128, NT], F32)
        if full:
            k.op("pool", (lambda pre=pre, ci=ci: nc.gpsimd.tensor_copy(out=pre[:, 0:3], in_=hal[:, ci, :])),
                 reads=[r_hal], writes=[r_pre])
        else:
            k.op("pool", (lambda pre=pre: nc.gpsimd.memset(pre[:, 0:3], 0.0)), writes=[r_pre])
        for th in range(2):
            ts = slice(th * 512, (th + 1) * 512)
            b = k.bank(); bt, rb = k.banks[b]
            for kc in range(NKC):
                k.op("pe", (lambda kc=kc, ts=ts, bt=bt, wt=wt, co=co: nc.tensor.matmul(
                    bt[:], lhsT=wt[:, kc, co:co + 128], rhs=hnT[:, kc, ts], start=(kc == 0), stop=(kc == NKC - 1))),
                    reads=[r_wt, r_hn], writes=[rb])
            k.op("act", (lambda bt=bt, pre=pre, th=th: nc.scalar.copy(out=pre[:, 3 + th * 512: 3 + (th + 1) * 512], in_=bt[:])),
                 reads=[rb], writes=[r_pre])
            k.op("act", (lambda bt=bt, acc=acc, ts=ts, ci=ci: nc.scalar.activation(
                out=acc[:, ts], in_=bt[:], func=AF.Identity, bias=cbh[:, ci:ci + 1], scale=cwh[:, ci, 3:4])),
                reads=[rb, r_cwh, r_cbh], writes=[r_acc])
            k.free(b)
        for tap in range(3):
            k.op("dve", (lambda pre=pre, acc=acc, ci=ci, tap=tap: nc.vector.scalar_tensor_tensor(
                out=acc[:], in0=pre[:, tap:tap + NT], scalar=cwh[:, ci, tap:tap + 1], in1=acc[:], op0=ALU.mult, op1=ALU.add)),
                reads=[r_pre, r_acc, r_cwh], writes=[r_acc])
        if not full:
            k.op("pool", (lambda pre=pre, ci=ci: nc.gpsimd.tensor_copy(out=hal[:, ci, :], in_=pre[:, NT:NT + 3])),
                 reads=[r_pre], writes=[r_hal])
        k.op("act", (lambda acc=acc, pre=pre: nc.scalar.activation(out=pre[:, 0:NT], in_=acc[:], func=AF.Tanh)),
             reads=[r_acc], writes=[r_pre])
        k.op("dve", (lambda acc=acc, pre=pre, dst=dst: nc.vector.scalar_tensor_tensor(
            out=dst, in0=pre[:, 0:NT], scalar=1.0, in1=acc[:], op0=ALU.add, op1=ALU.mult)),
            reads=[r_pre, r_acc], writes=[r_dstt])

    if full:
        (wz0, r_wz0), = k.wget(seq[3])
        (wz1, r_wz1), = k.wget(seq[4])
        ss, r_ss = k.sb("sg_ss", [128, 8], F32)
        k.op("dve", lambda: nc.vector.memset(ss[:], 0.0), writes=[r_ss])
        k.op("dve", lambda: nc.vector.tensor_scalar(out=Sg[:], in0=S_in[:, g, :], scalar1=flag[:], scalar2=None, op0=ALU.mult),
             reads=[r_Sin, r_flag], writes=[r_S])
        k.op("act", lambda: nc.scalar.copy(out=Sbf[:], in_=Sg[:]), reads=[r_S], writes=[r_Sbf])
    else:
        k.op("pool", lambda: nc.gpsimd.memset(Sg[:], 0.0), writes=[r_S])

    for cch in range(8):
        cs = slice(cch * 128, (cch + 1) * 128)
        bx = k.bank(); btx, rbx = k.banks[bx]
        for fc in range(4):
            k.op("pe", (lambda fc=fc, cs=cs, btx=btx: nc.tensor.matmul(btx[:, fc * 128:(fc + 1) * 128], lhsT=xcT[:, fc, cs],
                                                                       rhs=identb[:], start=True, stop=True)),
                 reads=[r_xc, r_identb], writes=[rbx])
        xdt, r_xdt = k.sb(f"sg_xdt{cch % 2}", [128, 512], BF16)
        k.op("dve", (lambda btx=btx, xdt=xdt, cch=cch: nc.vector.tensor_tensor(
            out=h8(xdt[:]), in0=h8(btx[:]), in1=dt[:, cch, hs].unsqueeze(2).to_broadcast([128, 8, 64]), op=ALU.mult)),
            reads=[rbx, r_dt], writes=[r_xdt])
        if full:
            xD, r_xD = k.sb("sg_xD", [128, 512], F32)
            k.op("dve", (lambda btx=btx, xD=xD: nc.vector.tensor_tensor(
                out=h8(xD[:]), in0=h8(btx[:]), in1=D_bc[:, hs].unsqueeze(2).to_broadcast([128, 8, 64]), op=ALU.mult)),
                reads=[rbx, r_D], writes=[r_xD])
        k.free(bx)
        bB = k.bank(); btB, rbB = k.banks[bB]
        k.op("pe", (lambda cs=cs, btB=btB: nc.tensor.matmul(btB[:, 0:128], lhsT=BcT[:, cs], rhs=identb[:], start=True, stop=True)),
             reads=[r_Bc, r_identb], writes=[rbB])
        if full:
            k.op("pe", (lambda cs=cs, btB=btB: nc.tensor.matmul(btB[:, 128:256], lhsT=BcT[:, cs], rhs=CcT[:, cs], start=True, stop=True)),
                 reads=[r_Bc, r_Cc], writes=[rbB])
        Btok, r_Bt = k.sb(f"sg_Btok{cch % 2}", [128, 128], BF16)
        k.op("act", (lambda btB=btB, Btok=Btok: nc.scalar.copy(out=Btok[:], in_=btB[:, 0:128])), reads=[rbB], writes=[r_Bt])
        if full:
            cbm, r_cbm = k.sb("sg_cbm", [128, 128], F32)
            k.op("dve", (lambda btB=btB, cbm=cbm: nc.vector.tensor_tensor(out=cbm[:], in0=btB[:, 128:256], in1=mle[:], op=ALU.mult)),
                 reads=[rbB, r_mle], writes=[r_cbm])
        k.free(bB)
        if full:
            MT, r_MT = k.sb("sg_MT", [128, 8, 128], BF16)
            for hh in range(2):
                lta, r_lta = k.sb(f"sg_lta", [128, 4, 128], F32)
                k.op("dve", (lambda lta=lta, cch=cch, hh=hh: nc.vector.tensor_tensor(
                    out=lta[:], in0=ustr[:].unsqueeze(1).to_broadcast([128, 4, 128]),
                    in1=a[:, cch, 8 * g + 4 * hh:8 * g + 4 * hh + 4].unsqueeze(2).to_broadcast([128, 4, 128]), op=ALU.mult)),
                    reads=[r_ustr, r_a], writes=[r_lta])
                ba = k.bank(); bta, rba = k.banks[ba]
                for h4 in range(4):
                    k.op("pe", (lambda h4=h4, bta=bta, lta=lta: nc.tensor.matmul(
                        bta[:, h4 * 128:(h4 + 1) * 128], lhsT=lta[:, h4, :], rhs=tri[:], start=True, stop=True)),
                        reads=[r_lta, r_tri], writes=[rba])
                dec, r_dec = k.sb(f"sg_dec{hh}", [128, 512], BF16)
                k.op("act", (lambda bta=bta, dec=dec: nc.scalar.activation(out=dec[:], in_=bta[:], func=AF.Exp)),
                     reads=[rba], writes=[r_dec])
                k.free(ba)
                k.op("dve", (lambda dec=dec, MT=MT, hh=hh, cbm=cbm: nc.vector.tensor_tensor(
                    out=MT[:, hh * 4:(hh + 1) * 4, :], in0=dec[:].rearrange("p (h l) -> p h l", h=4),
                    in1=cbm[:].unsqueeze(1).to_broadcast([128, 4, 128]), op=ALU.mult)), reads=[r_dec, r_cbm], writes=[r_MT])
            by = k.bank(); bty, rby = k.banks[by]
            for h in range(8):
                k.op("pe", (lambda h=h, bty=bty, MT=MT, xdt=xdt: nc.tensor.matmul(
                    bty[:, h * 64:(h + 1) * 64], lhsT=MT[:, h, :], rhs=xdt[:, h * 64:(h + 1) * 64], start=True, stop=True)),
                    reads=[r_MT, r_xdt], writes=[rby])
            bo = k.bank(); bto, rbo = k.banks[bo]
            k.op("pe", (lambda cs=cs, bto=bto: nc.tensor.matmul(bto[:], lhsT=CcT[:, cs], rhs=Sbf[:], start=True, stop=True)),
                 reads=[r_Cc, r_Sbf], writes=[rbo])
            t1, r_t1 = k.sb("sg_t1", [128, 512], F32)
            k.op("dve", (lambda bto=bto, t1=t1, cch=cch: nc.vector.tensor_tensor(
                out=h8(t1[:]), in0=h8(bto[:]), in1=eacs[:, cch, hs].unsqueeze(2).to_broadcast([128, 8, 64]), op=ALU.mult)),
                reads=[rbo, r_eacs], writes=[r_t1])
            k.free(bo)
            k.op("pool", (lambda t1=t1, xD=xD: nc.gpsimd.tensor_tensor(out=t1[:], in0=t1[:], in1=xD[:], op=ALU.add)),
                 reads=[r_t1, r_xD], writes=[r_t1])
            ysb, r_ysb = k.sb("sg_ysb", [128, 512], F32)
            k.op("dve", (lambda bty=bty, t1=t1, ysb=ysb: nc.vector.tensor_tensor(out=ysb[:], in0=t1[:], in1=bty[:], op=ALU.add)),
                 reads=[rby, r_t1], writes=[r_ysb])
            k.free(by)
            bz = k.bank(); btz, rbz = k.banks[bz]
            for zi, (wz, r_wz) in enumerate(((wz0, r_wz0), (wz1, r_wz1))):
                for kc in range(NKC):
                    k.op("pe", (lambda kc=kc, cs=cs, btz=btz, wz=wz, zi=zi: nc.tensor.matmul(
                        btz[:, zi * 256:(zi + 1) * 256], lhsT=hnT[:, kc, cs], rhs=wz[:, kc, :],
                        start=(kc == 0), stop=(kc == NKC - 1))), reads=[r_hn, r_wz], writes=[rbz])
            zs, r_zs = k.sb("sg_zs", [128, 512], F32)
            k.op("act", (lambda btz=btz, zs=zs: nc.scalar.activation(out=zs[:], in_=btz[:], func=AF.Tanh, scale=0.5)),
                 reads=[rbz], writes=[r_zs])
            k.op("dve", (lambda btz=btz, zs=zs: nc.vector.scalar_tensor_tensor(out=zs[:], in0=zs[:], scalar=1.0, in1=btz[:],
                                                                               op0=ALU.add, op1=ALU.mult)),
                 reads=[r_zs, rbz], writes=[r_zs])
            k.free(bz)
            ygb, r_ygb = k.sb(f"sg_ygb{cch % 2}", [128, 512], BF16)
            k.op("dve", (lambda ysb=ysb, zs=zs, ygb=ygb: nc.vector.tensor_tensor(out=ygb[:], in0=ysb[:], in1=zs[:], op=ALU.mult)),
                 reads=[r_ysb, r_zs], writes=[r_ygb])
            k.op("act", (lambda cch=cch, zs=zs, ygb=ygb: nc.scalar.activation(out=zs[:], in_=ygb[:], func=AF.Square, scale=0.5,
                                                                             accum_out=ss[:, cch:cch + 1])),
                 reads=[r_ygb], writes=[r_zs, r_ss])
            btt = k.bank(); bttt, rbt = k.banks[btt]
            for fc in range(4):
                k.op("pe", (lambda fc=fc, bttt=bttt, ygb=ygb: nc.tensor.matmul(bttt[:, fc * 128:(fc + 1) * 128], lhsT=ygb[:, fc * 128:(fc + 1) * 128],
                                                                               rhs=identb[:], start=True, stop=True)),
                     reads=[r_ygb, r_identb], writes=[rbt])
            k.op("act", (lambda bttt=bttt, cs=cs: nc.scalar.copy(out=ynT_all[:, 4 * g:4 * g + 4, cs], in_=bttt[:].rearrange("p (f t) -> p f t", f=4))),
                 reads=[rbt], writes=[r_yn])
            k.free(btt)
        if (not full) or cch < 7:
            xd, r_xd = k.sb("sg_xd", [128, 512], BF16)
            k.op("pool", (lambda xd=xd, xdt=xdt, cch=cch: nc.gpsimd.tensor_tensor(
                out=h8(xd[:]), in0=h8(xdt[:]), in1=dstate[:, cch, hs].unsqueeze(2).to_broadcast([128, 8, 64]), op=ALU.mult)),
                reads=[r_xdt, r_dst], writes=[r_xd])
            bs = k.bank(); bts, rbs = k.banks[bs]
            k.op("pe", (lambda bts=bts, Btok=Btok, xd=xd: nc.tensor.matmul(bts[:], lhsT=Btok[:], rhs=xd[:], start=True, stop=True)),
                 reads=[r_Bt, r_xd], writes=[rbs])
            k.op("pool", (lambda cch=cch: nc.gpsimd.tensor_tensor(
                out=h8(Sg[:]), in0=h8(Sg[:]), in1=cdec[:, cch, hs].unsqueeze(2).to_broadcast([128, 8, 64]), op=ALU.mult)),
                reads=[r_S, r_cdec], writes=[r_S])
            k.op("dve", (lambda bts=bts: nc.vector.tensor_tensor(out=Sg[:], in0=Sg[:], in1=bts[:], op=ALU.add)), reads=[r_S, rbs], writes=[r_S])
            k.free(bs)
            if full:
                k.op("act", lambda: nc.scalar.copy(out=Sbf[:], in_=Sg[:]), reads=[r_S], writes=[r_Sbf])

    if not full:
        k.op("act", lambda: nc.scalar.copy(out=S_in[:, g, :], in_=Sg[:]), reads=[r_S], writes=[r_Sin])
        return
    eps4, r_eps4 = c["eps4_col"]
    pre_t, r_rbc = k.sb("sg_pre", [128, NT + 8], F32)
    rbc = pre_t[:, 0:NT]
    for hh in range(2):
        b = k.bank(); bt, rb = k.banks[b]
        for c4 in range(4):
            cch = hh * 4 + c4
            dg, r_dg = k.sb(f"sg_diag{c4 % 2}", [128, 128], F32)
            k.op("dve", (lambda dg=dg, cch=cch: nc.vector.tensor_scalar(out=dg[:], in0=identf[:], scalar1=ss[:, cch:cch + 1], scalar2=None,
                                                                       op0=ALU.mult)), reads=[r_identf, r_ss], writes=[r_dg])
            k.op("pe", (lambda dg=dg, bt=bt, c4=c4: nc.tensor.matmul(bt[:, c4 * 128:(c4 + 1) * 128], lhsT=ones[:], rhs=dg[:], start=True, stop=True)),
                 reads=[r_dg, r_ones], writes=[rb])
        k.op("act", (lambda bt=bt, hh=hh: nc.scalar.activation(out=rbc[:, hh * 512:(hh + 1) * 512], in_=bt[:], func=AF.Ln, bias=eps4[:],
                                                               scale=4.0 / 512)), reads=[rb, r_eps4], writes=[r_rbc])
        k.free(b)
    k.op("act", lambda: nc.scalar.activation(out=rbc, in_=rbc, func=AF.Exp, scale=-0.5), reads=[r_rbc], writes=[r_rbc])
    for fc in range(4):
        eng = "dve"
        E = nc.vector
        k.op(eng, (lambda fc=fc, E=E: E.scalar_tensor_tensor(
            out=ynT_all[:, 4 * g + fc, :], in0=ynT_all[:, 4 * g + fc, :], scalar=nw[:, 4 * g + fc:4 * g + fc + 1], in1=rbc,
            op0=ALU.mult, op1=ALU.mult)), reads=[r_yn, r_nw, r_rbc], writes=[r_yn])


def wseq_outproj(prm):
    w = prm["w_out"]
    return [[(w[0:2048, dc * 128:(dc + 1) * 128], 16, 128), (w[2048:4096, dc * 128:(dc + 1) * 128], 16, 128)] for dc in range(16)]


def out_proj(k, hT, r_h, ynT_all, r_yn, prm):
    nc = k.nc
    for dc, pieces in enumerate(wseq_outproj(prm)):
        (wo0, r_wo0), (wo1, r_wo1) = k.wget(pieces)
        for th in range(2):
            ts = slice(th * 512, (th + 1) * 512)
            b = k.bank(); bt, rb = k.banks[b]
            for fc in range(32):
                wo, r_wo = (wo0, r_wo0) if fc < 16 else (wo1, r_wo1)
                k.op("pe", (lambda fc=fc, ts=ts, bt=bt, wo=wo: nc.tensor.matmul(
                    bt[:], lhsT=wo[:, fc % 16, :], rhs=ynT_all[:, fc, ts], start=(fc == 0), stop=(fc == 31))),
                    reads=[r_wo, r_yn], writes=[rb])
            k.op("dve", (lambda dc=dc, ts=ts, bt=bt: nc.vector.tensor_tensor(out=hT[:, dc, ts], in0=hT[:, dc, ts], in1=bt[:], op=ALU.add)),
                 reads=[rb, r_h], writes=[r_h])
            k.free(b)


W_IN_COLS = 10304
KB = 1024


def dram_in(nc, name, shape, dt=F32):
    return nc.dram_tensor(name, list(shape), dt, kind="ExternalInput").ap()


def load_small(k, name, src, shape):
    nc = k.nc
    t, r = k.sb("p_" + name, shape, F32)
    k.op("sp", (lambda t=t, src=src: nc.sync.dma_start(out=t[:], in_=src)), writes=[r], dma=True)
    return t, r


L1_INPUTS = [("xT_prev", [2048, NT]), ("xT_own", [2048, NT]), ("flag", [128, 1]),
             ("mix_g0", [128, 16]), ("ffn_g0", [128, 16]), ("kv_g", [128, 16]),
             ("w_in", [2048, W_IN_COLS]), ("conv_w", [128, 48, 4]), ("conv_b", [128, 48]),
             ("dt_bias", [128, 64]), ("a_log", [128, 64]), ("d_skip", [128, 64]), ("norm_w", [128, 32]),
             ("w_out", [4096, 2048]), ("w_k", [2048, 2048]), ("w_v", [2048, 2048]),
             ("w_router0", [2048, 20]), ("b_router0", [128, 20]),
             ("w_gate0", [16, 2048, 512]), ("w_up0", [16, 2048, 512]), ("w_down0", [16, 512, 2048])]


def wseq_kv(I):
    return ([[(I["w_k"][:, cb * 256:(cb + 1) * 256], NKC, 256)] for cb in range(8)] +
            [[(I["w_v"][:, cb * 256:(cb + 1) * 256], NKC, 256)] for cb in range(8)])


def build_launch1(stop="full"):
    nc = bass.Bass("TRN2", target_bir_lowering=False)
    dr = {n: dram_in(nc, n, s) for n, s in CONST_SHAPES.items()}
    I = {n: dram_in(nc, n, s) for n, s in L1_INPUTS}
    h_out = nc.dram_tensor("h_out", [2048, NT], F32, kind="ExternalOutput").ap()
    kT_out = nc.dram_tensor("kT_out", [2048, NT], F32, kind="ExternalOutput").ap()
    v_out = nc.dram_tensor("v_out", [NT, 2048], F32, kind="ExternalOutput").ap()
    k = K(nc)
    k.setup()
    outs = []

    load_consts(k, dr, ["ident_bf", "ident_f", "ones_f", "tri_incl", "ustrict", "mask_le", "inv128"])
    hnT, r_hn = k.sb("hnT", [128, 16, NT], BF16)
    prm = {}
    mixg = load_small(k, "mix_g0", I["mix_g0"], [128, 16])
    prm["ffn_g"] = [load_small(k, "ffn_g0", I["ffn_g0"], [128, 16])]
    kvg = load_small(k, "kv_g", I["kv_g"], [128, 16])
    prm["b_router"] = [load_small(k, "b_router0", I["b_router0"], [128, 20])]
    prm["w_router"] = [I["w_router0"]]
    prm["w_gate"] = [[I["w_gate0"][e] for e in range(16)]]
    prm["w_up"] = [[I["w_up0"][e] for e in range(16)]]
    prm["w_down"] = [[I["w_down0"][e] for e in range(16)]]
    prm["w_in"] = I["w_in"]
    prm["w_out"] = I["w_out"]
    prm["flag"] = load_small(k, "flag", I["flag"], [128, 1])
    prm["dtb"] = load_small(k, "dt_bias", I["dt_bias"], [128, 64])
    prm["D_bc"] = load_small(k, "d_skip", I["d_skip"], [128, 64])
    prm["nw"] = load_small(k, "norm_w", I["norm_w"], [128, 32])
    A_bc, r_A = load_small(k, "a_log", I["a_log"], [128, 64])
    k.op("act", lambda: nc.scalar.activation(out=A_bc[:], in_=A_bc[:], func=AF.Exp), reads=[r_A], writes=[r_A])
    k.op("dve", lambda: nc.vector.tensor_scalar(out=A_bc[:], in0=A_bc[:], scalar1=-1.0, scalar2=None, op0=ALU.mult), reads=[r_A], writes=[r_A])
    prm["A_bc"] = (A_bc, r_A)
    cwh, r_cwh = k.sb("p_cwh", [128, 48, 4], F32)
    k.op("sp", lambda: nc.sync.dma_start(out=cwh[:], in_=I["conv_w"]), writes=[r_cwh], dma=True)
    k.op("dve", lambda: nc.vector.tensor_scalar(out=cwh[:], in0=cwh[:], scalar1=0.5, scalar2=None, op0=ALU.mult), reads=[r_cwh], writes=[r_cwh])
    prm["cwh"] = (cwh, r_cwh)
    cbh, r_cbh = load_small(k, "conv_b", I["conv_b"], [128, 48])
    k.op("dve", lambda: nc.vector.tensor_scalar(out=cbh[:], in0=cbh[:], scalar1=0.5, scalar2=None, op0=ALU.mult), reads=[r_cbh], writes=[r_cbh])
    prm["cbh"] = (cbh, r_cbh)
    wdt, r_wdt = k.sb("p_wdt", [128, 16, 64], BF16)
    k.op("pool", lambda: nc.gpsimd.dma_start(out=wdt[:], in_=I["w_in"][:, 10240:10304].rearrange("(kc p) f -> p kc f", p=128)),
         writes=[r_wdt], dma=True)
    prm["wdt"] = (wdt, r_wdt)
    prm["hal"] = k.sb("p_hal", [128, 48, 3], F32)
    prm["S_in"] = k.sb("p_Sin", [128, 8, 512], BF16)
    BIG = (k.bump + 63) // 64 * 64
    print("perm end", BIG - SB_BASE, "big bytes", SB_TOP - BIG)
    assert SB_TOP - BIG >= 128 * KB

    LV = ["normA", "prepA", "sA1", "sA", "normB", "sB1", "sB", "mixer", "moe", "full"]
    lv = LV.index(stop)
    nA = 0 if lv < 2 else (1 if lv == 2 else 8)
    nB = 0 if lv < 5 else (1 if lv == 5 else 8)
    for g in range(nA):
        k.wdeclare(wseq_ssd_group(prm, g, False))
    for g in range(nB):
        k.wdeclare(wseq_ssd_group(prm, g, True))
    if lv >= 7:
        k.wdeclare(wseq_outproj(prm))
    if lv >= 8:
        k.wdeclare(wseq_moe(prm, 0))
    if lv >= 9:
        k.wdeclare(wseq_kv(I))

    def early_out():
        o = k.op("pool", lambda: nc.gpsimd.dma_start(out=h_out.rearrange("(kc p) t -> p kc t", p=128), in_=hnT[:]), reads=[r_hn], dma=True)
        cnt = k.p.emit()
        print("launch1(early): ops", len(k.p.ops), "wtiles", len(k.wlist), "signals", cnt)
        k.p.final_wait("pool", [o])
        nc._knames = k.names
        return nc

    k.begin_phase("nA", BIG)
    rmsnorm(k, src_staged(k, I["xT_prev"]), mixg[0], mixg[1], hnT, r_hn)
    if lv == 0:
        return early_out()
    k.begin_phase("sA", BIG)
    P = ssd_prep(k, hnT, r_hn, prm)
    for g in range(nA):
        ssd_group(k, g, "A", hnT, r_hn, P, prm)
    if lv <= 3:
        return early_out()
    k.begin_phase("nB", BIG)
    rmsnorm(k, src_staged(k, I["xT_own"]), mixg[0], mixg[1], hnT, r_hn)
    k.begin_phase("sB", BIG)
    ynT_all, r_yn = k.sb("ynT_all", [128, 32, NT], BF16)
    assert k.bump == BIG + 64 * KB
    if lv == 4:
        return early_out()
    P = ssd_prep(k, hnT, r_hn, prm)
    for g in range(nB):
        ssd_group(k, g, "B", hnT, r_hn, P, prm, ynT_all, r_yn)
    print("sB scratch used", k.bump - BIG - 64 * KB)
    if lv <= 6:
        return early_out()
    k.begin_phase("op", BIG + 64 * KB)
    hT, r_h = k.sb("hT", [128, 16, NT], F32)
    k.op("sp", lambda: nc.sync.dma_start(out=hT[:], in_=I["xT_own"].rearrange("(kc p) t -> p kc t", p=128)), writes=[r_h], dma=True)
    out_proj(k, hT, r_h, ynT_all, r_yn, prm)
    if lv >= 8:
        k.begin_phase("moe0", BIG, BIG + 64 * KB)
        load_consts(k, dr, ["sel16"])
        moe(k, hT, r_h, hnT, r_hn, prm, 0)
        print("moe scratch used", k.bump - BIG)
    if lv >= 9:
        k.begin_phase("kv", BIG, BIG + 64 * KB)
        rmsnorm(k, src_resident(hT, r_h), kvg[0], kvg[1], hnT, r_hn)
        seq = wseq_kv(I)
        for cb in range(8):
            (wk, r_wk), = k.wget(seq[cb])
            st, r_st = k.sb(f"kv_st{cb % 2}", [128, 2, NT], F32)
            for c2 in range(2):
                for th in range(2):
                    ts = slice(th * 512, (th + 1) * 512)
                    b = k.bank(); bt, rb = k.banks[b]
                    for kc in range(NKC):
                        k.op("pe", (lambda kc=kc, ts=ts, bt=bt, wk=wk, c2=c2: nc.tensor.matmul(
                            bt[:], lhsT=wk[:, kc, c2 * 128:(c2 + 1) * 128], rhs=hnT[:, kc, ts],
                            start=(kc == 0), stop=(kc == NKC - 1))), reads=[r_wk, r_hn], writes=[rb])
                    k.op("act", (lambda bt=bt, st=st, c2=c2, ts=ts: nc.scalar.copy(out=st[:, c2, ts], in_=bt[:])), reads=[rb], writes=[r_st])
                    k.free(b)
            o = k.op("sp", (lambda st=st, cb=cb: nc.sync.dma_start(
                out=kT_out[cb * 256:(cb + 1) * 256, :].rearrange("(c p) t -> p c t", p=128), in_=st[:])), reads=[r_st], dma=True)
            outs.append(o)
        for cb in range(8):
            (wv, r_wv), = k.wget(seq[8 + cb])
            st, r_st = k.sb(f"kv_st{cb % 2}", [128, 2, NT], F32)
            stv = st[:].rearrange("p c t -> p (c t)").rearrange("p (t f) -> p t f", f=256)
            for t2 in range(4):
                b = k.bank(); bt, rb = k.banks[b]
                for ti in range(2):
                    tb = t2 * 2 + ti
                    for kc in range(NKC):
                        k.op("pe", (lambda kc=kc, tb=tb, ti=ti, bt=bt, wv=wv: nc.tensor.matmul(
                            bt[:, ti * 256:(ti + 1) * 256], lhsT=hnT[:, kc, tb * 128:(tb + 1) * 128], rhs=wv[:, kc, :],
                            start=(kc == 0), stop=(kc == NKC - 1))), reads=[r_wv, r_hn], writes=[rb])
                k.op("act", (lambda bt=bt, stv=stv, t2=t2: nc.scalar.copy(
                    out=stv[:, 2 * t2:2 * t2 + 2, :], in_=bt[:].rearrange("p (t f) -> p t f", f=256))), reads=[rb], writes=[r_st])
                k.free(b)
            o = k.op("sp", (lambda stv=stv, cb=cb: nc.sync.dma_start(
                out=v_out[:, cb * 256:(cb + 1) * 256].rearrange("(t p) f -> p t f", p=128), in_=stv)), reads=[r_st], dma=True)
            outs.append(o)
    o = k.op("sp", lambda: nc.sync.dma_start(out=h_out.rearrange("(kc p) t -> p kc t", p=128), in_=hT[:]), reads=[r_h], dma=True)
    outs.append(o)
    assert k.widx == len(k.wlist), (k.widx, len(k.wlist))
    cnt = k.p.emit()
    print("launch1: ops", len(k.p.ops), "wtiles", len(k.wlist), "signals", cnt)
    k.p.final_wait("sp", outs)
    return nc


def fm(v, n):
    return np.ascontiguousarray(np.asarray(v, np.float32).reshape(n, 128).T)


def rep(v):
    return np.ascontiguousarray(np.tile(np.asarray(v, np.float32)[None, :], (128, 1)))


def host_inputs_l1(inp, core):
    b, half = core // 2, core % 2
    x = np.asarray(inp["x"], np.float32)
    m = dict(host_consts())
    own = x[b, half * NT:(half + 1) * NT]
    prev = x[b, 0:NT] if half == 1 else np.zeros((NT, D), np.float32)
    m["xT_own"] = np.ascontiguousarray(own.T)
    m["xT_prev"] = np.ascontiguousarray(prev.T)
    m["flag"] = np.full((128, 1), float(half), np.float32)
    m["mix_g0"] = fm(inp["mix_norm"][0], 16)
    m["ffn_g0"] = fm(inp["ffn_norm"][0], 16)
    m["kv_g"] = fm(inp["kv_norm"], 16)
    m["w_in"] = np.asarray(inp["ssm_w_in"][0], np.float32)
    cw = np.asarray(inp["ssm_conv_w"][0], np.float32)
    m["conv_w"] = np.ascontiguousarray(cw.T.reshape(48, 128, 4).transpose(1, 0, 2))
    m["conv_b"] = fm(inp["ssm_conv_b"][0], 48)
    m["dt_bias"] = rep(inp["ssm_dt_bias"][0])
    m["a_log"] = rep(inp["ssm_a_log"][0])
    m["d_skip"] = rep(inp["ssm_d"][0])
    m["norm_w"] = fm(inp["ssm_norm_w"][0], 32)
    m["w_out"] = np.asarray(inp["ssm_w_out"][0], np.float32)
    m["w_k"] = np.asarray(inp["w_k"], np.float32)
    m["w_v"] = np.asarray(inp["w_v"], np.float32)
    m["w_router0"] = np.ascontiguousarray(np.concatenate(
        [np.asarray(inp["moe_w_coarse"][0], np.float32), np.asarray(inp["moe_w_fine"][0], np.float32).reshape(D, 16)], axis=1))
    m["b_router0"] = rep(np.concatenate([np.asarray(inp["moe_b_coarse"][0], np.float32),
                                         np.asarray(inp["moe_b_fine"][0], np.float32).reshape(16)]))
    m["w_gate0"] = np.asarray(inp["moe_w_gate"][0], np.float32)
    m["w_up0"] = np.asarray(inp["moe_w_up"][0], np.float32)
    m["w_down0"] = np.asarray(inp["moe_w_down"][0], np.float32)
    return m


L2_INPUTS = [("hT_in", [2048, NT]), ("kT_all", [2048, 2048]), ("v_all", [2048, 2048]),
             ("mix_g1", [128, 16]), ("ffn_g1", [128, 16]), ("final_g", [128, 16]),
             ("w_q", [2048, 2048]), ("w_o", [2048, 2048]),
             ("w_router1", [2048, 20]), ("b_router1", [128, 20]),
             ("w_gate1", [16, 2048, 512]), ("w_up1", [16, 2048, 512]), ("w_down1", [16, 512, 2048]),
             ("mask_rev", [128, 128])]


def wseq_sq(w):
    return [[(w[:, cb * 256:(cb + 1) * 256], NKC, 256)] for cb in range(8)]


def build_launch2(stop="full"):
    nc = bass.Bass("TRN2", target_bir_lowering=False)
    dr = {n: dram_in(nc, n, s) for n, s in CONST_SHAPES.items()}
    I = {n: dram_in(nc, n, s) for n, s in L2_INPUTS}
    out_d = nc.dram_tensor("outT", [2048, NT], F32, kind="ExternalOutput").ap()
    k = K(nc)
    k.setup()
    load_consts(k, dr, ["ident_bf", "ident_f", "ones_f", "inv128"])
    hn_off = k.bump
    hnT, r_hn = k.sb("hnT", [128, 16, NT], BF16)
    hn_end = k.bump
    prm = {}
    mixg = load_small(k, "mix_g1", I["mix_g1"], [128, 16])
    prm["ffn_g"] = [load_small(k, "ffn_g1", I["ffn_g1"], [128, 16])]
    fing = load_small(k, "final_g", I["final_g"], [128, 16])
    prm["b_router"] = [load_small(k, "b_router1", I["b_router1"], [128, 20])]
    prm["w_router"] = [I["w_router1"]]
    prm["w_gate"] = [[I["w_gate1"][e] for e in range(16)]]
    prm["w_up"] = [[I["w_up1"][e] for e in range(16)]]
    prm["w_down"] = [[I["w_down1"][e] for e in range(16)]]
    mrev, r_mrev = load_small(k, "mask_rev", I["mask_rev"], [128, 128])
    BIG = (k.bump + 63) // 64 * 64
    assert SB_TOP - BIG >= 128 * KB
    LV = ["q", "attn", "wo", "moe", "full"]
    lv = LV.index(stop)
    k.wdeclare(wseq_sq(I["w_q"]))
    if lv >= 2:
        k.wdeclare(wseq_sq(I["w_o"]))
    if lv >= 3:
        k.wdeclare(wseq_moe(prm, 0))

    k.begin_phase("q", BIG + 64 * KB)
    hT, r_h = k.sb("hT", [128, 16, NT], F32)
    k.op("sp", lambda: nc.sync.dma_start(out=hT[:], in_=I["hT_in"].rearrange("(kc p) t -> p kc t", p=128)), writes=[r_h], dma=True)
    k.bump = BIG
    k.limit = BIG + 64 * KB
    qT, r_q = k.sb("qT", [128, 16, NT], BF16)
    rmsnorm(k, src_resident(hT, r_h), mixg[0], mixg[1], hnT, r_hn)
    for cb, pieces in enumerate(wseq_sq(I["w_q"])):
        (wq, r_wq), = k.wget(pieces)
        for c2 in range(2):
            hd = cb * 2 + c2
            for th in range(2):
                ts = slice(th * 512, (th + 1) * 512)
                b = k.bank(); bt, rb = k.banks[b]
                for kc in range(NKC):
                    k.op("pe", (lambda kc=kc, ts=ts, bt=bt, wq=wq, c2=c2: nc.tensor.matmul(
                        bt[:], lhsT=wq[:, kc, c2 * 128:(c2 + 1) * 128], rhs=hnT[:, kc, ts], start=(kc == 0), stop=(kc == NKC - 1))),
                        reads=[r_wq, r_hn], writes=[rb])
                k.op("act", (lambda bt=bt, hd=hd, ts=ts: nc.scalar.activation(out=qT[:, hd, ts], in_=bt[:], func=AF.Copy, scale=128 ** -0.5)),
                     reads=[rb], writes=[r_q])
                k.free(b)

    def finish(src_t, r_src, bf):
        if bf:
            o = k.op("pool", lambda: nc.gpsimd.dma_start(out=out_d.rearrange("(kc p) t -> p kc t", p=128), in_=src_t[:]), reads=[r_src], dma=True)
            eng = "pool"
        else:
            o = k.op("sp", lambda: nc.sync.dma_start(out=out_d.rearrange("(kc p) t -> p kc t", p=128), in_=src_t[:]), reads=[r_src], dma=True)
            eng = "sp"
        assert k.widx == len(k.wlist), (k.widx, len(k.wlist))
        cnt = k.p.emit()
        print("launch2: ops", len(k.p.ops), "wtiles", len(k.wlist), "signals", cnt)
        k.p.final_wait(eng, [o])
        nc._knames = k.names
        return nc
    if lv == 0:
        return finish(qT, r_q, True)

    k.begin_phase("attn", BIG + 32 * KB, BIG + 64 * KB)
    es, r_es = k.sb("at_e", [128, 2048], F32)
    cs, r_cs = k.sb("at_cs", [128, 2048], F32)
    at, r_at = k.sb("at_attn", [128, 2048], BF16)
    atT, r_atT = k.sb("at_attnT", [128, 16, 128], BF16)
    kbuf = [k.sb(f"at_k{i}", [128, 2048], BF16) for i in range(2)]
    k.bump = hn_off
    k.limit = hn_end
    vbuf = [k.sb(f"at_v{i}", [128, 16, 128], BF16) for i in range(2)]
    ones_row, r_onr = k.sb("at_ones", [128, 2048], F32)
    k.op("sp", lambda: nc.sync.dma_start(out=ones_row[:], in_=dr["ones_row"]), writes=[r_onr], dma=True)
    onec, r_onec = k.c["one_col"]
    identb, r_identb = k.c["ident_bf"]
    for hd in range(16):
        kt, r_kt = kbuf[hd % 2]
        vt, r_vt = vbuf[hd % 2]
        k.op("pool", (lambda kt=kt, hd=hd: nc.gpsimd.dma_start(out=kt[:], in_=I["kT_all"][hd * 128:(hd + 1) * 128, :])), writes=[r_kt], dma=True)
        k.op("pool", (lambda vt=vt, hd=hd: nc.gpsimd.dma_start(
            out=vt[:], in_=I["v_all"][:, hd * 128:(hd + 1) * 128].rearrange("(blk p) d -> p blk d", p=128))), writes=[r_vt], dma=True)
        for i in range(8):
            qs = slice(i * 128, (i + 1) * 128)
            c0 = (7 - i) * 128
            ncol = 2048 - c0
            nblk = ncol // 128
            nbank = (ncol + 511) // 512
            zb = []
            for j in range(nbank):
                b = k.bank(); bt, rb = k.banks[b]
                w = min(512, ncol - j * 512)
                k.op("pe", (lambda bt=bt, hd=hd, qs=qs, kt=kt, j=j, w=w, c0=c0: nc.tensor.matmul(
                    bt[:, 0:w], lhsT=qT[:, hd, qs], rhs=kt[:, c0 + j * 512: c0 + j * 512 + w], start=True, stop=True)),
                    reads=[r_q, r_kt], writes=[rb])
                zb.append((b, bt, rb, w))
            for j, (b, bt, rb, w) in enumerate(zb):
                k.op("act", (lambda bt=bt, j=j, w=w: nc.scalar.activation(out=es[:, j * 512: j * 512 + w], in_=bt[:, 0:w], func=AF.Exp)),
                     reads=[rb], writes=[r_es])
            k.op("act", (lambda ncol=ncol: nc.scalar.activation(out=es[:, 0:ncol], in_=es[:, 0:ncol], func=AF.Ln, bias=onec[:], scale=1.0)),
                 reads=[r_es, r_onec], writes=[r_es])
            k.op("dve", lambda: nc.vector.tensor_tensor(out=es[:, 0:128], in0=es[:, 0:128], in1=mrev[:], op=ALU.mult),
                 reads=[r_es, r_mrev], writes=[r_es])
            k.op("dve", (lambda ncol=ncol: nc.vector.tensor_tensor_scan(out=cs[:, 0:ncol], data0=ones_row[:, 0:ncol], data1=es[:, 0:ncol],
                                                                       initial=0.0, op0=ALU.mult, op1=ALU.add)),
                 reads=[r_es, r_onr], writes=[r_cs])
            for j, (b, bt, rb, w) in enumerate(zb):
                k.op("dve", (lambda bt=bt, j=j, w=w: nc.vector.tensor_tensor(out=cs[:, j * 512: j * 512 + w], in0=cs[:, j * 512: j * 512 + w],
                                                                            in1=bt[:, 0:w], op=ALU.subtract)),
                     reads=[r_cs, rb], writes=[r_cs])
                k.free(b)
            k.op("act", (lambda ncol=ncol: nc.scalar.activation(out=at[:, 0:ncol], in_=cs[:, 0:ncol], func=AF.Exp, scale=-1.0)),
                 reads=[r_cs], writes=[r_at])
            k.op("dve", lambda: nc.vector.tensor_tensor(out=at[:, 0:128], in0=at[:, 0:128], in1=mrev[:], op=ALU.mult),
                 reads=[r_at, r_mrev], writes=[r_at])
            for j4 in range((nblk + 3) // 4):
                b = k.bank(); bt, rb = k.banks[b]
                nb = min(4, nblk - j4 * 4)
                for jj in range(nb):
                    blk = j4 * 4 + jj
                    k.op("pe", (lambda bt=bt, jj=jj, blk=blk: nc.tensor.matmul(bt[:, jj * 128:(jj + 1) * 128], lhsT=at[:, blk * 128:(blk + 1) * 128],
                                                                               rhs=identb[:], start=True, stop=True)),
                         reads=[r_at, r_identb], writes=[rb])
                k.op("act", (lambda bt=bt, j4=j4, nb=nb: nc.scalar.copy(out=atT[:, j4 * 4:j4 * 4 + nb, :],
                                                                       in_=bt[:, 0:nb * 128].rearrange("p (b q) -> p b q", b=nb))),
                     reads=[rb], writes=[r_atT])
                k.free(b)
            b = k.bank(); bt, rb = k.banks[b]
            for blk in range(nblk):
                k.op("pe", (lambda bt=bt, blk=blk, vt=vt, i=i, nblk=nblk: nc.tensor.matmul(
                    bt[:, 0:128], lhsT=vt[:, (7 - i) + blk, :], rhs=atT[:, blk, :], start=(blk == 0), stop=(blk == nblk - 1))),
                    reads=[r_vt, r_atT], writes=[rb])
            k.op("act", (lambda bt=bt, hd=hd, qs=qs: nc.scalar.copy(out=qT[:, hd, qs], in_=bt[:, 0:128])), reads=[rb], writes=[r_q])
            k.free(b)
    if lv == 1:
        return finish(qT, r_q, True)

    k.begin_phase("wo", BIG + 32 * KB, BIG + 64 * KB)
    for cb, pieces in enumerate(wseq_sq(I["w_o"])):
        (wo, r_wo), = k.wget(pieces)
        for c2 in range(2):
            dc = cb * 2 + c2
            for th in range(2):
                ts = slice(th * 512, (th + 1) * 512)
                b = k.bank(); bt, rb = k.banks[b]
                for kc in range(NKC):
                    k.op("pe", (lambda kc=kc, ts=ts, bt=bt, wo=wo, c2=c2: nc.tensor.matmul(
                        bt[:], lhsT=wo[:, kc, c2 * 128:(c2 + 1) * 128], rhs=qT[:, kc, ts], start=(kc == 0), stop=(kc == NKC - 1))),
                        reads=[r_wo, r_q], writes=[rb])
                k.op("dve", (lambda dc=dc, ts=ts, bt=bt: nc.vector.tensor_tensor(out=hT[:, dc, ts], in0=hT[:, dc, ts], in1=bt[:], op=ALU.add)),
                     reads=[rb, r_h], writes=[r_h])
                k.free(b)
    if lv == 2:
        return finish(hT, r_h, False)
    k.begin_phase("moe1", BIG, BIG + 64 * KB)
    load_consts(k, dr, ["sel16"])
    moe(k, hT, r_h, hnT, r_hn, prm, 0)
    if lv == 3:
        return finish(hT, r_h, False)
    k.begin_phase("fin", BIG, BIG + 64 * KB)
    outT, r_o = k.sb("outT", [128, 16, NT // 2], F32)
    oo = []
    nc_ = nc
    ones, r_ones = k.c["ones_f"]
    epsc, r_eps = k.c["eps_col"]
    for th in range(2):
        ts = slice(th * 512, (th + 1) * 512)
        b = k.bank(); bt, rb = k.banks[b]
        for kc in range(NKC):
            sq, rsq = k.sb(f"rn_sq{kc % 2}", [128, 512], F32)
            k.op("act", (lambda sq=sq, kc=kc, ts=ts: nc.scalar.activation(out=sq[:], in_=hT[:, kc, ts], func=AF.Square)), reads=[r_h], writes=[rsq])
            k.op("pe", (lambda sq=sq, kc=kc, bt=bt: nc.tensor.matmul(bt[:], lhsT=ones[:], rhs=sq[:], start=(kc == 0), stop=(kc == NKC - 1))),
                 reads=[rsq, r_ones], writes=[rb])
        rt, rr = k.sb("rn_rstd", [128, 512], F32)
        k.op("act", (lambda bt=bt, rt=rt: nc.scalar.activation(out=rt[:], in_=bt[:], func=AF.Ln, bias=epsc[:], scale=1.0 / D)), reads=[rb, r_eps], writes=[rr])
        k.free(b)
        k.op("act", (lambda rt=rt: nc.scalar.activation(out=rt[:], in_=rt[:], func=AF.Exp, scale=-0.5)), reads=[rr], writes=[rr])
        for kc in range(NKC):
            k.op("dve", (lambda kc=kc, ts=ts, rt=rt: nc.vector.scalar_tensor_tensor(
                out=outT[:, kc, :], in0=hT[:, kc, ts], scalar=fing[0][:, kc:kc + 1], in1=rt[:], op0=ALU.mult, op1=ALU.mult)),
                reads=[r_h, fing[1], rr], writes=[r_o])
        oo.append(k.op("sp", (lambda ts=ts: nc.sync.dma_start(out=out_d[:, ts].rearrange("(kc p) t -> p kc t", p=128), in_=outT[:])),
                       reads=[r_o], dma=True))
    assert k.widx == len(k.wlist), (k.widx, len(k.wlist))
    cnt = k.p.emit()
    print("launch2: ops", len(k.p.ops), "wtiles", len(k.wlist), "signals", cnt)
    k.p.final_wait("sp", oo)
    return nc


def host_inputs_l2(inp, core, h_out, kT_outs, v_outs):
    b, half = core // 2, core % 2
    m = dict(host_consts())
    m["hT_in"] = np.ascontiguousarray(h_out)
    kown = kT_outs[core][:, ::-1]
    vown = v_outs[core][::-1, :]
    if half == 1:
        kprev = kT_outs[core - 1][:, ::-1]
        vprev = v_outs[core - 1][::-1, :]
    else:
        kprev = np.zeros_like(kown)
        vprev = np.zeros_like(vown)
    m["kT_all"] = np.ascontiguousarray(np.concatenate([kown, kprev], axis=1))
    m["v_all"] = np.ascontiguousarray(np.concatenate([vown, vprev], axis=0))
    m["mix_g1"] = fm(inp["mix_norm"][1], 16)
    m["ffn_g1"] = fm(inp["ffn_norm"][1], 16)
    m["final_g"] = fm(inp["final_norm"], 16)
    m["w_q"] = np.asarray(inp["sb_w_q"][0], np.float32)
    m["w_o"] = np.asarray(inp["sb_w_out"][0], np.float32)
    m["w_router1"] = np.ascontiguousarray(np.concatenate(
        [np.asarray(inp["moe_w_coarse"][1], np.float32), np.asarray(inp["moe_w_fine"][1], np.float32).reshape(D, 16)], axis=1))
    m["b_router1"] = rep(np.concatenate([np.asarray(inp["moe_b_coarse"][1], np.float32),
                                         np.asarray(inp["moe_b_fine"][1], np.float32).reshape(16)]))
    m["w_gate1"] = np.asarray(inp["moe_w_gate"][1], np.float32)
    m["w_up1"] = np.asarray(inp["moe_w_up"][1], np.float32)
    m["w_down1"] = np.asarray(inp["moe_w_down"][1], np.float32)
    i = np.arange(128)
    m["mask_rev"] = (i[None, :] > 127 - i[:, None]).astype(np.float32)
    return m


F_INPUTS = L1_INPUTS + [("mix_g1", [128, 16]), ("ffn_g1", [128, 16]), ("final_g", [128, 16]),
                        ("w_q", [2048, 2048]), ("w_o", [2048, 2048]),
                        ("w_router1", [2048, 20]), ("b_router1", [128, 20]),
                        ("w_gate1", [16, 2048, 512]), ("w_up1", [16, 2048, 512]), ("w_down1", [16, 512, 2048])]


def build_fused():
    nc = bass.Bass("TRN2", target_bir_lowering=False)
    dr = {n: dram_in(nc, n, s) for n, s in CONST_SHAPES.items()}
    I = {n: dram_in(nc, n, s) for n, s in F_INPUTS}
    out_d = nc.dram_tensor("outT", [2048, NT], F32, kind="ExternalOutput").ap()
    kT_loc = nc.dram_tensor("kT_loc", [2048, NT], BF16, kind="Internal").ap()
    v_loc = nc.dram_tensor("v_loc", [NT, 2048], BF16, kind="Internal").ap()
    kT_prev = nc.dram_tensor("kT_prev", [2048, NT], BF16, kind="Internal").ap()
    v_prev = nc.dram_tensor("v_prev", [NT, 2048], BF16, kind="Internal").ap()
    xloc = nc.dram_tensor("xloc", [2048, 512], BF16, kind="Internal").ap()
    xg = nc.dram_tensor("xg", [4096, 512], BF16, kind="Internal").ap()
    k = K(nc)
    k.setup()

    load_consts(k, dr, ["ident_bf", "ident_f", "ones_f", "tri_incl", "ustrict", "mask_le"])
    hn_off = k.bump
    hnT, r_hn = k.sb("hnT", [128, 16, NT], BF16)
    hn_end = k.bump
    prm = {}
    mixg = [load_small(k, "mix_g0", I["mix_g0"], [128, 16]), load_small(k, "mix_g1", I["mix_g1"], [128, 16])]
    prm["ffn_g"] = [load_small(k, "ffn_g0", I["ffn_g0"], [128, 16]), load_small(k, "ffn_g1", I["ffn_g1"], [128, 16])]
    kvg = load_small(k, "kv_g", I["kv_g"], [128, 16])
    fing = load_small(k, "final_g", I["final_g"], [128, 16])
    prm["b_router"] = [load_small(k, "b_router0", I["b_router0"], [128, 20]), load_small(k, "b_router1", I["b_router1"], [128, 20])]
    prm["w_router"] = [I["w_router0"], I["w_router1"]]
    prm["w_gate"] = [[I["w_gate0"][e] for e in range(16)], [I["w_gate1"][e] for e in range(16)]]
    prm["w_up"] = [[I["w_up0"][e] for e in range(16)], [I["w_up1"][e] for e in range(16)]]
    prm["w_down"] = [[I["w_down0"][e] for e in range(16)], [I["w_down1"][e] for e in range(16)]]
    prm["w_in"] = I["w_in"]
    prm["w_out"] = I["w_out"]
    prm["flag"] = load_small(k, "flag", I["flag"], [128, 1])
    flag, r_flag = prm["flag"]
    prm["dtb"] = load_small(k, "dt_bias", I["dt_bias"], [128, 64])
    prm["D_bc"] = load_small(k, "d_skip", I["d_skip"], [128, 64])
    prm["nw"] = load_small(k, "norm_w", I["norm_w"], [128, 32])
    A_bc, r_A = load_small(k, "a_log", I["a_log"], [128, 64])
    k.op("act", lambda: nc.scalar.activation(out=A_bc[:], in_=A_bc[:], func=AF.Exp), reads=[r_A], writes=[r_A])
    k.op("dve", lambda: nc.vector.tensor_scalar(out=A_bc[:], in0=A_bc[:], scalar1=-1.0, scalar2=None, op0=ALU.mult), reads=[r_A], writes=[r_A])
    prm["A_bc"] = (A_bc, r_A)
    cwh, r_cwh = k.sb("p_cwh", [128, 48, 4], F32)
    k.op("sp", lambda: nc.sync.dma_start(out=cwh[:], in_=I["conv_w"]), writes=[r_cwh], dma=True)
    k.op("dve", lambda: nc.vector.tensor_scalar(out=cwh[:], in0=cwh[:], scalar1=0.5, scalar2=None, op0=ALU.mult), reads=[r_cwh], writes=[r_cwh])
    prm["cwh"] = (cwh, r_cwh)
    cbh, r_cbh = load_small(k, "conv_b", I["conv_b"], [128, 48])
    k.op("dve", lambda: nc.vector.tensor_scalar(out=cbh[:], in0=cbh[:], scalar1=0.5, scalar2=None, op0=ALU.mult), reads=[r_cbh], writes=[r_cbh])
    prm["cbh"] = (cbh, r_cbh)
    wdt, r_wdt = k.sb("p_wdt", [128, 16, 64], BF16)
    k.op("pool", lambda: nc.gpsimd.dma_start(out=wdt[:], in_=I["w_in"][:, 10240:10304].rearrange("(kc p) f -> p kc f", p=128)),
         writes=[r_wdt], dma=True)
    prm["wdt"] = (wdt, r_wdt)
    prm["hal"] = k.sb("p_hal", [128, 48, 3], F32)
    sin_off = k.bump
    prm["S_in"] = k.sb("p_Sin", [128, 8, 512], BF16)
    sin_end = k.bump
    BIG = (k.bump + 63) // 64 * 64
    print("fused: perm end", BIG - SB_BASE, "big bytes", SB_TOP - BIG)
    assert SB_TOP - BIG >= 128 * KB

    for g in range(8):
        k.wdeclare(wseq_ssd_group(prm, g, False))
    for g in range(8):
        k.wdeclare(wseq_ssd_group(prm, g, True))
    k.wdeclare(wseq_outproj(prm))
    k.wdeclare(wseq_moe(prm, 0))
    k.wdeclare(wseq_kv(I))
    k.wdeclare(wseq_sq(I["w_q"]))
    k.wdeclare(wseq_sq(I["w_o"]))
    k.wdeclare(wseq_moe(prm, 1))

    k.begin_phase("nA", BIG)
    rmsnorm(k, src_staged(k, I["xT_prev"]), mixg[0][0], mixg[0][1], hnT, r_hn)
    k.begin_phase("sA", BIG)
    P = ssd_prep(k, hnT, r_hn, prm)
    for g in range(8):
        ssd_group(k, g, "A", hnT, r_hn, P, prm)
    k.begin_phase("nB", BIG)
    rmsnorm(k, src_staged(k, I["xT_own"]), mixg[0][0], mixg[0][1], hnT, r_hn)
    k.begin_phase("sB", BIG)
    ynT_all, r_yn = k.sb("ynT_all", [128, 32, NT], BF16)
    P = ssd_prep(k, hnT, r_hn, prm)
    for g in range(8):
        ssd_group(k, g, "B", hnT, r_hn, P, prm, ynT_all, r_yn)
    k.begin_phase("op", BIG + 64 * KB)
    hT, r_h = k.sb("hT", [128, 16, NT], F32)
    k.op("sp", lambda: nc.sync.dma_start(out=hT[:], in_=I["xT_own"].rearrange("(kc p) t -> p kc t", p=128)), writes=[r_h], dma=True)
    out_proj(k, hT, r_h, ynT_all, r_yn, prm)
    k.begin_phase("moe0", BIG, BIG + 64 * KB)
    load_consts(k, dr, ["sel16", "inv128"])
    moe(k, hT, r_h, hnT, r_hn, prm, 0)

    k.begin_phase("kv", BIG, BIG + 64 * KB)
    r_kloc = k.p.reg("kT_loc"); r_vloc = k.p.reg("v_loc"); r_kp = k.p.reg("kT_prev"); r_vp = k.p.reg("v_prev")
    r_xloc = k.p.reg("xloc"); r_xg = k.p.reg("xg")
    rmsnorm(k, src_resident(hT, r_h), kvg[0], kvg[1], hnT, r_hn)
    seq = wseq_kv(I)
    for cb in range(8):
        (wk, r_wk), = k.wget(seq[cb])
        st, r_st = k.sb(f"kv_st{cb % 2}", [128, 2, NT], BF16)
        for c2 in range(2):
            for th in range(2):
                ts = slice(th * 512, (th + 1) * 512)
                b = k.bank(); bt, rb = k.banks[b]
                for kc in range(NKC):
                    k.op("pe", (lambda kc=kc, ts=ts, bt=bt, wk=wk, c2=c2: nc.tensor.matmul(
                        bt[:], lhsT=wk[:, kc, c2 * 128:(c2 + 1) * 128], rhs=hnT[:, kc, ts],
                        start=(kc == 0), stop=(kc == NKC - 1))), reads=[r_wk, r_hn], writes=[rb])
                k.op("act", (lambda bt=bt, st=st, c2=c2, ts=ts: nc.scalar.copy(out=st[:, c2, ts], in_=bt[:])), reads=[rb], writes=[r_st])
                k.free(b)
        k.op("sp", (lambda st=st, cb=cb: nc.sync.dma_start(
            out=kT_loc[cb * 256:(cb + 1) * 256, :].rearrange("(c p) t -> p c t", p=128), in_=st[:])), reads=[r_st], writes=[r_kloc], dma=True)
    for cb in range(8):
        (wv, r_wv), = k.wget(seq[8 + cb])
        st, r_st = k.sb(f"kv_st{cb % 2}", [128, 2, NT], BF16)
        stv = st[:].rearrange("p c t -> p (c t)").rearrange("p (t f) -> p t f", f=256)
        for t2 in range(4):
            b = k.bank(); bt, rb = k.banks[b]
            for ti in range(2):
                tb = t2 * 2 + ti
                for kc in range(NKC):
                    k.op("pe", (lambda kc=kc, tb=tb, ti=ti, bt=bt, wv=wv: nc.tensor.matmul(
                        bt[:, ti * 256:(ti + 1) * 256], lhsT=hnT[:, kc, tb * 128:(tb + 1) * 128], rhs=wv[:, kc, :],
                        start=(kc == 0), stop=(kc == NKC - 1))), reads=[r_wv, r_hn], writes=[rb])
            k.op("act", (lambda bt=bt, stv=stv, t2=t2: nc.scalar.copy(
                out=stv[:, 2 * t2:2 * t2 + 2, :], in_=bt[:].rearrange("p (t f) -> p t f", f=256))), reads=[rb], writes=[r_st])
            k.free(b)
        k.op("sp", (lambda stv=stv, cb=cb: nc.sync.dma_start(
            out=v_loc[:, cb * 256:(cb + 1) * 256].rearrange("(t p) f -> p t f", p=128), in_=stv)), reads=[r_st], writes=[r_vloc], dma=True)

    k.begin_phase("q", BIG, BIG + 64 * KB)
    rg = [[0, 1], [2, 3], [4, 5], [6, 7]]
    xl_v = xloc.rearrange("(t a) c -> t (a c)", a=4)
    xg_v = xg[0:2048, :].rearrange("(t a) c -> t (a c)", a=4)
    rounds = [(kT_loc[:, 0:512], xloc, xg[0:2048, :], kT_prev[:, 0:512], r_kloc, r_kp),
              (kT_loc[:, 512:1024], xloc, xg[0:2048, :], kT_prev[:, 512:1024], r_kloc, r_kp),
              (v_loc[0:512, :], xl_v, xg_v, v_prev[0:512, :], r_vloc, r_vp),
              (v_loc[512:1024, :], xl_v, xg_v, v_prev[512:1024, :], r_vloc, r_vp)]
    for (src, xin, xout, dst, r_src, r_dst) in rounds:
        k.op("sp", (lambda src=src, xin=xin: nc.sync.dma_start(out=xin, in_=src)), reads=[r_src], writes=[r_xloc], dma=True)
        k.op("pool", lambda: nc.gpsimd.collective_compute("AllGather", op=ALU.bypass, replica_groups=rg, ins=[xloc], outs=[xg]),
             reads=[r_xloc], writes=[r_xg])
        k.op("sp", (lambda xout=xout, dst=dst: nc.sync.dma_start(out=dst, in_=xout)), reads=[r_xg], writes=[r_dst], dma=True)
    qT, r_q = k.sb("qT", [128, 16, NT], BF16)
    rmsnorm(k, src_resident(hT, r_h), mixg[1][0], mixg[1][1], hnT, r_hn)
    for cb, pieces in enumerate(wseq_sq(I["w_q"])):
        (wq, r_wq), = k.wget(pieces)
        for c2 in range(2):
            hd = cb * 2 + c2
            for th in range(2):
                ts = slice(th * 512, (th + 1) * 512)
                b = k.bank(); bt, rb = k.banks[b]
                for kc in range(NKC):
                    k.op("pe", (lambda kc=kc, ts=ts, bt=bt, wq=wq, c2=c2: nc.tensor.matmul(
                        bt[:], lhsT=wq[:, kc, c2 * 128:(c2 + 1) * 128], rhs=hnT[:, kc, ts], start=(kc == 0), stop=(kc == NKC - 1))),
                        reads=[r_wq, r_hn], writes=[rb])
                k.op("act", (lambda bt=bt, hd=hd, ts=ts: nc.scalar.activation(out=qT[:, hd, ts], in_=bt[:], func=AF.Copy, scale=128 ** -0.5)),
                     reads=[rb], writes=[r_q])
                k.free(b)

    k.begin_phase("attn", BIG + 32 * KB, BIG + 64 * KB)
    sets = [dict(), dict()]
    sets[0]["es"] = k.sb("at_e0", [128, 2048], F32)
    sets[0]["cs"] = k.sb("at_cs0", [128, 2048], F32)
    sets[0]["at"] = k.sb("at_attn0", [128, 2048], BF16)
    sets[0]["atT"] = k.sb("at_attnT0", [128, 16, 128], BF16)
    kbuf = [k.sb(f"at_k{i}", [128, 2048], BF16) for i in range(2)]
    k.bump = hn_off
    k.limit = hn_end
    sets[1]["es"] = k.sb("at_e1", [128, 2048], F32)
    sets[1]["cs"] = k.sb("at_cs1", [128, 2048], F32)
    sets[1]["at"] = k.sb("at_attn1", [128, 2048], BF16)
    sets[1]["atT"] = k.sb("at_attnT1", [128, 16, 128], BF16)
    vbuf = [k.sb(f"at_v{i}", [128, 16, 128], BF16) for i in range(2)]
    k.bump = sin_off
    k.limit = sin_end
    load_consts(k, dr, ["mask_lt"])
    sets[0]["negT"] = k.sb("at_negT0", [128, 1], F32)
    sets[1]["negT"] = k.sb("at_negT1", [128, 1], F32)
    onec, r_onec = k.c["one_col"]
    identb, r_identb = k.c["ident_bf"]
    mlt, r_mlt = k.c["mask_lt"]

    rq = {(hd_, i_): k.p.reg(f"q{hd_}_{i_}") for hd_ in range(16) for i_ in range(8)}

    def load_kv(hd):
        kt, r_kt = kbuf[hd % 2]
        vt, r_vt = vbuf[hd % 2]
        k.op("sp", (lambda: nc.sync.dma_start(out=kt[:, 0:NT], in_=kT_prev[hd * 128:(hd + 1) * 128, :])), writes=[r_kt], dma=True)
        k.op("sp", (lambda: nc.sync.dma_start(out=kt[:, NT:2 * NT], in_=kT_loc[hd * 128:(hd + 1) * 128, :])), writes=[r_kt], dma=True)
        k.op("sp", (lambda: nc.sync.dma_start(
            out=vt[:, 0:8, :], in_=v_prev[:, hd * 128:(hd + 1) * 128].rearrange("(blk p) d -> p blk d", p=128))), writes=[r_vt], dma=True)
        k.op("sp", (lambda: nc.sync.dma_start(
            out=vt[:, 8:16, :], in_=v_loc[:, hd * 128:(hd + 1) * 128].rearrange("(blk p) d -> p blk d", p=128))), writes=[r_vt], dma=True)
        k.op("pool", (lambda: nc.gpsimd.tensor_scalar(out=vt[:, 0:8, :], in0=vt[:, 0:8, :], scalar1=flag[:], scalar2=None, op0=ALU.mult)),
             reads=[r_vt, r_flag], writes=[r_vt])

    def stage1(n, hd, i):
        S = sets[n % 2]
        es, r_es = S["es"]; cs, r_cs = S["cs"]; ngT, r_ngT = S["negT"]
        kt, r_kt = kbuf[hd % 2]
        qs = slice(i * 128, (i + 1) * 128)
        nblk = 9 + i
        ncol = nblk * 128
        nbank = (ncol + 511) // 512
        zb = []
        for j in range(nbank):
            b = k.bank(); bt, rb = k.banks[b]
            w = min(512, ncol - j * 512)
            k.op("pe", (lambda bt=bt, j=j, w=w: nc.tensor.matmul(
                bt[:, 0:w], lhsT=qT[:, hd, qs], rhs=kt[:, j * 512: j * 512 + w], start=True, stop=True)),
                reads=[rq[(hd, i)], r_kt], writes=[rb])
            zb.append((b, bt, rb, w))
        for j, (b, bt, rb, w) in enumerate(zb):
            k.op("act", (lambda bt=bt, j=j, w=w: nc.scalar.activation(out=cs[:, j * 512: j * 512 + w], in_=bt[:, 0:w], func=AF.Exp)),
                 reads=[rb], writes=[r_cs])
        k.op("act", (lambda: nc.scalar.activation(out=es[:, 0:ncol], in_=cs[:, 0:ncol], func=AF.Ln, bias=onec[:], scale=1.0)),
             reads=[r_cs, r_onec], writes=[r_es])
        k.op("dve", (lambda: nc.vector.tensor_tensor(out=es[:, ncol - 128:ncol], in0=es[:, ncol - 128:ncol], in1=mlt[:], op=ALU.mult)),
             reads=[r_es, r_mlt], writes=[r_es])
        k.op("dve", (lambda: nc.vector.tensor_tensor_scan(out=cs[:, 0:ncol], data0=onec[:].to_broadcast([128, ncol]), data1=es[:, 0:ncol],
                                                          initial=0.0, op0=ALU.mult, op1=ALU.add)),
             reads=[r_es, r_onec], writes=[r_cs])
        k.op("dve", (lambda: nc.vector.tensor_scalar(out=ngT[:], in0=cs[:, ncol - 1:ncol], scalar1=-1.0, scalar2=None, op0=ALU.mult)),
             reads=[r_cs], writes=[r_ngT])
        for j, (b, bt, rb, w) in enumerate(zb):
            lo = j * 512
            if j == 0:
                k.op("dve", (lambda bt=bt: nc.vector.tensor_copy(out=es[:, 0:1], in_=bt[:, 0:1])), reads=[rb], writes=[r_es])
                k.op("dve", (lambda bt=bt, w=w: nc.vector.tensor_tensor(out=es[:, 1:w], in0=cs[:, 0:w - 1], in1=bt[:, 1:w], op=ALU.add)),
                     reads=[r_cs, rb], writes=[r_es])
            else:
                k.op("dve", (lambda bt=bt, w=w, lo=lo: nc.vector.tensor_tensor(out=es[:, lo:lo + w], in0=cs[:, lo - 1:lo + w - 1], in1=bt[:, 0:w], op=ALU.add)),
                     reads=[r_cs, rb], writes=[r_es])
            k.free(b)

    def stage2(n, hd, i):
        S = sets[n % 2]
        es, r_es = S["es"]; at, r_at = S["at"]; atT, r_atT = S["atT"]; ngT, r_ngT = S["negT"]
        nblk = 9 + i
        ncol = nblk * 128
        k.op("act", (lambda: nc.scalar.activation(out=at[:, 0:ncol], in_=es[:, 0:ncol], func=AF.Exp, bias=ngT[:], scale=1.0)),
             reads=[r_es, r_ngT], writes=[r_at])
        k.op("dve", (lambda: nc.vector.tensor_tensor(out=at[:, ncol - 128:ncol], in0=at[:, ncol - 128:ncol], in1=mlt[:], op=ALU.mult)),
             reads=[r_at, r_mlt], writes=[r_at])
        for j4 in range((nblk + 3) // 4):
            b = k.bank(); bt, rb = k.banks[b]
            nb = min(4, nblk - j4 * 4)
            for jj in range(nb):
                blk = j4 * 4 + jj
                k.op("pe", (lambda bt=bt, jj=jj, blk=blk: nc.tensor.matmul(bt[:, jj * 128:(jj + 1) * 128], lhsT=at[:, blk * 128:(blk + 1) * 128],
                                                                           rhs=identb[:], start=True, stop=True)),
                     reads=[r_at, r_identb], writes=[rb])
            eng = "act" if j4 % 2 == 0 else "dve"
            if eng == "act":
                k.op("act", (lambda bt=bt, j4=j4, nb=nb: nc.scalar.copy(out=atT[:, j4 * 4:j4 * 4 + nb, :],
                                                                       in_=bt[:, 0:nb * 128].rearrange("p (b q) -> p b q", b=nb))),
                     reads=[rb], writes=[r_atT])
            else:
                k.op("dve", (lambda bt=bt, j4=j4, nb=nb: nc.vector.tensor_copy(out=atT[:, j4 * 4:j4 * 4 + nb, :],
                                                                              in_=bt[:, 0:nb * 128].rearrange("p (b q) -> p b q", b=nb))),
                     reads=[rb], writes=[r_atT])
            k.free(b)

    def stage3(n, hd, i):
        S = sets[n % 2]
        atT, r_atT = S["atT"]
        vt, r_vt = vbuf[hd % 2]
        qs = slice(i * 128, (i + 1) * 128)
        nblk = 9 + i
        b = k.bank(); bt, rb = k.banks[b]
        for blk in range(nblk):
            k.op("pe", (lambda bt=bt, blk=blk: nc.tensor.matmul(
                bt[:, 0:128], lhsT=vt[:, blk, :], rhs=atT[:, blk, :], start=(blk == 0), stop=(blk == nblk - 1))),
                reads=[r_vt, r_atT], writes=[rb])
        k.op("act", (lambda bt=bt: nc.scalar.copy(out=qT[:, hd, qs], in_=bt[:, 0:128])), reads=[rb], writes=[rq[(hd, i)]])
        k.free(b)

    its = [(hd, i) for hd in range(16) for i in range(8)]
    load_kv(0)
    stage1(0, *its[0])
    for n, (hd, i) in enumerate(its):
        if i == 2 and hd + 1 < 16:
            load_kv(hd + 1)
        if n + 1 < len(its):
            stage1(n + 1, *its[n + 1])
        stage2(n, hd, i)
        stage3(n, hd, i)

    k.begin_phase("wo", BIG + 32 * KB, BIG + 64 * KB)
    for cb, pieces in enumerate(wseq_sq(I["w_o"])):
        (wo, r_wo), = k.wget(pieces)
        for c2 in range(2):
            dc = cb * 2 + c2
            for th in range(2):
                ts = slice(th * 512, (th + 1) * 512)
                b = k.bank(); bt, rb = k.banks[b]
                for kc in range(NKC):
                    k.op("pe", (lambda kc=kc, ts=ts, bt=bt, wo=wo, c2=c2: nc.tensor.matmul(
                        bt[:], lhsT=wo[:, kc, c2 * 128:(c2 + 1) * 128], rhs=qT[:, kc, ts], start=(kc == 0), stop=(kc == NKC - 1))),
                        reads=[r_wo, r_q], writes=[rb])
                k.op("dve", (lambda dc=dc, ts=ts, bt=bt: nc.vector.tensor_tensor(out=hT[:, dc, ts], in0=hT[:, dc, ts], in1=bt[:], op=ALU.add)),
                     reads=[rb, r_h], writes=[r_h])
                k.free(b)
    k.begin_phase("moe1", BIG, BIG + 64 * KB)
    load_consts(k, dr, ["sel16", "inv128"])
    moe(k, hT, r_h, hnT, r_hn, prm, 1)
    k.begin_phase("fin", BIG, BIG + 64 * KB)
    outT, r_o = k.sb("outT", [128, 16, NT // 2], F32)
    oo = []
    ones, r_ones = k.c["ones_f"]
    epsc, r_eps = k.c["eps_col"]
    for th in range(2):
        ts = slice(th * 512, (th + 1) * 512)
        b = k.bank(); bt, rb = k.banks[b]
        for kc in range(NKC):
            sq, rsq = k.sb(f"rn_sq{kc % 2}", [128, 512], F32)
            k.op("act", (lambda sq=sq, kc=kc, ts=ts: nc.scalar.activation(out=sq[:], in_=hT[:, kc, ts], func=AF.Square)), reads=[r_h], writes=[rsq])
            k.op("pe", (lambda sq=sq, kc=kc, bt=bt: nc.tensor.matmul(bt[:], lhsT=ones[:], rhs=sq[:], start=(kc == 0), stop=(kc == NKC - 1))),
                 reads=[rsq, r_ones], writes=[rb])
        rt, rr = k.sb("rn_rstd", [128, 512], F32)
        k.op("act", (lambda bt=bt, rt=rt: nc.scalar.activation(out=rt[:], in_=bt[:], func=AF.Ln, bias=epsc[:], scale=1.0 / D)), reads=[rb, r_eps], writes=[rr])
        k.free(b)
        k.op("act", (lambda rt=rt: nc.scalar.activation(out=rt[:], in_=rt[:], func=AF.Exp, scale=-0.5)), reads=[rr], writes=[rr])
        for kc in range(NKC):
            k.op("dve", (lambda kc=kc, ts=ts, rt=rt: nc.vector.scalar_tensor_tensor(
                out=outT[:, kc, :], in0=hT[:, kc, ts], scalar=fing[0][:, kc:kc + 1], in1=rt[:], op0=ALU.mult, op1=ALU.mult)),
                reads=[r_h, fing[1], rr], writes=[r_o])
        oo.append(k.op("sp", (lambda ts=ts: nc.sync.dma_start(out=out_d[:, ts].rearrange("(kc p) t -> p kc t", p=128), in_=outT[:])),
                       reads=[r_o], dma=True))
    assert k.widx == len(k.wlist), (k.widx, len(k.wlist))
    cnt = k.p.emit()
    print("fused: ops", len(k.p.ops), "wtiles", len(k.wlist), "signals", cnt)
    k.p.final_wait("sp", oo)
    return nc


def host_inputs_fused(inp, core):
    m = host_inputs_l1(inp, core)
    m["mix_g1"] = fm(inp["mix_norm"][1], 16)
    m["ffn_g1"] = fm(inp["ffn_norm"][1], 16)
    m["final_g"] = fm(inp["final_norm"], 16)
    m["w_q"] = np.asarray(inp["sb_w_q"][0], np.float32)
    m["w_o"] = np.asarray(inp["sb_w_out"][0], np.float32)
    m["w_router1"] = np.ascontiguousarray(np.concatenate(
        [np.asarray(inp["moe_w_coarse"][1], np.float32), np.asarray(inp["moe_w_fine"][1], np.float32).reshape(D, 16)], axis=1))
    m["b_router1"] = rep(np.concatenate([np.asarray(inp["moe_b_coarse"][1], np.float32),
                                         np.asarray(inp["moe_b_fine"][1], np.float32).reshape(16)]))
    m["w_gate1"] = np.asarray(inp["moe_w_gate"][1], np.float32)
    m["w_up1"] = np.asarray(inp["moe_w_up"][1], np.float32)
    m["w_down1"] = np.asarray(inp["moe_w_down"][1], np.float32)
    return m


_PROGS = {}


def kernel(**inputs):
    inp = {k_: np.asarray(v) for k_, v in inputs.items()}
    ncores = 8
    names = set(CONST_SHAPES) | {n for n, _ in F_INPUTS}
    if "fused" not in _PROGS:
        _PROGS["fused"] = build_fused()
    nc = _PROGS["fused"]
    ims = []
    for c in range(ncores):
        m = host_inputs_fused(inp, c)
        ims.append({n: v for n, v in m.items() if n in names})
    res = run_bass_kernel_spmd(nc, ims, core_ids=list(range(ncores)))
    out = np.zeros((4, 2048, 2048), np.float32)
    for c in range(ncores):
        b, half = c // 2, c % 2
        out[b, half * NT:(half + 1) * NT, :] = np.asarray(res.results[c]["outT"], np.float32).T
    return out
```

```python
from concourse.bass_utils import run_bass_kernel_spmd
import numpy as np
from contextlib import ExitStack
import concourse.bass as bass
import concourse.mybir as mybir

F32 = mybir.dt.float32
BF16 = mybir.dt.bfloat16
AF = mybir.ActivationFunctionType
ALU = mybir.AluOpType
AX = mybir.AxisListType


class Reg:
    __slots__ = ("name", "w", "r")

    def __init__(self, name):
        self.name = name
        self.w = None
        self.r = []


class Prog:
    ENGS = ("pe", "act", "dve", "pool", "sp")

    def __init__(self, nc, n_dma_sems=20):
        self.nc = nc
        self.es = ExitStack()
        self.ops = []
        self.eng_obj = {"pe": nc.tensor, "act": nc.scalar, "dve": nc.vector,
                        "pool": nc.gpsimd, "sp": nc.sync}
        self.n_dma_sems = n_dma_sems
        self._uid = 0
        self.allregs = []
        self.bar_from = 0
        self.tag = None
        self.scopes = False

    def sb(self, shape, dt, name=None):
        self._uid += 1
        return self.es.enter_context(self.nc.sbuf_tensor(name or f"sb{self._uid}", list(shape), dt))

    def ps(self, shape, dt, name=None):
        self._uid += 1
        return self.es.enter_context(self.nc.psum_tensor(name or f"ps{self._uid}", list(shape), dt))

    def reg(self, name=None):
        self._uid += 1
        r = Reg(name or f"r{self._uid}")
        self.allregs.append(r)
        return r

    def barrier(self):
        last = {}
        dmas = []
        for oid in range(self.bar_from, len(self.ops)):
            o = self.ops[oid]
            if o["dma"]:
                dmas.append(oid)
            else:
                last[o["eng"]] = oid
        deps = set(last.values()) | set(dmas)
        for e in self.ENGS:
            self.ops.append(dict(eng=e, fn=(lambda: None), deps=set(deps), dma=False, tag=self.tag))
        self.bar_from = len(self.ops)
        for r in self.allregs:
            r.w = None
            r.r = []

    def op(self, eng, fn, reads=(), writes=(), dma=False, nosync_same_pe=True):
        oid = len(self.ops)
        deps = set()
        for r in reads:
            if r.w is not None:
                deps.add(r.w)
        for w in writes:
            if w.w is not None:
                deps.add(w.w)
            deps.update(w.r)
        for r in reads:
            r.r.append(oid)
        for w in writes:
            w.w = oid
            w.r = []
        deps.discard(oid)
        self.ops.append(dict(eng=eng, fn=fn, deps=deps, dma=dma, tag=self.tag))
        return oid

    def emit(self):
        nc = self.nc
        ops = self.ops
        need = [False] * len(ops)
        for o in ops:
            for d in list(o["deps"]):
                od = ops[d]
                if od["eng"] == "pe" and o["eng"] == "pe" and not od["dma"] and not o["dma"]:
                    o["deps"].discard(d)
                    continue
                need[d] = True
        esem = {e: self.es.enter_context(nc.semaphore(f"s_{e}")) for e in self.ENGS}
        dsem = {}
        for q in ("sp", "pool"):
            dsem[q] = [self.es.enter_context(nc.semaphore(f"d_{q}{i}")) for i in range(self.n_dma_sems)]
        ecount = {e: 0 for e in self.ENGS}
        dcount = {q: [0] * self.n_dma_sems for q in dsem}
        drr = {q: 0 for q in dsem}
        sig = [None] * len(ops)
        waited = {}
        cur_tag = None
        for oid, o in enumerate(ops):
            eng = o["eng"]
            E = self.eng_obj[eng]
            if self.scopes and o.get("tag") != cur_tag:
                if cur_tag is not None:
                    nc.leave_named_scope(cur_tag, cur_sid, False)
                cur_tag = o.get("tag")
                if cur_tag is not None:
                    cur_sid, _ = nc.enter_named_scope(cur_tag, False)
            wl = {}
            for d in o["deps"]:
                s, v = sig[d]
                k = id(s)
                if k not in wl or wl[k][1] < v:
                    wl[k] = (s, v)
            if o["dma"]:
                slot = drr[eng]
                drr[eng] = (slot + 1) % self.n_dma_sems
                ds = dsem[eng][slot]
                prev = dcount[eng][slot]
                if prev > 0:
                    k = id(ds)
                    if k not in wl or wl[k][1] < prev:
                        wl[k] = (ds, prev)
            for k, (ws, wv) in wl.items():
                if waited.get((eng, k), 0) >= wv:
                    continue
                waited[(eng, k)] = wv
                E.wait_ge(ws, wv)
            inst = o["fn"]()
            if inst is None:
                continue
            if o["dma"]:
                dcount[eng][slot] = prev + 16
                inst.then_inc(ds, 16)
                sig[oid] = (ds, prev + 16)
            elif need[oid]:
                ecount[eng] += 1
                inst.then_inc(esem[eng], 1)
                sig[oid] = (esem[eng], ecount[eng])
        if self.scopes and cur_tag is not None:
            nc.leave_named_scope(cur_tag, cur_sid, False)
        self.sig = sig
        self.esem = esem
        return ecount

    def final_wait(self, eng, oids):
        E = self.eng_obj[eng]
        for oid in oids:
            s, v = self.sig[oid]
            E.wait_ge(s, v)


import numpy as np

D = 2048
NT = 1024
NKC = 16
EPS = 1e-5
NE = 16
DE = 512


SB_BASE = 16512
SB_TOP = 229344


class K:
    def __init__(self, nc, nw=4, look=2):
        self.nc = nc
        self.p = Prog(nc)
        self.cache = {}
        self.NW = nw
        self.LOOK = look
        self.wlist = []
        self.widx = 0
        self.wloaded = 0
        self.bank_live = [False] * 8
        self.bank_rr = 0
        self.bump = SB_BASE
        self.limit = SB_TOP
        self.phase = "perm"
        self.uid = 0

    def sb(self, name, shape, dt):
        key = (self.phase, name)
        if key not in self.cache:
            n = 1
            for d in shape[1:]:
                n *= d
            nbytes = n * (4 if dt == F32 else 2)
            nbytes = (nbytes + 31) // 32 * 32
            off = self.bump
            assert off + nbytes <= self.limit, f"SBUF overflow allocating {name} in phase {self.phase}: {off}+{nbytes} > {self.limit}"
            self.bump = off + nbytes
            self.uid += 1
            t = self.nc.alloc_sbuf_tensor_at(f"{self.phase}_{name}_{self.uid}", list(shape), dt, offset=off)
            self.names = getattr(self, "names", {})
            self.names[key] = t.name
            self.cache[key] = (t, self.p.reg(name))
        return self.cache[key]

    def begin_phase(self, name, start, limit=SB_TOP):
        self.p.barrier()
        assert not any(self.bank_live)
        self.p.tag = name
        self.phase = name
        self.bump = start
        self.limit = limit

    def setup(self):
        p = self.p
        self.banks = [(p.ps([128, 512], F32, name=f"bank{i}"), p.reg(f"bank{i}")) for i in range(8)]
        self.bankregs = {id(r) for _, r in self.banks}
        self.wring = []
        for i in range(self.NW):
            t, _ = self.sb(f"wr{i}", [128, 4096], BF16)
            r1 = p.reg(f"wr{i}")
            self.wring.append((t, [r1, r1]))

    def op(self, eng, fn, reads=(), writes=(), dma=False):
        if eng != "pe":
            bs = self.bankregs
            extra = [r for r in reads if id(r) in bs]
            if extra:
                writes = list(writes) + [r for r in extra if r not in writes]
        return self.p.op(eng, fn, reads=reads, writes=writes, dma=dma)

    def bank(self):
        for i in range(8):
            j = (self.bank_rr + i) % 8
            if not self.bank_live[j]:
                self.bank_live[j] = True
                self.bank_rr = (j + 1) % 8
                return j
        raise RuntimeError("out of PSUM banks")

    def free(self, j):
        assert self.bank_live[j]
        self.bank_live[j] = False

    def wdeclare(self, seq):
        self.wlist.extend(seq)

    def wget(self, pieces):
        key = [(str(pc[0]), pc[1], pc[2]) for pc in pieces]
        i = self.widx
        self.widx += 1
        assert [(str(pc[0]), pc[1], pc[2]) for pc in self.wlist[i]] == key, f"weight order mismatch at {i}"
        hi = min(len(self.wlist), i + 1 + self.LOOK)
        while self.wloaded < hi:
            self._wload(self.wloaded)
            self.wloaded += 1
        t, regs = self.wring[i % self.NW]
        return self._wviews(t, regs, pieces)

    def _wviews(self, t, regs, pieces):
        out = []
        off = 0
        for pi, (src, kc, nco) in enumerate(pieces):
            v = t[:, off:off + kc * nco].rearrange("p (kc f) -> p kc f", kc=kc)
            out.append((v, regs[pi]))
            off += kc * nco
        assert off <= 4096
        return out

    def _wload(self, j):
        nc = self.nc
        t, regs = self.wring[j % self.NW]
        views = self._wviews(t, regs, self.wlist[j])
        for (v, r), (src, kc, nco) in zip(views, self.wlist[j]):
            self.p.op("pool", (lambda v=v, src=src: nc.gpsimd.dma_start(
                out=v, in_=src.rearrange("(kc p) f -> p kc f", p=128))), writes=[r], dma=True)


def load_consts(k, dr, names):
    nc = k.nc
    c = getattr(k, "c", {})
    for name in names:
        shape = CONST_SHAPES[name]
        dt = BF16 if name == "ident_bf" else F32
        t, r = k.sb("c_" + name, shape, dt)
        if dt == BF16:
            k.op("pool", (lambda t=t, name=name: nc.gpsimd.dma_start(out=t[:], in_=dr[name])), writes=[r], dma=True)
        else:
            k.op("sp", (lambda t=t, name=name: nc.sync.dma_start(out=t[:], in_=dr[name])), writes=[r], dma=True)
        c[name] = (t, r)
    for nm, val in (("eps_col", EPS), ("one_col", 1.0), ("eps4_col", 4 * EPS)):
        if nm not in c:
            t, r = k.sb("c_" + nm, [128, 1], F32)
            k.op("pool", (lambda t=t, val=val: nc.gpsimd.memset(t[:], val)), writes=[r])
            c[nm] = (t, r)
    k.c = c


def host_consts():
    i = np.arange(128)
    h = {}
    h["ident_bf"] = np.eye(128, dtype=np.float32)
    h["ident_f"] = np.eye(128, dtype=np.float32)
    h["ones_f"] = np.ones((128, 128), np.float32)
    h["tri_incl"] = (i[:, None] <= i[None, :]).astype(np.float32)
    h["ustrict"] = (i[:, None] > i[None, :]).astype(np.float32)
    h["mask_le"] = (i[:, None] <= i[None, :]).astype(np.float32)
    h["mask_lt"] = (i[None, :] < i[:, None]).astype(np.float32)
    sel = np.zeros((16, 16, 128), np.float32)
    for e in range(16):
        sel[e, e, :] = 1.0
    h["sel16"] = sel.reshape(16, 16 * 128)
    h["inv128"] = np.full((128, 1), 1.0 / 128, np.float32)
    h["ones_row"] = np.ones((128, 2048), np.float32)
    return h


CONST_SHAPES = {"ident_bf": [128, 128], "ident_f": [128, 128], "ones_f": [128, 128], "tri_incl": [128, 128],
                "ustrict": [128, 128], "mask_le": [128, 128], "mask_lt": [128, 128], "sel16": [16, 2048],
                "inv128": [128, 1], "ones_row": [128, 2048]}


def rmsnorm(k, src, g, r_g, outT, r_out, ntok=NT, want_rstd=None):
    nc = k.nc
    ones, r_ones = k.c["ones_f"]
    epsc, r_eps = k.c["eps_col"]
    for th in range(ntok // 512):
        ts = slice(th * 512, (th + 1) * 512)
        view, r_h = src(th)
        b = k.bank()
        bt, rb = k.banks[b]
        for kc in range(NKC):
            sq, rsq = k.sb(f"rn_sq{kc % 2}", [128, 512], F32)
            k.op("act", (lambda sq=sq, kc=kc, view=view: nc.scalar.activation(out=sq[:], in_=view(kc), func=AF.Square)),
                 reads=[r_h], writes=[rsq])
            k.op("pe", (lambda sq=sq, kc=kc, bt=bt: nc.tensor.matmul(bt[:], lhsT=ones[:], rhs=sq[:],
                                                                     start=(kc == 0), stop=(kc == NKC - 1))),
                 reads=[rsq, r_ones], writes=[rb])
        if want_rstd is not None:
            rt, rr = want_rstd
            rview = rt[:, ts]
        else:
            rt, rr = k.sb("rn_rstd", [128, 512], F32)
            rview = rt[:]
        k.op("act", (lambda bt=bt, rview=rview: nc.scalar.activation(out=rview, in_=bt[:], func=AF.Ln, bias=epsc[:], scale=1.0 / D)),
             reads=[rb, r_eps], writes=[rr])
        k.free(b)
        k.op("act", (lambda rview=rview: nc.scalar.activation(out=rview, in_=rview, func=AF.Exp, scale=-0.5)),
             reads=[rr], writes=[rr])
        for kc in range(NKC):
            k.op("dve", (lambda kc=kc, ts=ts, rview=rview, view=view: nc.vector.scalar_tensor_tensor(
                out=outT[:, kc, ts], in0=view(kc), scalar=g[:, kc:kc + 1], in1=rview,
                op0=ALU.mult, op1=ALU.mult)), reads=[r_h, r_g, rr], writes=[r_out])


def src_resident(hT, r_h):
    return lambda th: ((lambda kc, th=th: hT[:, kc, th * 512:(th + 1) * 512]), r_h)


def src_staged(k, dram_xT):
    nc = k.nc

    def f(th):
        st, r_st = k.sb("rn_stage", [128, NKC, 512], F32)
        k.op("sp", (lambda st=st, th=th: nc.sync.dma_start(
            out=st[:], in_=dram_xT[:, th * 512:(th + 1) * 512].rearrange("(kc p) t -> p kc t", p=128))), writes=[r_st], dma=True)
        return (lambda kc, st=st: st[:, kc, :]), r_st
    return f


def wseq_moe(prm, l):
    wg, wu, wd = prm["w_gate"][l], prm["w_up"][l], prm["w_down"][l]
    seq = []
    for e in range(NE):
        for fh in range(2):
            seq.append([(wg[e][:, fh * 256:(fh + 1) * 256], NKC, 256)])
            seq.append([(wu[e][:, fh * 256:(fh + 1) * 256], NKC, 256)])
        for dq in range(2):
            seq.append([(wd[e][:, dq * 1024:(dq + 1) * 1024], 4, 1024)])
    return seq


def moe(k, hT, r_h, hnT, r_hn, prm, l):
    nc = k.nc
    c = k.c
    g, r_g = prm["ffn_g"][l]
    rstd, r_rstd = k.sb("moe_rstd", [128, NT], F32)
    rmsnorm(k, src_resident(hT, r_h), g, r_g, hnT, r_hn, want_rstd=(rstd, r_rstd))

    wr, r_wr = k.sb("moe_wr", [128, NKC, 20], F32)
    k.op("sp", lambda: nc.sync.dma_start(out=wr[:], in_=prm["w_router"][l].rearrange("(kc p) e -> p kc e", p=128)),
         writes=[r_wr], dma=True)
    k.op("dve", lambda: nc.vector.tensor_tensor(out=wr[:], in0=wr[:], in1=g[:].unsqueeze(2).to_broadcast([128, NKC, 20]),
                                               op=ALU.mult), reads=[r_wr, r_g], writes=[r_wr])
    rb_bias, r_bias = prm["b_router"][l]
    gT, r_gT = k.sb("moe_gT", [16, NT], F32)
    inv128, r_inv = c["inv128"]
    identf, r_identf = c["ident_f"]
    for tb in range(NT // 128):
        tsl = slice(tb * 128, (tb + 1) * 128)
        b = k.bank()
        bt, rb = k.banks[b]
        for kc in range(NKC):
            k.op("pe", (lambda kc=kc, tsl=tsl, bt=bt: nc.tensor.matmul(bt[:, 0:20], lhsT=hT[:, kc, tsl], rhs=wr[:, kc, :],
                                                                       start=(kc == 0), stop=(kc == NKC - 1))),
                 reads=[r_h, r_wr], writes=[rb])
        k.op("pe", (lambda tsl=tsl, bt=bt: nc.tensor.matmul(bt[:, 32:33], lhsT=rstd[:, tsl], rhs=inv128[:],
                                                            start=True, stop=True)),
             reads=[r_rstd, r_inv], writes=[rb])
        sm, r_sm = k.sb(f"moe_sm{tb % 2}", [128, 96], F32)
        rs = sm[:, 0:1]
        k.op("act", (lambda bt=bt, rs=rs: nc.scalar.copy(out=rs, in_=bt[:, 32:33])), reads=[rb], writes=[r_sm])
        lg = sm[:, 4:24]
        k.op("dve", (lambda bt=bt, lg=lg, rs=rs: nc.vector.scalar_tensor_tensor(
            out=lg, in0=bt[:, 0:20], scalar=rs, in1=rb_bias[:], op0=ALU.mult, op1=ALU.add)),
            reads=[rb, r_sm, r_bias], writes=[r_sm])
        k.free(b)
        lc = sm[:, 4:8]
        mx = sm[:, 1:2]
        k.op("dve", (lambda lc=lc, mx=mx: nc.vector.reduce_max(out=mx, in_=lc, axis=AX.X)), reads=[r_sm], writes=[r_sm])
        nmx = sm[:, 2:3]
        k.op("dve", (lambda mx=mx, nmx=nmx: nc.vector.tensor_scalar(out=nmx, in0=mx, scalar1=-1.0, scalar2=None, op0=ALU.mult)),
             reads=[r_sm], writes=[r_sm])
        ec = sm[:, 24:28]
        se = sm[:, 3:4]
        k.op("act", (lambda lc=lc, ec=ec, nmx=nmx, se=se: nc.scalar.activation(out=ec, in_=lc, func=AF.Exp, bias=nmx, scale=1.0,
                                                                               accum_out=se)),
             reads=[r_sm], writes=[r_sm])
        oh = sm[:, 28:32]
        k.op("dve", (lambda lc=lc, mx=mx, oh=oh: nc.vector.tensor_scalar(out=oh, in0=lc, scalar1=mx, scalar2=None, op0=ALU.is_ge)),
             reads=[r_sm], writes=[r_sm])
        fa = sm[:, 8:24].rearrange("p (g e) -> p g e", g=4)
        tmp = sm[:, 32:48]
        k.op("dve", (lambda fa=fa, oh=oh, tmp=tmp: nc.vector.tensor_tensor(
            out=tmp.rearrange("p (g e) -> p g e", g=4), in0=fa, in1=oh.unsqueeze(2).to_broadcast([128, 4, 4]), op=ALU.mult)),
            reads=[r_sm], writes=[r_sm])
        fn = sm[:, 48:52]
        k.op("dve", (lambda tmp=tmp, fn=fn: nc.vector.tensor_reduce(out=fn, in_=tmp.rearrange("p (g e) -> p e g", g=4),
                                                                    axis=AX.X, op=ALU.add)),
             reads=[r_sm], writes=[r_sm])
        mf = sm[:, 52:53]
        k.op("dve", (lambda fn=fn, mf=mf: nc.vector.reduce_max(out=mf, in_=fn, axis=AX.X)), reads=[r_sm], writes=[r_sm])
        nmf = sm[:, 53:54]
        k.op("dve", (lambda mf=mf, nmf=nmf: nc.vector.tensor_scalar(out=nmf, in0=mf, scalar1=-1.0, scalar2=None, op0=ALU.mult)),
             reads=[r_sm], writes=[r_sm])
        ef = sm[:, 56:60]
        k.op("act", (lambda fn=fn, ef=ef, nmf=nmf: nc.scalar.activation(out=ef, in_=fn, func=AF.Exp, bias=nmf, scale=1.0)),
             reads=[r_sm], writes=[r_sm])
        m1 = sm[:, 60:64]
        k.op("dve", (lambda fn=fn, mf=mf, m1=m1: nc.vector.tensor_scalar(out=m1, in0=fn, scalar1=mf, scalar2=None, op0=ALU.is_ge)),
             reads=[r_sm], writes=[r_sm])
        ef2 = sm[:, 64:68]
        k.op("dve", (lambda ef=ef, m1=m1, ef2=ef2: nc.vector.tensor_tensor(out=ef2, in0=ef, in1=m1, op=ALU.mult)),
             reads=[r_sm], writes=[r_sm])
        k.op("dve", (lambda ef=ef, ef2=ef2: nc.vector.tensor_tensor(out=ef2, in0=ef, in1=ef2, op=ALU.subtract)),
             reads=[r_sm], writes=[r_sm])
        e2 = sm[:, 54:55]
        k.op("dve", (lambda ef2=ef2, e2=e2: nc.vector.reduce_max(out=e2, in_=ef2, axis=AX.X)), reads=[r_sm], writes=[r_sm])
        m2 = sm[:, 68:72]
        k.op("dve", (lambda ef2=ef2, e2=e2, m2=m2: nc.vector.tensor_scalar(out=m2, in0=ef2, scalar1=e2, scalar2=None, op0=ALU.is_ge)),
             reads=[r_sm], writes=[r_sm])
        tv = sm[:, 72:76]
        k.op("dve", (lambda ef2=ef2, m2=m2, tv=tv: nc.vector.tensor_tensor(out=tv, in0=ef2, in1=m2, op=ALU.mult)),
             reads=[r_sm], writes=[r_sm])
        t1 = sm[:, 76:80]
        k.op("dve", (lambda ef=ef, m1=m1, t1=t1: nc.vector.tensor_tensor(out=t1, in0=ef, in1=m1, op=ALU.mult)),
             reads=[r_sm], writes=[r_sm])
        k.op("dve", (lambda tv=tv, t1=t1: nc.vector.tensor_tensor(out=tv, in0=tv, in1=t1, op=ALU.add)),
             reads=[r_sm], writes=[r_sm])
        den = sm[:, 55:56]
        k.op("dve", (lambda tv=tv, den=den: nc.vector.reduce_sum(out=den, in_=tv, axis=AX.X)), reads=[r_sm], writes=[r_sm])
        k.op("dve", (lambda den=den, se=se: nc.vector.tensor_tensor(out=den, in0=den, in1=se, op=ALU.mult)),
             reads=[r_sm], writes=[r_sm])
        k.op("dve", (lambda den=den: nc.vector.reciprocal(out=den, in_=den)), reads=[r_sm], writes=[r_sm])
        k.op("dve", (lambda tv=tv, den=den: nc.vector.tensor_scalar(out=tv, in0=tv, scalar1=den, scalar2=None, op0=ALU.mult)),
             reads=[r_sm], writes=[r_sm])
        gt = sm[:, 80:96]
        k.op("dve", (lambda gt=gt, oh=oh, tv=tv: nc.vector.tensor_tensor(
            out=gt.rearrange("p (g e) -> p g e", g=4), in0=oh.unsqueeze(2).to_broadcast([128, 4, 4]),
            in1=tv.unsqueeze(1).to_broadcast([128, 4, 4]), op=ALU.mult)), reads=[r_sm], writes=[r_sm])
        b2 = k.bank()
        bt2, rb2 = k.banks[b2]
        k.op("pe", (lambda gt=gt, bt2=bt2: nc.tensor.matmul(bt2[0:16, 0:128], lhsT=gt, rhs=identf[:], start=True, stop=True)),
             reads=[r_sm, r_identf], writes=[rb2])
        k.op("act", (lambda bt2=bt2, tsl=tsl: nc.scalar.copy(out=gT[:, tsl], in_=bt2[0:16, 0:128])), reads=[rb2], writes=[r_gT])
        k.free(b2)

    sel, r_sel = c["sel16"]
    wg, wu, wd = prm["w_gate"][l], prm["w_up"][l], prm["w_down"][l]
    for e in range(NE):
        gb, r_gb = k.sb(f"moe_gb{e % 2}", [128, NT], F32)
        for th in range(2):
            ts = slice(th * 512, (th + 1) * 512)
            b = k.bank()
            bt, rb = k.banks[b]
            k.op("pe", (lambda e=e, ts=ts, bt=bt: nc.tensor.matmul(bt[:], lhsT=sel[:, e * 128:(e + 1) * 128], rhs=gT[:, ts],
                                                                   start=True, stop=True)),
                 reads=[r_sel, r_gT], writes=[rb])
            k.op("act", (lambda gb=gb, ts=ts, bt=bt: nc.scalar.copy(out=gb[:, ts], in_=bt[:])), reads=[rb], writes=[r_gb])
            k.free(b)
        hid, r_hid = k.sb(f"moe_hid{e % 2}", [128, 4, NT], BF16)
        for fh in range(2):
            (wgt, r_wgt), = k.wget([(wg[e][:, fh * 256:(fh + 1) * 256], NKC, 256)])
            (wut, r_wut), = k.wget([(wu[e][:, fh * 256:(fh + 1) * 256], NKC, 256)])
            for f2 in range(2):
                fc = fh * 2 + f2
                for th in range(2):
                    ts = slice(th * 512, (th + 1) * 512)
                    bg = k.bank(); bu = k.bank()
                    btg, rbg = k.banks[bg]
                    btu, rbu = k.banks[bu]
                    for kc in range(NKC):
                        k.op("pe", (lambda kc=kc, ts=ts, btg=btg, wgt=wgt, f2=f2: nc.tensor.matmul(
                            btg[:], lhsT=wgt[:, kc, f2 * 128:(f2 + 1) * 128], rhs=hnT[:, kc, ts],
                            start=(kc == 0), stop=(kc == NKC - 1))), reads=[r_wgt, r_hn], writes=[rbg])
                    for kc in range(NKC):
                        k.op("pe", (lambda kc=kc, ts=ts, btu=btu, wut=wut, f2=f2: nc.tensor.matmul(
                            btu[:], lhsT=wut[:, kc, f2 * 128:(f2 + 1) * 128], rhs=hnT[:, kc, ts],
                            start=(kc == 0), stop=(kc == NKC - 1))), reads=[r_wut, r_hn], writes=[rbu])
                    sg, r_sg = k.sb(f"moe_sg{(fc * 2 + th) % 2}", [128, 512], F32)
                    k.op("act", (lambda sg=sg, btg=btg: nc.scalar.activation(out=sg[:], in_=btg[:], func=AF.Silu)),
                         reads=[rbg], writes=[r_sg])
                    k.free(bg)
                    k.op("dve", (lambda sg=sg, btu=btu: nc.vector.tensor_tensor(out=sg[:], in0=sg[:], in1=btu[:], op=ALU.mult)),
                         reads=[r_sg, rbu], writes=[r_sg])
                    k.free(bu)
                    k.op("dve", (lambda sg=sg, gb=gb, ts=ts, hid=hid, fc=fc: nc.vector.tensor_tensor(
                        out=hid[:, fc, ts], in0=sg[:], in1=gb[:, ts], op=ALU.mult)), reads=[r_sg, r_gb], writes=[r_hid])
        for dq in range(2):
            (wdt, r_wdt), = k.wget([(wd[e][:, dq * 1024:(dq + 1) * 1024], 4, 1024)])
            for d2 in range(8):
                dc = dq * 8 + d2
                for th in range(2):
                    ts = slice(th * 512, (th + 1) * 512)
                    b = k.bank()
                    bt, rb = k.banks[b]
                    for fc in range(4):
                        k.op("pe", (lambda fc=fc, ts=ts, bt=bt, wdt=wdt, d2=d2, hid=hid: nc.tensor.matmul(
                            bt[:], lhsT=wdt[:, fc, d2 * 128:(d2 + 1) * 128], rhs=hid[:, fc, ts],
                            start=(fc == 0), stop=(fc == 3))), reads=[r_wdt, r_hid], writes=[rb])
                    k.op("dve", (lambda dc=dc, ts=ts, bt=bt: nc.vector.tensor_tensor(out=hT[:, dc, ts], in0=hT[:, dc, ts], in1=bt[:],
                                                                                     op=ALU.add)), reads=[rb, r_h], writes=[r_h])
                    k.free(b)


def ssd_prep(k, hnT, r_hn, prm):
    nc = k.nc
    c = k.c
    wdt, r_wdt = prm["wdt"]
    dtb, r_dtb = prm["dtb"]
    A_bc, r_A = prm["A_bc"]
    onec, r_onec = c["one_col"]
    tri, r_tri = c["tri_incl"]
    ones, r_ones = c["ones_f"]
    P = {}
    for nm in ("dt", "a", "eacs", "dstate", "cdec"):
        P[nm] = k.sb("sp_" + nm, [128, 8, 64], F32)
    dt, r_dt = P["dt"]; a, r_a = P["a"]; eacs, r_eacs = P["eacs"]
    dstate, r_dst = P["dstate"]; cdec, r_cdec = P["cdec"]
    b = k.bank(); bt, rb = k.banks[b]
    for cch in range(8):
        for kc in range(NKC):
            k.op("pe", (lambda cch=cch, kc=kc, bt=bt: nc.tensor.matmul(
                bt[:, cch * 64:(cch + 1) * 64], lhsT=hnT[:, kc, cch * 128:(cch + 1) * 128], rhs=wdt[:, kc, :],
                start=(kc == 0), stop=(kc == NKC - 1))), reads=[r_hn, r_wdt], writes=[rb])
    k.op("dve", (lambda bt=bt: nc.vector.tensor_tensor(out=dt[:], in0=bt[:].rearrange("p (c h) -> p c h", c=8),
                                                      in1=dtb[:].unsqueeze(1).to_broadcast([128, 8, 64]), op=ALU.add)),
         reads=[rb, r_dtb], writes=[r_dt])
    k.free(b)
    k.op("act", lambda: nc.scalar.activation(out=dt[:], in_=dt[:], func=AF.Exp), reads=[r_dt], writes=[r_dt])
    k.op("act", lambda: nc.scalar.activation(out=dt[:], in_=dt[:], func=AF.Ln, bias=onec[:], scale=1.0),
         reads=[r_dt, r_onec], writes=[r_dt])
    k.op("dve", lambda: nc.vector.tensor_tensor(out=a[:], in0=dt[:], in1=A_bc[:].unsqueeze(1).to_broadcast([128, 8, 64]),
                                               op=ALU.mult), reads=[r_dt, r_A], writes=[r_a])
    b2 = k.bank(); bt2, rb2 = k.banks[b2]
    b3 = k.bank(); bt3, rb3 = k.banks[b3]
    for cch in range(8):
        k.op("pe", (lambda cch=cch, bt2=bt2: nc.tensor.matmul(bt2[:, cch * 64:(cch + 1) * 64], lhsT=tri[:], rhs=a[:, cch, :],
                                                              start=True, stop=True)), reads=[r_a, r_tri], writes=[rb2])
        k.op("pe", (lambda cch=cch, bt3=bt3: nc.tensor.matmul(bt3[:, cch * 64:(cch + 1) * 64], lhsT=ones[:], rhs=a[:, cch, :],
                                                              start=True, stop=True)), reads=[r_a, r_ones], writes=[rb3])
    f3 = lambda t: t[:].rearrange("p c h -> p (c h)")
    k.op("act", (lambda bt2=bt2: nc.scalar.activation(out=f3(eacs), in_=bt2[:], func=AF.Exp)), reads=[rb2], writes=[r_eacs])
    k.op("act", (lambda bt2=bt2: nc.scalar.copy(out=f3(dstate), in_=bt2[:])), reads=[rb2], writes=[r_dst])
    k.free(b2)
    k.op("act", (lambda bt3=bt3: nc.scalar.activation(out=f3(cdec), in_=bt3[:], func=AF.Exp)), reads=[rb3], writes=[r_cdec])
    k.op("dve", (lambda bt3=bt3: nc.vector.tensor_tensor(out=f3(dstate), in0=f3(dstate), in1=bt3[:], op=ALU.subtract)),
         reads=[rb3, r_dst], writes=[r_dst])
    k.free(b3)
    k.op("act", lambda: nc.scalar.activation(out=f3(dstate), in_=f3(dstate), func=AF.Exp, scale=-1.0), reads=[r_dst], writes=[r_dst])
    return P


def wseq_ssd_group(prm, g, full):
    w_in = prm["w_in"]
    seq = [[(w_in[:, 4096 + 512 * g: 4096 + 512 * g + 256], NKC, 256)],
           [(w_in[:, 4096 + 512 * g + 256: 4096 + 512 * g + 512], NKC, 256)],
           [(w_in[:, 8192 + 128 * g: 8192 + 128 * g + 128], NKC, 128), (w_in[:, 9216 + 128 * g: 9216 + 128 * g + 128], NKC, 128)]]
    if full:
        seq += [[(w_in[:, 512 * g: 512 * g + 256], NKC, 256)], [(w_in[:, 512 * g + 256: 512 * g + 512], NKC, 256)]]
    return seq


def ssd_group(k, g, mode, hnT, r_hn, P, prm, ynT_all=None, r_yn=None):
    nc = k.nc
    c = k.c
    cwh, r_cwh = prm["cwh"]
    cbh, r_cbh = prm["cbh"]
    D_bc, r_D = prm["D_bc"]
    nw, r_nw = prm["nw"]
    hal, r_hal = prm["hal"]
    S_in, r_Sin = prm["S_in"]
    flag, r_flag = prm["flag"]
    identb, r_identb = c["ident_bf"]
    identf, r_identf = c["ident_f"]
    ones, r_ones = c["ones_f"]
    tri, r_tri = c["tri_incl"]
    ustr, r_ustr = c["ustrict"]
    mle, r_mle = c["mask_le"]
    dt, r_dt = P["dt"]; a, r_a = P["a"]; eacs, r_eacs = P["eacs"]
    dstate, r_dst = P["dstate"]; cdec, r_cdec = P["cdec"]
    full = (mode == "B")
    hs = slice(8 * g, 8 * g + 8)
    seq = wseq_ssd_group(prm, g, full)

    xcT, r_xc = k.sb("sg_xcT", [128, 4, NT], BF16)
    BcT, r_Bc = k.sb("sg_BcT", [128, NT], BF16)
    CcT, r_Cc = k.sb("sg_CcT", [128, NT], BF16)
    Sg, r_S = k.sb("sg_S", [128, 512], F32)
    Sbf, r_Sbf = k.sb("sg_Sbf", [128, 512], BF16)
    h8 = lambda ap: ap.rearrange("p (h q) -> p h q", h=8)

    items = [(0, 0, 4 * g + 0, xcT[:, 0, :], r_xc), (0, 128, 4 * g + 1, xcT[:, 1, :], r_xc),
             (1, 0, 4 * g + 2, xcT[:, 2, :], r_xc), (1, 128, 4 * g + 3, xcT[:, 3, :], r_xc),
             (2, 0, 32 + g, BcT[:], r_Bc), (3, 0, 40 + g, CcT[:], r_Cc)]
    wcur = {}
    for ii, (wi, co, ci, dst, r_dstt) in enumerate(items):
        if wi == 0 and 0 not in wcur:
            wcur[0], = k.wget(seq[0])
        elif wi == 1 and 1 not in wcur:
            wcur[1], = k.wget(seq[1])
        elif wi == 2 and 2 not in wcur:
            wcur[2], wcur[3] = k.wget(seq[2])
        wt, r_wt = wcur[wi]
        pre, r_pre = k.sb("sg_pre", [128, NT + 8], F32)
        acc, r_acc = k.sb("sg_acc", [128, NT], F32)
        if full:
            k.op("pool", (lambda pre=pre, ci=ci: nc.gpsimd.tensor_copy(out=pre[:, 0:3], in_=hal[:, ci, :])),
                 reads=[r_hal], writes=[r_pre])
        else:
            k.op("pool", (lambda pre=pre: nc.gpsimd.memset(pre[:, 0:3], 0.0)), writes=[r_pre])
        for th in range(2):
            ts = slice(th * 512, (th + 1) * 512)
            b = k.bank(); bt, rb = k.banks[b]
            for kc in range(NKC):
                k.op("pe", (lambda kc=kc, ts=ts, bt=bt, wt=wt, co=co: nc.tensor.matmul(
                    bt[:], lhsT=wt[:, kc, co:co + 128], rhs=hnT[:, kc, ts], start=(kc == 0), stop=(kc == NKC - 1))),
                    reads=[r_wt, r_hn], writes=[rb])
            k.op("act", (lambda bt=bt, pre=pre, th=th: nc.scalar.copy(out=pre[:, 3 + th * 512: 3 + (th + 1) * 512], in_=bt[:])),
                 reads=[rb], writes=[r_pre])
            k.op("act", (lambda bt=bt, acc=acc, ts=ts, ci=ci: nc.scalar.activation(
                out=acc[:, ts], in_=bt[:], func=AF.Identity, bias=cbh[:, ci:ci + 1], scale=cwh[:, ci, 3:4])),
                reads=[rb, r_cwh, r_cbh], writes=[r_acc])
            k.free(b)
        for tap in range(3):
            k.op("dve", (lambda pre=pre, acc=acc, ci=ci, tap=tap: nc.vector.scalar_tensor_tensor(
                out=acc[:], in0=pre[:, tap:tap + NT], scalar=cwh[:, ci, tap:tap + 1], in1=acc[:], op0=ALU.mult, op1=ALU.add)),
                reads=[r_pre, r_acc, r_cwh], writes=[r_acc])
        if not full:
            k.op("pool", (lambda pre=pre, ci=ci: nc.gpsimd.tensor_copy(out=hal[:, ci, :], in_=pre[:, NT:NT + 3])),
                 reads=[r_pre], writes=[r_hal])
        k.op("act", (lambda acc=acc, pre=pre: nc.scalar.activation(out=pre[:, 0:NT], in_=acc[:], func=AF.Tanh)),
             reads=[r_acc], writes=[r_pre])
        k.op("dve", (lambda acc=acc, pre=pre, dst=dst: nc.vector.scalar_tensor_tensor(
            out=dst, in0=pre[:, 0:NT], scalar=1.0, in1=acc[:], op0=ALU.add, op1=ALU.mult)),
            reads=[r_pre, r_acc], writes=[r_dstt])

    if full:
        (wz0, r_wz0), = k.wget(seq[3])
        (wz1, r_wz1), = k.wget(seq[4])
        ss, r_ss = k.sb("sg_ss", [128, 8], F32)
        k.op("dve", lambda: nc.vector.memset(ss[:], 0.0), writes=[r_ss])
        k.op("dve", lambda: nc.vector.tensor_scalar(out=Sg[:], in0=S_in[:, g, :], scalar1=flag[:], scalar2=None, op0=ALU.mult),
             reads=[r_Sin, r_flag], writes=[r_S])
        k.op("act", lambda: nc.scalar.copy(out=Sbf[:], in_=Sg[:]), reads=[r_S], writes=[r_Sbf])
    else:
        k.op("pool", lambda: nc.gpsimd.memset(Sg[:], 0.0), writes=[r_S])

    for cch in range(8):
        cs = slice(cch * 128, (cch + 1) * 128)
        bx = k.bank(); btx, rbx = k.banks[bx]
        for fc in range(4):
            k.op("pe", (lambda fc=fc, cs=cs, btx=btx: nc.tensor.matmul(btx[:, fc * 128:(fc + 1) * 128], lhsT=xcT[:, fc, cs],
                                                                       rhs=identb[:], start=True, stop=True)),
                 reads=[r_xc, r_identb], writes=[rbx])
        xdt, r_xdt = k.sb(f"sg_xdt{cch % 2}", [128, 512], BF16)
        k.op("dve", (lambda btx=btx, xdt=xdt, cch=cch: nc.vector.tensor_tensor(
            out=h8(xdt[:]), in0=h8(btx[:]), in1=dt[:, cch, hs].unsqueeze(2).to_broadcast([128, 8, 64]), op=ALU.mult)),
            reads=[rbx, r_dt], writes=[r_xdt])
        if full:
            xD, r_xD = k.sb("sg_xD", [128, 512], F32)
            k.op("dve", (lambda btx=btx, xD=xD: nc.vector.tensor_tensor(
                out=h8(xD[:]), in0=h8(btx[:]), in1=D_bc[:, hs].unsqueeze(2).to_broadcast([128, 8, 64]), op=ALU.mult)),
                reads=[rbx, r_D], writes=[r_xD])
        k.free(bx)
        bB = k.bank(); btB, rbB = k.banks[bB]
        k.op("pe", (lambda cs=cs, btB=btB: nc.tensor.matmul(btB[:, 0:128], lhsT=BcT[:, cs], rhs=identb[:], start=True, stop=True)),
             reads=[r_Bc, r_identb], writes=[rbB])
        if full:
            k.op("pe", (lambda cs=cs, btB=btB: nc.tensor.matmul(btB[:, 128:256], lhsT=BcT[:, cs], rhs=CcT[:, cs], start=True, stop=True)),
                 reads=[r_Bc, r_Cc], writes=[rbB])
        Btok, r_Bt = k.sb(f"sg_Btok{cch % 2}", [128, 128], BF16)
        k.op("act", (lambda btB=btB, Btok=Btok: nc.scalar.copy(out=Btok[:], in_=btB[:, 0:128])), reads=[rbB], writes=[r_Bt])
        if full:
            cbm, r_cbm = k.sb("sg_cbm", [128, 128], F32)
            k.op("dve", (lambda btB=btB, cbm=cbm: nc.vector.tensor_tensor(out=cbm[:], in0=btB[:, 128:256], in1=mle[:], op=ALU.mult)),
                 reads=[rbB, r_mle], writes=[r_cbm])
        k.free(bB)
        if full:
            MT, r_MT = k.sb("sg_MT", [128, 8, 128], BF16)
            for hh in range(2):
                lta, r_lta = k.sb(f"sg_lta", [128, 4, 128], F32)
                k.op("dve", (lambda lta=lta, cch=cch, hh=hh: nc.vector.tensor_tensor(
                    out=lta[:], in0=ustr[:].unsqueeze(1).to_broadcast([128, 4, 128]),
                    in1=a[:, cch, 8 * g + 4 * hh:8 * g + 4 * hh + 4].unsqueeze(2).to_broadcast([128, 4, 128]), op=ALU.mult)),
                    reads=[r_ustr, r_a], writes=[r_lta])
                ba = k.bank(); bta, rba = k.banks[ba]
                for h4 in range(4):
                    k.op("pe", (lambda h4=h4, bta=bta, lta=lta: nc.tensor.matmul(
                        bta[:, h4 * 128:(h4 + 1) * 128], lhsT=lta[:, h4, :], rhs=tri[:], start=True, stop=True)),
                        reads=[r_lta, r_tri], writes=[rba])
                dec, r_dec = k.sb(f"sg_dec{hh}", [128, 512], BF16)
                k.op("act", (lambda bta=bta, dec=dec: nc.scalar.activation(out=dec[:], in_=bta[:], func=AF.Exp)),
                     reads=[rba], writes=[r_dec])
                k.free(ba)
                k.op("dve", (lambda dec=dec, MT=MT, hh=hh, cbm=cbm: nc.vector.tensor_tensor(
                    out=MT[:, hh * 4:(hh + 1) * 4, :], in0=dec[:].rearrange("p (h l) -> p h l", h=4),
                    in1=cbm[:].unsqueeze(1).to_broadcast([128, 4, 128]), op=ALU.mult)), reads=[r_dec, r_cbm], writes=[r_MT])
            by = k.bank(); bty, rby = k.banks[by]
            for h in range(8):
                k.op("pe", (lambda h=h, bty=bty, MT=MT, xdt=xdt: nc.tensor.matmul(
                    bty[:, h * 64:(h + 1) * 64], lhsT=MT[:, h, :], rhs=xdt[:, h * 64:(h + 1) * 64], start=True, stop=True)),
                    reads=[r_MT, r_xdt], writes=[rby])
            bo = k.bank(); bto, rbo = k.banks[bo]
            k.op("pe", (lambda cs=cs, bto=bto: nc.tensor.matmul(bto[:], lhsT=CcT[:, cs], rhs=Sbf[:], start=True, stop=True)),
                 reads=[r_Cc, r_Sbf], writes=[rbo])
            t1, r_t1 = k.sb("sg_t1", [128, 512], F32)
            k.op("dve", (lambda bto=bto, t1=t1, cch=cch: nc.vector.tensor_tensor(
                out=h8(t1[:]), in0=h8(bto[:]), in1=eacs[:, cch, hs].unsqueeze(2).to_broadcast([128, 8, 64]), op=ALU.mult)),
                reads=[rbo, r_eacs], writes=[r_t1])
            k.free(bo)
            k.op("pool", (lambda t1=t1, xD=xD: nc.gpsimd.tensor_tensor(out=t1[:], in0=t1[:], in1=xD[:], op=ALU.add)),
                 reads=[r_t1, r_xD], writes=[r_t1])
            ysb, r_ysb = k.sb("sg_ysb", [128, 512], F32)
            k.op("dve", (lambda bty=bty, t1=t1, ysb=ysb: nc.vector.tensor_tensor(out=ysb[:], in0=t1[:], in1=bty[:], op=ALU.add)),
                 reads=[rby, r_t1], writes=[r_ysb])
            k.free(by)
            bz = k.bank(); btz, rbz = k.banks[bz]
            for zi, (wz, r_wz) in enumerate(((wz0, r_wz0), (wz1, r_wz1))):
                for kc in range(NKC):
                    k.op("pe", (lambda kc=kc, cs=cs, btz=btz, wz=wz, zi=zi: nc.tensor.matmul(
                        btz[:, zi * 256:(zi + 1) * 256], lhsT=hnT[:, kc, cs], rhs=wz[:, kc, :],
                        start=(kc == 0), stop=(kc == NKC - 1))), reads=[r_hn, r_wz], writes=[rbz])
            zs, r_zs = k.sb("sg_zs", [128, 512], F32)
            k.op("act", (lambda btz=btz, zs=zs: nc.scalar.activation(out=zs[:], in_=btz[:], func=AF.Tanh, scale=0.5)),
                 reads=[rbz], writes=[r_zs])
            k.op("dve", (lambda btz=btz, zs=zs: nc.vector.scalar_tensor_tensor(out=zs[:], in0=zs[:], scalar=1.0, in1=btz[:],
                                                                               op0=ALU.add, op1=ALU.mult)),
                 reads=[r_zs, rbz], writes=[r_zs])
            k.free(bz)
            ygb, r_ygb = k.sb(f"sg_ygb{cch % 2}", [128, 512], BF16)
            k.op("dve", (lambda ysb=ysb, zs=zs, ygb=ygb: nc.vector.tensor_tensor(out=ygb[:], in0=ysb[:], in1=zs[:], op=ALU.mult)),
                 reads=[r_ysb, r_zs], writes=[r_ygb])
            k.op("act", (lambda cch=cch, zs=zs, ygb=ygb: nc.scalar.activation(out=zs[:], in_=ygb[:], func=AF.Square, scale=0.5,
                                                                             accum_out=ss[:, cch:cch + 1])),
                 reads=[r_ygb], writes=[r_zs, r_ss])
            btt = k.bank(); bttt, rbt = k.banks[btt]
            for fc in range(4):
                k.op("pe", (lambda fc=fc, bttt=bttt, ygb=ygb: nc.tensor.matmul(bttt[:, fc * 128:(fc + 1) * 128], lhsT=ygb[:, fc * 128:(fc + 1) * 128],
                                                                               rhs=identb[:], start=True, stop=True)),
                     reads=[r_ygb, r_identb], writes=[rbt])
            k.op("act", (lambda bttt=bttt, cs=cs: nc.scalar.copy(out=ynT_all[:, 4 * g:4 * g + 4, cs], in_=bttt[:].rearrange("p (f t) -> p f t", f=4))),
                 reads=[rbt], writes=[r_yn])
            k.free(btt)
        if (not full) or cch < 7:
            xd, r_xd = k.sb("sg_xd", [128, 512], BF16)
            k.op("pool", (lambda xd=xd, xdt=xdt, cch=cch: nc.gpsimd.tensor_tensor(
                out=h8(xd[:]), in0=h8(xdt[:]), in1=dstate[:, cch, hs].unsqueeze(2).to_broadcast([128, 8, 64]), op=ALU.mult)),
                reads=[r_xdt, r_dst], writes=[r_xd])
            bs = k.bank(); bts, rbs = k.banks[bs]
            k.op("pe", (lambda bts=bts, Btok=Btok, xd=xd: nc.tensor.matmul(bts[:], lhsT=Btok[:], rhs=xd[:], start=True, stop=True)),
                 reads=[r_Bt, r_xd], writes=[rbs])
            k.op("pool", (lambda cch=cch: nc.gpsimd.tensor_tensor(
                out=h8(Sg[:]), in0=h8(Sg[:]), in1=cdec[:, cch, hs].unsqueeze(2).to_broadcast([128, 8, 64]), op=ALU.mult)),
                reads=[r_S, r_cdec], writes=[r_S])
            k.op("dve", (lambda bts=bts: nc.vector.tensor_tensor(out=Sg[:], in0=Sg[:], in1=bts[:], op=ALU.add)), reads=[r_S, rbs], writes=[r_S])
            k.free(bs)
            if full:
                k.op("act", lambda: nc.scalar.copy(out=Sbf[:], in_=Sg[:]), reads=[r_S], writes=[r_Sbf])

    if not full:
        k.op("act", lambda: nc.scalar.copy(out=S_in[:, g, :], in_=Sg[:]), reads=[r_S], writes=[r_Sin])
        return
    eps4, r_eps4 = c["eps4_col"]
    pre_t, r_rbc = k.sb("sg_pre", [128, NT + 8], F32)
    rbc = pre_t[:, 0:NT]
    for hh in range(2):
        b = k.bank(); bt, rb = k.banks[b]
        for c4 in range(4):
            cch = hh * 4 + c4
            dg, r_dg = k.sb(f"sg_diag{c4 % 2}", [128, 128], F32)
            k.op("dve", (lambda dg=dg, cch=cch: nc.vector.tensor_scalar(out=dg[:], in0=identf[:], scalar1=ss[:, cch:cch + 1], scalar2=None,
                                                                       op0=ALU.mult)), reads=[r_identf, r_ss], writes=[r_dg])
            k.op("pe", (lambda dg=dg, bt=bt, c4=c4: nc.tensor.matmul(bt[:, c4 * 128:(c4 + 1) * 128], lhsT=ones[:], rhs=dg[:], start=True, stop=True)),
                 reads=[r_dg, r_ones], writes=[rb])
        k.op("act", (lambda bt=bt, hh=hh: nc.scalar.activation(out=rbc[:, hh * 512:(hh + 1) * 512], in_=bt[:], func=AF.Ln, bias=eps4[:],
                                                               scale=4.0 / 512)), reads=[rb, r_eps4], writes=[r_rbc])
        k.free(b)
    k.op("act", lambda: nc.scalar.activation(out=rbc, in_=rbc, func=AF.Exp, scale=-0.5), reads=[r_rbc], writes=[r_rbc])
    for fc in range(4):
        eng = "dve"
        E = nc.vector
        k.op(eng, (lambda fc=fc, E=E: E.scalar_tensor_tensor(
            out=ynT_all[:, 4 * g + fc, :], in0=ynT_all[:, 4 * g + fc, :], scalar=nw[:, 4 * g + fc:4 * g + fc + 1], in1=rbc,
            op0=ALU.mult, op1=ALU.mult)), reads=[r_yn, r_nw, r_rbc], writes=[r_yn])


def wseq_outproj(prm):
    w = prm["w_out"]
    return [[(w[0:2048, dc * 128:(dc + 1) * 128], 16, 128), (w[2048:4096, dc * 128:(dc + 1) * 128], 16, 128)] for dc in range(16)]


def out_proj(k, hT, r_h, ynT_all, r_yn, prm):
    nc = k.nc
    for dc, pieces in enumerate(wseq_outproj(prm)):
        (wo0, r_wo0), (wo1, r_wo1) = k.wget(pieces)
        for th in range(2):
            ts = slice(th * 512, (th + 1) * 512)
            b = k.bank(); bt, rb = k.banks[b]
            for fc in range(32):
                wo, r_wo = (wo0, r_wo0) if fc < 16 else (wo1, r_wo1)
                k.op("pe", (lambda fc=fc, ts=ts, bt=bt, wo=wo: nc.tensor.matmul(
                    bt[:], lhsT=wo[:, fc % 16, :], rhs=ynT_all[:, fc, ts], start=(fc == 0), stop=(fc == 31))),
                    reads=[r_wo, r_yn], writes=[rb])
            k.op("dve", (lambda dc=dc, ts=ts, bt=bt: nc.vector.tensor_tensor(out=hT[:, dc, ts], in0=hT[:, dc, ts], in1=bt[:], op=ALU.add)),
                 reads=[rb, r_h], writes=[r_h])
            k.free(b)


W_IN_COLS = 10304
KB = 1024


def dram_in(nc, name, shape, dt=F32):
    return nc.dram_tensor(name, list(shape), dt, kind="ExternalInput").ap()


def load_small(k, name, src, shape):
    nc = k.nc
    t, r = k.sb("p_" + name, shape, F32)
    k.op("sp", (lambda t=t, src=src: nc.sync.dma_start(out=t[:], in_=src)), writes=[r], dma=True)
    return t, r


L1_INPUTS = [("xT_prev", [2048, NT]), ("xT_own", [2048, NT]), ("flag", [128, 1]),
             ("mix_g0", [128, 16]), ("ffn_g0", [128, 16]), ("kv_g", [128, 16]),
             ("w_in", [2048, W_IN_COLS]), ("conv_w", [128, 48, 4]), ("conv_b", [128, 48]),
             ("dt_bias", [128, 64]), ("a_log", [128, 64]), ("d_skip", [128, 64]), ("norm_w", [128, 32]),
             ("w_out", [4096, 2048]), ("w_k", [2048, 2048]), ("w_v", [2048, 2048]),
             ("w_router0", [2048, 20]), ("b_router0", [128, 20]),
             ("w_gate0", [16, 2048, 512]), ("w_up0", [16, 2048, 512]), ("w_down0", [16, 512, 2048])]


def wseq_kv(I):
    return ([[(I["w_k"][:, cb * 256:(cb + 1) * 256], NKC, 256)] for cb in range(8)] +
            [[(I["w_v"][:, cb * 256:(cb + 1) * 256], NKC, 256)] for cb in range(8)])


def build_launch1(stop="full"):
    nc = bass.Bass("TRN2", target_bir_lowering=False)
    dr = {n: dram_in(nc, n, s) for n, s in CONST_SHAPES.items()}
    I = {n: dram_in(nc, n, s) for n, s in L1_INPUTS}
    h_out = nc.dram_tensor("h_out", [2048, NT], F32, kind="ExternalOutput").ap()
    kT_out = nc.dram_tensor("kT_out", [2048, NT], F32, kind="ExternalOutput").ap()
    v_out = nc.dram_tensor("v_out", [NT, 2048], F32, kind="ExternalOutput").ap()
    k = K(nc)
    k.setup()
    outs = []

    load_consts(k, dr, ["ident_bf", "ident_f", "ones_f", "tri_incl", "ustrict", "mask_le", "inv128"])
    hnT, r_hn = k.sb("hnT", [128, 16, NT], BF16)
    prm = {}
    mixg = load_small(k, "mix_g0", I["mix_g0"], [128, 16])
    prm["ffn_g"] = [load_small(k, "ffn_g0", I["ffn_g0"], [128, 16])]
    kvg = load_small(k, "kv_g", I["kv_g"], [128, 16])
    prm["b_router"] = [load_small(k, "b_router0", I["b_router0"], [128, 20])]
    prm["w_router"] = [I["w_router0"]]
    prm["w_gate"] = [[I["w_gate0"][e] for e in range(16)]]
    prm["w_up"] = [[I["w_up0"][e] for e in range(16)]]
    prm["w_down"] = [[I["w_down0"][e] for e in range(16)]]
    prm["w_in"] = I["w_in"]
    prm["w_out"] = I["w_out"]
    prm["flag"] = load_small(k, "flag", I["flag"], [128, 1])
    prm["dtb"] = load_small(k, "dt_bias", I["dt_bias"], [128, 64])
    prm["D_bc"] = load_small(k, "d_skip", I["d_skip"], [128, 64])
    prm["nw"] = load_small(k, "norm_w", I["norm_w"], [128, 32])
    A_bc, r_A = load_small(k, "a_log", I["a_log"], [128, 64])
    k.op("act", lambda: nc.scalar.activation(out=A_bc[:], in_=A_bc[:], func=AF.Exp), reads=[r_A], writes=[r_A])
    k.op("dve", lambda: nc.vector.tensor_scalar(out=A_bc[:], in0=A_bc[:], scalar1=-1.0, scalar2=None, op0=ALU.mult), reads=[r_A], writes=[r_A])
    prm["A_bc"] = (A_bc, r_A)
    cwh, r_cwh = k.sb("p_cwh", [128, 48, 4], F32)
    k.op("sp", lambda: nc.sync.dma_start(out=cwh[:], in_=I["conv_w"]), writes=[r_cwh], dma=True)
    k.op("dve", lambda: nc.vector.tensor_scalar(out=cwh[:], in0=cwh[:], scalar1=0.5, scalar2=None, op0=ALU.mult), reads=[r_cwh], writes=[r_cwh])
    prm["cwh"] = (cwh, r_cwh)
    cbh, r_cbh = load_small(k, "conv_b", I["conv_b"], [128, 48])
    k.op("dve", lambda: nc.vector.tensor_scalar(out=cbh[:], in0=cbh[:], scalar1=0.5, scalar2=None, op0=ALU.mult), reads=[r_cbh], writes=[r_cbh])
    prm["cbh"] = (cbh, r_cbh)
    wdt, r_wdt = k.sb("p_wdt", [128, 16, 64], BF16)
    k.op("pool", lambda: nc.gpsimd.dma_start(out=wdt[:], in_=I["w_in"][:, 10240:10304].rearrange("(kc p) f -> p kc f", p=128)),
         writes=[r_wdt], dma=True)
    prm["wdt"] = (wdt, r_wdt)
    prm["hal"] = k.sb("p_hal", [128, 48, 3], F32)
    prm["S_in"] = k.sb("p_Sin", [128, 8, 512], BF16)
    BIG = (k.bump + 63) // 64 * 64
    print("perm end", BIG - SB_BASE, "big bytes", SB_TOP - BIG)
    assert SB_TOP - BIG >= 128 * KB

    LV = ["normA", "prepA", "sA1", "sA", "normB", "sB1", "sB", "mixer", "moe", "full"]
    lv = LV.index(stop)
    nA = 0 if lv < 2 else (1 if lv == 2 else 8)
    nB = 0 if lv < 5 else (1 if lv == 5 else 8)
    for g in range(nA):
        k.wdeclare(wseq_ssd_group(prm, g, False))
    for g in range(nB):
        k.wdeclare(wseq_ssd_group(prm, g, True))
    if lv >= 7:
        k.wdeclare(wseq_outproj(prm))
    if lv >= 8:
        k.wdeclare(wseq_moe(prm, 0))
    if lv >= 9:
        k.wdeclare(wseq_kv(I))

    def early_out():
        o = k.op("pool", lambda: nc.gpsimd.dma_start(out=h_out.rearrange("(kc p) t -> p kc t", p=128), in_=hnT[:]), reads=[r_hn], dma=True)
        cnt = k.p.emit()
        print("launch1(early): ops", len(k.p.ops), "wtiles", len(k.wlist), "signals", cnt)
        k.p.final_wait("pool", [o])
        nc._knames = k.names
        return nc

    k.begin_phase("nA", BIG)
    rmsnorm(k, src_staged(k, I["xT_prev"]), mixg[0], mixg[1], hnT, r_hn)
    if lv == 0:
        return early_out()
    k.begin_phase("sA", BIG)
    P = ssd_prep(k, hnT, r_hn, prm)
    for g in range(nA):
        ssd_group(k, g, "A", hnT, r_hn, P, prm)
    if lv <= 3:
        return early_out()
    k.begin_phase("nB", BIG)
    rmsnorm(k, src_staged(k, I["xT_own"]), mixg[0], mixg[1], hnT, r_hn)
    k.begin_phase("sB", BIG)
    ynT_all, r_yn = k.sb("ynT_all", [128, 32, NT], BF16)
    assert k.bump == BIG + 64 * KB
    if lv == 4:
        return early_out()
    P = ssd_prep(k, hnT, r_hn, prm)
    for g in range(nB):
        ssd_group(k, g, "B", hnT, r_hn, P, prm, ynT_all, r_yn)
    print("sB scratch used", k.bump - BIG - 64 * KB)
    if lv <= 6:
        return early_out()
    k.begin_phase("op", BIG + 64 * KB)
    hT, r_h = k.sb("hT", [128, 16, NT], F32)
    k.op("sp", lambda: nc.sync.dma_start(out=hT[:], in_=I["xT_own"].rearrange("(kc p) t -> p kc t", p=128)), writes=[r_h], dma=True)
    out_proj(k, hT, r_h, ynT_all, r_yn, prm)
    if lv >= 8:
        k.begin_phase("moe0", BIG, BIG + 64 * KB)
        load_consts(k, dr, ["sel16"])
        moe(k, hT, r_h, hnT, r_hn, prm, 0)
        print("moe scratch used", k.bump - BIG)
    if lv >= 9:
        k.begin_phase("kv", BIG, BIG + 64 * KB)
        rmsnorm(k, src_resident(hT, r_h), kvg[0], kvg[1], hnT, r_hn)
        seq = wseq_kv(I)
        for cb in range(8):
            (wk, r_wk), = k.wget(seq[cb])
            st, r_st = k.sb(f"kv_st{cb % 2}", [128, 2, NT], F32)
            for c2 in range(2):
                for th in range(2):
                    ts = slice(th * 512, (th + 1) * 512)
                    b = k.bank(); bt, rb = k.banks[b]
                    for kc in range(NKC):
                        k.op("pe", (lambda kc=kc, ts=ts, bt=bt, wk=wk, c2=c2: nc.tensor.matmul(
                            bt[:], lhsT=wk[:, kc, c2 * 128:(c2 + 1) * 128], rhs=hnT[:, kc, ts],
                            start=(kc == 0), stop=(kc == NKC - 1))), reads=[r_wk, r_hn], writes=[rb])
                    k.op("act", (lambda bt=bt, st=st, c2=c2, ts=ts: nc.scalar.copy(out=st[:, c2, ts], in_=bt[:])), reads=[rb], writes=[r_st])
                    k.free(b)
            o = k.op("sp", (lambda st=st, cb=cb: nc.sync.dma_start(
                out=kT_out[cb * 256:(cb + 1) * 256, :].rearrange("(c p) t -> p c t", p=128), in_=st[:])), reads=[r_st], dma=True)
            outs.append(o)
        for cb in range(8):
            (wv, r_wv), = k.wget(seq[8 + cb])
            st, r_st = k.sb(f"kv_st{cb % 2}", [128, 2, NT], F32)
            stv = st[:].rearrange("p c t -> p (c t)").rearrange("p (t f) -> p t f", f=256)
            for t2 in range(4):
                b = k.bank(); bt, rb = k.banks[b]
                for ti in range(2):
                    tb = t2 * 2 + ti
                    for kc in range(NKC):
                        k.op("pe", (lambda kc=kc, tb=tb, ti=ti, bt=bt, wv=wv: nc.tensor.matmul(
                            bt[:, ti * 256:(ti + 1) * 256], lhsT=hnT[:, kc, tb * 128:(tb + 1) * 128], rhs=wv[:, kc, :],
                            start=(kc == 0), stop=(kc == NKC - 1))), reads=[r_wv, r_hn], writes=[rb])
                k.op("act", (lambda bt=bt, stv=stv, t2=t2: nc.scalar.copy(
                    out=stv[:, 2 * t2:2 * t2 + 2, :], in_=bt[:].rearrange("p (t f) -> p t f", f=256))), reads=[rb], writes=[r_st])
                k.free(b)
            o = k.op("sp", (lambda stv=stv, cb=cb: nc.sync.dma_start(
                out=v_out[:, cb * 256:(cb + 1) * 256].rearrange("(t p) f -> p t f", p=128), in_=stv)), reads=[r_st], dma=True)
            outs.append(o)
    o = k.op("sp", lambda: nc.sync.dma_start(out=h_out.rearrange("(kc p) t -> p kc t", p=128), in_=hT[:]), reads=[r_h], dma=True)
    outs.append(o)
    assert k.widx == len(k.wlist), (k.widx, len(k.wlist))
    cnt = k.p.emit()
    print("launch1: ops", len(k.p.ops), "wtiles", len(k.wlist), "signals", cnt)
    k.p.final_wait("sp", outs)
    return nc


def fm(v, n):
    return np.ascontiguousarray(np.asarray(v, np.float32).reshape(n, 128).T)


def rep(v):
    return np.ascontiguousarray(np.tile(np.asarray(v, np.float32)[None, :], (128, 1)))


def host_inputs_l1(inp, core):
    b, half = core // 2, core % 2
    x = np.asarray(inp["x"], np.float32)
    m = dict(host_consts())
    own = x[b, half * NT:(half + 1) * NT]
    prev = x[b, 0:NT] if half == 1 else np.zeros((NT, D), np.float32)
    m["xT_own"] = np.ascontiguousarray(own.T)
    m["xT_prev"] = np.ascontiguousarray(prev.T)
    m["flag"] = np.full((128, 1), float(half), np.float32)
    m["mix_g0"] = fm(inp["mix_norm"][0], 16)
    m["ffn_g0"] = fm(inp["ffn_norm"][0], 16)
    m["kv_g"] = fm(inp["kv_norm"], 16)
    m["w_in"] = np.asarray(inp["ssm_w_in"][0], np.float32)
    cw = np.asarray(inp["ssm_conv_w"][0], np.float32)
    m["conv_w"] = np.ascontiguousarray(cw.T.reshape(48, 128, 4).transpose(1, 0, 2))
    m["conv_b"] = fm(inp["ssm_conv_b"][0], 48)
    m["dt_bias"] = rep(inp["ssm_dt_bias"][0])
    m["a_log"] = rep(inp["ssm_a_log"][0])
    m["d_skip"] = rep(inp["ssm_d"][0])
    m["norm_w"] = fm(inp["ssm_norm_w"][0], 32)
    m["w_out"] = np.asarray(inp["ssm_w_out"][0], np.float32)
    m["w_k"] = np.asarray(inp["w_k"], np.float32)
    m["w_v"] = np.asarray(inp["w_v"], np.float32)
    m["w_router0"] = np.ascontiguousarray(np.concatenate(
        [np.asarray(inp["moe_w_coarse"][0], np.float32), np.asarray(inp["moe_w_fine"][0], np.float32).reshape(D, 16)], axis=1))
    m["b_router0"] = rep(np.concatenate([np.asarray(inp["moe_b_coarse"][0], np.float32),
                                         np.asarray(inp["moe_b_fine"][0], np.float32).reshape(16)]))
    m["w_gate0"] = np.asarray(inp["moe_w_gate"][0], np.float32)
    m["w_up0"] = np.asarray(inp["moe_w_up"][0], np.float32)
    m["w_down0"] = np.asarray(inp["moe_w_down"][0], np.float32)
    return m


L2_INPUTS = [("hT_in", [2048, NT]), ("kT_all", [2048, 2048]), ("v_all", [2048, 2048]),
             ("mix_g1", [128, 16]), ("ffn_g1", [128, 16]), ("final_g", [128, 16]),
             ("w_q", [2048, 2048]), ("w_o", [2048, 2048]),
             ("w_router1", [2048, 20]), ("b_router1", [128, 20]),
             ("w_gate1", [16, 2048, 512]), ("w_up1", [16, 2048, 512]), ("w_down1", [16, 512, 2048]),
             ("mask_rev", [128, 128])]


def wseq_sq(w):
    return [[(w[:, cb * 256:(cb + 1) * 256], NKC, 256)] for cb in range(8)]


def build_launch2(stop="full"):
    nc = bass.Bass("TRN2", target_bir_lowering=False)
    dr = {n: dram_in(nc, n, s) for n, s in CONST_SHAPES.items()}
    I = {n: dram_in(nc, n, s) for n, s in L2_INPUTS}
    out_d = nc.dram_tensor("outT", [2048, NT], F32, kind="ExternalOutput").ap()
    k = K(nc)
    k.setup()
    load_consts(k, dr, ["ident_bf", "ident_f", "ones_f", "inv128"])
    hn_off = k.bump
    hnT, r_hn = k.sb("hnT", [128, 16, NT], BF16)
    hn_end = k.bump
    prm = {}
    mixg = load_small(k, "mix_g1", I["mix_g1"], [128, 16])
    prm["ffn_g"] = [load_small(k, "ffn_g1", I["ffn_g1"], [128, 16])]
    fing = load_small(k, "final_g", I["final_g"], [128, 16])
    prm["b_router"] = [load_small(k, "b_router1", I["b_router1"], [128, 20])]
    prm["w_router"] = [I["w_router1"]]
    prm["w_gate"] = [[I["w_gate1"][e] for e in range(16)]]
    prm["w_up"] = [[I["w_up1"][e] for e in range(16)]]
    prm["w_down"] = [[I["w_down1"][e] for e in range(16)]]
    mrev, r_mrev = load_small(k, "mask_rev", I["mask_rev"], [128, 128])
    BIG = (k.bump + 63) // 64 * 64
    assert SB_TOP - BIG >= 128 * KB
    LV = ["q", "attn", "wo", "moe", "full"]
    lv = LV.index(stop)
    k.wdeclare(wseq_sq(I["w_q"]))
    if lv >= 2:
        k.wdeclare(wseq_sq(I["w_o"]))
    if lv >= 3:
        k.wdeclare(wseq_moe(prm, 0))

    k.begin_phase("q", BIG + 64 * KB)
    hT, r_h = k.sb("hT", [128, 16, NT], F32)
    k.op("sp", lambda: nc.sync.dma_start(out=hT[:], in_=I["hT_in"].rearrange("(kc p) t -> p kc t", p=128)), writes=[r_h], dma=True)
    k.bump = BIG
    k.limit = BIG + 64 * KB
    qT, r_q = k.sb("qT", [128, 16, NT], BF16)
    rmsnorm(k, src_resident(hT, r_h), mixg[0], mixg[1], hnT, r_hn)
    for cb, pieces in enumerate(wseq_sq(I["w_q"])):
        (wq, r_wq), = k.wget(pieces)
        for c2 in range(2):
            hd = cb * 2 + c2
            for th in range(2):
                ts = slice(th * 512, (th + 1) * 512)
                b = k.bank(); bt, rb = k.banks[b]
                for kc in range(NKC):
                    k.op("pe", (lambda kc=kc, ts=ts, bt=bt, wq=wq, c2=c2: nc.tensor.matmul(
                        bt[:], lhsT=wq[:, kc, c2 * 128:(c2 + 1) * 128], rhs=hnT[:, kc, ts], start=(kc == 0), stop=(kc == NKC - 1))),
                        reads=[r_wq, r_hn], writes=[rb])
                k.op("act", (lambda bt=bt, hd=hd, ts=ts: nc.scalar.activation(out=qT[:, hd, ts], in_=bt[:], func=AF.Copy, scale=128 ** -0.5)),
                     reads=[rb], writes=[r_q])
                k.free(b)

    def finish(src_t, r_src, bf):
        if bf:
            o = k.op("pool", lambda: nc.gpsimd.dma_start(out=out_d.rearrange("(kc p) t -> p kc t", p=128), in_=src_t[:]), reads=[r_src], dma=True)
            eng = "pool"
        else:
            o = k.op("sp", lambda: nc.sync.dma_start(out=out_d.rearrange("(kc p) t -> p kc t", p=128), in_=src_t[:]), reads=[r_src], dma=True)
            eng = "sp"
        assert k.widx == len(k.wlist), (k.widx, len(k.wlist))
        cnt = k.p.emit()
        print("launch2: ops", len(k.p.ops), "wtiles", len(k.wlist), "signals", cnt)
        k.p.final_wait(eng, [o])
        nc._knames = k.names
        return nc
    if lv == 0:
        return finish(qT, r_q, True)

    k.begin_phase("attn", BIG + 32 * KB, BIG + 64 * KB)
    es, r_es = k.sb("at_e", [128, 2048], F32)
    cs, r_cs = k.sb("at_cs", [128, 2048], F32)
    at, r_at = k.sb("at_attn", [128, 2048], BF16)
    atT, r_atT = k.sb("at_attnT", [128, 16, 128], BF16)
    kbuf = [k.sb(f"at_k{i}", [128, 2048], BF16) for i in range(2)]
    k.bump = hn_off
    k.limit = hn_end
    vbuf = [k.sb(f"at_v{i}", [128, 16, 128], BF16) for i in range(2)]
    ones_row, r_onr = k.sb("at_ones", [128, 2048], F32)
    k.op("sp", lambda: nc.sync.dma_start(out=ones_row[:], in_=dr["ones_row"]), writes=[r_onr], dma=True)
    onec, r_onec = k.c["one_col"]
    identb, r_identb = k.c["ident_bf"]
    for hd in range(16):
        kt, r_kt = kbuf[hd % 2]
        vt, r_vt = vbuf[hd % 2]
        k.op("pool", (lambda kt=kt, hd=hd: nc.gpsimd.dma_start(out=kt[:], in_=I["kT_all"][hd * 128:(hd + 1) * 128, :])), writes=[r_kt], dma=True)
        k.op("pool", (lambda vt=vt, hd=hd: nc.gpsimd.dma_start(
            out=vt[:], in_=I["v_all"][:, hd * 128:(hd + 1) * 128].rearrange("(blk p) d -> p blk d", p=128))), writes=[r_vt], dma=True)
        for i in range(8):
            qs = slice(i * 128, (i + 1) * 128)
            c0 = (7 - i) * 128
            ncol = 2048 - c0
            nblk = ncol // 128
            nbank = (ncol + 511) // 512
            zb = []
            for j in range(nbank):
                b = k.bank(); bt, rb = k.banks[b]
                w = min(512, ncol - j * 512)
                k.op("pe", (lambda bt=bt, hd=hd, qs=qs, kt=kt, j=j, w=w, c0=c0: nc.tensor.matmul(
                    bt[:, 0:w], lhsT=qT[:, hd, qs], rhs=kt[:, c0 + j * 512: c0 + j * 512 + w], start=True, stop=True)),
                    reads=[r_q, r_kt], writes=[rb])
                zb.append((b, bt, rb, w))
            for j, (b, bt, rb, w) in enumerate(zb):
                k.op("act", (lambda bt=bt, j=j, w=w: nc.scalar.activation(out=es[:, j * 512: j * 512 + w], in_=bt[:, 0:w], func=AF.Exp)),
                     reads=[rb], writes=[r_es])
            k.op("act", (lambda ncol=ncol: nc.scalar.activation(out=es[:, 0:ncol], in_=es[:, 0:ncol], func=AF.Ln, bias=onec[:], scale=1.0)),
                 reads=[r_es, r_onec], writes=[r_es])
            k.op("dve", lambda: nc.vector.tensor_tensor(out=es[:, 0:128], in0=es[:, 0:128], in1=mrev[:], op=ALU.mult),
                 reads=[r_es, r_mrev], writes=[r_es])
            k.op("dve", (lambda ncol=ncol: nc.vector.tensor_tensor_scan(out=cs[:, 0:ncol], data0=ones_row[:, 0:ncol], data1=es[:, 0:ncol],
                                                                       initial=0.0, op0=ALU.mult, op1=ALU.add)),
                 reads=[r_es, r_onr], writes=[r_cs])
            for j, (b, bt, rb, w) in enumerate(zb):
                k.op("dve", (lambda bt=bt, j=j, w=w: nc.vector.tensor_tensor(out=cs[:, j * 512: j * 512 + w], in0=cs[:, j * 512: j * 512 + w],
                                                                            in1=bt[:, 0:w], op=ALU.subtract)),
                     reads=[r_cs, rb], writes=[r_cs])
                k.free(b)
            k.op("act", (lambda ncol=ncol: nc.scalar.activation(out=at[:, 0:ncol], in_=cs[:, 0:ncol], func=AF.Exp, scale=-1.0)),
                 reads=[r_cs], writes=[r_at])
            k.op("dve", lambda: nc.vector.tensor_tensor(out=at[:, 0:128], in0=at[:, 0:128], in1=mrev[:], op=ALU.mult),
                 reads=[r_at, r_mrev], writes=[r_at])
            for j4 in range((nblk + 3) // 4):
                b = k.bank(); bt, rb = k.banks[b]
                nb = min(4, nblk - j4 * 4)
                for jj in range(nb):
                    blk = j4 * 4 + jj
                    k.op("pe", (lambda bt=bt, jj=jj, blk=blk: nc.tensor.matmul(bt[:, jj * 128:(jj + 1) * 128], lhsT=at[:, blk * 128:(blk + 1) * 128],
                                                                               rhs=identb[:], start=True, stop=True)),
                         reads=[r_at, r_identb], writes=[rb])
                k.op("act", (lambda bt=bt, j4=j4, nb=nb: nc.scalar.copy(out=atT[:, j4 * 4:j4 * 4 + nb, :],
                                                                       in_=bt[:, 0:nb * 128].rearrange("p (b q) -> p b q", b=nb))),
                     reads=[rb], writes=[r_atT])
                k.free(b)
            b = k.bank(); bt, rb = k.banks[b]
            for blk in range(nblk):
                k.op("pe", (lambda bt=bt, blk=blk, vt=vt, i=i, nblk=nblk: nc.tensor.matmul(
                    bt[:, 0:128], lhsT=vt[:, (7 - i) + blk, :], rhs=atT[:, blk, :], start=(blk == 0), stop=(blk == nblk - 1))),
                    reads=[r_vt, r_atT], writes=[rb])
            k.op("act", (lambda bt=bt, hd=hd, qs=qs: nc.scalar.copy(out=qT[:, hd, qs], in_=bt[:, 0:128])), reads=[rb], writes=[r_q])
            k.free(b)
    if lv == 1:
        return finish(qT, r_q, True)

    k.begin_phase("wo", BIG + 32 * KB, BIG + 64 * KB)
    for cb, pieces in enumerate(wseq_sq(I["w_o"])):
        (wo, r_wo), = k.wget(pieces)
        for c2 in range(2):
            dc = cb * 2 + c2
            for th in range(2):
                ts = slice(th * 512, (th + 1) * 512)
                b = k.bank(); bt, rb = k.banks[b]
                for kc in range(NKC):
                    k.op("pe", (lambda kc=kc, ts=ts, bt=bt, wo=wo, c2=c2: nc.tensor.matmul(
                        bt[:], lhsT=wo[:, kc, c2 * 128:(c2 + 1) * 128], rhs=qT[:, kc, ts], start=(kc == 0), stop=(kc == NKC - 1))),
                        reads=[r_wo, r_q], writes=[rb])
                k.op("dve", (lambda dc=dc, ts=ts, bt=bt: nc.vector.tensor_tensor(out=hT[:, dc, ts], in0=hT[:, dc, ts], in1=bt[:], op=ALU.add)),
                     reads=[rb, r_h], writes=[r_h])
                k.free(b)
    if lv == 2:
        return finish(hT, r_h, False)
    k.begin_phase("moe1", BIG, BIG + 64 * KB)
    load_consts(k, dr, ["sel16"])
    moe(k, hT, r_h, hnT, r_hn, prm, 0)
    if lv == 3:
        return finish(hT, r_h, False)
    k.begin_phase("fin", BIG, BIG + 64 * KB)
    outT, r_o = k.sb("outT", [128, 16, NT // 2], F32)
    oo = []
    nc_ = nc
    ones, r_ones = k.c["ones_f"]
    epsc, r_eps = k.c["eps_col"]
    for th in range(2):
        ts = slice(th * 512, (th + 1) * 512)
        b = k.bank(); bt, rb = k.banks[b]
        for kc in range(NKC):
            sq, rsq = k.sb(f"rn_sq{kc % 2}", [128, 512], F32)
            k.op("act", (lambda sq=sq, kc=kc, ts=ts: nc.scalar.activation(out=sq[:], in_=hT[:, kc, ts], func=AF.Square)), reads=[r_h], writes=[rsq])
            k.op("pe", (lambda sq=sq, kc=kc, bt=bt: nc.tensor.matmul(bt[:], lhsT=ones[:], rhs=sq[:], start=(kc == 0), stop=(kc == NKC - 1))),
                 reads=[rsq, r_ones], writes=[rb])
        rt, rr = k.sb("rn_rstd", [128, 512], F32)
        k.op("act", (lambda bt=bt, rt=rt: nc.scalar.activation(out=rt[:], in_=bt[:], func=AF.Ln, bias=epsc[:], scale=1.0 / D)), reads=[rb, r_eps], writes=[rr])
        k.free(b)
        k.op("act", (lambda rt=rt: nc.scalar.activation(out=rt[:], in_=rt[:], func=AF.Exp, scale=-0.5)), reads=[rr], writes=[rr])
        for kc in range(NKC):
            k.op("dve", (lambda kc=kc, ts=ts, rt=rt: nc.vector.scalar_tensor_tensor(
                out=outT[:, kc, :], in0=hT[:, kc, ts], scalar=fing[0][:, kc:kc + 1], in1=rt[:], op0=ALU.mult, op1=ALU.mult)),
                reads=[r_h, fing[1], rr], writes=[r_o])
        oo.append(k.op("sp", (lambda ts=ts: nc.sync.dma_start(out=out_d[:, ts].rearrange("(kc p) t -> p kc t", p=128), in_=outT[:])),
                       reads=[r_o], dma=True))
    assert k.widx == len(k.wlist), (k.widx, len(k.wlist))
    cnt = k.p.emit()
    print("launch2: ops", len(k.p.ops), "wtiles", len(k.wlist), "signals", cnt)
    k.p.final_wait("sp", oo)
    return nc


def host_inputs_l2(inp, core, h_out, kT_outs, v_outs):
    b, half = core // 2, core % 2
    m = dict(host_consts())
    m["hT_in"] = np.ascontiguousarray(h_out)
    kown = kT_outs[core][:, ::-1]
    vown = v_outs[core][::-1, :]
    if half == 1:
        kprev = kT_outs[core - 1][:, ::-1]
        vprev = v_outs[core - 1][::-1, :]
    else:
        kprev = np.zeros_like(kown)
        vprev = np.zeros_like(vown)
    m["kT_all"] = np.ascontiguousarray(np.concatenate([kown, kprev], axis=1))
    m["v_all"] = np.ascontiguousarray(np.concatenate([vown, vprev], axis=0))
    m["mix_g1"] = fm(inp["mix_norm"][1], 16)
    m["ffn_g1"] = fm(inp["ffn_norm"][1], 16)
    m["final_g"] = fm(inp["final_norm"], 16)
    m["w_q"] = np.asarray(inp["sb_w_q"][0], np.float32)
    m["w_o"] = np.asarray(inp["sb_w_out"][0], np.float32)
    m["w_router1"] = np.ascontiguousarray(np.concatenate(
        [np.asarray(inp["moe_w_coarse"][1], np.float32), np.asarray(inp["moe_w_fine"][1], np.float32).reshape(D, 16)], axis=1))
    m["b_router1"] = rep(np.concatenate([np.asarray(inp["moe_b_coarse"][1], np.float32),
                                         np.asarray(inp["moe_b_fine"][1], np.float32).reshape(16)]))
    m["w_gate1"] = np.asarray(inp["moe_w_gate"][1], np.float32)
    m["w_up1"] = np.asarray(inp["moe_w_up"][1], np.float32)
    m["w_down1"] = np.asarray(inp["moe_w_down"][1], np.float32)
    i = np.arange(128)
    m["mask_rev"] = (i[None, :] > 127 - i[:, None]).astype(np.float32)
    return m


F_INPUTS = L1_INPUTS + [("mix_g1", [128, 16]), ("ffn_g1", [128, 16]), ("final_g", [128, 16]),
                        ("w_q", [2048, 2048]), ("w_o", [2048, 2048]),
                        ("w_router1", [2048, 20]), ("b_router1", [128, 20]),
                        ("w_gate1", [16, 2048, 512]), ("w_up1", [16, 2048, 512]), ("w_down1", [16, 512, 2048])]


def build_fused():
    nc = bass.Bass("TRN2", target_bir_lowering=False)
    dr = {n: dram_in(nc, n, s) for n, s in CONST_SHAPES.items()}
    I = {n: dram_in(nc, n, s) for n, s in F_INPUTS}
    out_d = nc.dram_tensor("outT", [2048, NT], F32, kind="ExternalOutput").ap()
    kT_loc = nc.dram_tensor("kT_loc", [2048, NT], BF16, kind="Internal").ap()
    v_loc = nc.dram_tensor("v_loc", [NT, 2048], BF16, kind="Internal").ap()
    kT_prev = nc.dram_tensor("kT_prev", [2048, NT], BF16, kind="Internal").ap()
    v_prev = nc.dram_tensor("v_prev", [NT, 2048], BF16, kind="Internal").ap()
    xloc = nc.dram_tensor("xloc", [2048, 512], BF16, kind="Internal").ap()
    xg = nc.dram_tensor("xg", [4096, 512], BF16, kind="Internal").ap()
    k = K(nc)
    k.setup()

    load_consts(k, dr, ["ident_bf", "ident_f", "ones_f", "tri_incl", "ustrict", "mask_le"])
    hn_off = k.bump
    hnT, r_hn = k.sb("hnT", [128, 16, NT], BF16)
    hn_end = k.bump
    prm = {}
    mixg = [load_small(k, "mix_g0", I["mix_g0"], [128, 16]), load_small(k, "mix_g1", I["mix_g1"], [128, 16])]
    prm["ffn_g"] = [load_small(k, "ffn_g0", I["ffn_g0"], [128, 16]), load_small(k, "ffn_g1", I["ffn_g1"], [128, 16])]
    kvg = load_small(k, "kv_g", I["kv_g"], [128, 16])
    fing = load_small(k, "final_g", I["final_g"], [128, 16])
    prm["b_router"] = [load_small(k, "b_router0", I["b_router0"], [128, 20]), load_small(k, "b_router1", I["b_router1"], [128, 20])]
    prm["w_router"] = [I["w_router0"], I["w_router1"]]
    prm["w_gate"] = [[I["w_gate0"][e] for e in range(16)], [I["w_gate1"][e] for e in range(16)]]
    prm["w_up"] = [[I["w_up0"][e] for e in range(16)], [I["w_up1"][e] for e in range(16)]]
    prm["w_down"] = [[I["w_down0"][e] for e in range(16)], [I["w_down1"][e] for e in range(16)]]
    prm["w_in"] = I["w_in"]
    prm["w_out"] = I["w_out"]
    prm["flag"] = load_small(k, "flag", I["flag"], [128, 1])
    flag, r_flag = prm["flag"]
    prm["dtb"] = load_small(k, "dt_bias", I["dt_bias"], [128, 64])
    prm["D_bc"] = load_small(k, "d_skip", I["d_skip"], [128, 64])
    prm["nw"] = load_small(k, "norm_w", I["norm_w"], [128, 32])
    A_bc, r_A = load_small(k, "a_log", I["a_log"], [128, 64])
    k.op("act", lambda: nc.scalar.activation(out=A_bc[:], in_=A_bc[:], func=AF.Exp), reads=[r_A], writes=[r_A])
    k.op("dve", lambda: nc.vector.tensor_scalar(out=A_bc[:], in0=A_bc[:], scalar1=-1.0, scalar2=None, op0=ALU.mult), reads=[r_A], writes=[r_A])
    prm["A_bc"] = (A_bc, r_A)
    cwh, r_cwh = k.sb("p_cwh", [128, 48, 4], F32)
    k.op("sp", lambda: nc.sync.dma_start(out=cwh[:], in_=I["conv_w"]), writes=[r_cwh], dma=True)
    k.op("dve", lambda: nc.vector.tensor_scalar(out=cwh[:], in0=cwh[:], scalar1=0.5, scalar2=None, op0=ALU.mult), reads=[r_cwh], writes=[r_cwh])
    prm["cwh"] = (cwh, r_cwh)
    cbh, r_cbh = load_small(k, "conv_b", I["conv_b"], [128, 48])
    k.op("dve", lambda: nc.vector.tensor_scalar(out=cbh[:], in0=cbh[:], scalar1=0.5, scalar2=None, op0=ALU.mult), reads=[r_cbh], writes=[r_cbh])
    prm["cbh"] = (cbh, r_cbh)
    wdt, r_wdt = k.sb("p_wdt", [128, 16, 64], BF16)
    k.op("pool", lambda: nc.gpsimd.dma_start(out=wdt[:], in_=I["w_in"][:, 10240:10304].rearrange("(kc p) f -> p kc f", p=128)),
         writes=[r_wdt], dma=True)
    prm["wdt"] = (wdt, r_wdt)
    prm["hal"] = k.sb("p_hal", [128, 48, 3], F32)
    sin_off = k.bump
    prm["S_in"] = k.sb("p_Sin", [128, 8, 512], BF16)
    sin_end = k.bump
    BIG = (k.bump + 63) // 64 * 64
    print("fused: perm end", BIG - SB_BASE, "big bytes", SB_TOP - BIG)
    assert SB_TOP - BIG >= 128 * KB

    for g in range(8):
        k.wdeclare(wseq_ssd_group(prm, g, False))
    for g in range(8):
        k.wdeclare(wseq_ssd_group(prm, g, True))
    k.wdeclare(wseq_outproj(prm))
    k.wdeclare(wseq_moe(prm, 0))
    k.wdeclare(wseq_kv(I))
    k.wdeclare(wseq_sq(I["w_q"]))
    k.wdeclare(wseq_sq(I["w_o"]))
    k.wdeclare(wseq_moe(prm, 1))

    k.begin_phase("nA", BIG)
    rmsnorm(k, src_staged(k, I["xT_prev"]), mixg[0][0], mixg[0][1], hnT, r_hn)
    k.begin_phase("sA", BIG)
    P = ssd_prep(k, hnT, r_hn, prm)
    for g in range(8):
        ssd_group(k, g, "A", hnT, r_hn, P, prm)
    k.begin_phase("nB", BIG)
    rmsnorm(k, src_staged(k, I["xT_own"]), mixg[0][0], mixg[0][1], hnT, r_hn)
    k.begin_phase("sB", BIG)
    ynT_all, r_yn = k.sb("ynT_all", [128, 32, NT], BF16)
    P = ssd_prep(k, hnT, r_hn, prm)
    for g in range(8):
        ssd_group(k, g, "B", hnT, r_hn, P, prm, ynT_all, r_yn)
    k.begin_phase("op", BIG + 64 * KB)
    hT, r_h = k.sb("hT", [128, 16, NT], F32)
    k.op("sp", lambda: nc.sync.dma_start(out=hT[:], in_=I["xT_own"].rearrange("(kc p) t -> p kc t", p=128)), writes=[r_h], dma=True)
    out_proj(k, hT, r_h, ynT_all, r_yn, prm)
    k.begin_phase("moe0", BIG, BIG + 64 * KB)
    load_consts(k, dr, ["sel16", "inv128"])
    moe(k, hT, r_h, hnT, r_hn, prm, 0)

    k.begin_phase("kv", BIG, BIG + 64 * KB)
    r_kloc = k.p.reg("kT_loc"); r_vloc = k.p.reg("v_loc"); r_kp = k.p.reg("kT_prev"); r_vp = k.p.reg("v_prev")
    r_xloc = k.p.reg("xloc"); r_xg = k.p.reg("xg")
    rmsnorm(k, src_resident(hT, r_h), kvg[0], kvg[1], hnT, r_hn)
    seq = wseq_kv(I)
    for cb in range(8):
        (wk, r_wk), = k.wget(seq[cb])
        st, r_st = k.sb(f"kv_st{cb % 2}", [128, 2, NT], BF16)
        for c2 in range(2):
            for th in range(2):
                ts = slice(th * 512, (th + 1) * 512)
                b = k.bank(); bt, rb = k.banks[b]
                for kc in range(NKC):
                    k.op("pe", (lambda kc=kc, ts=ts, bt=bt, wk=wk, c2=c2: nc.tensor.matmul(
                        bt[:], lhsT=wk[:, kc, c2 * 128:(c2 + 1) * 128], rhs=hnT[:, kc, ts],
                        start=(kc == 0), stop=(kc == NKC - 1))), reads=[r_wk, r_hn], writes=[rb])
                k.op("act", (lambda bt=bt, st=st, c2=c2, ts=ts: nc.scalar.copy(out=st[:, c2, ts], in_=bt[:])), reads=[rb], writes=[r_st])
                k.free(b)
        k.op("sp", (lambda st=st, cb=cb: nc.sync.dma_start(
            out=kT_loc[cb * 256:(cb + 1) * 256, :].rearrange("(c p) t -> p c t", p=128), in_=st[:])), reads=[r_st], writes=[r_kloc], dma=True)
    for cb in range(8):
        (wv, r_wv), = k.wget(seq[8 + cb])
        st, r_st = k.sb(f"kv_st{cb % 2}", [128, 2, NT], BF16)
        stv = st[:].rearrange("p c t -> p (c t)").rearrange("p (t f) -> p t f", f=256)
        for t2 in range(4):
            b = k.bank(); bt, rb = k.banks[b]
            for ti in range(2):
                tb = t2 * 2 + ti
                for kc in range(NKC):
                    k.op("pe", (lambda kc=kc, tb=tb, ti=ti, bt=bt, wv=wv: nc.tensor.matmul(
                        bt[:, ti * 256:(ti + 1) * 256], lhsT=hnT[:, kc, tb * 128:(tb + 1) * 128], rhs=wv[:, kc, :],
                        start=(kc == 0), stop=(kc == NKC - 1))), reads=[r_wv, r_hn], writes=[rb])
            k.op("act", (lambda bt=bt, stv=stv, t2=t2: nc.scalar.copy(
                out=stv[:, 2 * t2:2 * t2 + 2, :], in_=bt[:].rearrange("p (t f) -> p t f", f=256))), reads=[rb], writes=[r_st])
            k.free(b)
        k.op("sp", (lambda stv=stv, cb=cb: nc.sync.dma_start(
            out=v_loc[:, cb * 256:(cb + 1) * 256].rearrange("(t p) f -> p t f", p=128), in_=stv)), reads=[r_st], writes=[r_vloc], dma=True)

    k.begin_phase("q", BIG, BIG + 64 * KB)
    rg = [[0, 1], [2, 3], [4, 5], [6, 7]]
    xl_v = xloc.rearrange("(t a) c -> t (a c)", a=4)
    xg_v = xg[0:2048, :].rearrange("(t a) c -> t (a c)", a=4)
    rounds = [(kT_loc[:, 0:512], xloc, xg[0:2048, :], kT_prev[:, 0:512], r_kloc, r_kp),
              (kT_loc[:, 512:1024], xloc, xg[0:2048, :], kT_prev[:, 512:1024], r_kloc, r_kp),
              (v_loc[0:512, :], xl_v, xg_v, v_prev[0:512, :], r_vloc, r_vp),
              (v_loc[512:1024, :], xl_v, xg_v, v_prev[512:1024, :], r_vloc, r_vp)]
    for (src, xin, xout, dst, r_src, r_dst) in rounds:
        k.op("sp", (lambda src=src, xin=xin: nc.sync.dma_start(out=xin, in_=src)), reads=[r_src], writes=[r_xloc], dma=True)
        k.op("pool", lambda: nc.gpsimd.collective_compute("AllGather", op=ALU.bypass, replica_groups=rg, ins=[xloc], outs=[xg]),
             reads=[r_xloc], writes=[r_xg])
        k.op("sp", (lambda xout=xout, dst=dst: nc.sync.dma_start(out=dst, in_=xout)), reads=[r_xg], writes=[r_dst], dma=True)
    qT, r_q = k.sb("qT", [128, 16, NT], BF16)
    rmsnorm(k, src_resident(hT, r_h), mixg[1][0], mixg[1][1], hnT, r_hn)
    for cb, pieces in enumerate(wseq_sq(I["w_q"])):
        (wq, r_wq), = k.wget(pieces)
        for c2 in range(2):
            hd = cb * 2 + c2
            for th in range(2):
                ts = slice(th * 512, (th + 1) * 512)
                b = k.bank(); bt, rb = k.banks[b]
                for kc in range(NKC):
                    k.op("pe", (lambda kc=kc, ts=ts, bt=bt, wq=wq, c2=c2: nc.tensor.matmul(
                        bt[:], lhsT=wq[:, kc, c2 * 128:(c2 + 1) * 128], rhs=hnT[:, kc, ts], start=(kc == 0), stop=(kc == NKC - 1))),
                        reads=[r_wq, r_hn], writes=[rb])
                k.op("act", (lambda bt=bt, hd=hd, ts=ts: nc.scalar.activation(out=qT[:, hd, ts], in_=bt[:], func=AF.Copy, scale=128 ** -0.5)),
                     reads=[rb], writes=[r_q])
                k.free(b)

    k.begin_phase("attn", BIG + 32 * KB, BIG + 64 * KB)
    sets = [dict(), dict()]
    sets[0]["es"] = k.sb("at_e0", [128, 2048], F32)
    sets[0]["cs"] = k.sb("at_cs0", [128, 2048], F32)
    sets[0]["at"] = k.sb("at_attn0", [128, 2048], BF16)
    sets[0]["atT"] = k.sb("at_attnT0", [128, 16, 128], BF16)
    kbuf = [k.sb(f"at_k{i}", [128, 2048], BF16) for i in range(2)]
    k.bump = hn_off
    k.limit = hn_end
    sets[1]["es"] = k.sb("at_e1", [128, 2048], F32)
    sets[1]["cs"] = k.sb("at_cs1", [128, 2048], F32)
    sets[1]["at"] = k.sb("at_attn1", [128, 2048], BF16)
    sets[1]["atT"] = k.sb("at_attnT1", [128, 16, 128], BF16)
    vbuf = [k.sb(f"at_v{i}", [128, 16, 128], BF16) for i in range(2)]
    k.bump = sin_off
    k.limit = sin_end
    load_consts(k, dr, ["mask_lt"])
    sets[0]["negT"] = k.sb("at_negT0", [128, 1], F32)
    sets[1]["negT"] = k.sb("at_negT1", [128, 1], F32)
    onec, r_onec = k.c["one_col"]
    identb, r_identb = k.c["ident_bf"]
    mlt, r_mlt = k.c["mask_lt"]

    rq = {(hd_, i_): k.p.reg(f"q{hd_}_{i_}") for hd_ in range(16) for i_ in range(8)}

    def load_kv(hd):
        kt, r_kt = kbuf[hd % 2]
        vt, r_vt = vbuf[hd % 2]
        k.op("sp", (lambda: nc.sync.dma_start(out=kt[:, 0:NT], in_=kT_prev[hd * 128:(hd + 1) * 128, :])), writes=[r_kt], dma=True)
        k.op("sp", (lambda: nc.sync.dma_start(out=kt[:, NT:2 * NT], in_=kT_loc[hd * 128:(hd + 1) * 128, :])), writes=[r_kt], dma=True)
        k.op("sp", (lambda: nc.sync.dma_start(
            out=vt[:, 0:8, :], in_=v_prev[:, hd * 128:(hd + 1) * 128].rearrange("(blk p) d -> p blk d", p=128))), writes=[r_vt], dma=True)
        k.op("sp", (lambda: nc.sync.dma_start(
            out=vt[:, 8:16, :], in_=v_loc[:, hd * 128:(hd + 1) * 128].rearrange("(blk p) d -> p blk d", p=128))), writes=[r_vt], dma=True)
        k.op("pool", (lambda: nc.gpsimd.tensor_scalar(out=vt[:, 0:8, :], in0=vt[:, 0:8, :], scalar1=flag[:], scalar2=None, op0=ALU.mult)),
             reads=[r_vt, r_flag], writes=[r_vt])

    zstate = {}

    def s1_pe(n, hd, i):
        kt, r_kt = kbuf[hd % 2]
        qs = slice(i * 128, (i + 1) * 128)
        ncol = (9 + i) * 128
        zb = []
        for j in range((ncol + 511) // 512):
            b = k.bank(); bt, rb = k.banks[b]
            w = min(512, ncol - j * 512)
            k.op("pe", (lambda bt=bt, j=j, w=w: nc.tensor.matmul(
                bt[:, 0:w], lhsT=qT[:, hd, qs], rhs=kt[:, j * 512: j * 512 + w], start=True, stop=True)),
                reads=[rq[(hd, i)], r_kt], writes=[rb])
            zb.append((b, bt, rb, w))
        zstate[n] = zb

    def s1_act(n, hd, i):
        S = sets[n % 2]
        es, r_es = S["es"]; cs, r_cs = S["cs"]
        ncol = (9 + i) * 128
        for j, (b, bt, rb, w) in enumerate(zstate[n]):
            k.op("act", (lambda bt=bt, j=j, w=w: nc.scalar.activation(out=cs[:, j * 512: j * 512 + w], in_=bt[:, 0:w], func=AF.Exp)),
                 reads=[rb], writes=[r_cs])
        k.op("act", (lambda: nc.scalar.activation(out=es[:, 0:ncol], in_=cs[:, 0:ncol], func=AF.Ln, bias=onec[:], scale=1.0)),
             reads=[r_cs, r_onec], writes=[r_es])

    def s1_dve(n, hd, i):
        S = sets[n % 2]
        es, r_es = S["es"]; cs, r_cs = S["cs"]; ngT, r_ngT = S["negT"]
        ncol = (9 + i) * 128
        k.op("dve", (lambda: nc.vector.tensor_tensor(out=es[:, ncol - 128:ncol], in0=es[:, ncol - 128:ncol], in1=mlt[:], op=ALU.mult)),
             reads=[r_es, r_mlt], writes=[r_es])
        k.op("dve", (lambda: nc.vector.tensor_tensor_scan(out=cs[:, 0:ncol], data0=onec[:].to_broadcast([128, ncol]), data1=es[:, 0:ncol],
                                                          initial=0.0, op0=ALU.mult, op1=ALU.add)),
             reads=[r_es, r_onec], writes=[r_cs])
        k.op("dve", (lambda: nc.vector.tensor_scalar(out=ngT[:], in0=cs[:, ncol - 1:ncol], scalar1=-1.0, scalar2=None, op0=ALU.mult)),
             reads=[r_cs], writes=[r_ngT])
        for j, (b, bt, rb, w) in enumerate(zstate.pop(n)):
            lo = j * 512
            if j == 0:
                k.op("dve", (lambda bt=bt: nc.vector.tensor_copy(out=es[:, 0:1], in_=bt[:, 0:1])), reads=[rb], writes=[r_es])
                k.op("dve", (lambda bt=bt, w=w: nc.vector.tensor_tensor(out=es[:, 1:w], in0=cs[:, 0:w - 1], in1=bt[:, 1:w], op=ALU.add)),
                     reads=[r_cs, rb], writes=[r_es])
            else:
                k.op("dve", (lambda bt=bt, w=w, lo=lo: nc.vector.tensor_tensor(out=es[:, lo:lo + w], in0=cs[:, lo - 1:lo + w - 1], in1=bt[:, 0:w], op=ALU.add)),
                     reads=[r_cs, rb], writes=[r_es])
            k.free(b)

    def s2_exp(n, hd, i):
        S = sets[n % 2]
        es, r_es = S["es"]; at, r_at = S["at"]; ngT, r_ngT = S["negT"]
        ncol = (9 + i) * 128
        k.op("act", (lambda: nc.scalar.activation(out=at[:, 0:ncol], in_=es[:, 0:ncol], func=AF.Exp, bias=ngT[:], scale=1.0)),
             reads=[r_es, r_ngT], writes=[r_at])
        k.op("dve", (lambda: nc.vector.tensor_tensor(out=at[:, ncol - 128:ncol], in0=at[:, ncol - 128:ncol], in1=mlt[:], op=ALU.mult)),
             reads=[r_at, r_mlt], writes=[r_at])

    def s2_tr(n, hd, i):
        S = sets[n % 2]
        at, r_at = S["at"]; atT, r_atT = S["atT"]
        nblk = 9 + i
        for j4 in range((nblk + 3) // 4):
            b = k.bank(); bt, rb = k.banks[b]
            nb = min(4, nblk - j4 * 4)
            for jj in range(nb):
                blk = j4 * 4 + jj
                k.op("pe", (lambda bt=bt, jj=jj, blk=blk: nc.tensor.matmul(bt[:, jj * 128:(jj + 1) * 128], lhsT=at[:, blk * 128:(blk + 1) * 128],
                                                                           rhs=identb[:], start=True, stop=True)),
                     reads=[r_at, r_identb], writes=[rb])
            if j4 % 2 == 0:
                k.op("act", (lambda bt=bt, j4=j4, nb=nb: nc.scalar.copy(out=atT[:, j4 * 4:j4 * 4 + nb, :],
                                                                       in_=bt[:, 0:nb * 128].rearrange("p (b q) -> p b q", b=nb))),
                     reads=[rb], writes=[r_atT])
            else:
                k.op("dve", (lambda bt=bt, j4=j4, nb=nb: nc.vector.tensor_copy(out=atT[:, j4 * 4:j4 * 4 + nb, :],
                                                                              in_=bt[:, 0:nb * 128].rearrange("p (b q) -> p b q", b=nb))),
                     reads=[rb], writes=[r_atT])
            k.free(b)

    def s3(n, hd, i):
        S = sets[n % 2]
        atT, r_atT = S["atT"]
        vt, r_vt = vbuf[hd % 2]
        qs = slice(i * 128, (i + 1) * 128)
        nblk = 9 + i
        b = k.bank(); bt, rb = k.banks[b]
        for blk in range(nblk):
            k.op("pe", (lambda bt=bt, blk=blk: nc.tensor.matmul(
                bt[:, 0:128], lhsT=vt[:, blk, :], rhs=atT[:, blk, :], start=(blk == 0), stop=(blk == nblk - 1))),
                reads=[r_vt, r_atT], writes=[rb])
        k.op("act", (lambda bt=bt: nc.scalar.copy(out=qT[:, hd, qs], in_=bt[:, 0:128])), reads=[rb], writes=[rq[(hd, i)]])
        k.free(b)

    its = [(hd, i) for hd in range(16) for i in range(8)]
    load_kv(0)
    s1_pe(0, *its[0]); s1_act(0, *its[0]); s1_dve(0, *its[0])
    for n, (hd, i) in enumerate(its):
        nxt = its[n + 1] if n + 1 < len(its) else None
        if i == 2 and hd + 1 < 16:
            load_kv(hd + 1)
        if nxt:
            s1_pe(n + 1, *nxt)
        s2_exp(n, hd, i)
        if nxt:
            s1_act(n + 1, *nxt)
        s2_tr(n, hd, i)
        if nxt:
            s1_dve(n + 1, *nxt)
        s3(n, hd, i)

    k.begin_phase("wo", BIG + 32 * KB, BIG + 64 * KB)
    for cb, pieces in enumerate(wseq_sq(I["w_o"])):
        (wo, r_wo), = k.wget(pieces)
        for c2 in range(2):
            dc = cb * 2 + c2
            for th in range(2):
                ts = slice(th * 512, (th + 1) * 512)
                b = k.bank(); bt, rb = k.banks[b]
                for kc in range(NKC):
                    k.op("pe", (lambda kc=kc, ts=ts, bt=bt, wo=wo, c2=c2: nc.tensor.matmul(
                        bt[:], lhsT=wo[:, kc, c2 * 128:(c2 + 1) * 128], rhs=qT[:, kc, ts], start=(kc == 0), stop=(kc == NKC - 1))),
                        reads=[r_wo, r_q], writes=[rb])
                k.op("dve", (lambda dc=dc, ts=ts, bt=bt: nc.vector.tensor_tensor(out=hT[:, dc, ts], in0=hT[:, dc, ts], in1=bt[:], op=ALU.add)),
                     reads=[rb, r_h], writes=[r_h])
                k.free(b)
    k.begin_phase("moe1", BIG, BIG + 64 * KB)
    load_consts(k, dr, ["sel16", "inv128"])
    moe(k, hT, r_h, hnT, r_hn, prm, 1)
    k.begin_phase("fin", BIG, BIG + 64 * KB)
    outT, r_o = k.sb("outT", [128, 16, NT // 2], F32)
    oo = []
    ones, r_ones = k.c["ones_f"]
    epsc, r_eps = k.c["eps_col"]
    for th in range(2):
        ts = slice(th * 512, (th + 1) * 512)
        b = k.bank(); bt, rb = k.banks[b]
        for kc in range(NKC):
            sq, rsq = k.sb(f"rn_sq{kc % 2}", [128, 512], F32)
            k.op("act", (lambda sq=sq, kc=kc, ts=ts: nc.scalar.activation(out=sq[:], in_=hT[:, kc, ts], func=AF.Square)), reads=[r_h], writes=[rsq])
            k.op("pe", (lambda sq=sq, kc=kc, bt=bt: nc.tensor.matmul(bt[:], lhsT=ones[:], rhs=sq[:], start=(kc == 0), stop=(kc == NKC - 1))),
                 reads=[rsq, r_ones], writes=[rb])
        rt, rr = k.sb("rn_rstd", [128, 512], F32)
        k.op("act", (lambda bt=bt, rt=rt: nc.scalar.activation(out=rt[:], in_=bt[:], func=AF.Ln, bias=epsc[:], scale=1.0 / D)), reads=[rb, r_eps], writes=[rr])
        k.free(b)
        k.op("act", (lambda rt=rt: nc.scalar.activation(out=rt[:], in_=rt[:], func=AF.Exp, scale=-0.5)), reads=[rr], writes=[rr])
        for kc in range(NKC):
            k.op("dve", (lambda kc=kc, ts=ts, rt=rt: nc.vector.scalar_tensor_tensor(
                out=outT[:, kc, :], in0=hT[:, kc, ts], scalar=fing[0][:, kc:kc + 1], in1=rt[:], op0=ALU.mult, op1=ALU.mult)),
                reads=[r_h, fing[1], rr], writes=[r_o])
        oo.append(k.op("sp", (lambda ts=ts: nc.sync.dma_start(out=out_d[:, ts].rearrange("(kc p) t -> p kc t", p=128), in_=outT[:])),
                       reads=[r_o], dma=True))
    assert k.widx == len(k.wlist), (k.widx, len(k.wlist))
    cnt = k.p.emit()
    print("fused: ops", len(k.p.ops), "wtiles", len(k.wlist), "signals", cnt)
    k.p.final_wait("sp", oo)
    return nc


def host_inputs_fused(inp, core):
    m = host_inputs_l1(inp, core)
    m["mix_g1"] = fm(inp["mix_norm"][1], 16)
    m["ffn_g1"] = fm(inp["ffn_norm"][1], 16)
    m["final_g"] = fm(inp["final_norm"], 16)
    m["w_q"] = np.asarray(inp["sb_w_q"][0], np.float32)
    m["w_o"] = np.asarray(inp["sb_w_out"][0], np.float32)
    m["w_router1"] = np.ascontiguousarray(np.concatenate(
        [np.asarray(inp["moe_w_coarse"][1], np.float32), np.asarray(inp["moe_w_fine"][1], np.float32).reshape(D, 16)], axis=1))
    m["b_router1"] = rep(np.concatenate([np.asarray(inp["moe_b_coarse"][1], np.float32),
                                         np.asarray(inp["moe_b_fine"][1], np.float32).reshape(16)]))
    m["w_gate1"] = np.asarray(inp["moe_w_gate"][1], np.float32)
    m["w_up1"] = np.asarray(inp["moe_w_up"][1], np.float32)
    m["w_down1"] = np.asarray(inp["moe_w_down"][1], np.float32)
    return m


_PROGS = {}


def kernel(**inputs):
    inp = {k_: np.asarray(v) for k_, v in inputs.items()}
    ncores = 8
    names = set(CONST_SHAPES) | {n for n, _ in F_INPUTS}
    if "fused" not in _PROGS:
        _PROGS["fused"] = build_fused()
    nc = _PROGS["fused"]
    ims = []
    for c in range(ncores):
        m = host_inputs_fused(inp, c)
        ims.append({n: v for n, v in m.items() if n in names})
    res = run_bass_kernel_spmd(nc, ims, core_ids=list(range(ncores)))
    out = np.zeros((4, 2048, 2048), np.float32)
    for c in range(ncores):
        b, half = c // 2, c % 2
        out[b, half * NT:(half + 1) * NT, :] = np.asarray(res.results[c]["outT"], np.float32).T
    return out
```

```python
from concourse.bass_utils import run_bass_kernel_spmd
import numpy as np
from contextlib import ExitStack
import concourse.bass as bass
import concourse.mybir as mybir

F32 = mybir.dt.float32
BF16 = mybir.dt.bfloat16
AF = mybir.ActivationFunctionType
ALU = mybir.AluOpType
AX = mybir.AxisListType


class Reg:
    __slots__ = ("name", "w", "r")

    def __init__(self, name):
        self.name = name
        self.w = None
        self.r = []


class Prog:
    ENGS = ("pe", "act", "dve", "pool", "sp")

    def __init__(self, nc, n_dma_sems=20):
        self.nc = nc
        self.es = ExitStack()
        self.ops = []
        self.eng_obj = {"pe": nc.tensor, "act": nc.scalar, "dve": nc.vector,
                        "pool": nc.gpsimd, "sp": nc.sync}
        self.n_dma_sems = n_dma_sems
        self._uid = 0
        self.allregs = []
        self.bar_from = 0
        self.tag = None
        self.scopes = False

    def sb(self, shape, dt, name=None):
        self._uid += 1
        return self.es.enter_context(self.nc.sbuf_tensor(name or f"sb{self._uid}", list(shape), dt))

    def ps(self, shape, dt, name=None):
        self._uid += 1
        return self.es.enter_context(self.nc.psum_tensor(name or f"ps{self._uid}", list(shape), dt))

    def reg(self, name=None):
        self._uid += 1
        r = Reg(name or f"r{self._uid}")
        self.allregs.append(r)
        return r

    def barrier(self):
        last = {}
        dmas = []
        for oid in range(self.bar_from, len(self.ops)):
            o = self.ops[oid]
            if o["dma"]:
                dmas.append(oid)
            else:
                last[o["eng"]] = oid
        deps = set(last.values()) | set(dmas)
        for e in self.ENGS:
            self.ops.append(dict(eng=e, fn=(lambda: None), deps=set(deps), dma=False, tag=self.tag))
        self.bar_from = len(self.ops)
        for r in self.allregs:
            r.w = None
            r.r = []

    def op(self, eng, fn, reads=(), writes=(), dma=False, nosync_same_pe=True):
        oid = len(self.ops)
        deps = set()
        for r in reads:
            if r.w is not None:
                deps.add(r.w)
        for w in writes:
            if w.w is not None:
                deps.add(w.w)
            deps.update(w.r)
        for r in reads:
            r.r.append(oid)
        for w in writes:
            w.w = oid
            w.r = []
        deps.discard(oid)
        self.ops.append(dict(eng=eng, fn=fn, deps=deps, dma=dma, tag=self.tag))
        return oid

    def emit(self):
        nc = self.nc
        ops = self.ops
        need = [False] * len(ops)
        for o in ops:
            for d in list(o["deps"]):
                od = ops[d]
                if od["eng"] == "pe" and o["eng"] == "pe" and not od["dma"] and not o["dma"]:
                    o["deps"].discard(d)
                    continue
                need[d] = True
        esem = {e: self.es.enter_context(nc.semaphore(f"s_{e}")) for e in self.ENGS}
        dsem = {}
        for q in ("sp", "pool"):
            dsem[q] = [self.es.enter_context(nc.semaphore(f"d_{q}{i}")) for i in range(self.n_dma_sems)]
        ecount = {e: 0 for e in self.ENGS}
        dcount = {q: [0] * self.n_dma_sems for q in dsem}
        drr = {q: 0 for q in dsem}
        sig = [None] * len(ops)
        waited = {}
        cur_tag = None
        for oid, o in enumerate(ops):
            eng = o["eng"]
            E = self.eng_obj[eng]
            if self.scopes and o.get("tag") != cur_tag:
                if cur_tag is not None:
                    nc.leave_named_scope(cur_tag, cur_sid, False)
                cur_tag = o.get("tag")
                if cur_tag is not None:
                    cur_sid, _ = nc.enter_named_scope(cur_tag, False)
            wl = {}
            for d in o["deps"]:
                s, v = sig[d]
                k = id(s)
                if k not in wl or wl[k][1] < v:
                    wl[k] = (s, v)
            if o["dma"]:
                slot = drr[eng]
                drr[eng] = (slot + 1) % self.n_dma_sems
                ds = dsem[eng][slot]
                prev = dcount[eng][slot]
                if prev > 0:
                    k = id(ds)
                    if k not in wl or wl[k][1] < prev:
                        wl[k] = (ds, prev)
            for k, (ws, wv) in wl.items():
                if waited.get((eng, k), 0) >= wv:
                    continue
                waited[(eng, k)] = wv
                E.wait_ge(ws, wv)
            inst = o["fn"]()
            if inst is None:
                continue
            if o["dma"]:
                dcount[eng][slot] = prev + 16
                inst.then_inc(ds, 16)
                sig[oid] = (ds, prev + 16)
            elif need[oid]:
                ecount[eng] += 1
                inst.then_inc(esem[eng], 1)
                sig[oid] = (esem[eng], ecount[eng])
        if self.scopes and cur_tag is not None:
            nc.leave_named_scope(cur_tag, cur_sid, False)
        self.sig = sig
        self.esem = esem
        return ecount

    def final_wait(self, eng, oids):
        E = self.eng_obj[eng]
        for oid in oids:
            s, v = self.sig[oid]
            E.wait_ge(s, v)


import numpy as np

D = 2048
NT = 1024
NKC = 16
EPS = 1e-5
NE = 16
DE = 512


SB_BASE = 16512
SB_TOP = 229344


class K:
    def __init__(self, nc, nw=4, look=2):
        self.nc = nc
        self.p = Prog(nc)
        self.cache = {}
        self.NW = nw
        self.LOOK = look
        self.wlist = []
        self.widx = 0
        self.wloaded = 0
        self.bank_live = [False] * 8
        self.bank_rr = 0
        self.bump = SB_BASE
        self.limit = SB_TOP
        self.phase = "perm"
        self.uid = 0

    def sb(self, name, shape, dt):
        key = (self.phase, name)
        if key not in self.cache:
            n = 1
            for d in shape[1:]:
                n *= d
            nbytes = n * (4 if dt == F32 else 2)
            nbytes = (nbytes + 31) // 32 * 32
            off = self.bump
            assert off + nbytes <= self.limit, f"SBUF overflow allocating {name} in phase {self.phase}: {off}+{nbytes} > {self.limit}"
            self.bump = off + nbytes
            self.uid += 1
            t = self.nc.alloc_sbuf_tensor_at(f"{self.phase}_{name}_{self.uid}", list(shape), dt, offset=off)
            self.names = getattr(self, "names", {})
            self.names[key] = t.name
            self.cache[key] = (t, self.p.reg(name))
        return self.cache[key]

    def begin_phase(self, name, start, limit=SB_TOP):
        self.p.barrier()
        assert not any(self.bank_live)
        self.p.tag = name
        self.phase = name
        self.bump = start
        self.limit = limit

    def setup(self):
        p = self.p
        self.banks = [(p.ps([128, 512], F32, name=f"bank{i}"), p.reg(f"bank{i}")) for i in range(8)]
        self.bankregs = {id(r) for _, r in self.banks}
        self.wring = []
        for i in range(self.NW):
            t, _ = self.sb(f"wr{i}", [128, 4096], BF16)
            r1 = p.reg(f"wr{i}")
            self.wring.append((t, [r1, r1]))

    def op(self, eng, fn, reads=(), writes=(), dma=False):
        if eng != "pe":
            bs = self.bankregs
            extra = [r for r in reads if id(r) in bs]
            if extra:
                writes = list(writes) + [r for r in extra if r not in writes]
        return self.p.op(eng, fn, reads=reads, writes=writes, dma=dma)

    def bank(self):
        for i in range(8):
            j = (self.bank_rr + i) % 8
            if not self.bank_live[j]:
                self.bank_live[j] = True
                self.bank_rr = (j + 1) % 8
                return j
        raise RuntimeError("out of PSUM banks")

    def free(self, j):
        assert self.bank_live[j]
        self.bank_live[j] = False

    def wdeclare(self, seq):
        self.wlist.extend(seq)

    def wget(self, pieces):
        key = [(str(pc[0]), pc[1], pc[2]) for pc in pieces]
        i = self.widx
        self.widx += 1
        assert [(str(pc[0]), pc[1], pc[2]) for pc in self.wlist[i]] == key, f"weight order mismatch at {i}"
        hi = min(len(self.wlist), i + 1 + self.LOOK)
        while self.wloaded < hi:
            self._wload(self.wloaded)
            self.wloaded += 1
        t, regs = self.wring[i % self.NW]
        return self._wviews(t, regs, pieces)

    def _wviews(self, t, regs, pieces):
        out = []
        off = 0
        for pi, (src, kc, nco) in enumerate(pieces):
            v = t[:, off:off + kc * nco].rearrange("p (kc f) -> p kc f", kc=kc)
            out.append((v, regs[pi]))
            off += kc * nco
        assert off <= 4096
        return out

    def _wload(self, j):
        nc = self.nc
        t, regs = self.wring[j % self.NW]
        views = self._wviews(t, regs, self.wlist[j])
        for (v, r), (src, kc, nco) in zip(views, self.wlist[j]):
            self.p.op("pool", (lambda v=v, src=src: nc.gpsimd.dma_start(
                out=v, in_=src.rearrange("(kc p) f -> p kc f", p=128))), writes=[r], dma=True)


def load_consts(k, dr, names):
    nc = k.nc
    c = getattr(k, "c", {})
    for name in names:
        shape = CONST_SHAPES[name]
        dt = BF16 if name == "ident_bf" else F32
        t, r = k.sb("c_" + name, shape, dt)
        if dt == BF16:
            k.op("pool", (lambda t=t, name=name: nc.gpsimd.dma_start(out=t[:], in_=dr[name])), writes=[r], dma=True)
        else:
            k.op("sp", (lambda t=t, name=name: nc.sync.dma_start(out=t[:], in_=dr[name])), writes=[r], dma=True)
        c[name] = (t, r)
    for nm, val in (("eps_col", EPS), ("one_col", 1.0), ("eps4_col", 4 * EPS)):
        if nm not in c:
            t, r = k.sb("c_" + nm, [128, 1], F32)
            k.op("pool", (lambda t=t, val=val: nc.gpsimd.memset(t[:], val)), writes=[r])
            c[nm] = (t, r)
    k.c = c


def host_consts():
    i = np.arange(128)
    h = {}
    h["ident_bf"] = np.eye(128, dtype=np.float32)
    h["ident_f"] = np.eye(128, dtype=np.float32)
    h["ones_f"] = np.ones((128, 128), np.float32)
    h["tri_incl"] = (i[:, None] <= i[None, :]).astype(np.float32)
    h["ustrict"] = (i[:, None] > i[None, :]).astype(np.float32)
    h["mask_le"] = (i[:, None] <= i[None, :]).astype(np.float32)
    h["mask_lt"] = (i[None, :] < i[:, None]).astype(np.float32)
    sel = np.zeros((16, 16, 128), np.float32)
    for e in range(16):
        sel[e, e, :] = 1.0
    h["sel16"] = sel.reshape(16, 16 * 128)
    h["inv128"] = np.full((128, 1), 1.0 / 128, np.float32)
    h["ones_row"] = np.ones((128, 2048), np.float32)
    return h


CONST_SHAPES = {"ident_bf": [128, 128], "ident_f": [128, 128], "ones_f": [128, 128], "tri_incl": [128, 128],
                "ustrict": [128, 128], "mask_le": [128, 128], "mask_lt": [128, 128], "sel16": [16, 2048],
                "inv128": [128, 1], "ones_row": [128, 2048]}


def rmsnorm(k, src, g, r_g, outT, r_out, ntok=NT, want_rstd=None):
    nc = k.nc
    ones, r_ones = k.c["ones_f"]
    epsc, r_eps = k.c["eps_col"]
    for th in range(ntok // 512):
        ts = slice(th * 512, (th + 1) * 512)
        view, r_h = src(th)
        b = k.bank()
        bt, rb = k.banks[b]
        for kc in range(NKC):
            sq, rsq = k.sb(f"rn_sq{kc % 2}", [128, 512], F32)
            k.op("act", (lambda sq=sq, kc=kc, view=view: nc.scalar.activation(out=sq[:], in_=view(kc), func=AF.Square)),
                 reads=[r_h], writes=[rsq])
            k.op("pe", (lambda sq=sq, kc=kc, bt=bt: nc.tensor.matmul(bt[:], lhsT=ones[:], rhs=sq[:],
                                                                     start=(kc == 0), stop=(kc == NKC - 1))),
                 reads=[rsq, r_ones], writes=[rb])
        if want_rstd is not None:
            rt, rr = want_rstd
            rview = rt[:, ts]
        else:
            rt, rr = k.sb("rn_rstd", [128, 512], F32)
            rview = rt[:]
        k.op("act", (lambda bt=bt, rview=rview: nc.scalar.activation(out=rview, in_=bt[:], func=AF.Ln, bias=epsc[:], scale=1.0 / D)),
             reads=[rb, r_eps], writes=[rr])
        k.free(b)
        k.op("act", (lambda rview=rview: nc.scalar.activation(out=rview, in_=rview, func=AF.Exp, scale=-0.5)),
             reads=[rr], writes=[rr])
        for kc in range(NKC):
            k.op("dve", (lambda kc=kc, ts=ts, rview=rview, view=view: nc.vector.scalar_tensor_tensor(
                out=outT[:, kc, ts], in0=view(kc), scalar=g[:, kc:kc + 1], in1=rview,
                op0=ALU.mult, op1=ALU.mult)), reads=[r_h, r_g, rr], writes=[r_out])


def src_resident(hT, r_h):
    return lambda th: ((lambda kc, th=th: hT[:, kc, th * 512:(th + 1) * 512]), r_h)


def src_staged(k, dram_xT):
    nc = k.nc

    def f(th):
        st, r_st = k.sb("rn_stage", [128, NKC, 512], F32)
        k.op("sp", (lambda st=st, th=th: nc.sync.dma_start(
            out=st[:], in_=dram_xT[:, th * 512:(th + 1) * 512].rearrange("(kc p) t -> p kc t", p=128))), writes=[r_st], dma=True)
        return (lambda kc, st=st: st[:, kc, :]), r_st
    return f


def wseq_moe(prm, l):
    wg, wu, wd = prm["w_gate"][l], prm["w_up"][l], prm["w_down"][l]
    seq = []
    for e in range(NE):
        for fh in range(2):
            seq.append([(wg[e][:, fh * 256:(fh + 1) * 256], NKC, 256)])
            seq.append([(wu[e][:, fh * 256:(fh + 1) * 256], NKC, 256)])
        for dq in range(2):
            seq.append([(wd[e][:, dq * 1024:(dq + 1) * 1024], 4, 1024)])
    return seq


def moe(k, hT, r_h, hnT, r_hn, prm, l):
    nc = k.nc
    c = k.c
    g, r_g = prm["ffn_g"][l]
    rstd, r_rstd = k.sb("moe_rstd", [128, NT], F32)
    rmsnorm(k, src_resident(hT, r_h), g, r_g, hnT, r_hn, want_rstd=(rstd, r_rstd))

    wr, r_wr = k.sb("moe_wr", [128, NKC, 20], F32)
    k.op("sp", lambda: nc.sync.dma_start(out=wr[:], in_=prm["w_router"][l].rearrange("(kc p) e -> p kc e", p=128)),
         writes=[r_wr], dma=True)
    k.op("dve", lambda: nc.vector.tensor_tensor(out=wr[:], in0=wr[:], in1=g[:].unsqueeze(2).to_broadcast([128, NKC, 20]),
                                               op=ALU.mult), reads=[r_wr, r_g], writes=[r_wr])
    rb_bias, r_bias = prm["b_router"][l]
    gT, r_gT = k.sb("moe_gT", [16, NT], F32)
    inv128, r_inv = c["inv128"]
    identf, r_identf = c["ident_f"]
    for tb in range(NT // 128):
        tsl = slice(tb * 128, (tb + 1) * 128)
        b = k.bank()
        bt, rb = k.banks[b]
        for kc in range(NKC):
            k.op("pe", (lambda kc=kc, tsl=tsl, bt=bt: nc.tensor.matmul(bt[:, 0:20], lhsT=hT[:, kc, tsl], rhs=wr[:, kc, :],
                                                                       start=(kc == 0), stop=(kc == NKC - 1))),
                 reads=[r_h, r_wr], writes=[rb])
        k.op("pe", (lambda tsl=tsl, bt=bt: nc.tensor.matmul(bt[:, 32:33], lhsT=rstd[:, tsl], rhs=inv128[:],
                                                            start=True, stop=True)),
             reads=[r_rstd, r_inv], writes=[rb])
        sm, r_sm = k.sb(f"moe_sm{tb % 2}", [128, 96], F32)
        rs = sm[:, 0:1]
        k.op("act", (lambda bt=bt, rs=rs: nc.scalar.copy(out=rs, in_=bt[:, 32:33])), reads=[rb], writes=[r_sm])
        lg = sm[:, 4:24]
        k.op("dve", (lambda bt=bt, lg=lg, rs=rs: nc.vector.scalar_tensor_tensor(
            out=lg, in0=bt[:, 0:20], scalar=rs, in1=rb_bias[:], op0=ALU.mult, op1=ALU.add)),
            reads=[rb, r_sm, r_bias], writes=[r_sm])
        k.free(b)
        lc = sm[:, 4:8]
        mx = sm[:, 1:2]
        k.op("dve", (lambda lc=lc, mx=mx: nc.vector.reduce_max(out=mx, in_=lc, axis=AX.X)), reads=[r_sm], writes=[r_sm])
        nmx = sm[:, 2:3]
        k.op("dve", (lambda mx=mx, nmx=nmx: nc.vector.tensor_scalar(out=nmx, in0=mx, scalar1=-1.0, scalar2=None, op0=ALU.mult)),
             reads=[r_sm], writes=[r_sm])
        ec = sm[:, 24:28]
        se = sm[:, 3:4]
        k.op("act", (lambda lc=lc, ec=ec, nmx=nmx, se=se: nc.scalar.activation(out=ec, in_=lc, func=AF.Exp, bias=nmx, scale=1.0,
                                                                               accum_out=se)),
             reads=[r_sm], writes=[r_sm])
        oh = sm[:, 28:32]
        k.op("dve", (lambda lc=lc, mx=mx, oh=oh: nc.vector.tensor_scalar(out=oh, in0=lc, scalar1=mx, scalar2=None, op0=ALU.is_ge)),
             reads=[r_sm], writes=[r_sm])
        fa = sm[:, 8:24].rearrange("p (g e) -> p g e", g=4)
        tmp = sm[:, 32:48]
        k.op("dve", (lambda fa=fa, oh=oh, tmp=tmp: nc.vector.tensor_tensor(
            out=tmp.rearrange("p (g e) -> p g e", g=4), in0=fa, in1=oh.unsqueeze(2).to_broadcast([128, 4, 4]), op=ALU.mult)),
            reads=[r_sm], writes=[r_sm])
        fn = sm[:, 48:52]
        k.op("dve", (lambda tmp=tmp, fn=fn: nc.vector.tensor_reduce(out=fn, in_=tmp.rearrange("p (g e) -> p e g", g=4),
                                                                    axis=AX.X, op=ALU.add)),
             reads=[r_sm], writes=[r_sm])
        mf = sm[:, 52:53]
        k.op("dve", (lambda fn=fn, mf=mf: nc.vector.reduce_max(out=mf, in_=fn, axis=AX.X)), reads=[r_sm], writes=[r_sm])
        nmf = sm[:, 53:54]
        k.op("dve", (lambda mf=mf, nmf=nmf: nc.vector.tensor_scalar(out=nmf, in0=mf, scalar1=-1.0, scalar2=None, op0=ALU.mult)),
             reads=[r_sm], writes=[r_sm])
        ef = sm[:, 56:60]
        k.op("act", (lambda fn=fn, ef=ef, nmf=nmf: nc.scalar.activation(out=ef, in_=fn, func=AF.Exp, bias=nmf, scale=1.0)),
             reads=[r_sm], writes=[r_sm])
        m1 = sm[:, 60:64]
        k.op("dve", (lambda fn=fn, mf=mf, m1=m1: nc.vector.tensor_scalar(out=m1, in0=fn, scalar1=mf, scalar2=None, op0=ALU.is_ge)),
             reads=[r_sm], writes=[r_sm])
        ef2 = sm[:, 64:68]
        k.op("dve", (lambda ef=ef, m1=m1, ef2=ef2: nc.vector.tensor_tensor(out=ef2, in0=ef, in1=m1, op=ALU.mult)),
             reads=[r_sm], writes=[r_sm])
        k.op("dve", (lambda ef=ef, ef2=ef2: nc.vector.tensor_tensor(out=ef2, in0=ef, in1=ef2, op=ALU.subtract)),
             reads=[r_sm], writes=[r_sm])
        e2 = sm[:, 54:55]
        k.op("dve", (lambda ef2=ef2, e2=e2: nc.vector.reduce_max(out=e2, in_=ef2, axis=AX.X)), reads=[r_sm], writes=[r_sm])
        m2 = sm[:, 68:72]
        k.op("dve", (lambda ef2=ef2, e2=e2, m2=m2: nc.vector.tensor_scalar(out=m2, in0=ef2, scalar1=e2, scalar2=None, op0=ALU.is_ge)),
             reads=[r_sm], writes=[r_sm])
        tv = sm[:, 72:76]
        k.op("dve", (lambda ef2=ef2, m2=m2, tv=tv: nc.vector.tensor_tensor(out=tv, in0=ef2, in1=m2, op=ALU.mult)),
             reads=[r_sm], writes=[r_sm])
        t1 = sm[:, 76:80]
        k.op("dve", (lambda ef=ef, m1=m1, t1=t1: nc.vector.tensor_tensor(out=t1, in0=ef, in1=m1, op=ALU.mult)),
             reads=[r_sm], writes=[r_sm])
        k.op("dve", (lambda tv=tv, t1=t1: nc.vector.tensor_tensor(out=tv, in0=tv, in1=t1, op=ALU.add)),
             reads=[r_sm], writes=[r_sm])
        den = sm[:, 55:56]
        k.op("dve", (lambda tv=tv, den=den: nc.vector.reduce_sum(out=den, in_=tv, axis=AX.X)), reads=[r_sm], writes=[r_sm])
        k.op("dve", (lambda den=den, se=se: nc.vector.tensor_tensor(out=den, in0=den, in1=se, op=ALU.mult)),
             reads=[r_sm], writes=[r_sm])
        k.op("dve", (lambda den=den: nc.vector.reciprocal(out=den, in_=den)), reads=[r_sm], writes=[r_sm])
        k.op("dve", (lambda tv=tv, den=den: nc.vector.tensor_scalar(out=tv, in0=tv, scalar1=den, scalar2=None, op0=ALU.mult)),
             reads=[r_sm], writes=[r_sm])
        gt = sm[:, 80:96]
        k.op("dve", (lambda gt=gt, oh=oh, tv=tv: nc.vector.tensor_tensor(
            out=gt.rearrange("p (g e) -> p g e", g=4), in0=oh.unsqueeze(2).to_broadcast([128, 4, 4]),
            in1=tv.unsqueeze(1).to_broadcast([128, 4, 4]), op=ALU.mult)), reads=[r_sm], writes=[r_sm])
        b2 = k.bank()
        bt2, rb2 = k.banks[b2]
        k.op("pe", (lambda gt=gt, bt2=bt2: nc.tensor.matmul(bt2[0:16, 0:128], lhsT=gt, rhs=identf[:], start=True, stop=True)),
             reads=[r_sm, r_identf], writes=[rb2])
        k.op("act", (lambda bt2=bt2, tsl=tsl: nc.scalar.copy(out=gT[:, tsl], in_=bt2[0:16, 0:128])), reads=[rb2], writes=[r_gT])
        k.free(b2)

    sel, r_sel = c["sel16"]
    wg, wu, wd = prm["w_gate"][l], prm["w_up"][l], prm["w_down"][l]
    for e in range(NE):
        gb, r_gb = k.sb(f"moe_gb{e % 2}", [128, NT], F32)
        for th in range(2):
            ts = slice(th * 512, (th + 1) * 512)
            b = k.bank()
            bt, rb = k.banks[b]
            k.op("pe", (lambda e=e, ts=ts, bt=bt: nc.tensor.matmul(bt[:], lhsT=sel[:, e * 128:(e + 1) * 128], rhs=gT[:, ts],
                                                                   start=True, stop=True)),
                 reads=[r_sel, r_gT], writes=[rb])
            k.op("act", (lambda gb=gb, ts=ts, bt=bt: nc.scalar.copy(out=gb[:, ts], in_=bt[:])), reads=[rb], writes=[r_gb])
            k.free(b)
        hid, r_hid = k.sb(f"moe_hid{e % 2}", [128, 4, NT], BF16)
        for fh in range(2):
            (wgt, r_wgt), = k.wget([(wg[e][:, fh * 256:(fh + 1) * 256], NKC, 256)])
            (wut, r_wut), = k.wget([(wu[e][:, fh * 256:(fh + 1) * 256], NKC, 256)])
            for f2 in range(2):
                fc = fh * 2 + f2
                for th in range(2):
                    ts = slice(th * 512, (th + 1) * 512)
                    bg = k.bank(); bu = k.bank()
                    btg, rbg = k.banks[bg]
                    btu, rbu = k.banks[bu]
                    for kc in range(NKC):
                        k.op("pe", (lambda kc=kc, ts=ts, btg=btg, wgt=wgt, f2=f2: nc.tensor.matmul(
                            btg[:], lhsT=wgt[:, kc, f2 * 128:(f2 + 1) * 128], rhs=hnT[:, kc, ts],
                            start=(kc == 0), stop=(kc == NKC - 1))), reads=[r_wgt, r_hn], writes=[rbg])
                    for kc in range(NKC):
                        k.op("pe", (lambda kc=kc, ts=ts, btu=btu, wut=wut, f2=f2: nc.tensor.matmul(
                            btu[:], lhsT=wut[:, kc, f2 * 128:(f2 + 1) * 128], rhs=hnT[:, kc, ts],
                            start=(kc == 0), stop=(kc == NKC - 1))), reads=[r_wut, r_hn], writes=[rbu])
                    sg, r_sg = k.sb(f"moe_sg{(fc * 2 + th) % 2}", [128, 512], F32)
                    k.op("act", (lambda sg=sg, btg=btg: nc.scalar.activation(out=sg[:], in_=btg[:], func=AF.Silu)),
                         reads=[rbg], writes=[r_sg])
                    k.free(bg)
                    k.op("dve", (lambda sg=sg, btu=btu: nc.vector.tensor_tensor(out=sg[:], in0=sg[:], in1=btu[:], op=ALU.mult)),
                         reads=[r_sg, rbu], writes=[r_sg])
                    k.free(bu)
                    k.op("dve", (lambda sg=sg, gb=gb, ts=ts, hid=hid, fc=fc: nc.vector.tensor_tensor(
                        out=hid[:, fc, ts], in0=sg[:], in1=gb[:, ts], op=ALU.mult)), reads=[r_sg, r_gb], writes=[r_hid])
        for dq in range(2):
            (wdt, r_wdt), = k.wget([(wd[e][:, dq * 1024:(dq + 1) * 1024], 4, 1024)])
            for d2 in range(8):
                dc = dq * 8 + d2
                for th in range(2):
                    ts = slice(th * 512, (th + 1) * 512)
                    b = k.bank()
                    bt, rb = k.banks[b]
                    for fc in range(4):
                        k.op("pe", (lambda fc=fc, ts=ts, bt=bt, wdt=wdt, d2=d2, hid=hid: nc.tensor.matmul(
                            bt[:], lhsT=wdt[:, fc, d2 * 128:(d2 + 1) * 128], rhs=hid[:, fc, ts],
                            start=(fc == 0), stop=(fc == 3))), reads=[r_wdt, r_hid], writes=[rb])
                    k.op("dve", (lambda dc=dc, ts=ts, bt=bt: nc.vector.tensor_tensor(out=hT[:, dc, ts], in0=hT[:, dc, ts], in1=bt[:],
                                                                                     op=ALU.add)), reads=[rb, r_h], writes=[r_h])
                    k.free(b)


def ssd_prep(k, hnT, r_hn, prm):
    nc = k.nc
    c = k.c
    wdt, r_wdt = prm["wdt"]
    dtb, r_dtb = prm["dtb"]
    A_bc, r_A = prm["A_bc"]
    onec, r_onec = c["one_col"]
    tri, r_tri = c["tri_incl"]
    ones, r_ones = c["ones_f"]
    P = {}
    for nm in ("dt", "a", "eacs", "dstate", "cdec"):
        P[nm] = k.sb("sp_" + nm, [128, 8, 64], F32)
    dt, r_dt = P["dt"]; a, r_a = P["a"]; eacs, r_eacs = P["eacs"]
    dstate, r_dst = P["dstate"]; cdec, r_cdec = P["cdec"]
    b = k.bank(); bt, rb = k.banks[b]
    for cch in range(8):
        for kc in range(NKC):
            k.op("pe", (lambda cch=cch, kc=kc, bt=bt: nc.tensor.matmul(
                bt[:, cch * 64:(cch + 1) * 64], lhsT=hnT[:, kc, cch * 128:(cch + 1) * 128], rhs=wdt[:, kc, :],
                start=(kc == 0), stop=(kc == NKC - 1))), reads=[r_hn, r_wdt], writes=[rb])
    k.op("dve", (lambda bt=bt: nc.vector.tensor_tensor(out=dt[:], in0=bt[:].rearrange("p (c h) -> p c h", c=8),
                                                      in1=dtb[:].unsqueeze(1).to_broadcast([128, 8, 64]), op=ALU.add)),
         reads=[rb, r_dtb], writes=[r_dt])
    k.free(b)
    k.op("act", lambda: nc.scalar.activation(out=dt[:], in_=dt[:], func=AF.Exp), reads=[r_dt], writes=[r_dt])
    k.op("act", lambda: nc.scalar.activation(out=dt[:], in_=dt[:], func=AF.Ln, bias=onec[:], scale=1.0),
         reads=[r_dt, r_onec], writes=[r_dt])
    k.op("dve", lambda: nc.vector.tensor_tensor(out=a[:], in0=dt[:], in1=A_bc[:].unsqueeze(1).to_broadcast([128, 8, 64]),
                                               op=ALU.mult), reads=[r_dt, r_A], writes=[r_a])
    b2 = k.bank(); bt2, rb2 = k.banks[b2]
    b3 = k.bank(); bt3, rb3 = k.banks[b3]
    for cch in range(8):
        k.op("pe", (lambda cch=cch, bt2=bt2: nc.tensor.matmul(bt2[:, cch * 64:(cch + 1) * 64], lhsT=tri[:], rhs=a[:, cch, :],
                                                              start=True, stop=True)), reads=[r_a, r_tri], writes=[rb2])
        k.op("pe", (lambda cch=cch, bt3=bt3: nc.tensor.matmul(bt3[:, cch * 64:(cch + 1) * 64], lhsT=ones[:], rhs=a[:, cch, :],
                                                              start=True, stop=True)), reads=[r_a, r_ones], writes=[rb3])
    f3 = lambda t: t[:].rearrange("p c h -> p (c h)")
    k.op("act", (lambda bt2=bt2: nc.scalar.activation(out=f3(eacs), in_=bt2[:], func=AF.Exp)), reads=[rb2], writes=[r_eacs])
    k.op("act", (lambda bt2=bt2: nc.scalar.copy(out=f3(dstate), in_=bt2[:])), reads=[rb2], writes=[r_dst])
    k.free(b2)
    k.op("act", (lambda bt3=bt3: nc.scalar.activation(out=f3(cdec), in_=bt3[:], func=AF.Exp)), reads=[rb3], writes=[r_cdec])
    k.op("dve", (lambda bt3=bt3: nc.vector.tensor_tensor(out=f3(dstate), in0=f3(dstate), in1=bt3[:], op=ALU.subtract)),
         reads=[rb3, r_dst], writes=[r_dst])
    k.free(b3)
    k.op("act", lambda: nc.scalar.activation(out=f3(dstate), in_=f3(dstate), func=AF.Exp, scale=-1.0), reads=[r_dst], writes=[r_dst])
    return P


def wseq_ssd_group(prm, g, full):
    w_in = prm["w_in"]
    seq = [[(w_in[:, 4096 + 512 * g: 4096 + 512 * g + 256], NKC, 256)],
           [(w_in[:, 4096 + 512 * g + 256: 4096 + 512 * g + 512], NKC, 256)],
           [(w_in[:, 8192 + 128 * g: 8192 + 128 * g + 128], NKC, 128), (w_in[:, 9216 + 128 * g: 9216 + 128 * g + 128], NKC, 128)]]
    if full:
        seq += [[(w_in[:, 512 * g: 512 * g + 256], NKC, 256)], [(w_in[:, 512 * g + 256: 512 * g + 512], NKC, 256)]]
    return seq


def ssd_group(k, g, mode, hnT, r_hn, P, prm, ynT_all=None, r_yn=None):
    nc = k.nc
    c = k.c
    cwh, r_cwh = prm["cwh"]
    cbh, r_cbh = prm["cbh"]
    D_bc, r_D = prm["D_bc"]
    nw, r_nw = prm["nw"]
    hal, r_hal = prm["hal"]
    S_in, r_Sin = prm["S_in"]
    flag, r_flag = prm["flag"]
    identb, r_identb = c["ident_bf"]
    identf, r_identf = c["ident_f"]
    ones, r_ones = c["ones_f"]
    tri, r_tri = c["tri_incl"]
    ustr, r_ustr = c["ustrict"]
    mle, r_mle = c["mask_le"]
    dt, r_dt = P["dt"]; a, r_a = P["a"]; eacs, r_eacs = P["eacs"]
    dstate, r_dst = P["dstate"]; cdec, r_cdec = P["cdec"]
    full = (mode == "B")
    hs = slice(8 * g, 8 * g + 8)
    seq = wseq_ssd_group(prm, g, full)

    xcT, r_xc = k.sb("sg_xcT", [128, 4, NT], BF16)
    BcT, r_Bc = k.sb("sg_BcT", [128, NT], BF16)
    CcT, r_Cc = k.sb("sg_CcT", [128, NT], BF16)
    Sg, r_S = k.sb("sg_S", [128, 512], F32)
    Sbf, r_Sbf = k.sb("sg_Sbf", [128, 512], BF16)
    h8 = lambda ap: ap.rearrange("p (h q) -> p h q", h=8)

    items = [(0, 0, 4 * g + 0, xcT[:, 0, :], r_xc), (0, 128, 4 * g + 1, xcT[:, 1, :], r_xc),
             (1, 0, 4 * g + 2, xcT[:, 2, :], r_xc), (1, 128, 4 * g + 3, xcT[:, 3, :], r_xc),
             (2, 0, 32 + g, BcT[:], r_Bc), (3, 0, 40 + g, CcT[:], r_Cc)]
    wcur = {}
    for ii, (wi, co, ci, dst, r_dstt) in enumerate(items):
        if wi == 0 and 0 not in wcur:
            wcur[0], = k.wget(seq[0])
        elif wi == 1 and 1 not in wcur:
            wcur[1], = k.wget(seq[1])
        elif wi == 2 and 2 not in wcur:
            wcur[2], wcur[3] = k.wget(seq[2])
        wt, r_wt = wcur[wi]
        pre, r_pre = k.sb("sg_pre", [128, NT + 8], F32)
        acc, r_acc = k.sb("sg_acc", [128, NT], F32)
        if full:
            k.op("pool", (lambda pre=pre, ci=ci: nc.gpsimd.tensor_copy(out=pre[:, 0:3], in_=hal[:, ci, :])),
                 reads=[r_hal], writes=[r_pre])
        else:
            k.op("pool", (lambda pre=pre: nc.gpsimd.memset(pre[:, 0:3], 0.0)), writes=[r_pre])
        for th in range(2):
            ts = slice(th * 512, (th + 1) * 512)
            b = k.bank(); bt, rb = k.banks[b]
            for kc in range(NKC):
                k.op("pe", (lambda kc=kc, ts=ts, bt=bt, wt=wt, co=co: nc.tensor.matmul(
                    bt[:], lhsT=wt[:, kc, co:co + 128], rhs=hnT[:, kc, ts], start=(kc == 0), stop=(kc == NKC - 1))),
                    reads=[r_wt, r_hn], writes=[rb])
            k.op("act", (lambda bt=bt, pre=pre, th=th: nc.scalar.copy(out=pre[:, 3 + th * 512: 3 + (th + 1) * 512], in_=bt[:])),
                 reads=[rb], writes=[r_pre])
            k.op("act", (lambda bt=bt, acc=acc, ts=ts, ci=ci: nc.scalar.activation(
                out=acc[:, ts], in_=bt[:], func=AF.Identity, bias=cbh[:, ci:ci + 1], scale=cwh[:, ci, 3:4])),
                reads=[rb, r_cwh, r_cbh], writes=[r_acc])
            k.free(b)
        for tap in range(3):
            k.op("dve", (lambda pre=pre, acc=acc, ci=ci, tap=tap: nc.vector.scalar_tensor_tensor(
                out=acc[:], in0=pre[:, tap:tap + NT], scalar=cwh[:, ci, tap:tap + 1], in1=acc[:], op0=ALU.mult, op1=ALU.add)),
                reads=[r_pre, r_acc, r_cwh], writes=[r_acc])
        if not full:
            k.op("pool", (lambda pre=pre, ci=ci: nc.gpsimd.tensor_copy(out=hal[:, ci, :], in_=pre[:, NT:NT + 3])),
                 reads=[r_pre], writes=[r_hal])
        k.op("act", (lambda acc=acc, pre=pre: nc.scalar.activation(out=pre[:, 0:NT], in_=acc[:], func=AF.Tanh)),
             reads=[r_acc], writes=[r_pre])
        k.op("dve", (lambda acc=acc, pre=pre, dst=dst: nc.vector.scalar_tensor_tensor(
            out=dst, in0=pre[:, 0:NT], scalar=1.0, in1=acc[:], op0=ALU.add, op1=ALU.mult)),
            reads=[r_pre, r_acc], writes=[r_dstt])

    if full:
        (wz0, r_wz0), = k.wget(seq[3])
        (wz1, r_wz1), = k.wget(seq[4])
        ss, r_ss = k.sb("sg_ss", [128, 8], F32)
        k.op("dve", lambda: nc.vector.memset(ss[:], 0.0), writes=[r_ss])
        k.op("dve", lambda: nc.vector.tensor_scalar(out=Sg[:], in0=S_in[:, g, :], scalar1=flag[:], scalar2=None, op0=ALU.mult),
             reads=[r_Sin, r_flag], writes=[r_S])
        k.op("act", lambda: nc.scalar.copy(out=Sbf[:], in_=Sg[:]), reads=[r_S], writes=[r_Sbf])
    else:
        k.op("pool", lambda: nc.gpsimd.memset(Sg[:], 0.0), writes=[r_S])

    for cch in range(8):
        cs = slice(cch * 128, (cch + 1) * 128)
        bx = k.bank(); btx, rbx = k.banks[bx]
        for fc in range(4):
            k.op("pe", (lambda fc=fc, cs=cs, btx=btx: nc.tensor.matmul(btx[:, fc * 128:(fc + 1) * 128], lhsT=xcT[:, fc, cs],
                                                                       rhs=identb[:], start=True, stop=True)),
                 reads=[r_xc, r_identb], writes=[rbx])
        xdt, r_xdt = k.sb(f"sg_xdt{cch % 2}", [128, 512], BF16)
        k.op("dve", (lambda btx=btx, xdt=xdt, cch=cch: nc.vector.tensor_tensor(
            out=h8(xdt[:]), in0=h8(btx[:]), in1=dt[:, cch, hs].unsqueeze(2).to_broadcast([128, 8, 64]), op=ALU.mult)),
            reads=[rbx, r_dt], writes=[r_xdt])
        if full:
            xD, r_xD = k.sb("sg_xD", [128, 512], F32)
            k.op("dve", (lambda btx=btx, xD=xD: nc.vector.tensor_tensor(
                out=h8(xD[:]), in0=h8(btx[:]), in1=D_bc[:, hs].unsqueeze(2).to_broadcast([128, 8, 64]), op=ALU.mult)),
                reads=[rbx, r_D], writes=[r_xD])
        k.free(bx)
        bB = k.bank(); btB, rbB = k.banks[bB]
        k.op("pe", (lambda cs=cs, btB=btB: nc.tensor.matmul(btB[:, 0:128], lhsT=BcT[:, cs], rhs=identb[:], start=True, stop=True)),
             reads=[r_Bc, r_identb], writes=[rbB])
        if full:
            k.op("pe", (lambda cs=cs, btB=btB: nc.tensor.matmul(btB[:, 128:256], lhsT=BcT[:, cs], rhs=CcT[:, cs], start=True, stop=True)),
                 reads=[r_Bc, r_Cc], writes=[rbB])
        Btok, r_Bt = k.sb(f"sg_Btok{cch % 2}", [128, 128], BF16)
        k.op("act", (lambda btB=btB, Btok=Btok: nc.scalar.copy(out=Btok[:], in_=btB[:, 0:128])), reads=[rbB], writes=[r_Bt])
        if full:
            cbm, r_cbm = k.sb("sg_cbm", [128, 128], F32)
            k.op("dve", (lambda btB=btB, cbm=cbm: nc.vector.tensor_tensor(out=cbm[:], in0=btB[:, 128:256], in1=mle[:], op=ALU.mult)),
                 reads=[rbB, r_mle], writes=[r_cbm])
        k.free(bB)
        if full:
            MT, r_MT = k.sb("sg_MT", [128, 8, 128], BF16)
            for hh in range(2):
                lta, r_lta = k.sb(f"sg_lta", [128, 4, 128], F32)
                k.op("dve", (lambda lta=lta, cch=cch, hh=hh: nc.vector.tensor_tensor(
                    out=lta[:], in0=ustr[:].unsqueeze(1).to_broadcast([128, 4, 128]),
                    in1=a[:, cch, 8 * g + 4 * hh:8 * g + 4 * hh + 4].unsqueeze(2).to_broadcast([128, 4, 128]), op=ALU.mult)),
                    reads=[r_ustr, r_a], writes=[r_lta])
                ba = k.bank(); bta, rba = k.banks[ba]
                for h4 in range(4):
                    k.op("pe", (lambda h4=h4, bta=bta, lta=lta: nc.tensor.matmul(
                        bta[:, h4 * 128:(h4 + 1) * 128], lhsT=lta[:, h4, :], rhs=tri[:], start=True, stop=True)),
                        reads=[r_lta, r_tri], writes=[rba])
                dec, r_dec = k.sb(f"sg_dec{hh}", [128, 512], BF16)
                k.op("act", (lambda bta=bta, dec=dec: nc.scalar.activation(out=dec[:], in_=bta[:], func=AF.Exp)),
                     reads=[rba], writes=[r_dec])
                k.free(ba)
                k.op("dve", (lambda dec=dec, MT=MT, hh=hh, cbm=cbm: nc.vector.tensor_tensor(
                    out=MT[:, hh * 4:(hh + 1) * 4, :], in0=dec[:].rearrange("p (h l) -> p h l", h=4),
                    in1=cbm[:].unsqueeze(1).to_broadcast([128, 4, 128]), op=ALU.mult)), reads=[r_dec, r_cbm], writes=[r_MT])
            by = k.bank(); bty, rby = k.banks[by]
            for h in range(8):
                k.op("pe", (lambda h=h, bty=bty, MT=MT, xdt=xdt: nc.tensor.matmul(
                    bty[:, h * 64:(h + 1) * 64], lhsT=MT[:, h, :], rhs=xdt[:, h * 64:(h + 1) * 64], start=True, stop=True)),
                    reads=[r_MT, r_xdt], writes=[rby])
            bo = k.bank(); bto, rbo = k.banks[bo]
            k.op("pe", (lambda cs=cs, bto=bto: nc.tensor.matmul(bto[:], lhsT=CcT[:, cs], rhs=Sbf[:], start=True, stop=True)),
                 reads=[r_Cc, r_Sbf], writes=[rbo])
            t1, r_t1 = k.sb("sg_t1", [128, 512], F32)
            k.op("dve", (lambda bto=bto, t1=t1, cch=cch: nc.vector.tensor_tensor(
                out=h8(t1[:]), in0=h8(bto[:]), in1=eacs[:, cch, hs].unsqueeze(2).to_broadcast([128, 8, 64]), op=ALU.mult)),
                reads=[rbo, r_eacs], writes=[r_t1])
            k.free(bo)
            k.op("pool", (lambda t1=t1, xD=xD: nc.gpsimd.tensor_tensor(out=t1[:], in0=t1[:], in1=xD[:], op=ALU.add)),
                 reads=[r_t1, r_xD], writes=[r_t1])
            ysb, r_ysb = k.sb("sg_ysb", [128, 512], F32)
            k.op("dve", (lambda bty=bty, t1=t1, ysb=ysb: nc.vector.tensor_tensor(out=ysb[:], in0=t1[:], in1=bty[:], op=ALU.add)),
                 reads=[rby, r_t1], writes=[r_ysb])
            k.free(by)
            bz = k.bank(); btz, rbz = k.banks[bz]
            for zi, (wz, r_wz) in enumerate(((wz0, r_wz0), (wz1, r_wz1))):
                for kc in range(NKC):
                    k.op("pe", (lambda kc=kc, cs=cs, btz=btz, wz=wz, zi=zi: nc.tensor.matmul(
                        btz[:, zi * 256:(zi + 1) * 256], lhsT=hnT[:, kc, cs], rhs=wz[:, kc, :],
                        start=(kc == 0), stop=(kc == NKC - 1))), reads=[r_hn, r_wz], writes=[rbz])
            zs, r_zs = k.sb("sg_zs", [128, 512], F32)
            k.op("act", (lambda btz=btz, zs=zs: nc.scalar.activation(out=zs[:], in_=btz[:], func=AF.Tanh, scale=0.5)),
                 reads=[rbz], writes=[r_zs])
            k.op("dve", (lambda btz=btz, zs=zs: nc.vector.scalar_tensor_tensor(out=zs[:], in0=zs[:], scalar=1.0, in1=btz[:],
                                                                               op0=ALU.add, op1=ALU.mult)),
                 reads=[r_zs, rbz], writes=[r_zs])
            k.free(bz)
            ygb, r_ygb = k.sb(f"sg_ygb{cch % 2}", [128, 512], BF16)
            k.op("dve", (lambda ysb=ysb, zs=zs, ygb=ygb: nc.vector.tensor_tensor(out=ygb[:], in0=ysb[:], in1=zs[:], op=ALU.mult)),
                 reads=[r_ysb, r_zs], writes=[r_ygb])
            k.op("act", (lambda cch=cch, zs=zs, ygb=ygb: nc.scalar.activation(out=zs[:], in_=ygb[:], func=AF.Square, scale=0.5,
                                                                             accum_out=ss[:, cch:cch + 1])),
                 reads=[r_ygb], writes=[r_zs, r_ss])
            btt = k.bank(); bttt, rbt = k.banks[btt]
            for fc in range(4):
                k.op("pe", (lambda fc=fc, bttt=bttt, ygb=ygb: nc.tensor.matmul(bttt[:, fc * 128:(fc + 1) * 128], lhsT=ygb[:, fc * 128:(fc + 1) * 128],
                                                                               rhs=identb[:], start=True, stop=True)),
                     reads=[r_ygb, r_identb], writes=[rbt])
            k.op("act", (lambda bttt=bttt, cs=cs: nc.scalar.copy(out=ynT_all[:, 4 * g:4 * g + 4, cs], in_=bttt[:].rearrange("p (f t) -> p f t", f=4))),
                 reads=[rbt], writes=[r_yn])
            k.free(btt)
        if (not full) or cch < 7:
            xd, r_xd = k.sb("sg_xd", [128, 512], BF16)
            k.op("pool", (lambda xd=xd, xdt=xdt, cch=cch: nc.gpsimd.tensor_tensor(
                out=h8(xd[:]), in0=h8(xdt[:]), in1=dstate[:, cch, hs].unsqueeze(2).to_broadcast([128, 8, 64]), op=ALU.mult)),
                reads=[r_xdt, r_dst], writes=[r_xd])
            bs = k.bank(); bts, rbs = k.banks[bs]
            k.op("pe", (lambda bts=bts, Btok=Btok, xd=xd: nc.tensor.matmul(bts[:], lhsT=Btok[:], rhs=xd[:], start=True, stop=True)),
                 reads=[r_Bt, r_xd], writes=[rbs])
            k.op("pool", (lambda cch=cch: nc.gpsimd.tensor_tensor(
                out=h8(Sg[:]), in0=h8(Sg[:]), in1=cdec[:, cch, hs].unsqueeze(2).to_broadcast([128, 8, 64]), op=ALU.mult)),
                reads=[r_S, r_cdec], writes=[r_S])
            k.op("dve", (lambda bts=bts: nc.vector.tensor_tensor(out=Sg[:], in0=Sg[:], in1=bts[:], op=ALU.add)), reads=[r_S, rbs], writes=[r_S])
            k.free(bs)
            if full:
                k.op("act", lambda: nc.scalar.copy(out=Sbf[:], in_=Sg[:]), reads=[r_S], writes=[r_Sbf])

    if not full:
        k.op("act", lambda: nc.scalar.copy(out=S_in[:, g, :], in_=Sg[:]), reads=[r_S], writes=[r_Sin])
        return
    eps4, r_eps4 = c["eps4_col"]
    pre_t, r_rbc = k.sb("sg_pre", [128, NT + 8], F32)
    rbc = pre_t[:, 0:NT]
    for hh in range(2):
        b = k.bank(); bt, rb = k.banks[b]
        for c4 in range(4):
            cch = hh * 4 + c4
            dg, r_dg = k.sb(f"sg_diag{c4 % 2}", [128, 128], F32)
            k.op("dve", (lambda dg=dg, cch=cch: nc.vector.tensor_scalar(out=dg[:], in0=identf[:], scalar1=ss[:, cch:cch + 1], scalar2=None,
                                                                       op0=ALU.mult)), reads=[r_identf, r_ss], writes=[r_dg])
            k.op("pe", (lambda dg=dg, bt=bt, c4=c4: nc.tensor.matmul(bt[:, c4 * 128:(c4 + 1) * 128], lhsT=ones[:], rhs=dg[:], start=True, stop=True)),
                 reads=[r_dg, r_ones], writes=[rb])
        k.op("act", (lambda bt=bt, hh=hh: nc.scalar.activation(out=rbc[:, hh * 512:(hh + 1) * 512], in_=bt[:], func=AF.Ln, bias=eps4[:],
                                                               scale=4.0 / 512)), reads=[rb, r_eps4], writes=[r_rbc])
        k.free(b)
    k.op("act", lambda: nc.scalar.activation(out=rbc, in_=rbc, func=AF.Exp, scale=-0.5), reads=[r_rbc], writes=[r_rbc])
    for fc in range(4):
        eng = "dve"
        E = nc.vector
        k.op(eng, (lambda fc=fc, E=E: E.scalar_tensor_tensor(
            out=ynT_all[:, 4 * g + fc, :], in0=ynT_all[:, 4 * g + fc, :], scalar=nw[:, 4 * g + fc:4 * g + fc + 1], in1=rbc,
            op0=ALU.mult, op1=ALU.mult)), reads=[r_yn, r_nw, r_rbc], writes=[r_yn])


def wseq_outproj(prm):
    w = prm["w_out"]
    return [[(w[0:2048, dc * 128:(dc + 1) * 128], 16, 128), (w[2048:4096, dc * 128:(dc + 1) * 128], 16, 128)] for dc in range(16)]


def out_proj(k, hT, r_h, ynT_all, r_yn, prm):
    nc = k.nc
    for dc, pieces in enumerate(wseq_outproj(prm)):
        (wo0, r_wo0), (wo1, r_wo1) = k.wget(pieces)
        for th in range(2):
            ts = slice(th * 512, (th + 1) * 512)
            b = k.bank(); bt, rb = k.banks[b]
            for fc in range(32):
                wo, r_wo = (wo0, r_wo0) if fc < 16 else (wo1, r_wo1)
                k.op("pe", (lambda fc=fc, ts=ts, bt=bt, wo=wo: nc.tensor.matmul(
                    bt[:], lhsT=wo[:, fc % 16, :], rhs=ynT_all[:, fc, ts], start=(fc == 0), stop=(fc == 31))),
                    reads=[r_wo, r_yn], writes=[rb])
            k.op("dve", (lambda dc=dc, ts=ts, bt=bt: nc.vector.tensor_tensor(out=hT[:, dc, ts], in0=hT[:, dc, ts], in1=bt[:], op=ALU.add)),
                 reads=[rb, r_h], writes=[r_h])
            k.free(b)


W_IN_COLS = 10304
KB = 1024


def dram_in(nc, name, shape, dt=F32):
    return nc.dram_tensor(name, list(shape), dt, kind="ExternalInput").ap()


def load_small(k, name, src, shape):
    nc = k.nc
    t, r = k.sb("p_" + name, shape, F32)
    k.op("sp", (lambda t=t, src=src: nc.sync.dma_start(out=t[:], in_=src)), writes=[r], dma=True)
    return t, r


L1_INPUTS = [("xT_prev", [2048, NT]), ("xT_own", [2048, NT]), ("flag", [128, 1]),
             ("mix_g0", [128, 16]), ("ffn_g0", [128, 16]), ("kv_g", [128, 16]),
             ("w_in", [2048, W_IN_COLS]), ("conv_w", [128, 48, 4]), ("conv_b", [128, 48]),
             ("dt_bias", [128, 64]), ("a_log", [128, 64]), ("d_skip", [128, 64]), ("norm_w", [128, 32]),
             ("w_out", [4096, 2048]), ("w_k", [2048, 2048]), ("w_v", [2048, 2048]),
             ("w_router0", [2048, 20]), ("b_router0", [128, 20]),
             ("w_gate0", [16, 2048, 512]), ("w_up0", [16, 2048, 512]), ("w_down0", [16, 512, 2048])]


def wseq_kv(I):
    return ([[(I["w_k"][:, cb * 256:(cb + 1) * 256], NKC, 256)] for cb in range(8)] +
            [[(I["w_v"][:, cb * 256:(cb + 1) * 256], NKC, 256)] for cb in range(8)])


def build_launch1(stop="full"):
    nc = bass.Bass("TRN2", target_bir_lowering=False)
    dr = {n: dram_in(nc, n, s) for n, s in CONST_SHAPES.items()}
    I = {n: dram_in(nc, n, s) for n, s in L1_INPUTS}
    h_out = nc.dram_tensor("h_out", [2048, NT], F32, kind="ExternalOutput").ap()
    kT_out = nc.dram_tensor("kT_out", [2048, NT], F32, kind="ExternalOutput").ap()
    v_out = nc.dram_tensor("v_out", [NT, 2048], F32, kind="ExternalOutput").ap()
    k = K(nc)
    k.setup()
    outs = []

    load_consts(k, dr, ["ident_bf", "ident_f", "ones_f", "tri_incl", "ustrict", "mask_le", "inv128"])
    hnT, r_hn = k.sb("hnT", [128, 16, NT], BF16)
    prm = {}
    mixg = load_small(k, "mix_g0", I["mix_g0"], [128, 16])
    prm["ffn_g"] = [load_small(k, "ffn_g0", I["ffn_g0"], [128, 16])]
    kvg = load_small(k, "kv_g", I["kv_g"], [128, 16])
    prm["b_router"] = [load_small(k, "b_router0", I["b_router0"], [128, 20])]
    prm["w_router"] = [I["w_router0"]]
    prm["w_gate"] = [[I["w_gate0"][e] for e in range(16)]]
    prm["w_up"] = [[I["w_up0"][e] for e in range(16)]]
    prm["w_down"] = [[I["w_down0"][e] for e in range(16)]]
    prm["w_in"] = I["w_in"]
    prm["w_out"] = I["w_out"]
    prm["flag"] = load_small(k, "flag", I["flag"], [128, 1])
    prm["dtb"] = load_small(k, "dt_bias", I["dt_bias"], [128, 64])
    prm["D_bc"] = load_small(k, "d_skip", I["d_skip"], [128, 64])
    prm["nw"] = load_small(k, "norm_w", I["norm_w"], [128, 32])
    A_bc, r_A = load_small(k, "a_log", I["a_log"], [128, 64])
    k.op("act", lambda: nc.scalar.activation(out=A_bc[:], in_=A_bc[:], func=AF.Exp), reads=[r_A], writes=[r_A])
    k.op("dve", lambda: nc.vector.tensor_scalar(out=A_bc[:], in0=A_bc[:], scalar1=-1.0, scalar2=None, op0=ALU.mult), reads=[r_A], writes=[r_A])
    prm["A_bc"] = (A_bc, r_A)
    cwh, r_cwh = k.sb("p_cwh", [128, 48, 4], F32)
    k.op("sp", lambda: nc.sync.dma_start(out=cwh[:], in_=I["conv_w"]), writes=[r_cwh], dma=True)
    k.op("dve", lambda: nc.vector.tensor_scalar(out=cwh[:], in0=cwh[:], scalar1=0.5, scalar2=None, op0=ALU.mult), reads=[r_cwh], writes=[r_cwh])
    prm["cwh"] = (cwh, r_cwh)
    cbh, r_cbh = load_small(k, "conv_b", I["conv_b"], [128, 48])
    k.op("dve", lambda: nc.vector.tensor_scalar(out=cbh[:], in0=cbh[:], scalar1=0.5, scalar2=None, op0=ALU.mult), reads=[r_cbh], writes=[r_cbh])
    prm["cbh"] = (cbh, r_cbh)
    wdt, r_wdt = k.sb("p_wdt", [128, 16, 64], BF16)
    k.op("pool", lambda: nc.gpsimd.dma_start(out=wdt[:], in_=I["w_in"][:, 10240:10304].rearrange("(kc p) f -> p kc f", p=128)),
         writes=[r_wdt], dma=True)
    prm["wdt"] = (wdt, r_wdt)
    prm["hal"] = k.sb("p_hal", [128, 48, 3], F32)
    prm["S_in"] = k.sb("p_Sin", [128, 8, 512], BF16)
    BIG = (k.bump + 63) // 64 * 64
    print("perm end", BIG - SB_BASE, "big bytes", SB_TOP - BIG)
    assert SB_TOP - BIG >= 128 * KB

    LV = ["normA", "prepA", "sA1", "sA", "normB", "sB1", "sB", "mixer", "moe", "full"]
    lv = LV.index(stop)
    nA = 0 if lv < 2 else (1 if lv == 2 else 8)
    nB = 0 if lv < 5 else (1 if lv == 5 else 8)
    for g in range(nA):
        k.wdeclare(wseq_ssd_group(prm, g, False))
    for g in range(nB):
        k.wdeclare(wseq_ssd_group(prm, g, True))
    if lv >= 7:
        k.wdeclare(wseq_outproj(prm))
    if lv >= 8:
        k.wdeclare(wseq_moe(prm, 0))
    if lv >= 9:
        k.wdeclare(wseq_kv(I))

    def early_out():
        o = k.op("pool", lambda: nc.gpsimd.dma_start(out=h_out.rearrange("(kc p) t -> p kc t", p=128), in_=hnT[:]), reads=[r_hn], dma=True)
        cnt = k.p.emit()
        print("launch1(early): ops", len(k.p.ops), "wtiles", len(k.wlist), "signals", cnt)
        k.p.final_wait("pool", [o])
        nc._knames = k.names
        return nc

    k.begin_phase("nA", BIG)
    rmsnorm(k, src_staged(k, I["xT_prev"]), mixg[0], mixg[1], hnT, r_hn)
    if lv == 0:
        return early_out()
    k.begin_phase("sA", BIG)
    P = ssd_prep(k, hnT, r_hn, prm)
    for g in range(nA):
        ssd_group(k, g, "A", hnT, r_hn, P, prm)
    if lv <= 3:
        return early_out()
    k.begin_phase("nB", BIG)
    rmsnorm(k, src_staged(k, I["xT_own"]), mixg[0], mixg[1], hnT, r_hn)
    k.begin_phase("sB", BIG)
    ynT_all, r_yn = k.sb("ynT_all", [128, 32, NT], BF16)
    assert k.bump == BIG + 64 * KB
    if lv == 4:
        return early_out()
    P = ssd_prep(k, hnT, r_hn, prm)
    for g in range(nB):
        ssd_group(k, g, "B", hnT, r_hn, P, prm, ynT_all, r_yn)
    print("sB scratch used", k.bump - BIG - 64 * KB)
    if lv <= 6:
        return early_out()
    k.begin_phase("op", BIG + 64 * KB)
    hT, r_h = k.sb("hT", [128, 16, NT], F32)
    k.op("sp", lambda: nc.sync.dma_start(out=hT[:], in_=I["xT_own"].rearrange("(kc p) t -> p kc t", p=128)), writes=[r_h], dma=True)
    out_proj(k, hT, r_h, ynT_all, r_yn, prm)
    if lv >= 8:
        k.begin_phase("moe0", BIG, BIG + 64 * KB)
        load_consts(k, dr, ["sel16"])
        moe(k, hT, r_h, hnT, r_hn, prm, 0)
        print("moe scratch used", k.bump - BIG)
    if lv >= 9:
        k.begin_phase("kv", BIG, BIG + 64 * KB)
        rmsnorm(k, src_resident(hT, r_h), kvg[0], kvg[1], hnT, r_hn)
        seq = wseq_kv(I)
        for cb in range(8):
            (wk, r_wk), = k.wget(seq[cb])
            st, r_st = k.sb(f"kv_st{cb % 2}", [128, 2, NT], F32)
            for c2 in range(2):
                for th in range(2):
                    ts = slice(th * 512, (th + 1) * 512)
                    b = k.bank(); bt, rb = k.banks[b]
                    for kc in range(NKC):
                        k.op("pe", (lambda kc=kc, ts=ts, bt=bt, wk=wk, c2=c2: nc.tensor.matmul(
                            bt[:], lhsT=wk[:, kc, c2 * 128:(c2 + 1) * 128], rhs=hnT[:, kc, ts],
                            start=(kc == 0), stop=(kc == NKC - 1))), reads=[r_wk, r_hn], writes=[rb])
                    k.op("act", (lambda bt=bt, st=st, c2=c2, ts=ts: nc.scalar.copy(out=st[:, c2, ts], in_=bt[:])), reads=[rb], writes=[r_st])
                    k.free(b)
            o = k.op("sp", (lambda st=st, cb=cb: nc.sync.dma_start(
                out=kT_out[cb * 256:(cb + 1) * 256, :].rearrange("(c p) t -> p c t", p=128), in_=st[:])), reads=[r_st], dma=True)
            outs.append(o)
        for cb in range(8):
            (wv, r_wv), = k.wget(seq[8 + cb])
            st, r_st = k.sb(f"kv_st{cb % 2}", [128, 2, NT], F32)
            stv = st[:].rearrange("p c t -> p (c t)").rearrange("p (t f) -> p t f", f=256)
            for t2 in range(4):
                b = k.bank(); bt, rb = k.banks[b]
                for ti in range(2):
                    tb = t2 * 2 + ti
                    for kc in range(NKC):
                        k.op("pe", (lambda kc=kc, tb=tb, ti=ti, bt=bt, wv=wv: nc.tensor.matmul(
                            bt[:, ti * 256:(ti + 1) * 256], lhsT=hnT[:, kc, tb * 128:(tb + 1) * 128], rhs=wv[:, kc, :],
                            start=(kc == 0), stop=(kc == NKC - 1))), reads=[r_wv, r_hn], writes=[rb])
                k.op("act", (lambda bt=bt, stv=stv, t2=t2: nc.scalar.copy(
                    out=stv[:, 2 * t2:2 * t2 + 2, :], in_=bt[:].rearrange("p (t f) -> p t f", f=256))), reads=[rb], writes=[r_st])
                k.free(b)
            o = k.op("sp", (lambda stv=stv, cb=cb: nc.sync.dma_start(
                out=v_out[:, cb * 256:(cb + 1) * 256].rearrange("(t p) f -> p t f", p=128), in_=stv)), reads=[r_st], dma=True)
            outs.append(o)
    o = k.op("sp", lambda: nc.sync.dma_start(out=h_out.rearrange("(kc p) t -> p kc t", p=128), in_=hT[:]), reads=[r_h], dma=True)
    outs.append(o)
    assert k.widx == len(k.wlist), (k.widx, len(k.wlist))
    cnt = k.p.emit()
    print("launch1: ops", len(k.p.ops), "wtiles", len(k.wlist), "signals", cnt)
    k.p.final_wait("sp", outs)
    return nc


def fm(v, n):
    return np.ascontiguousarray(np.asarray(v, np.float32).reshape(n, 128).T)


def rep(v):
    return np.ascontiguousarray(np.tile(np.asarray(v, np.float32)[None, :], (128, 1)))


def host_inputs_l1(inp, core):
    b, half = core // 2, core % 2
    x = np.asarray(inp["x"], np.float32)
    m = dict(host_consts())
    own = x[b, half * NT:(half + 1) * NT]
    prev = x[b, 0:NT] if half == 1 else np.zeros((NT, D), np.float32)
    m["xT_own"] = np.ascontiguousarray(own.T)
    m["xT_prev"] = np.ascontiguousarray(prev.T)
    m["flag"] = np.full((128, 1), float(half), np.float32)
    m["mix_g0"] = fm(inp["mix_norm"][0], 16)
    m["ffn_g0"] = fm(inp["ffn_norm"][0], 16)
    m["kv_g"] = fm(inp["kv_norm"], 16)
    m["w_in"] = np.asarray(inp["ssm_w_in"][0], np.float32)
    cw = np.asarray(inp["ssm_conv_w"][0], np.float32)
    m["conv_w"] = np.ascontiguousarray(cw.T.reshape(48, 128, 4).transpose(1, 0, 2))
    m["conv_b"] = fm(inp["ssm_conv_b"][0], 48)
    m["dt_bias"] = rep(inp["ssm_dt_bias"][0])
    m["a_log"] = rep(inp["ssm_a_log"][0])
    m["d_skip"] = rep(inp["ssm_d"][0])
    m["norm_w"] = fm(inp["ssm_norm_w"][0], 32)
    m["w_out"] = np.asarray(inp["ssm_w_out"][0], np.float32)
    m["w_k"] = np.asarray(inp["w_k"], np.float32)
    m["w_v"] = np.asarray(inp["w_v"], np.float32)
    m["w_router0"] = np.ascontiguousarray(np.concatenate(
        [np.asarray(inp["moe_w_coarse"][0], np.float32), np.asarray(inp["moe_w_fine"][0], np.float32).reshape(D, 16)], axis=1))
    m["b_router0"] = rep(np.concatenate([np.asarray(inp["moe_b_coarse"][0], np.float32),
                                         np.asarray(inp["moe_b_fine"][0], np.float32).reshape(16)]))
    m["w_gate0"] = np.asarray(inp["moe_w_gate"][0], np.float32)
    m["w_up0"] = np.asarray(inp["moe_w_up"][0], np.float32)
    m["w_down0"] = np.asarray(inp["moe_w_down"][0], np.float32)
    return m


L2_INPUTS = [("hT_in", [2048, NT]), ("kT_all", [2048, 2048]), ("v_all", [2048, 2048]),
             ("mix_g1", [128, 16]), ("ffn_g1", [128, 16]), ("final_g", [128, 16]),
             ("w_q", [2048, 2048]), ("w_o", [2048, 2048]),
             ("w_router1", [2048, 20]), ("b_router1", [128, 20]),
             ("w_gate1", [16, 2048, 512]), ("w_up1", [16, 2048, 512]), ("w_down1", [16, 512, 2048]),
             ("mask_rev", [128, 128])]


def wseq_sq(w):
    return [[(w[:, cb * 256:(cb + 1) * 256], NKC, 256)] for cb in range(8)]


def build_launch2(stop="full"):
    nc = bass.Bass("TRN2", target_bir_lowering=False)
    dr = {n: dram_in(nc, n, s) for n, s in CONST_SHAPES.items()}
    I = {n: dram_in(nc, n, s) for n, s in L2_INPUTS}
    out_d = nc.dram_tensor("outT", [2048, NT], F32, kind="ExternalOutput").ap()
    k = K(nc)
    k.setup()
    load_consts(k, dr, ["ident_bf", "ident_f", "ones_f", "inv128"])
    hn_off = k.bump
    hnT, r_hn = k.sb("hnT", [128, 16, NT], BF16)
    hn_end = k.bump
    prm = {}
    mixg = load_small(k, "mix_g1", I["mix_g1"], [128, 16])
    prm["ffn_g"] = [load_small(k, "ffn_g1", I["ffn_g1"], [128, 16])]
    fing = load_small(k, "final_g", I["final_g"], [128, 16])
    prm["b_router"] = [load_small(k, "b_router1", I["b_router1"], [128, 20])]
    prm["w_router"] = [I["w_router1"]]
    prm["w_gate"] = [[I["w_gate1"][e] for e in range(16)]]
    prm["w_up"] = [[I["w_up1"][e] for e in range(16)]]
    prm["w_down"] = [[I["w_down1"][e] for e in range(16)]]
    mrev, r_mrev = load_small(k, "mask_rev", I["mask_rev"], [128, 128])
    BIG = (k.bump + 63) // 64 * 64
    assert SB_TOP - BIG >= 128 * KB
    LV = ["q", "attn", "wo", "moe", "full"]
    lv = LV.index(stop)
    k.wdeclare(wseq_sq(I["w_q"]))
    if lv >= 2:
        k.wdeclare(wseq_sq(I["w_o"]))
    if lv >= 3:
        k.wdeclare(wseq_moe(prm, 0))

    k.begin_phase("q", BIG + 64 * KB)
    hT, r_h = k.sb("hT", [128, 16, NT], F32)
    k.op("sp", lambda: nc.sync.dma_start(out=hT[:], in_=I["hT_in"].rearrange("(kc p) t -> p kc t", p=128)), writes=[r_h], dma=True)
    k.bump = BIG
    k.limit = BIG + 64 * KB
    qT, r_q = k.sb("qT", [128, 16, NT], BF16)
    rmsnorm(k, src_resident(hT, r_h), mixg[0], mixg[1], hnT, r_hn)
    for cb, pieces in enumerate(wseq_sq(I["w_q"])):
        (wq, r_wq), = k.wget(pieces)
        for c2 in range(2):
            hd = cb * 2 + c2
            for th in range(2):
                ts = slice(th * 512, (th + 1) * 512)
                b = k.bank(); bt, rb = k.banks[b]
                for kc in range(NKC):
                    k.op("pe", (lambda kc=kc, ts=ts, bt=bt, wq=wq, c2=c2: nc.tensor.matmul(
                        bt[:], lhsT=wq[:, kc, c2 * 128:(c2 + 1) * 128], rhs=hnT[:, kc, ts], start=(kc == 0), stop=(kc == NKC - 1))),
                        reads=[r_wq, r_hn], writes=[rb])
                k.op("act", (lambda bt=bt, hd=hd, ts=ts: nc.scalar.activation(out=qT[:, hd, ts], in_=bt[:], func=AF.Copy, scale=128 ** -0.5)),
                     reads=[rb], writes=[r_q])
                k.free(b)

    def finish(src_t, r_src, bf):
        if bf:
            o = k.op("pool", lambda: nc.gpsimd.dma_start(out=out_d.rearrange("(kc p) t -> p kc t", p=128), in_=src_t[:]), reads=[r_src], dma=True)
            eng = "pool"
        else:
            o = k.op("sp", lambda: nc.sync.dma_start(out=out_d.rearrange("(kc p) t -> p kc t", p=128), in_=src_t[:]), reads=[r_src], dma=True)
            eng = "sp"
        assert k.widx == len(k.wlist), (k.widx, len(k.wlist))
        cnt = k.p.emit()
        print("launch2: ops", len(k.p.ops), "wtiles", len(k.wlist), "signals", cnt)
        k.p.final_wait(eng, [o])
        nc._knames = k.names
        return nc
    if lv == 0:
        return finish(qT, r_q, True)

    k.begin_phase("attn", BIG + 32 * KB, BIG + 64 * KB)
    es, r_es = k.sb("at_e", [128, 2048], F32)
    cs, r_cs = k.sb("at_cs", [128, 2048], F32)
    at, r_at = k.sb("at_attn", [128, 2048], BF16)
    atT, r_atT = k.sb("at_attnT", [128, 16, 128], BF16)
    kbuf = [k.sb(f"at_k{i}", [128, 2048], BF16) for i in range(2)]
    k.bump = hn_off
    k.limit = hn_end
    vbuf = [k.sb(f"at_v{i}", [128, 16, 128], BF16) for i in range(2)]
    ones_row, r_onr = k.sb("at_ones", [128, 2048], F32)
    k.op("sp", lambda: nc.sync.dma_start(out=ones_row[:], in_=dr["ones_row"]), writes=[r_onr], dma=True)
    onec, r_onec = k.c["one_col"]
    identb, r_identb = k.c["ident_bf"]
    for hd in range(16):
        kt, r_kt = kbuf[hd % 2]
        vt, r_vt = vbuf[hd % 2]
        k.op("pool", (lambda kt=kt, hd=hd: nc.gpsimd.dma_start(out=kt[:], in_=I["kT_all"][hd * 128:(hd + 1) * 128, :])), writes=[r_kt], dma=True)
        k.op("pool", (lambda vt=vt, hd=hd: nc.gpsimd.dma_start(
            out=vt[:], in_=I["v_all"][:, hd * 128:(hd + 1) * 128].rearrange("(blk p) d -> p blk d", p=128))), writes=[r_vt], dma=True)
        for i in range(8):
            qs = slice(i * 128, (i + 1) * 128)
            c0 = (7 - i) * 128
            ncol = 2048 - c0
            nblk = ncol // 128
            nbank = (ncol + 511) // 512
            zb = []
            for j in range(nbank):
                b = k.bank(); bt, rb = k.banks[b]
                w = min(512, ncol - j * 512)
                k.op("pe", (lambda bt=bt, hd=hd, qs=qs, kt=kt, j=j, w=w, c0=c0: nc.tensor.matmul(
                    bt[:, 0:w], lhsT=qT[:, hd, qs], rhs=kt[:, c0 + j * 512: c0 + j * 512 + w], start=True, stop=True)),
                    reads=[r_q, r_kt], writes=[rb])
                zb.append((b, bt, rb, w))
            for j, (b, bt, rb, w) in enumerate(zb):
                k.op("act", (lambda bt=bt, j=j, w=w: nc.scalar.activation(out=es[:, j * 512: j * 512 + w], in_=bt[:, 0:w], func=AF.Exp)),
                     reads=[rb], writes=[r_es])
            k.op("act", (lambda ncol=ncol: nc.scalar.activation(out=es[:, 0:ncol], in_=es[:, 0:ncol], func=AF.Ln, bias=onec[:], scale=1.0)),
                 reads=[r_es, r_onec], writes=[r_es])
            k.op("dve", lambda: nc.vector.tensor_tensor(out=es[:, 0:128], in0=es[:, 0:128], in1=mrev[:], op=ALU.mult),
                 reads=[r_es, r_mrev], writes=[r_es])
            k.op("dve", (lambda ncol=ncol: nc.vector.tensor_tensor_scan(out=cs[:, 0:ncol], data0=ones_row[:, 0:ncol], data1=es[:, 0:ncol],
                                                                       initial=0.0, op0=ALU.mult, op1=ALU.add)),
                 reads=[r_es, r_onr], writes=[r_cs])
            for j, (b, bt, rb, w) in enumerate(zb):
                k.op("dve", (lambda bt=bt, j=j, w=w: nc.vector.tensor_tensor(out=cs[:, j * 512: j * 512 + w], in0=cs[:, j * 512: j * 512 + w],
                                                                            in1=bt[:, 0:w], op=ALU.subtract)),
                     reads=[r_cs, rb], writes=[r_cs])
                k.free(b)
            k.op("act", (lambda ncol=ncol: nc.scalar.activation(out=at[:, 0:ncol], in_=cs[:, 0:ncol], func=AF.Exp, scale=-1.0)),
                 reads=[r_cs], writes=[r_at])
            k.op("dve", lambda: nc.vector.tensor_tensor(out=at[:, 0:128], in0=at[:, 0:128], in1=mrev[:], op=ALU.mult),
                 reads=[r_at, r_mrev], writes=[r_at])
            for j4 in range((nblk + 3) // 4):
                b = k.bank(); bt, rb = k.banks[b]
                nb = min(4, nblk - j4 * 4)
                for jj in range(nb):
                    blk = j4 * 4 + jj
                    k.op("pe", (lambda bt=bt, jj=jj, blk=blk: nc.tensor.matmul(bt[:, jj * 128:(jj + 1) * 128], lhsT=at[:, blk * 128:(blk + 1) * 128],
                                                                               rhs=identb[:], start=True, stop=True)),
                         reads=[r_at, r_identb], writes=[rb])
                k.op("act", (lambda bt=bt, j4=j4, nb=nb: nc.scalar.copy(out=atT[:, j4 * 4:j4 * 4 + nb, :],
                                                                       in_=bt[:, 0:nb * 128].rearrange("p (b q) -> p b q", b=nb))),
                     reads=[rb], writes=[r_atT])
                k.free(b)
            b = k.bank(); bt, rb = k.banks[b]
            for blk in range(nblk):
                k.op("pe", (lambda bt=bt, blk=blk, vt=vt, i=i, nblk=nblk: nc.tensor.matmul(
                    bt[:, 0:128], lhsT=vt[:, (7 - i) + blk, :], rhs=atT[:, blk, :], start=(blk == 0), stop=(blk == nblk - 1))),
                    reads=[r_vt, r_atT], writes=[rb])
            k.op("act", (lambda bt=bt, hd=hd, qs=qs: nc.scalar.copy(out=qT[:, hd, qs], in_=bt[:, 0:128])), reads=[rb], writes=[r_q])
            k.free(b)
    if lv == 1:
        return finish(qT, r_q, True)

    k.begin_phase("wo", BIG + 32 * KB, BIG + 64 * KB)
    for cb, pieces in enumerate(wseq_sq(I["w_o"])):
        (wo, r_wo), = k.wget(pieces)
        for c2 in range(2):
            dc = cb * 2 + c2
            for th in range(2):
                ts = slice(th * 512, (th + 1) * 512)
                b = k.bank(); bt, rb = k.banks[b]
                for kc in range(NKC):
                    k.op("pe", (lambda kc=kc, ts=ts, bt=bt, wo=wo, c2=c2: nc.tensor.matmul(
                        bt[:], lhsT=wo[:, kc, c2 * 128:(c2 + 1) * 128], rhs=qT[:, kc, ts], start=(kc == 0), stop=(kc == NKC - 1))),
                        reads=[r_wo, r_q], writes=[rb])
                k.op("dve", (lambda dc=dc, ts=ts, bt=bt: nc.vector.tensor_tensor(out=hT[:, dc, ts], in0=hT[:, dc, ts], in1=bt[:], op=ALU.add)),
                     reads=[rb, r_h], writes=[r_h])
                k.free(b)
    if lv == 2:
        return finish(hT, r_h, False)
    k.begin_phase("moe1", BIG, BIG + 64 * KB)
    load_consts(k, dr, ["sel16"])
    moe(k, hT, r_h, hnT, r_hn, prm, 0)
    if lv == 3:
        return finish(hT, r_h, False)
    k.begin_phase("fin", BIG, BIG + 64 * KB)
    outT, r_o = k.sb("outT", [128, 16, NT // 2], F32)
    oo = []
    nc_ = nc
    ones, r_ones = k.c["ones_f"]
    epsc, r_eps = k.c["eps_col"]
    for th in range(2):
        ts = slice(th * 512, (th + 1) * 512)
        b = k.bank(); bt, rb = k.banks[b]
        for kc in range(NKC):
            sq, rsq = k.sb(f"rn_sq{kc % 2}", [128, 512], F32)
            k.op("act", (lambda sq=sq, kc=kc, ts=ts: nc.scalar.activation(out=sq[:], in_=hT[:, kc, ts], func=AF.Square)), reads=[r_h], writes=[rsq])
            k.op("pe", (lambda sq=sq, kc=kc, bt=bt: nc.tensor.matmul(bt[:], lhsT=ones[:], rhs=sq[:], start=(kc == 0), stop=(kc == NKC - 1))),
                 reads=[rsq, r_ones], writes=[rb])
        rt, rr = k.sb("rn_rstd", [128, 512], F32)
        k.op("act", (lambda bt=bt, rt=rt: nc.scalar.activation(out=rt[:], in_=bt[:], func=AF.Ln, bias=epsc[:], scale=1.0 / D)), reads=[rb, r_eps], writes=[rr])
        k.free(b)
        k.op("act", (lambda rt=rt: nc.scalar.activation(out=rt[:], in_=rt[:], func=AF.Exp, scale=-0.5)), reads=[rr], writes=[rr])
        for kc in range(NKC):
            k.op("dve", (lambda kc=kc, ts=ts, rt=rt: nc.vector.scalar_tensor_tensor(
                out=outT[:, kc, :], in0=hT[:, kc, ts], scalar=fing[0][:, kc:kc + 1], in1=rt[:], op0=ALU.mult, op1=ALU.mult)),
                reads=[r_h, fing[1], rr], writes=[r_o])
        oo.append(k.op("sp", (lambda ts=ts: nc.sync.dma_start(out=out_d[:, ts].rearrange("(kc p) t -> p kc t", p=128), in_=outT[:])),
                       reads=[r_o], dma=True))
    assert k.widx == len(k.wlist), (k.widx, len(k.wlist))
    cnt = k.p.emit()
    print("launch2: ops", len(k.p.ops), "wtiles", len(k.wlist), "signals", cnt)
    k.p.final_wait("sp", oo)
    return nc


def host_inputs_l2(inp, core, h_out, kT_outs, v_outs):
    b, half = core // 2, core % 2
    m = dict(host_consts())
    m["hT_in"] = np.ascontiguousarray(h_out)
    kown = kT_outs[core][:, ::-1]
    vown = v_outs[core][::-1, :]
    if half == 1:
        kprev = kT_outs[core - 1][:, ::-1]
        vprev = v_outs[core - 1][::-1, :]
    else:
        kprev = np.zeros_like(kown)
        vprev = np.zeros_like(vown)
    m["kT_all"] = np.ascontiguousarray(np.concatenate([kown, kprev], axis=1))
    m["v_all"] = np.ascontiguousarray(np.concatenate([vown, vprev], axis=0))
    m["mix_g1"] = fm(inp["mix_norm"][1], 16)
    m["ffn_g1"] = fm(inp["ffn_norm"][1], 16)
    m["final_g"] = fm(inp["final_norm"], 16)
    m["w_q"] = np.asarray(inp["sb_w_q"][0], np.float32)
    m["w_o"] = np.asarray(inp["sb_w_out"][0], np.float32)
    m["w_router1"] = np.ascontiguousarray(np.concatenate(
        [np.asarray(inp["moe_w_coarse"][1], np.float32), np.asarray(inp["moe_w_fine"][1], np.float32).reshape(D, 16)], axis=1))
    m["b_router1"] = rep(np.concatenate([np.asarray(inp["moe_b_coarse"][1], np.float32),
                                         np.asarray(inp["moe_b_fine"][1], np.float32).reshape(16)]))
    m["w_gate1"] = np.asarray(inp["moe_w_gate"][1], np.float32)
    m["w_up1"] = np.asarray(inp["moe_w_up"][1], np.float32)
    m["w_down1"] = np.asarray(inp["moe_w_down"][1], np.float32)
    i = np.arange(128)
    m["mask_rev"] = (i[None, :] > 127 - i[:, None]).astype(np.float32)
    return m


F_INPUTS = L1_INPUTS + [("mix_g1", [128, 16]), ("ffn_g1", [128, 16]), ("final_g", [128, 16]),
                        ("w_q", [2048, 2048]), ("w_o", [2048, 2048]),
                        ("w_router1", [2048, 20]), ("b_router1", [128, 20]),
                        ("w_gate1", [16, 2048, 512]), ("w_up1", [16, 2048, 512]), ("w_down1", [16, 512, 2048])]


def build_fused():
    nc = bass.Bass("TRN2", target_bir_lowering=False)
    dr = {n: dram_in(nc, n, s) for n, s in CONST_SHAPES.items()}
    I = {n: dram_in(nc, n, s) for n, s in F_INPUTS}
    out_d = nc.dram_tensor("outT", [2048, NT], F32, kind="ExternalOutput").ap()
    kT_loc = nc.dram_tensor("kT_loc", [2048, NT], BF16, kind="Internal").ap()
    v_loc = nc.dram_tensor("v_loc", [NT, 2048], BF16, kind="Internal").ap()
    kT_prev = nc.dram_tensor("kT_prev", [2048, NT], BF16, kind="Internal").ap()
    v_prev = nc.dram_tensor("v_prev", [NT, 2048], BF16, kind="Internal").ap()
    xloc = nc.dram_tensor("xloc", [2048, 512], BF16, kind="Internal").ap()
    xg = nc.dram_tensor("xg", [4096, 512], BF16, kind="Internal").ap()
    k = K(nc)
    k.setup()

    load_consts(k, dr, ["ident_bf", "ident_f", "ones_f", "tri_incl", "ustrict", "mask_le"])
    hn_off = k.bump
    hnT, r_hn = k.sb("hnT", [128, 16, NT], BF16)
    hn_end = k.bump
    prm = {}
    mixg = [load_small(k, "mix_g0", I["mix_g0"], [128, 16]), load_small(k, "mix_g1", I["mix_g1"], [128, 16])]
    prm["ffn_g"] = [load_small(k, "ffn_g0", I["ffn_g0"], [128, 16]), load_small(k, "ffn_g1", I["ffn_g1"], [128, 16])]
    kvg = load_small(k, "kv_g", I["kv_g"], [128, 16])
    fing = load_small(k, "final_g", I["final_g"], [128, 16])
    prm["b_router"] = [load_small(k, "b_router0", I["b_router0"], [128, 20]), load_small(k, "b_router1", I["b_router1"], [128, 20])]
    prm["w_router"] = [I["w_router0"], I["w_router1"]]
    prm["w_gate"] = [[I["w_gate0"][e] for e in range(16)], [I["w_gate1"][e] for e in range(16)]]
    prm["w_up"] = [[I["w_up0"][e] for e in range(16)], [I["w_up1"][e] for e in range(16)]]
    prm["w_down"] = [[I["w_down0"][e] for e in range(16)], [I["w_down1"][e] for e in range(16)]]
    prm["w_in"] = I["w_in"]
    prm["w_out"] = I["w_out"]
    prm["flag"] = load_small(k, "flag", I["flag"], [128, 1])
    flag, r_flag = prm["flag"]
    prm["dtb"] = load_small(k, "dt_bias", I["dt_bias"], [128, 64])
    prm["D_bc"] = load_small(k, "d_skip", I["d_skip"], [128, 64])
    prm["nw"] = load_small(k, "norm_w", I["norm_w"], [128, 32])
    A_bc, r_A = load_small(k, "a_log", I["a_log"], [128, 64])
    k.op("act", lambda: nc.scalar.activation(out=A_bc[:], in_=A_bc[:], func=AF.Exp), reads=[r_A], writes=[r_A])
    k.op("dve", lambda: nc.vector.tensor_scalar(out=A_bc[:], in0=A_bc[:], scalar1=-1.0, scalar2=None, op0=ALU.mult), reads=[r_A], writes=[r_A])
    prm["A_bc"] = (A_bc, r_A)
    cwh, r_cwh = k.sb("p_cwh", [128, 48, 4], F32)
    k.op("sp", lambda: nc.sync.dma_start(out=cwh[:], in_=I["conv_w"]), writes=[r_cwh], dma=True)
    k.op("dve", lambda: nc.vector.tensor_scalar(out=cwh[:], in0=cwh[:], scalar1=0.5, scalar2=None, op0=ALU.mult), reads=[r_cwh], writes=[r_cwh])
    prm["cwh"] = (cwh, r_cwh)
    cbh, r_cbh = load_small(k, "conv_b", I["conv_b"], [128, 48])
    k.op("dve", lambda: nc.vector.tensor_scalar(out=cbh[:], in0=cbh[:], scalar1=0.5, scalar2=None, op0=ALU.mult), reads=[r_cbh], writes=[r_cbh])
    prm["cbh"] = (cbh, r_cbh)
    wdt, r_wdt = k.sb("p_wdt", [128, 16, 64], BF16)
    k.op("pool", lambda: nc.gpsimd.dma_start(out=wdt[:], in_=I["w_in"][:, 10240:10304].rearrange("(kc p) f -> p kc f", p=128)),
         writes=[r_wdt], dma=True)
    prm["wdt"] = (wdt, r_wdt)
    prm["hal"] = k.sb("p_hal", [128, 48, 3], F32)
    sin_off = k.bump
    prm["S_in"] = k.sb("p_Sin", [128, 8, 512], BF16)
    sin_end = k.bump
    BIG = (k.bump + 63) // 64 * 64
    print("fused: perm end", BIG - SB_BASE, "big bytes", SB_TOP - BIG)
    assert SB_TOP - BIG >= 128 * KB

    for g in range(8):
        k.wdeclare(wseq_ssd_group(prm, g, False))
    for g in range(8):
        k.wdeclare(wseq_ssd_group(prm, g, True))
    k.wdeclare(wseq_outproj(prm))
    k.wdeclare(wseq_moe(prm, 0))
    k.wdeclare(wseq_kv(I))
    k.wdeclare(wseq_sq(I["w_q"]))
    k.wdeclare(wseq_sq(I["w_o"]))
    k.wdeclare(wseq_moe(prm, 1))

    k.begin_phase("nA", BIG)
    rmsnorm(k, src_staged(k, I["xT_prev"]), mixg[0][0], mixg[0][1], hnT, r_hn)
    k.begin_phase("sA", BIG)
    P = ssd_prep(k, hnT, r_hn, prm)
    for g in range(8):
        ssd_group(k, g, "A", hnT, r_hn, P, prm)
    k.begin_phase("nB", BIG)
    rmsnorm(k, src_staged(k, I["xT_own"]), mixg[0][0], mixg[0][1], hnT, r_hn)
    k.begin_phase("sB", BIG)
    ynT_all, r_yn = k.sb("ynT_all", [128, 32, NT], BF16)
    P = ssd_prep(k, hnT, r_hn, prm)
    for g in range(8):
        ssd_group(k, g, "B", hnT, r_hn, P, prm, ynT_all, r_yn)
    k.begin_phase("op", BIG + 64 * KB)
    hT, r_h = k.sb("hT", [128, 16, NT], F32)
    k.op("sp", lambda: nc.sync.dma_start(out=hT[:], in_=I["xT_own"].rearrange("(kc p) t -> p kc t", p=128)), writes=[r_h], dma=True)
    out_proj(k, hT, r_h, ynT_all, r_yn, prm)
    k.begin_phase("moe0", BIG, BIG + 64 * KB)
    load_consts(k, dr, ["sel16", "inv128"])
    moe(k, hT, r_h, hnT, r_hn, prm, 0)

    k.begin_phase("kv", BIG, BIG + 64 * KB)
    r_kloc = k.p.reg("kT_loc"); r_vloc = k.p.reg("v_loc"); r_kp = k.p.reg("kT_prev"); r_vp = k.p.reg("v_prev")
    r_xloc = k.p.reg("xloc"); r_xg = k.p.reg("xg")
    rmsnorm(k, src_resident(hT, r_h), kvg[0], kvg[1], hnT, r_hn)
    seq = wseq_kv(I)
    for cb in range(8):
        (wk, r_wk), = k.wget(seq[cb])
        st, r_st = k.sb(f"kv_st{cb % 2}", [128, 2, NT], BF16)
        for c2 in range(2):
            for th in range(2):
                ts = slice(th * 512, (th + 1) * 512)
                b = k.bank(); bt, rb = k.banks[b]
                for kc in range(NKC):
                    k.op("pe", (lambda kc=kc, ts=ts, bt=bt, wk=wk, c2=c2: nc.tensor.matmul(
                        bt[:], lhsT=wk[:, kc, c2 * 128:(c2 + 1) * 128], rhs=hnT[:, kc, ts],
                        start=(kc == 0), stop=(kc == NKC - 1))), reads=[r_wk, r_hn], writes=[rb])
                k.op("act", (lambda bt=bt, st=st, c2=c2, ts=ts: nc.scalar.copy(out=st[:, c2, ts], in_=bt[:])), reads=[rb], writes=[r_st])
                k.free(b)
        k.op("sp", (lambda st=st, cb=cb: nc.sync.dma_start(
            out=kT_loc[cb * 256:(cb + 1) * 256, :].rearrange("(c p) t -> p c t", p=128), in_=st[:])), reads=[r_st], writes=[r_kloc], dma=True)
    for cb in range(8):
        (wv, r_wv), = k.wget(seq[8 + cb])
        st, r_st = k.sb(f"kv_st{cb % 2}", [128, 2, NT], BF16)
        stv = st[:].rearrange("p c t -> p (c t)").rearrange("p (t f) -> p t f", f=256)
        for t2 in range(4):
            b = k.bank(); bt, rb = k.banks[b]
            for ti in range(2):
                tb = t2 * 2 + ti
                for kc in range(NKC):
                    k.op("pe", (lambda kc=kc, tb=tb, ti=ti, bt=bt, wv=wv: nc.tensor.matmul(
                        bt[:, ti * 256:(ti + 1) * 256], lhsT=hnT[:, kc, tb * 128:(tb + 1) * 128], rhs=wv[:, kc, :],
                        start=(kc == 0), stop=(kc == NKC - 1))), reads=[r_wv, r_hn], writes=[rb])
            k.op("act", (lambda bt=bt, stv=stv, t2=t2: nc.scalar.copy(
                out=stv[:, 2 * t2:2 * t2 + 2, :], in_=bt[:].rearrange("p (t f) -> p t f", f=256))), reads=[rb], writes=[r_st])
            k.free(b)
        k.op("sp", (lambda stv=stv, cb=cb: nc.sync.dma_start(
            out=v_loc[:, cb * 256:(cb + 1) * 256].rearrange("(t p) f -> p t f", p=128), in_=stv)), reads=[r_st], writes=[r_vloc], dma=True)

    k.begin_phase("q", BIG, BIG + 64 * KB)
    rg = [[0, 1], [2, 3], [4, 5], [6, 7]]
    xl_v = xloc.rearrange("(t a) c -> t (a c)", a=4)
    xg_v = xg[0:2048, :].rearrange("(t a) c -> t (a c)", a=4)
    rounds = [(kT_loc[:, 0:512], xloc, xg[0:2048, :], kT_prev[:, 0:512], r_kloc, r_kp),
              (kT_loc[:, 512:1024], xloc, xg[0:2048, :], kT_prev[:, 512:1024], r_kloc, r_kp),
              (v_loc[0:512, :], xl_v, xg_v, v_prev[0:512, :], r_vloc, r_vp),
              (v_loc[512:1024, :], xl_v, xg_v, v_prev[512:1024, :], r_vloc, r_vp)]
    for (src, xin, xout, dst, r_src, r_dst) in rounds:
        k.op("sp", (lambda src=src, xin=xin: nc.sync.dma_start(out=xin, in_=src)), reads=[r_src], writes=[r_xloc], dma=True)
        k.op("pool", lambda: nc.gpsimd.collective_compute("AllGather", op=ALU.bypass, replica_groups=rg, ins=[xloc], outs=[xg]),
             reads=[r_xloc], writes=[r_xg])
        k.op("sp", (lambda xout=xout, dst=dst: nc.sync.dma_start(out=dst, in_=xout)), reads=[r_xg], writes=[r_dst], dma=True)
    qT, r_q = k.sb("qT", [128, 16, NT], BF16)
    rmsnorm(k, src_resident(hT, r_h), mixg[1][0], mixg[1][1], hnT, r_hn)
    for cb, pieces in enumerate(wseq_sq(I["w_q"])):
        (wq, r_wq), = k.wget(pieces)
        for c2 in range(2):
            hd = cb * 2 + c2
            for th in range(2):
                ts = slice(th * 512, (th + 1) * 512)
                b = k.bank(); bt, rb = k.banks[b]
                for kc in range(NKC):
                    k.op("pe", (lambda kc=kc, ts=ts, bt=bt, wq=wq, c2=c2: nc.tensor.matmul(
                        bt[:], lhsT=wq[:, kc, c2 * 128:(c2 + 1) * 128], rhs=hnT[:, kc, ts], start=(kc == 0), stop=(kc == NKC - 1))),
                        reads=[r_wq, r_hn], writes=[rb])
                k.op("act", (lambda bt=bt, hd=hd, ts=ts: nc.scalar.activation(out=qT[:, hd, ts], in_=bt[:], func=AF.Copy, scale=128 ** -0.5)),
                     reads=[rb], writes=[r_q])
                k.free(b)

    k.begin_phase("attn", BIG + 32 * KB, BIG + 64 * KB)
    sets = [dict(), dict()]
    sets[0]["es"] = k.sb("at_e0", [128, 2048], F32)
    sets[0]["cs"] = k.sb("at_cs0", [128, 2048], F32)
    sets[0]["at"] = k.sb("at_attn0", [128, 2048], BF16)
    sets[0]["atT"] = k.sb("at_attnT0", [128, 16, 128], BF16)
    kbuf = [k.sb(f"at_k{i}", [128, 2048], BF16) for i in range(2)]
    k.bump = hn_off
    k.limit = hn_end
    sets[1]["es"] = k.sb("at_e1", [128, 2048], F32)
    sets[1]["cs"] = k.sb("at_cs1", [128, 2048], F32)
    sets[1]["at"] = k.sb("at_attn1", [128, 2048], BF16)
    sets[1]["atT"] = k.sb("at_attnT1", [128, 16, 128], BF16)
    vbuf = [k.sb(f"at_v{i}", [128, 16, 128], BF16) for i in range(2)]
    k.bump = sin_off
    k.limit = sin_end
    load_consts(k, dr, ["mask_lt"])
    sets[0]["negT"] = k.sb("at_negT0", [128, 1], F32)
    sets[1]["negT"] = k.sb("at_negT1", [128, 1], F32)
    onec, r_onec = k.c["one_col"]
    identb, r_identb = k.c["ident_bf"]
    mlt, r_mlt = k.c["mask_lt"]

    rq = {(hd_, i_): k.p.reg(f"q{hd_}_{i_}") for hd_ in range(16) for i_ in range(8)}

    def load_kv(hd):
        kt, r_kt = kbuf[hd % 2]
        vt, r_vt = vbuf[hd % 2]
        k.op("sp", (lambda: nc.sync.dma_start(out=kt[:, 0:NT], in_=kT_prev[hd * 128:(hd + 1) * 128, :])), writes=[r_kt], dma=True)
        k.op("sp", (lambda: nc.sync.dma_start(out=kt[:, NT:2 * NT], in_=kT_loc[hd * 128:(hd + 1) * 128, :])), writes=[r_kt], dma=True)
        k.op("sp", (lambda: nc.sync.dma_start(
            out=vt[:, 0:8, :], in_=v_prev[:, hd * 128:(hd + 1) * 128].rearrange("(blk p) d -> p blk d", p=128))), writes=[r_vt], dma=True)
        k.op("sp", (lambda: nc.sync.dma_start(
            out=vt[:, 8:16, :], in_=v_loc[:, hd * 128:(hd + 1) * 128].rearrange("(blk p) d -> p blk d", p=128))), writes=[r_vt], dma=True)
        k.op("pool", (lambda: nc.gpsimd.tensor_scalar(out=vt[:, 0:8, :], in0=vt[:, 0:8, :], scalar1=flag[:], scalar2=None, op0=ALU.mult)),
             reads=[r_vt, r_flag], writes=[r_vt])

    zstate = {}

    def s1_pe(n, hd, i):
        kt, r_kt = kbuf[hd % 2]
        qs = slice(i * 128, (i + 1) * 128)
        ncol = (9 + i) * 128
        zb = []
        for j in range((ncol + 511) // 512):
            b = k.bank(); bt, rb = k.banks[b]
            w = min(512, ncol - j * 512)
            k.op("pe", (lambda bt=bt, j=j, w=w: nc.tensor.matmul(
                bt[:, 0:w], lhsT=qT[:, hd, qs], rhs=kt[:, j * 512: j * 512 + w], start=True, stop=True)),
                reads=[rq[(hd, i)], r_kt], writes=[rb])
            zb.append((b, bt, rb, w))
        zstate[n] = zb

    def s1_act(n, hd, i):
        S = sets[n % 2]
        es, r_es = S["es"]; cs, r_cs = S["cs"]
        ncol = (9 + i) * 128
        for j, (b, bt, rb, w) in enumerate(zstate[n]):
            k.op("act", (lambda bt=bt, j=j, w=w: nc.scalar.activation(out=cs[:, j * 512: j * 512 + w], in_=bt[:, 0:w], func=AF.Exp)),
                 reads=[rb], writes=[r_cs])
        k.op("act", (lambda: nc.scalar.activation(out=es[:, 0:ncol], in_=cs[:, 0:ncol], func=AF.Ln, bias=onec[:], scale=1.0)),
             reads=[r_cs, r_onec], writes=[r_es])

    def s1_dve(n, hd, i):
        S = sets[n % 2]
        es, r_es = S["es"]; cs, r_cs = S["cs"]; ngT, r_ngT = S["negT"]
        ncol = (9 + i) * 128
        k.op("dve", (lambda: nc.vector.tensor_tensor(out=es[:, ncol - 128:ncol], in0=es[:, ncol - 128:ncol], in1=mlt[:], op=ALU.mult)),
             reads=[r_es, r_mlt], writes=[r_es])
        k.op("dve", (lambda: nc.vector.tensor_tensor_scan(out=cs[:, 0:ncol], data0=onec[:].to_broadcast([128, ncol]), data1=es[:, 0:ncol],
                                                          initial=0.0, op0=ALU.mult, op1=ALU.add)),
             reads=[r_es, r_onec], writes=[r_cs])
        k.op("dve", (lambda: nc.vector.tensor_scalar(out=ngT[:], in0=cs[:, ncol - 1:ncol], scalar1=-1.0, scalar2=None, op0=ALU.mult)),
             reads=[r_cs], writes=[r_ngT])
        for j, (b, bt, rb, w) in enumerate(zstate.pop(n)):
            lo = j * 512
            if j == 0:
                k.op("dve", (lambda bt=bt: nc.vector.tensor_copy(out=es[:, 0:1], in_=bt[:, 0:1])), reads=[rb], writes=[r_es])
                k.op("dve", (lambda bt=bt, w=w: nc.vector.tensor_tensor(out=es[:, 1:w], in0=cs[:, 0:w - 1], in1=bt[:, 1:w], op=ALU.add)),
                     reads=[r_cs, rb], writes=[r_es])
            else:
                k.op("dve", (lambda bt=bt, w=w, lo=lo: nc.vector.tensor_tensor(out=es[:, lo:lo + w], in0=cs[:, lo - 1:lo + w - 1], in1=bt[:, 0:w], op=ALU.add)),
                     reads=[r_cs, rb], writes=[r_es])
            k.free(b)

    def s2_exp(n, hd, i):
        S = sets[n % 2]
        es, r_es = S["es"]; at, r_at = S["at"]; ngT, r_ngT = S["negT"]
        ncol = (9 + i) * 128
        k.op("act", (lambda: nc.scalar.activation(out=at[:, 0:ncol], in_=es[:, 0:ncol], func=AF.Exp, bias=ngT[:], scale=1.0)),
             reads=[r_es, r_ngT], writes=[r_at])
        k.op("dve", (lambda: nc.vector.tensor_tensor(out=at[:, ncol - 128:ncol], in0=at[:, ncol - 128:ncol], in1=mlt[:], op=ALU.mult)),
             reads=[r_at, r_mlt], writes=[r_at])

    def s2_tr(n, hd, i):
        S = sets[n % 2]
        at, r_at = S["at"]; atT, r_atT = S["atT"]
        nblk = 9 + i
        for j4 in range((nblk + 3) // 4):
            b = k.bank(); bt, rb = k.banks[b]
            nb = min(4, nblk - j4 * 4)
            for jj in range(nb):
                blk = j4 * 4 + jj
                k.op("pe", (lambda bt=bt, jj=jj, blk=blk: nc.tensor.matmul(bt[:, jj * 128:(jj + 1) * 128], lhsT=at[:, blk * 128:(blk + 1) * 128],
                                                                           rhs=identb[:], start=True, stop=True)),
                     reads=[r_at, r_identb], writes=[rb])
            if j4 % 2 == 0:
                k.op("act", (lambda bt=bt, j4=j4, nb=nb: nc.scalar.copy(out=atT[:, j4 * 4:j4 * 4 + nb, :],
                                                                       in_=bt[:, 0:nb * 128].rearrange("p (b q) -> p b q", b=nb))),
                     reads=[rb], writes=[r_atT])
            else:
                k.op("dve", (lambda bt=bt, j4=j4, nb=nb: nc.vector.tensor_copy(out=atT[:, j4 * 4:j4 * 4 + nb, :],
                                                                              in_=bt[:, 0:nb * 128].rearrange("p (b q) -> p b q", b=nb))),
                     reads=[rb], writes=[r_atT])
            k.free(b)

    def s3(n, hd, i):
        S = sets[n % 2]
        atT, r_atT = S["atT"]
        vt, r_vt = vbuf[hd % 2]
        qs = slice(i * 128, (i + 1) * 128)
        nblk = 9 + i
        b = k.bank(); bt, rb = k.banks[b]
        for blk in range(nblk):
            k.op("pe", (lambda bt=bt, blk=blk: nc.tensor.matmul(
                bt[:, 0:128], lhsT=vt[:, blk, :], rhs=atT[:, blk, :], start=(blk == 0), stop=(blk == nblk - 1))),
                reads=[r_vt, r_atT], writes=[rb])
        k.op("act", (lambda bt=bt: nc.scalar.copy(out=qT[:, hd, qs], in_=bt[:, 0:128])), reads=[rb], writes=[rq[(hd, i)]])
        k.free(b)

    its = [(hd, i) for hd in range(16) for i in range(8)]
    load_kv(0)
    for n0 in (0, 1):
        s1_pe(n0, *its[n0]); s1_act(n0, *its[n0]); s1_dve(n0, *its[n0])
    for n, (hd, i) in enumerate(its):
        nxt = its[n + 2] if n + 2 < len(its) else None
        if i == 2 and hd + 1 < 16:
            load_kv(hd + 1)
        if nxt:
            s1_pe(n + 2, *nxt)
        s2_exp(n, hd, i)
        if nxt:
            s1_act(n + 2, *nxt)
        s2_tr(n, hd, i)
        if nxt:
            s1_dve(n + 2, *nxt)
        s3(n, hd, i)

    k.begin_phase("wo", BIG + 32 * KB, BIG + 64 * KB)
    for cb, pieces in enumerate(wseq_sq(I["w_o"])):
        (wo, r_wo), = k.wget(pieces)
        for c2 in range(2):
            dc = cb * 2 + c2
            for th in range(2):
                ts = slice(th * 512, (th + 1) * 512)
                b = k.bank(); bt, rb = k.banks[b]
                for kc in range(NKC):
                    k.op("pe", (lambda kc=kc, ts=ts, bt=bt, wo=wo, c2=c2: nc.tensor.matmul(
                        bt[:], lhsT=wo[:, kc, c2 * 128:(c2 + 1) * 128], rhs=qT[:, kc, ts], start=(kc == 0), stop=(kc == NKC - 1))),
                        reads=[r_wo, r_q], writes=[rb])
                k.op("dve", (lambda dc=dc, ts=ts, bt=bt: nc.vector.tensor_tensor(out=hT[:, dc, ts], in0=hT[:, dc, ts], in1=bt[:], op=ALU.add)),
                     reads=[rb, r_h], writes=[r_h])
                k.free(b)
    k.begin_phase("moe1", BIG, BIG + 64 * KB)
    load_consts(k, dr, ["sel16", "inv128"])
    moe(k, hT, r_h, hnT, r_hn, prm, 1)
    k.begin_phase("fin", BIG, BIG + 64 * KB)
    outT, r_o = k.sb("outT", [128, 16, NT // 2], F32)
    oo = []
    ones, r_ones = k.c["ones_f"]
    epsc, r_eps = k.c["eps_col"]
    for th in range(2):
        ts = slice(th * 512, (th + 1) * 512)
        b = k.bank(); bt, rb = k.banks[b]
        for kc in range(NKC):
            sq, rsq = k.sb(f"rn_sq{kc % 2}", [128, 512], F32)
            k.op("act", (lambda sq=sq, kc=kc, ts=ts: nc.scalar.activation(out=sq[:], in_=hT[:, kc, ts], func=AF.Square)), reads=[r_h], writes=[rsq])
            k.op("pe", (lambda sq=sq, kc=kc, bt=bt: nc.tensor.matmul(bt[:], lhsT=ones[:], rhs=sq[:], start=(kc == 0), stop=(kc == NKC - 1))),
                 reads=[rsq, r_ones], writes=[rb])
        rt, rr = k.sb("rn_rstd", [128, 512], F32)
        k.op("act", (lambda bt=bt, rt=rt: nc.scalar.activation(out=rt[:], in_=bt[:], func=AF.Ln, bias=epsc[:], scale=1.0 / D)), reads=[rb, r_eps], writes=[rr])
        k.free(b)
        k.op("act", (lambda rt=rt: nc.scalar.activation(out=rt[:], in_=rt[:], func=AF.Exp, scale=-0.5)), reads=[rr], writes=[rr])
        for kc in range(NKC):
            k.op("dve", (lambda kc=kc, ts=ts, rt=rt: nc.vector.scalar_tensor_tensor(
                out=outT[:, kc, :], in0=hT[:, kc, ts], scalar=fing[0][:, kc:kc + 1], in1=rt[:], op0=ALU.mult, op1=ALU.mult)),
                reads=[r_h, fing[1], rr], writes=[r_o])
        oo.append(k.op("sp", (lambda ts=ts: nc.sync.dma_start(out=out_d[:, ts].rearrange("(kc p) t -> p kc t", p=128), in_=outT[:])),
                       reads=[r_o], dma=True))
    assert k.widx == len(k.wlist), (k.widx, len(k.wlist))
    cnt = k.p.emit()
    print("fused: ops", len(k.p.ops), "wtiles", len(k.wlist), "signals", cnt)
    k.p.final_wait("sp", oo)
    return nc


def host_inputs_fused(inp, core):
    m = host_inputs_l1(inp, core)
    m["mix_g1"] = fm(inp["mix_norm"][1], 16)
    m["ffn_g1"] = fm(inp["ffn_norm"][1], 16)
    m["final_g"] = fm(inp["final_norm"], 16)
    m["w_q"] = np.asarray(inp["sb_w_q"][0], np.float32)
    m["w_o"] = np.asarray(inp["sb_w_out"][0], np.float32)
    m["w_router1"] = np.ascontiguousarray(np.concatenate(
        [np.asarray(inp["moe_w_coarse"][1], np.float32), np.asarray(inp["moe_w_fine"][1], np.float32).reshape(D, 16)], axis=1))
    m["b_router1"] = rep(np.concatenate([np.asarray(inp["moe_b_coarse"][1], np.float32),
                                         np.asarray(inp["moe_b_fine"][1], np.float32).reshape(16)]))
    m["w_gate1"] = np.asarray(inp["moe_w_gate"][1], np.float32)
    m["w_up1"] = np.asarray(inp["moe_w_up"][1], np.float32)
    m["w_down1"] = np.asarray(inp["moe_w_down"][1], np.float32)
    return m


_PROGS = {}


def kernel(**inputs):
    inp = {k_: np.asarray(v) for k_, v in inputs.items()}
    ncores = 8
    names = set(CONST_SHAPES) | {n for n, _ in F_INPUTS}
    if "fused" not in _PROGS:
        _PROGS["fused"] = build_fused()
    nc = _PROGS["fused"]
    ims = []
    for c in range(ncores):
        m = host_inputs_fused(inp, c)
        ims.append({n: v for n, v in m.items() if n in names})
    res = run_bass_kernel_spmd(nc, ims, core_ids=list(range(ncores)))
    out = np.zeros((4, 2048, 2048), np.float32)
    for c in range(ncores):
        b, half = c // 2, c % 2
        out[b, half * NT:(half + 1) * NT, :] = np.asarray(res.results[c]["outT"], np.float32).T
    return out
```

```python
from concourse.bass_utils import run_bass_kernel_spmd
import numpy as np
from contextlib import ExitStack
import concourse.bass as bass
import concourse.mybir as mybir

F32 = mybir.dt.float32
BF16 = mybir.dt.bfloat16
AF = mybir.ActivationFunctionType
ALU = mybir.AluOpType
AX = mybir.AxisListType


class Reg:
    __slots__ = ("name", "w", "r")

    def __init__(self, name):
        self.name = name
        self.w = None
        self.r = []


class Prog:
    ENGS = ("pe", "act", "dve", "pool", "sp")

    def __init__(self, nc, n_dma_sems=20):
        self.nc = nc
        self.es = ExitStack()
        self.ops = []
        self.eng_obj = {"pe": nc.tensor, "act": nc.scalar, "dve": nc.vector,
                        "pool": nc.gpsimd, "sp": nc.sync}
        self.n_dma_sems = n_dma_sems
        self._uid = 0
        self.allregs = []
        self.bar_from = 0
        self.tag = None
        self.scopes = False

    def sb(self, shape, dt, name=None):
        self._uid += 1
        return self.es.enter_context(self.nc.sbuf_tensor(name or f"sb{self._uid}", list(shape), dt))

    def ps(self, shape, dt, name=None):
        self._uid += 1
        return self.es.enter_context(self.nc.psum_tensor(name or f"ps{self._uid}", list(shape), dt))

    def reg(self, name=None):
        self._uid += 1
        r = Reg(name or f"r{self._uid}")
        self.allregs.append(r)
        return r

    def barrier(self):
        last = {}
        dmas = []
        for oid in range(self.bar_from, len(self.ops)):
            o = self.ops[oid]
            if o["dma"]:
                dmas.append(oid)
            else:
                last[o["eng"]] = oid
        deps = set(last.values()) | set(dmas)
        for e in self.ENGS:
            self.ops.append(dict(eng=e, fn=(lambda: None), deps=set(deps), dma=False, tag=self.tag))
        self.bar_from = len(self.ops)
        for r in self.allregs:
            r.w = None
            r.r = []

    def op(self, eng, fn, reads=(), writes=(), dma=False, nosync_same_pe=True):
        oid = len(self.ops)
        deps = set()
        for r in reads:
            if r.w is not None:
                deps.add(r.w)
        for w in writes:
            if w.w is not None:
                deps.add(w.w)
            deps.update(w.r)
        for r in reads:
            r.r.append(oid)
        for w in writes:
            w.w = oid
            w.r = []
        deps.discard(oid)
        self.ops.append(dict(eng=eng, fn=fn, deps=deps, dma=dma, tag=self.tag))
        return oid

    def emit(self):
        nc = self.nc
        ops = self.ops
        need = [False] * len(ops)
        for o in ops:
            for d in list(o["deps"]):
                od = ops[d]
                if od["eng"] == "pe" and o["eng"] == "pe" and not od["dma"] and not o["dma"]:
                    o["deps"].discard(d)
                    continue
                need[d] = True
        esem = {e: self.es.enter_context(nc.semaphore(f"s_{e}")) for e in self.ENGS}
        dsem = {}
        for q in ("sp", "pool"):
            dsem[q] = [self.es.enter_context(nc.semaphore(f"d_{q}{i}")) for i in range(self.n_dma_sems)]
        ecount = {e: 0 for e in self.ENGS}
        dcount = {q: [0] * self.n_dma_sems for q in dsem}
        drr = {q: 0 for q in dsem}
        sig = [None] * len(ops)
        waited = {}
        cur_tag = None
        for oid, o in enumerate(ops):
            eng = o["eng"]
            E = self.eng_obj[eng]
            if self.scopes and o.get("tag") != cur_tag:
                if cur_tag is not None:
                    nc.leave_named_scope(cur_tag, cur_sid, False)
                cur_tag = o.get("tag")
                if cur_tag is not None:
                    cur_sid, _ = nc.enter_named_scope(cur_tag, False)
            wl = {}
            for d in o["deps"]:
                s, v = sig[d]
                k = id(s)
                if k not in wl or wl[k][1] < v:
                    wl[k] = (s, v)
            if o["dma"]:
                slot = drr[eng]
                drr[eng] = (slot + 1) % self.n_dma_sems
                ds = dsem[eng][slot]
                prev = dcount[eng][slot]
                if prev > 0:
                    k = id(ds)
                    if k not in wl or wl[k][1] < prev:
                        wl[k] = (ds, prev)
            for k, (ws, wv) in wl.items():
                if waited.get((eng, k), 0) >= wv:
                    continue
                waited[(eng, k)] = wv
                E.wait_ge(ws, wv)
            inst = o["fn"]()
            if inst is None:
                continue
            if o["dma"]:
                dcount[eng][slot] = prev + 16
                inst.then_inc(ds, 16)
                sig[oid] = (ds, prev + 16)
            elif need[oid]:
                ecount[eng] += 1
                inst.then_inc(esem[eng], 1)
                sig[oid] = (esem[eng], ecount[eng])
        if self.scopes and cur_tag is not None:
            nc.leave_named_scope(cur_tag, cur_sid, False)
        self.sig = sig
        self.esem = esem
        return ecount

    def final_wait(self, eng, oids):
        E = self.eng_obj[eng]
        for oid in oids:
            s, v = self.sig[oid]
            E.wait_ge(s, v)


import numpy as np

D = 2048
NT = 1024
NKC = 16
EPS = 1e-5
NE = 16
DE = 512


SB_BASE = 16512
SB_TOP = 229344


class K:
    def __init__(self, nc, nw=4, look=2):
        self.nc = nc
        self.p = Prog(nc)
        self.cache = {}
        self.NW = nw
        self.LOOK = look
        self.wlist = []
        self.widx = 0
        self.wloaded = 0
        self.bank_live = [False] * 8
        self.bank_rr = 0
        self.bump = SB_BASE
        self.limit = SB_TOP
        self.phase = "perm"
        self.uid = 0

    def sb(self, name, shape, dt):
        key = (self.phase, name)
        if key not in self.cache:
            n = 1
            for d in shape[1:]:
                n *= d
            nbytes = n * (4 if dt == F32 else 2)
            nbytes = (nbytes + 31) // 32 * 32
            off = self.bump
            assert off + nbytes <= self.limit, f"SBUF overflow allocating {name} in phase {self.phase}: {off}+{nbytes} > {self.limit}"
            self.bump = off + nbytes
            self.uid += 1
            t = self.nc.alloc_sbuf_tensor_at(f"{self.phase}_{name}_{self.uid}", list(shape), dt, offset=off)
            self.names = getattr(self, "names", {})
            self.names[key] = t.name
            self.cache[key] = (t, self.p.reg(name))
        return self.cache[key]

    def begin_phase(self, name, start, limit=SB_TOP):
        self.p.barrier()
        assert not any(self.bank_live)
        self.p.tag = name
        self.phase = name
        self.bump = start
        self.limit = limit

    def setup(self):
        p = self.p
        self.banks = [(p.ps([128, 512], F32, name=f"bank{i}"), p.reg(f"bank{i}")) for i in range(8)]
        self.bankregs = {id(r) for _, r in self.banks}
        self.wring = []
        for i in range(self.NW):
            t, _ = self.sb(f"wr{i}", [128, 4096], BF16)
            r1 = p.reg(f"wr{i}")
            self.wring.append((t, [r1, r1]))

    def op(self, eng, fn, reads=(), writes=(), dma=False):
        if eng != "pe":
            bs = self.bankregs
            extra = [r for r in reads if id(r) in bs]
            if extra:
                writes = list(writes) + [r for r in extra if r not in writes]
        return self.p.op(eng, fn, reads=reads, writes=writes, dma=dma)

    def bank(self):
        for i in range(8):
            j = (self.bank_rr + i) % 8
            if not self.bank_live[j]:
                self.bank_live[j] = True
                self.bank_rr = (j + 1) % 8
                return j
        raise RuntimeError("out of PSUM banks")

    def free(self, j):
        assert self.bank_live[j]
        self.bank_live[j] = False

    def wdeclare(self, seq):
        self.wlist.extend(seq)

    def wget(self, pieces):
        key = [(str(pc[0]), pc[1], pc[2]) for pc in pieces]
        i = self.widx
        self.widx += 1
        assert [(str(pc[0]), pc[1], pc[2]) for pc in self.wlist[i]] == key, f"weight order mismatch at {i}"
        hi = min(len(self.wlist), i + 1 + self.LOOK)
        while self.wloaded < hi:
            self._wload(self.wloaded)
            self.wloaded += 1
        t, regs = self.wring[i % self.NW]
        return self._wviews(t, regs, pieces)

    def _wviews(self, t, regs, pieces):
        out = []
        off = 0
        for pi, (src, kc, nco) in enumerate(pieces):
            v = t[:, off:off + kc * nco].rearrange("p (kc f) -> p kc f", kc=kc)
            out.append((v, regs[pi]))
            off += kc * nco
        assert off <= 4096
        return out

    def _wload(self, j):
        nc = self.nc
        t, regs = self.wring[j % self.NW]
        views = self._wviews(t, regs, self.wlist[j])
        for (v, r), (src, kc, nco) in zip(views, self.wlist[j]):
            self.p.op("pool", (lambda v=v, src=src: nc.gpsimd.dma_start(
                out=v, in_=src.rearrange("(kc p) f -> p kc f", p=128))), writes=[r], dma=True)


def load_consts(k, dr, names):
    nc = k.nc
    c = getattr(k, "c", {})
    for name in names:
        shape = CONST_SHAPES[name]
        dt = BF16 if name == "ident_bf" else F32
        t, r = k.sb("c_" + name, shape, dt)
        if dt == BF16:
            k.op("pool", (lambda t=t, name=name: nc.gpsimd.dma_start(out=t[:], in_=dr[name])), writes=[r], dma=True)
        else:
            k.op("sp", (lambda t=t, name=name: nc.sync.dma_start(out=t[:], in_=dr[name])), writes=[r], dma=True)
        c[name] = (t, r)
    for nm, val in (("eps_col", EPS), ("one_col", 1.0), ("eps4_col", 4 * EPS)):
        if nm not in c:
            t, r = k.sb("c_" + nm, [128, 1], F32)
            k.op("pool", (lambda t=t, val=val: nc.gpsimd.memset(t[:], val)), writes=[r])
            c[nm] = (t, r)
    k.c = c


def host_consts():
    i = np.arange(128)
    h = {}
    h["ident_bf"] = np.eye(128, dtype=np.float32)
    h["ident_f"] = np.eye(128, dtype=np.float32)
    h["ones_f"] = np.ones((128, 128), np.float32)
    h["tri_incl"] = (i[:, None] <= i[None, :]).astype(np.float32)
    h["ustrict"] = (i[:, None] > i[None, :]).astype(np.float32)
    h["mask_le"] = (i[:, None] <= i[None, :]).astype(np.float32)
    h["mask_lt"] = (i[None, :] < i[:, None]).astype(np.float32)
    sel = np.zeros((16, 16, 128), np.float32)
    for e in range(16):
        sel[e, e, :] = 1.0
    h["sel16"] = sel.reshape(16, 16 * 128)
    h["inv128"] = np.full((128, 1), 1.0 / 128, np.float32)
    h["ones_row"] = np.ones((128, 2048), np.float32)
    return h


CONST_SHAPES = {"ident_bf": [128, 128], "ident_f": [128, 128], "ones_f": [128, 128], "tri_incl": [128, 128],
                "ustrict": [128, 128], "mask_le": [128, 128], "mask_lt": [128, 128], "sel16": [16, 2048],
                "inv128": [128, 1], "ones_row": [128, 2048]}


def rmsnorm(k, src, g, r_g, outT, r_out, ntok=NT, want_rstd=None):
    nc = k.nc
    ones, r_ones = k.c["ones_f"]
    epsc, r_eps = k.c["eps_col"]
    for th in range(ntok // 512):
        ts = slice(th * 512, (th + 1) * 512)
        view, r_h = src(th)
        b = k.bank()
        bt, rb = k.banks[b]
        for kc in range(NKC):
            sq, rsq = k.sb(f"rn_sq{kc % 2}", [128, 512], F32)
            k.op("act", (lambda sq=sq, kc=kc, view=view: nc.scalar.activation(out=sq[:], in_=view(kc), func=AF.Square)),
                 reads=[r_h], writes=[rsq])
            k.op("pe", (lambda sq=sq, kc=kc, bt=bt: nc.tensor.matmul(bt[:], lhsT=ones[:], rhs=sq[:],
                                                                     start=(kc == 0), stop=(kc == NKC - 1))),
                 reads=[rsq, r_ones], writes=[rb])
        if want_rstd is not None:
            rt, rr = want_rstd
            rview = rt[:, ts]
        else:
            rt, rr = k.sb("rn_rstd", [128, 512], F32)
            rview = rt[:]
        k.op("act", (lambda bt=bt, rview=rview: nc.scalar.activation(out=rview, in_=bt[:], func=AF.Ln, bias=epsc[:], scale=1.0 / D)),
             reads=[rb, r_eps], writes=[rr])
        k.free(b)
        k.op("act", (lambda rview=rview: nc.scalar.activation(out=rview, in_=rview, func=AF.Exp, scale=-0.5)),
             reads=[rr], writes=[rr])
        for kc in range(NKC):
            k.op("dve", (lambda kc=kc, ts=ts, rview=rview, view=view: nc.vector.scalar_tensor_tensor(
                out=outT[:, kc, ts], in0=view(kc), scalar=g[:, kc:kc + 1], in1=rview,
                op0=ALU.mult, op1=ALU.mult)), reads=[r_h, r_g, rr], writes=[r_out])


def src_resident(hT, r_h):
    return lambda th: ((lambda kc, th=th: hT[:, kc, th * 512:(th + 1) * 512]), r_h)


def src_staged(k, dram_xT):
    nc = k.nc

    def f(th):
        st, r_st = k.sb("rn_stage", [128, NKC, 512], F32)
        k.op("sp", (lambda st=st, th=th: nc.sync.dma_start(
            out=st[:], in_=dram_xT[:, th * 512:(th + 1) * 512].rearrange("(kc p) t -> p kc t", p=128))), writes=[r_st], dma=True)
        return (lambda kc, st=st: st[:, kc, :]), r_st
    return f


def wseq_moe(prm, l):
    wg, wu, wd = prm["w_gate"][l], prm["w_up"][l], prm["w_down"][l]
    seq = []
    for e in range(NE):
        for fh in range(2):
            seq.append([(wg[e][:, fh * 256:(fh + 1) * 256], NKC, 256)])
            seq.append([(wu[e][:, fh * 256:(fh + 1) * 256], NKC, 256)])
        for dq in range(2):
            seq.append([(wd[e][:, dq * 1024:(dq + 1) * 1024], 4, 1024)])
    return seq


def moe(k, hT, r_h, hnT, r_hn, prm, l):
    nc = k.nc
    c = k.c
    g, r_g = prm["ffn_g"][l]
    rstd, r_rstd = k.sb("moe_rstd", [128, NT], F32)
    rmsnorm(k, src_resident(hT, r_h), g, r_g, hnT, r_hn, want_rstd=(rstd, r_rstd))

    wr, r_wr = k.sb("moe_wr", [128, NKC, 20], F32)
    k.op("sp", lambda: nc.sync.dma_start(out=wr[:], in_=prm["w_router"][l].rearrange("(kc p) e -> p kc e", p=128)),
         writes=[r_wr], dma=True)
    k.op("dve", lambda: nc.vector.tensor_tensor(out=wr[:], in0=wr[:], in1=g[:].unsqueeze(2).to_broadcast([128, NKC, 20]),
                                               op=ALU.mult), reads=[r_wr, r_g], writes=[r_wr])
    rb_bias, r_bias = prm["b_router"][l]
    gT, r_gT = k.sb("moe_gT", [16, NT], F32)
    inv128, r_inv = c["inv128"]
    identf, r_identf = c["ident_f"]
    for tb in range(NT // 128):
        tsl = slice(tb * 128, (tb + 1) * 128)
        b = k.bank()
        bt, rb = k.banks[b]
        for kc in range(NKC):
            k.op("pe", (lambda kc=kc, tsl=tsl, bt=bt: nc.tensor.matmul(bt[:, 0:20], lhsT=hT[:, kc, tsl], rhs=wr[:, kc, :],
                                                                       start=(kc == 0), stop=(kc == NKC - 1))),
                 reads=[r_h, r_wr], writes=[rb])
        k.op("pe", (lambda tsl=tsl, bt=bt: nc.tensor.matmul(bt[:, 32:33], lhsT=rstd[:, tsl], rhs=inv128[:],
                                                            start=True, stop=True)),
             reads=[r_rstd, r_inv], writes=[rb])
        sm, r_sm = k.sb(f"moe_sm{tb % 2}", [128, 96], F32)
        rs = sm[:, 0:1]
        k.op("act", (lambda bt=bt, rs=rs: nc.scalar.copy(out=rs, in_=bt[:, 32:33])), reads=[rb], writes=[r_sm])
        lg = sm[:, 4:24]
        k.op("dve", (lambda bt=bt, lg=lg, rs=rs: nc.vector.scalar_tensor_tensor(
            out=lg, in0=bt[:, 0:20], scalar=rs, in1=rb_bias[:], op0=ALU.mult, op1=ALU.add)),
            reads=[rb, r_sm, r_bias], writes=[r_sm])
        k.free(b)
        lc = sm[:, 4:8]
        mx = sm[:, 1:2]
        k.op("dve", (lambda lc=lc, mx=mx: nc.vector.reduce_max(out=mx, in_=lc, axis=AX.X)), reads=[r_sm], writes=[r_sm])
        nmx = sm[:, 2:3]
        k.op("dve", (lambda mx=mx, nmx=nmx: nc.vector.tensor_scalar(out=nmx, in0=mx, scalar1=-1.0, scalar2=None, op0=ALU.mult)),
             reads=[r_sm], writes=[r_sm])
        ec = sm[:, 24:28]
        se = sm[:, 3:4]
        k.op("act", (lambda lc=lc, ec=ec, nmx=nmx, se=se: nc.scalar.activation(out=ec, in_=lc, func=AF.Exp, bias=nmx, scale=1.0,
                                                                               accum_out=se)),
             reads=[r_sm], writes=[r_sm])
        oh = sm[:, 28:32]
        k.op("dve", (lambda lc=lc, mx=mx, oh=oh: nc.vector.tensor_scalar(out=oh, in0=lc, scalar1=mx, scalar2=None, op0=ALU.is_ge)),
             reads=[r_sm], writes=[r_sm])
        fa = sm[:, 8:24].rearrange("p (g e) -> p g e", g=4)
        tmp = sm[:, 32:48]
        k.op("dve", (lambda fa=fa, oh=oh, tmp=tmp: nc.vector.tensor_tensor(
            out=tmp.rearrange("p (g e) -> p g e", g=4), in0=fa, in1=oh.unsqueeze(2).to_broadcast([128, 4, 4]), op=ALU.mult)),
            reads=[r_sm], writes=[r_sm])
        fn = sm[:, 48:52]
        k.op("dve", (lambda tmp=tmp, fn=fn: nc.vector.tensor_reduce(out=fn, in_=tmp.rearrange("p (g e) -> p e g", g=4),
                                                                    axis=AX.X, op=ALU.add)),
             reads=[r_sm], writes=[r_sm])
        mf = sm[:, 52:53]
        k.op("dve", (lambda fn=fn, mf=mf: nc.vector.reduce_max(out=mf, in_=fn, axis=AX.X)), reads=[r_sm], writes=[r_sm])
        nmf = sm[:, 53:54]
        k.op("dve", (lambda mf=mf, nmf=nmf: nc.vector.tensor_scalar(out=nmf, in0=mf, scalar1=-1.0, scalar2=None, op0=ALU.mult)),
             reads=[r_sm], writes=[r_sm])
        ef = sm[:, 56:60]
        k.op("act", (lambda fn=fn, ef=ef, nmf=nmf: nc.scalar.activation(out=ef, in_=fn, func=AF.Exp, bias=nmf, scale=1.0)),
             reads=[r_sm], writes=[r_sm])
        m1 = sm[:, 60:64]
        k.op("dve", (lambda fn=fn, mf=mf, m1=m1: nc.vector.tensor_scalar(out=m1, in0=fn, scalar1=mf, scalar2=None, op0=ALU.is_ge)),
             reads=[r_sm], writes=[r_sm])
        ef2 = sm[:, 64:68]
        k.op("dve", (lambda ef=ef, m1=m1, ef2=ef2: nc.vector.tensor_tensor(out=ef2, in0=ef, in1=m1, op=ALU.mult)),
             reads=[r_sm], writes=[r_sm])
        k.op("dve", (lambda ef=ef, ef2=ef2: nc.vector.tensor_tensor(out=ef2, in0=ef, in1=ef2, op=ALU.subtract)),
             reads=[r_sm], writes=[r_sm])
        e2 = sm[:, 54:55]
        k.op("dve", (lambda ef2=ef2, e2=e2: nc.vector.reduce_max(out=e2, in_=ef2, axis=AX.X)), reads=[r_sm], writes=[r_sm])
        m2 = sm[:, 68:72]
        k.op("dve", (lambda ef2=ef2, e2=e2, m2=m2: nc.vector.tensor_scalar(out=m2, in0=ef2, scalar1=e2, scalar2=None, op0=ALU.is_ge)),
             reads=[r_sm], writes=[r_sm])
        tv = sm[:, 72:76]
        k.op("dve", (lambda ef2=ef2, m2=m2, tv=tv: nc.vector.tensor_tensor(out=tv, in0=ef2, in1=m2, op=ALU.mult)),
             reads=[r_sm], writes=[r_sm])
        t1 = sm[:, 76:80]
        k.op("dve", (lambda ef=ef, m1=m1, t1=t1: nc.vector.tensor_tensor(out=t1, in0=ef, in1=m1, op=ALU.mult)),
             reads=[r_sm], writes=[r_sm])
        k.op("dve", (lambda tv=tv, t1=t1: nc.vector.tensor_tensor(out=tv, in0=tv, in1=t1, op=ALU.add)),
             reads=[r_sm], writes=[r_sm])
        den = sm[:, 55:56]
        k.op("dve", (lambda tv=tv, den=den: nc.vector.reduce_sum(out=den, in_=tv, axis=AX.X)), reads=[r_sm], writes=[r_sm])
        k.op("dve", (lambda den=den, se=se: nc.vector.tensor_tensor(out=den, in0=den, in1=se, op=ALU.mult)),
             reads=[r_sm], writes=[r_sm])
        k.op("dve", (lambda den=den: nc.vector.reciprocal(out=den, in_=den)), reads=[r_sm], writes=[r_sm])
        k.op("dve", (lambda tv=tv, den=den: nc.vector.tensor_scalar(out=tv, in0=tv, scalar1=den, scalar2=None, op0=ALU.mult)),
             reads=[r_sm], writes=[r_sm])
        gt = sm[:, 80:96]
        k.op("dve", (lambda gt=gt, oh=oh, tv=tv: nc.vector.tensor_tensor(
            out=gt.rearrange("p (g e) -> p g e", g=4), in0=oh.unsqueeze(2).to_broadcast([128, 4, 4]),
            in1=tv.unsqueeze(1).to_broadcast([128, 4, 4]), op=ALU.mult)), reads=[r_sm], writes=[r_sm])
        b2 = k.bank()
        bt2, rb2 = k.banks[b2]
        k.op("pe", (lambda gt=gt, bt2=bt2: nc.tensor.matmul(bt2[0:16, 0:128], lhsT=gt, rhs=identf[:], start=True, stop=True)),
             reads=[r_sm, r_identf], writes=[rb2])
        k.op("act", (lambda bt2=bt2, tsl=tsl: nc.scalar.copy(out=gT[:, tsl], in_=bt2[0:16, 0:128])), reads=[rb2], writes=[r_gT])
        k.free(b2)

    sel, r_sel = c["sel16"]
    wg, wu, wd = prm["w_gate"][l], prm["w_up"][l], prm["w_down"][l]
    for e in range(NE):
        gb, r_gb = k.sb(f"moe_gb{e % 2}", [128, NT], F32)
        for th in range(2):
            ts = slice(th * 512, (th + 1) * 512)
            b = k.bank()
            bt, rb = k.banks[b]
            k.op("pe", (lambda e=e, ts=ts, bt=bt: nc.tensor.matmul(bt[:], lhsT=sel[:, e * 128:(e + 1) * 128], rhs=gT[:, ts],
                                                                   start=True, stop=True)),
                 reads=[r_sel, r_gT], writes=[rb])
            k.op("act", (lambda gb=gb, ts=ts, bt=bt: nc.scalar.copy(out=gb[:, ts], in_=bt[:])), reads=[rb], writes=[r_gb])
            k.free(b)
        hid, r_hid = k.sb(f"moe_hid{e % 2}", [128, 4, NT], BF16)
        for fh in range(2):
            (wgt, r_wgt), = k.wget([(wg[e][:, fh * 256:(fh + 1) * 256], NKC, 256)])
            (wut, r_wut), = k.wget([(wu[e][:, fh * 256:(fh + 1) * 256], NKC, 256)])
            for f2 in range(2):
                fc = fh * 2 + f2
                for th in range(2):
                    ts = slice(th * 512, (th + 1) * 512)
                    bg = k.bank(); bu = k.bank()
                    btg, rbg = k.banks[bg]
                    btu, rbu = k.banks[bu]
                    for kc in range(NKC):
                        k.op("pe", (lambda kc=kc, ts=ts, btg=btg, wgt=wgt, f2=f2: nc.tensor.matmul(
                            btg[:], lhsT=wgt[:, kc, f2 * 128:(f2 + 1) * 128], rhs=hnT[:, kc, ts],
                            start=(kc == 0), stop=(kc == NKC - 1))), reads=[r_wgt, r_hn], writes=[rbg])
                    for kc in range(NKC):
                        k.op("pe", (lambda kc=kc, ts=ts, btu=btu, wut=wut, f2=f2: nc.tensor.matmul(
                            btu[:], lhsT=wut[:, kc, f2 * 128:(f2 + 1) * 128], rhs=hnT[:, kc, ts],
                            start=(kc == 0), stop=(kc == NKC - 1))), reads=[r_wut, r_hn], writes=[rbu])
                    sg, r_sg = k.sb(f"moe_sg{(fc * 2 + th) % 2}", [128, 512], F32)
                    k.op("act", (lambda sg=sg, btg=btg: nc.scalar.activation(out=sg[:], in_=btg[:], func=AF.Silu)),
                         reads=[rbg], writes=[r_sg])
                    k.free(bg)
                    k.op("dve", (lambda sg=sg, btu=btu: nc.vector.tensor_tensor(out=sg[:], in0=sg[:], in1=btu[:], op=ALU.mult)),
                         reads=[r_sg, rbu], writes=[r_sg])
                    k.free(bu)
                    k.op("dve", (lambda sg=sg, gb=gb, ts=ts, hid=hid, fc=fc: nc.vector.tensor_tensor(
                        out=hid[:, fc, ts], in0=sg[:], in1=gb[:, ts], op=ALU.mult)), reads=[r_sg, r_gb], writes=[r_hid])
        for dq in range(2):
            (wdt, r_wdt), = k.wget([(wd[e][:, dq * 1024:(dq + 1) * 1024], 4, 1024)])
            for d2 in range(8):
                dc = dq * 8 + d2
                for th in range(2):
                    ts = slice(th * 512, (th + 1) * 512)
                    b = k.bank()
                    bt, rb = k.banks[b]
                    for fc in range(4):
                        k.op("pe", (lambda fc=fc, ts=ts, bt=bt, wdt=wdt, d2=d2, hid=hid: nc.tensor.matmul(
                            bt[:], lhsT=wdt[:, fc, d2 * 128:(d2 + 1) * 128], rhs=hid[:, fc, ts],
                            start=(fc == 0), stop=(fc == 3))), reads=[r_wdt, r_hid], writes=[rb])
                    k.op("dve", (lambda dc=dc, ts=ts, bt=bt: nc.vector.tensor_tensor(out=hT[:, dc, ts], in0=hT[:, dc, ts], in1=bt[:],
                                                                                     op=ALU.add)), reads=[rb, r_h], writes=[r_h])
                    k.free(b)


def ssd_prep(k, hnT, r_hn, prm):
    nc = k.nc
    c = k.c
    wdt, r_wdt = prm["wdt"]
    dtb, r_dtb = prm["dtb"]
    A_bc, r_A = prm["A_bc"]
    onec, r_onec = c["one_col"]
    tri, r_tri = c["tri_incl"]
    ones, r_ones = c["ones_f"]
    P = {}
    for nm in ("dt", "a", "eacs", "dstate", "cdec"):
        P[nm] = k.sb("sp_" + nm, [128, 8, 64], F32)
    dt, r_dt = P["dt"]; a, r_a = P["a"]; eacs, r_eacs = P["eacs"]
    dstate, r_dst = P["dstate"]; cdec, r_cdec = P["cdec"]
    b = k.bank(); bt, rb = k.banks[b]
    for cch in range(8):
        for kc in range(NKC):
            k.op("pe", (lambda cch=cch, kc=kc, bt=bt: nc.tensor.matmul(
                bt[:, cch * 64:(cch + 1) * 64], lhsT=hnT[:, kc, cch * 128:(cch + 1) * 128], rhs=wdt[:, kc, :],
                start=(kc == 0), stop=(kc == NKC - 1))), reads=[r_hn, r_wdt], writes=[rb])
    k.op("dve", (lambda bt=bt: nc.vector.tensor_tensor(out=dt[:], in0=bt[:].rearrange("p (c h) -> p c h", c=8),
                                                      in1=dtb[:].unsqueeze(1).to_broadcast([128, 8, 64]), op=ALU.add)),
         reads=[rb, r_dtb], writes=[r_dt])
    k.free(b)
    k.op("act", lambda: nc.scalar.activation(out=dt[:], in_=dt[:], func=AF.Exp), reads=[r_dt], writes=[r_dt])
    k.op("act", lambda: nc.scalar.activation(out=dt[:], in_=dt[:], func=AF.Ln, bias=onec[:], scale=1.0),
         reads=[r_dt, r_onec], writes=[r_dt])
    k.op("dve", lambda: nc.vector.tensor_tensor(out=a[:], in0=dt[:], in1=A_bc[:].unsqueeze(1).to_broadcast([128, 8, 64]),
                                               op=ALU.mult), reads=[r_dt, r_A], writes=[r_a])
    b2 = k.bank(); bt2, rb2 = k.banks[b2]
    b3 = k.bank(); bt3, rb3 = k.banks[b3]
    for cch in range(8):
        k.op("pe", (lambda cch=cch, bt2=bt2: nc.tensor.matmul(bt2[:, cch * 64:(cch + 1) * 64], lhsT=tri[:], rhs=a[:, cch, :],
                                                              start=True, stop=True)), reads=[r_a, r_tri], writes=[rb2])
        k.op("pe", (lambda cch=cch, bt3=bt3: nc.tensor.matmul(bt3[:, cch * 64:(cch + 1) * 64], lhsT=ones[:], rhs=a[:, cch, :],
                                                              start=True, stop=True)), reads=[r_a, r_ones], writes=[rb3])
    f3 = lambda t: t[:].rearrange("p c h -> p (c h)")
    k.op("act", (lambda bt2=bt2: nc.scalar.activation(out=f3(eacs), in_=bt2[:], func=AF.Exp)), reads=[rb2], writes=[r_eacs])
    k.op("act", (lambda bt2=bt2: nc.scalar.copy(out=f3(dstate), in_=bt2[:])), reads=[rb2], writes=[r_dst])
    k.free(b2)
    k.op("act", (lambda bt3=bt3: nc.scalar.activation(out=f3(cdec), in_=bt3[:], func=AF.Exp)), reads=[rb3], writes=[r_cdec])
    k.op("dve", (lambda bt3=bt3: nc.vector.tensor_tensor(out=f3(dstate), in0=f3(dstate), in1=bt3[:], op=ALU.subtract)),
         reads=[rb3, r_dst], writes=[r_dst])
    k.free(b3)
    k.op("act", lambda: nc.scalar.activation(out=f3(dstate), in_=f3(dstate), func=AF.Exp, scale=-1.0), reads=[r_dst], writes=[r_dst])
    return P


def wseq_ssd_group(prm, g, full):
    w_in = prm["w_in"]
    seq = [[(w_in[:, 4096 + 512 * g: 4096 + 512 * g + 256], NKC, 256)],
           [(w_in[:, 4096 + 512 * g + 256: 4096 + 512 * g + 512], NKC, 256)],
           [(w_in[:, 8192 + 128 * g: 8192 + 128 * g + 128], NKC, 128), (w_in[:, 9216 + 128 * g: 9216 + 128 * g + 128], NKC, 128)]]
    if full:
        seq += [[(w_in[:, 512 * g: 512 * g + 256], NKC, 256)], [(w_in[:, 512 * g + 256: 512 * g + 512], NKC, 256)]]
    return seq


def ssd_group(k, g, mode, hnT, r_hn, P, prm, ynT_all=None, r_yn=None):
    nc = k.nc
    c = k.c
    cwh, r_cwh = prm["cwh"]
    cbh, r_cbh = prm["cbh"]
    D_bc, r_D = prm["D_bc"]
    nw, r_nw = prm["nw"]
    hal, r_hal = prm["hal"]
    S_in, r_Sin = prm["S_in"]
    flag, r_flag = prm["flag"]
    identb, r_identb = c["ident_bf"]
    identf, r_identf = c["ident_f"]
    ones, r_ones = c["ones_f"]
    tri, r_tri = c["tri_incl"]
    ustr, r_ustr = c["ustrict"]
    mle, r_mle = c["mask_le"]
    dt, r_dt = P["dt"]; a, r_a = P["a"]; eacs, r_eacs = P["eacs"]
    dstate, r_dst = P["dstate"]; cdec, r_cdec = P["cdec"]
    full = (mode == "B")
    hs = slice(8 * g, 8 * g + 8)
    seq = wseq_ssd_group(prm, g, full)

    xcT, r_xc = k.sb("sg_xcT", [128, 4, NT], BF16)
    BcT, r_Bc = k.sb("sg_BcT", [128, NT], BF16)
    CcT, r_Cc = k.sb("sg_CcT", [128, NT], BF16)
    Sg, r_S = k.sb("sg_S", [128, 512], F32)
    Sbf, r_Sbf = k.sb("sg_Sbf", [128, 512], BF16)
    h8 = lambda ap: ap.rearrange("p (h q) -> p h q", h=8)

    items = [(0, 0, 4 * g + 0, xcT[:, 0, :], r_xc), (0, 128, 4 * g + 1, xcT[:, 1, :], r_xc),
             (1, 0, 4 * g + 2, xcT[:, 2, :], r_xc), (1, 128, 4 * g + 3, xcT[:, 3, :], r_xc),
             (2, 0, 32 + g, BcT[:], r_Bc), (3, 0, 40 + g, CcT[:], r_Cc)]
    wcur = {}
    for ii, (wi, co, ci, dst, r_dstt) in enumerate(items):
        if wi == 0 and 0 not in wcur:
            wcur[0], = k.wget(seq[0])
        elif wi == 1 and 1 not in wcur:
            wcur[1], = k.wget(seq[1])
        elif wi == 2 and 2 not in wcur:
            wcur[2], wcur[3] = k.wget(seq[2])
        wt, r_wt = wcur[wi]
        pre, r_pre = k.sb("sg_pre", [128, NT + 8], F32)
        acc, r_acc = k.sb("sg_acc", [128, NT], F32)
        if full:
            k.op("pool", (lambda pre=pre, ci=ci: nc.gpsimd.tensor_copy(out=pre[:, 0:3], in_=hal[:, ci, :])),
                 reads=[r_hal], writes=[r_pre])
        else:
            k.op("pool", (lambda pre=pre: nc.gpsimd.memset(pre[:, 0:3], 0.0)), writes=[r_pre])
        for th in range(2):
            ts = slice(th * 512, (th + 1) * 512)
            b = k.bank(); bt, rb = k.banks[b]
            for kc in range(NKC):
                k.op("pe", (lambda kc=kc, ts=ts, bt=bt, wt=wt, co=co: nc.tensor.matmul(
                    bt[:], lhsT=wt[:, kc, co:co + 128], rhs=hnT[:, kc, ts], start=(kc == 0), stop=(kc == NKC - 1))),
                    reads=[r_wt, r_hn], writes=[rb])
            k.op("act", (lambda bt=bt, pre=pre, th=th: nc.scalar.copy(out=pre[:, 3 + th * 512: 3 + (th + 1) * 512], in_=bt[:])),
                 reads=[rb], writes=[r_pre])
            k.op("act", (lambda bt=bt, acc=acc, ts=ts, ci=ci: nc.scalar.activation(
                out=acc[:, ts], in_=bt[:], func=AF.Identity, bias=cbh[:, ci:ci + 1], scale=cwh[:, ci, 3:4])),
                reads=[rb, r_cwh, r_cbh], writes=[r_acc])
            k.free(b)
        for tap in range(3):
            k.op("dve", (lambda pre=pre, acc=acc, ci=ci, tap=tap: nc.vector.scalar_tensor_tensor(
                out=acc[:], in0=pre[:, tap:tap + NT], scalar=cwh[:, ci, tap:tap + 1], in1=acc[:], op0=ALU.mult, op1=ALU.add)),
                reads=[r_pre, r_acc, r_cwh], writes=[r_acc])
        if not full:
            k.op("pool", (lambda pre=pre, ci=ci: nc.gpsimd.tensor_copy(out=hal[:, ci, :], in_=pre[:, NT:NT + 3])),
                 reads=[r_pre], writes=[r_hal])
        k.op("act", (lambda acc=acc, pre=pre: nc.scalar.activation(out=pre[:, 0:NT], in_=acc[:], func=AF.Tanh)),
             reads=[r_acc], writes=[r_pre])
        k.op("dve", (lambda acc=acc, pre=pre, dst=dst: nc.vector.scalar_tensor_tensor(
            out=dst, in0=pre[:, 0:NT], scalar=1.0, in1=acc[:], op0=ALU.add, op1=ALU.mult)),
            reads=[r_pre, r_acc], writes=[r_dstt])

    if full:
        (wz0, r_wz0), = k.wget(seq[3])
        (wz1, r_wz1), = k.wget(seq[4])
        ss, r_ss = k.sb("sg_ss", [128, 8], F32)
        k.op("dve", lambda: nc.vector.memset(ss[:], 0.0), writes=[r_ss])
        k.op("dve", lambda: nc.vector.tensor_scalar(out=Sg[:], in0=S_in[:, g, :], scalar1=flag[:], scalar2=None, op0=ALU.mult),
             reads=[r_Sin, r_flag], writes=[r_S])
        k.op("act", lambda: nc.scalar.copy(out=Sbf[:], in_=Sg[:]), reads=[r_S], writes=[r_Sbf])
    else:
        k.op("pool", lambda: nc.gpsimd.memset(Sg[:], 0.0), writes=[r_S])

    for cch in range(8):
        cs = slice(cch * 128, (cch + 1) * 128)
        bx = k.bank(); btx, rbx = k.banks[bx]
        for fc in range(4):
            k.op("pe", (lambda fc=fc, cs=cs, btx=btx: nc.tensor.matmul(btx[:, fc * 128:(fc + 1) * 128], lhsT=xcT[:, fc, cs],
                                                                       rhs=identb[:], start=True, stop=True)),
                 reads=[r_xc, r_identb], writes=[rbx])
        xdt, r_xdt = k.sb(f"sg_xdt{cch % 2}", [128, 512], BF16)
        k.op("dve", (lambda btx=btx, xdt=xdt, cch=cch: nc.vector.tensor_tensor(
            out=h8(xdt[:]), in0=h8(btx[:]), in1=dt[:, cch, hs].unsqueeze(2).to_broadcast([128, 8, 64]), op=ALU.mult)),
            reads=[rbx, r_dt], writes=[r_xdt])
        if full:
            xD, r_xD = k.sb("sg_xDb", [128, 512], BF16)
            k.op("dve", (lambda btx=btx, xD=xD: nc.vector.tensor_tensor(
                out=h8(xD[:]), in0=h8(btx[:]), in1=D_bc[:, hs].unsqueeze(2).to_broadcast([128, 8, 64]), op=ALU.mult)),
                reads=[rbx, r_D], writes=[r_xD])
        k.free(bx)
        bB = k.bank(); btB, rbB = k.banks[bB]
        k.op("pe", (lambda cs=cs, btB=btB: nc.tensor.matmul(btB[:, 0:128], lhsT=BcT[:, cs], rhs=identb[:], start=True, stop=True)),
             reads=[r_Bc, r_identb], writes=[rbB])
        if full:
            k.op("pe", (lambda cs=cs, btB=btB: nc.tensor.matmul(btB[:, 128:256], lhsT=BcT[:, cs], rhs=CcT[:, cs], start=True, stop=True)),
                 reads=[r_Bc, r_Cc], writes=[rbB])
        Btok, r_Bt = k.sb(f"sg_Btok{cch % 2}", [128, 128], BF16)
        k.op("act", (lambda btB=btB, Btok=Btok: nc.scalar.copy(out=Btok[:], in_=btB[:, 0:128])), reads=[rbB], writes=[r_Bt])
        if full:
            cbm, r_cbm = k.sb("sg_cbm", [128, 128], F32)
            k.op("dve", (lambda btB=btB, cbm=cbm: nc.vector.tensor_tensor(out=cbm[:], in0=btB[:, 128:256], in1=mle[:], op=ALU.mult)),
                 reads=[rbB, r_mle], writes=[r_cbm])
        k.free(bB)
        if full:
            bz = k.bank(); btz, rbz = k.banks[bz]
            for zi, (wz, r_wz) in enumerate(((wz0, r_wz0), (wz1, r_wz1))):
                for kc in range(NKC):
                    k.op("pe", (lambda kc=kc, cs=cs, btz=btz, wz=wz, zi=zi: nc.tensor.matmul(
                        btz[:, zi * 256:(zi + 1) * 256], lhsT=hnT[:, kc, cs], rhs=wz[:, kc, :],
                        start=(kc == 0), stop=(kc == NKC - 1))), reads=[r_hn, r_wz], writes=[rbz])
            zs, r_zs = k.sb("sg_zs", [128, 512], F32)
            k.op("act", (lambda btz=btz, zs=zs: nc.scalar.activation(out=zs[:], in_=btz[:], func=AF.Tanh, scale=0.5)),
                 reads=[rbz], writes=[r_zs])
            k.op("dve", (lambda btz=btz, zs=zs: nc.vector.scalar_tensor_tensor(out=zs[:], in0=zs[:], scalar=1.0, in1=btz[:],
                                                                               op0=ALU.add, op1=ALU.mult)),
                 reads=[r_zs, rbz], writes=[r_zs])
            k.free(bz)
            bo = k.bank(); bto, rbo = k.banks[bo]
            k.op("pe", (lambda cs=cs, bto=bto: nc.tensor.matmul(bto[:], lhsT=CcT[:, cs], rhs=Sbf[:], start=True, stop=True)),
                 reads=[r_Cc, r_Sbf], writes=[rbo])
            t1, r_t1 = k.sb("sg_t1", [128, 512], F32)
            k.op("dve", (lambda bto=bto, t1=t1, cch=cch: nc.vector.tensor_tensor(
                out=h8(t1[:]), in0=h8(bto[:]), in1=eacs[:, cch, hs].unsqueeze(2).to_broadcast([128, 8, 64]), op=ALU.mult)),
                reads=[rbo, r_eacs], writes=[r_t1])
            k.free(bo)
            MT, r_MT = k.sb("sg_MT", [128, 8, 128], BF16)
            ltas = []
            for hh in range(2):
                lta, r_lta = k.sb(f"sg_lta{hh}", [128, 4, 128], F32)
                k.op("dve", (lambda lta=lta, cch=cch, hh=hh: nc.vector.tensor_tensor(
                    out=lta[:], in0=ustr[:].unsqueeze(1).to_broadcast([128, 4, 128]),
                    in1=a[:, cch, 8 * g + 4 * hh:8 * g + 4 * hh + 4].unsqueeze(2).to_broadcast([128, 4, 128]), op=ALU.mult)),
                    reads=[r_ustr, r_a], writes=[r_lta])
                ltas.append((lta, r_lta))
            bas = []
            for hh in range(2):
                lta, r_lta = ltas[hh]
                ba = k.bank(); bta, rba = k.banks[ba]
                for h4 in range(4):
                    k.op("pe", (lambda h4=h4, bta=bta, lta=lta: nc.tensor.matmul(
                        bta[:, h4 * 128:(h4 + 1) * 128], lhsT=lta[:, h4, :], rhs=tri[:], start=True, stop=True)),
                        reads=[r_lta, r_tri], writes=[rba])
                bas.append((ba, bta, rba))
            for hh in range(2):
                ba, bta, rba = bas[hh]
                dec, r_dec = k.sb(f"sg_dec{hh}", [128, 512], BF16)
                k.op("act", (lambda bta=bta, dec=dec: nc.scalar.activation(out=dec[:], in_=bta[:], func=AF.Exp)),
                     reads=[rba], writes=[r_dec])
                k.free(ba)
                k.op("dve", (lambda dec=dec, MT=MT, hh=hh, cbm=cbm: nc.vector.tensor_tensor(
                    out=MT[:, hh * 4:(hh + 1) * 4, :], in0=dec[:].rearrange("p (h l) -> p h l", h=4),
                    in1=cbm[:].unsqueeze(1).to_broadcast([128, 4, 128]), op=ALU.mult)), reads=[r_dec, r_cbm], writes=[r_MT])
            by = k.bank(); bty, rby = k.banks[by]
            k.op("pe", (lambda bty=bty, xD=xD: nc.tensor.matmul(bty[:], lhsT=identb[:], rhs=xD[:], start=True, stop=False)),
                 reads=[r_xD, r_identb], writes=[rby])
            for h in range(8):
                k.op("pe", (lambda h=h, bty=bty, MT=MT, xdt=xdt: nc.tensor.matmul(
                    bty[:, h * 64:(h + 1) * 64], lhsT=MT[:, h, :], rhs=xdt[:, h * 64:(h + 1) * 64], start=False, stop=(h == 7))),
                    reads=[r_MT, r_xdt], writes=[rby])
            ysb, r_ysb = k.sb("sg_ysb", [128, 512], F32)
            k.op("dve", (lambda bty=bty, t1=t1, ysb=ysb: nc.vector.tensor_tensor(out=ysb[:], in0=t1[:], in1=bty[:], op=ALU.add)),
                 reads=[rby, r_t1], writes=[r_ysb])
            k.free(by)
            ygb, r_ygb = k.sb(f"sg_ygb{cch % 2}", [128, 512], BF16)
            k.op("dve", (lambda ysb=ysb, zs=zs, ygb=ygb: nc.vector.tensor_tensor(out=ygb[:], in0=ysb[:], in1=zs[:], op=ALU.mult)),
                 reads=[r_ysb, r_zs], writes=[r_ygb])
            k.op("act", (lambda cch=cch, t1=t1, ygb=ygb: nc.scalar.activation(out=t1[:], in_=ygb[:], func=AF.Square, scale=0.5,
                                                                             accum_out=ss[:, cch:cch + 1])),
                 reads=[r_ygb], writes=[r_t1, r_ss])
            btt = k.bank(); bttt, rbt = k.banks[btt]
            for fc in range(4):
                k.op("pe", (lambda fc=fc, bttt=bttt, ygb=ygb: nc.tensor.matmul(bttt[:, fc * 128:(fc + 1) * 128], lhsT=ygb[:, fc * 128:(fc + 1) * 128],
                                                                               rhs=identb[:], start=True, stop=True)),
                     reads=[r_ygb, r_identb], writes=[rbt])
            k.op("act", (lambda bttt=bttt, cs=cs: nc.scalar.copy(out=ynT_all[:, 4 * g:4 * g + 4, cs], in_=bttt[:].rearrange("p (f t) -> p f t", f=4))),
                 reads=[rbt], writes=[r_yn])
            k.free(btt)
        if (not full) or cch < 7:
            xd, r_xd = k.sb("sg_xd", [128, 512], BF16)
            k.op("pool", (lambda xd=xd, xdt=xdt, cch=cch: nc.gpsimd.tensor_tensor(
                out=h8(xd[:]), in0=h8(xdt[:]), in1=dstate[:, cch, hs].unsqueeze(2).to_broadcast([128, 8, 64]), op=ALU.mult)),
                reads=[r_xdt, r_dst], writes=[r_xd])
            bs = k.bank(); bts, rbs = k.banks[bs]
            k.op("pe", (lambda bts=bts, Btok=Btok, xd=xd: nc.tensor.matmul(bts[:], lhsT=Btok[:], rhs=xd[:], start=True, stop=True)),
                 reads=[r_Bt, r_xd], writes=[rbs])
            k.op("pool", (lambda cch=cch: nc.gpsimd.tensor_tensor(
                out=h8(Sg[:]), in0=h8(Sg[:]), in1=cdec[:, cch, hs].unsqueeze(2).to_broadcast([128, 8, 64]), op=ALU.mult)),
                reads=[r_S, r_cdec], writes=[r_S])
            k.op("dve", (lambda bts=bts: nc.vector.tensor_tensor(out=Sg[:], in0=Sg[:], in1=bts[:], op=ALU.add)), reads=[r_S, rbs], writes=[r_S])
            k.free(bs)
            if full:
                k.op("act", lambda: nc.scalar.copy(out=Sbf[:], in_=Sg[:]), reads=[r_S], writes=[r_Sbf])

    if not full:
        k.op("act", lambda: nc.scalar.copy(out=S_in[:, g, :], in_=Sg[:]), reads=[r_S], writes=[r_Sin])
        return
    eps4, r_eps4 = c["eps4_col"]
    pre_t, r_rbc = k.sb("sg_pre", [128, NT + 8], F32)
    rbc = pre_t[:, 0:NT]
    for hh in range(2):
        b = k.bank(); bt, rb = k.banks[b]
        for c4 in range(4):
            cch = hh * 4 + c4
            dg, r_dg = k.sb(f"sg_diag{c4 % 2}", [128, 128], F32)
            k.op("dve", (lambda dg=dg, cch=cch: nc.vector.tensor_scalar(out=dg[:], in0=identf[:], scalar1=ss[:, cch:cch + 1], scalar2=None,
                                                                       op0=ALU.mult)), reads=[r_identf, r_ss], writes=[r_dg])
            k.op("pe", (lambda dg=dg, bt=bt, c4=c4: nc.tensor.matmul(bt[:, c4 * 128:(c4 + 1) * 128], lhsT=ones[:], rhs=dg[:], start=True, stop=True)),
                 reads=[r_dg, r_ones], writes=[rb])
        k.op("act", (lambda bt=bt, hh=hh: nc.scalar.activation(out=rbc[:, hh * 512:(hh + 1) * 512], in_=bt[:], func=AF.Ln, bias=eps4[:],
                                                               scale=4.0 / 512)), reads=[rb, r_eps4], writes=[r_rbc])
        k.free(b)
    k.op("act", lambda: nc.scalar.activation(out=rbc, in_=rbc, func=AF.Exp, scale=-0.5), reads=[r_rbc], writes=[r_rbc])
    for fc in range(4):
        eng = "dve"
        E = nc.vector
        k.op(eng, (lambda fc=fc, E=E: E.scalar_tensor_tensor(
            out=ynT_all[:, 4 * g + fc, :], in0=ynT_all[:, 4 * g + fc, :], scalar=nw[:, 4 * g + fc:4 * g + fc + 1], in1=rbc,
            op0=ALU.mult, op1=ALU.mult)), reads=[r_yn, r_nw, r_rbc], writes=[r_yn])


def wseq_outproj(prm):
    w = prm["w_out"]
    return [[(w[0:2048, dc * 128:(dc + 1) * 128], 16, 128), (w[2048:4096, dc * 128:(dc + 1) * 128], 16, 128)] for dc in range(16)]


def out_proj(k, hT, r_h, ynT_all, r_yn, prm):
    nc = k.nc
    for dc, pieces in enumerate(wseq_outproj(prm)):
        (wo0, r_wo0), (wo1, r_wo1) = k.wget(pieces)
        for th in range(2):
            ts = slice(th * 512, (th + 1) * 512)
            b = k.bank(); bt, rb = k.banks[b]
            for fc in range(32):
                wo, r_wo = (wo0, r_wo0) if fc < 16 else (wo1, r_wo1)
                k.op("pe", (lambda fc=fc, ts=ts, bt=bt, wo=wo: nc.tensor.matmul(
                    bt[:], lhsT=wo[:, fc % 16, :], rhs=ynT_all[:, fc, ts], start=(fc == 0), stop=(fc == 31))),
                    reads=[r_wo, r_yn], writes=[rb])
            k.op("dve", (lambda dc=dc, ts=ts, bt=bt: nc.vector.tensor_tensor(out=hT[:, dc, ts], in0=hT[:, dc, ts], in1=bt[:], op=ALU.add)),
                 reads=[rb, r_h], writes=[r_h])
            k.free(b)


W_IN_COLS = 10304
KB = 1024


def dram_in(nc, name, shape, dt=F32):
    return nc.dram_tensor(name, list(shape), dt, kind="ExternalInput").ap()


def load_small(k, name, src, shape):
    nc = k.nc
    t, r = k.sb("p_" + name, shape, F32)
    k.op("sp", (lambda t=t, src=src: nc.sync.dma_start(out=t[:], in_=src)), writes=[r], dma=True)
    return t, r


L1_INPUTS = [("xT_prev", [2048, NT]), ("xT_own", [2048, NT]), ("flag", [128, 1]),
             ("mix_g0", [128, 16]), ("ffn_g0", [128, 16]), ("kv_g", [128, 16]),
             ("w_in", [2048, W_IN_COLS]), ("conv_w", [128, 48, 4]), ("conv_b", [128, 48]),
             ("dt_bias", [128, 64]), ("a_log", [128, 64]), ("d_skip", [128, 64]), ("norm_w", [128, 32]),
             ("w_out", [4096, 2048]), ("w_k", [2048, 2048]), ("w_v", [2048, 2048]),
             ("w_router0", [2048, 20]), ("b_router0", [128, 20]),
             ("w_gate0", [16, 2048, 512]), ("w_up0", [16, 2048, 512]), ("w_down0", [16, 512, 2048])]


def wseq_kv(I):
    return ([[(I["w_k"][:, cb * 256:(cb + 1) * 256], NKC, 256)] for cb in range(8)] +
            [[(I["w_v"][:, cb * 256:(cb + 1) * 256], NKC, 256)] for cb in range(8)])


def build_launch1(stop="full"):
    nc = bass.Bass("TRN2", target_bir_lowering=False)
    dr = {n: dram_in(nc, n, s) for n, s in CONST_SHAPES.items()}
    I = {n: dram_in(nc, n, s) for n, s in L1_INPUTS}
    h_out = nc.dram_tensor("h_out", [2048, NT], F32, kind="ExternalOutput").ap()
    kT_out = nc.dram_tensor("kT_out", [2048, NT], F32, kind="ExternalOutput").ap()
    v_out = nc.dram_tensor("v_out", [NT, 2048], F32, kind="ExternalOutput").ap()
    k = K(nc)
    k.setup()
    outs = []

    load_consts(k, dr, ["ident_bf", "ident_f", "ones_f", "tri_incl", "ustrict", "mask_le", "inv128"])
    hnT, r_hn = k.sb("hnT", [128, 16, NT], BF16)
    prm = {}
    mixg = load_small(k, "mix_g0", I["mix_g0"], [128, 16])
    prm["ffn_g"] = [load_small(k, "ffn_g0", I["ffn_g0"], [128, 16])]
    kvg = load_small(k, "kv_g", I["kv_g"], [128, 16])
    prm["b_router"] = [load_small(k, "b_router0", I["b_router0"], [128, 20])]
    prm["w_router"] = [I["w_router0"]]
    prm["w_gate"] = [[I["w_gate0"][e] for e in range(16)]]
    prm["w_up"] = [[I["w_up0"][e] for e in range(16)]]
    prm["w_down"] = [[I["w_down0"][e] for e in range(16)]]
    prm["w_in"] = I["w_in"]
    prm["w_out"] = I["w_out"]
    prm["flag"] = load_small(k, "flag", I["flag"], [128, 1])
    prm["dtb"] = load_small(k, "dt_bias", I["dt_bias"], [128, 64])
    prm["D_bc"] = load_small(k, "d_skip", I["d_skip"], [128, 64])
    prm["nw"] = load_small(k, "norm_w", I["norm_w"], [128, 32])
    A_bc, r_A = load_small(k, "a_log", I["a_log"], [128, 64])
    k.op("act", lambda: nc.scalar.activation(out=A_bc[:], in_=A_bc[:], func=AF.Exp), reads=[r_A], writes=[r_A])
    k.op("dve", lambda: nc.vector.tensor_scalar(out=A_bc[:], in0=A_bc[:], scalar1=-1.0, scalar2=None, op0=ALU.mult), reads=[r_A], writes=[r_A])
    prm["A_bc"] = (A_bc, r_A)
    cwh, r_cwh = k.sb("p_cwh", [128, 48, 4], F32)
    k.op("sp", lambda: nc.sync.dma_start(out=cwh[:], in_=I["conv_w"]), writes=[r_cwh], dma=True)
    k.op("dve", lambda: nc.vector.tensor_scalar(out=cwh[:], in0=cwh[:], scalar1=0.5, scalar2=None, op0=ALU.mult), reads=[r_cwh], writes=[r_cwh])
    prm["cwh"] = (cwh, r_cwh)
    cbh, r_cbh = load_small(k, "conv_b", I["conv_b"], [128, 48])
    k.op("dve", lambda: nc.vector.tensor_scalar(out=cbh[:], in0=cbh[:], scalar1=0.5, scalar2=None, op0=ALU.mult), reads=[r_cbh], writes=[r_cbh])
    prm["cbh"] = (cbh, r_cbh)
    wdt, r_wdt = k.sb("p_wdt", [128, 16, 64], BF16)
    k.op("pool", lambda: nc.gpsimd.dma_start(out=wdt[:], in_=I["w_in"][:, 10240:10304].rearrange("(kc p) f -> p kc f", p=128)),
         writes=[r_wdt], dma=True)
    prm["wdt"] = (wdt, r_wdt)
    prm["hal"] = k.sb("p_hal", [128, 48, 3], F32)
    prm["S_in"] = k.sb("p_Sin", [128, 8, 512], BF16)
    BIG = (k.bump + 63) // 64 * 64
    print("perm end", BIG - SB_BASE, "big bytes", SB_TOP - BIG)
    assert SB_TOP - BIG >= 128 * KB

    LV = ["normA", "prepA", "sA1", "sA", "normB", "sB1", "sB", "mixer", "moe", "full"]
    lv = LV.index(stop)
    nA = 0 if lv < 2 else (1 if lv == 2 else 8)
    nB = 0 if lv < 5 else (1 if lv == 5 else 8)
    for g in range(nA):
        k.wdeclare(wseq_ssd_group(prm, g, False))
    for g in range(nB):
        k.wdeclare(wseq_ssd_group(prm, g, True))
    if lv >= 7:
        k.wdeclare(wseq_outproj(prm))
    if lv >= 8:
        k.wdeclare(wseq_moe(prm, 0))
    if lv >= 9:
        k.wdeclare(wseq_kv(I))

    def early_out():
        o = k.op("pool", lambda: nc.gpsimd.dma_start(out=h_out.rearrange("(kc p) t -> p kc t", p=128), in_=hnT[:]), reads=[r_hn], dma=True)
        cnt = k.p.emit()
        print("launch1(early): ops", len(k.p.ops), "wtiles", len(k.wlist), "signals", cnt)
        k.p.final_wait("pool", [o])
        nc._knames = k.names
        return nc

    k.begin_phase("nA", BIG)
    rmsnorm(k, src_staged(k, I["xT_prev"]), mixg[0], mixg[1], hnT, r_hn)
    if lv == 0:
        return early_out()
    k.begin_phase("sA", BIG)
    P = ssd_prep(k, hnT, r_hn, prm)
    for g in range(nA):
        ssd_group(k, g, "A", hnT, r_hn, P, prm)
    if lv <= 3:
        return early_out()
    k.begin_phase("nB", BIG)
    rmsnorm(k, src_staged(k, I["xT_own"]), mixg[0], mixg[1], hnT, r_hn)
    k.begin_phase("sB", BIG)
    ynT_all, r_yn = k.sb("ynT_all", [128, 32, NT], BF16)
    assert k.bump == BIG + 64 * KB
    if lv == 4:
        return early_out()
    P = ssd_prep(k, hnT, r_hn, prm)
    for g in range(nB):
        ssd_group(k, g, "B", hnT, r_hn, P, prm, ynT_all, r_yn)
    print("sB scratch used", k.bump - BIG - 64 * KB)
    if lv <= 6:
        return early_out()
    k.begin_phase("op", BIG + 64 * KB)
    hT, r_h = k.sb("hT", [128, 16, NT], F32)
    k.op("sp", lambda: nc.sync.dma_start(out=hT[:], in_=I["xT_own"].rearrange("(kc p) t -> p kc t", p=128)), writes=[r_h], dma=True)
    out_proj(k, hT, r_h, ynT_all, r_yn, prm)
    if lv >= 8:
        k.begin_phase("moe0", BIG, BIG + 64 * KB)
        load_consts(k, dr, ["sel16"])
        moe(k, hT, r_h, hnT, r_hn, prm, 0)
        print("moe scratch used", k.bump - BIG)
    if lv >= 9:
        k.begin_phase("kv", BIG, BIG + 64 * KB)
        rmsnorm(k, src_resident(hT, r_h), kvg[0], kvg[1], hnT, r_hn)
        seq = wseq_kv(I)
        for cb in range(8):
            (wk, r_wk), = k.wget(seq[cb])
            st, r_st = k.sb(f"kv_st{cb % 2}", [128, 2, NT], F32)
            for c2 in range(2):
                for th in range(2):
                    ts = slice(th * 512, (th + 1) * 512)
                    b = k.bank(); bt, rb = k.banks[b]
                    for kc in range(NKC):
                        k.op("pe", (lambda kc=kc, ts=ts, bt=bt, wk=wk, c2=c2: nc.tensor.matmul(
                            bt[:], lhsT=wk[:, kc, c2 * 128:(c2 + 1) * 128], rhs=hnT[:, kc, ts],
                            start=(kc == 0), stop=(kc == NKC - 1))), reads=[r_wk, r_hn], writes=[rb])
                    k.op("act", (lambda bt=bt, st=st, c2=c2, ts=ts: nc.scalar.copy(out=st[:, c2, ts], in_=bt[:])), reads=[rb], writes=[r_st])
                    k.free(b)
            o = k.op("sp", (lambda st=st, cb=cb: nc.sync.dma_start(
                out=kT_out[cb * 256:(cb + 1) * 256, :].rearrange("(c p) t -> p c t", p=128), in_=st[:])), reads=[r_st], dma=True)
            outs.append(o)
        for cb in range(8):
            (wv, r_wv), = k.wget(seq[8 + cb])
            st, r_st = k.sb(f"kv_st{cb % 2}", [128, 2, NT], F32)
            stv = st[:].rearrange("p c t -> p (c t)").rearrange("p (t f) -> p t f", f=256)
            for t2 in range(4):
                b = k.bank(); bt, rb = k.banks[b]
                for ti in range(2):
                    tb = t2 * 2 + ti
                    for kc in range(NKC):
                        k.op("pe", (lambda kc=kc, tb=tb, ti=ti, bt=bt, wv=wv: nc.tensor.matmul(
                            bt[:, ti * 256:(ti + 1) * 256], lhsT=hnT[:, kc, tb * 128:(tb + 1) * 128], rhs=wv[:, kc, :],
                            start=(kc == 0), stop=(kc == NKC - 1))), reads=[r_wv, r_hn], writes=[rb])
                k.op("act", (lambda bt=bt, stv=stv, t2=t2: nc.scalar.copy(
                    out=stv[:, 2 * t2:2 * t2 + 2, :], in_=bt[:].rearrange("p (t f) -> p t f", f=256))), reads=[rb], writes=[r_st])
                k.free(b)
            o = k.op("sp", (lambda stv=stv, cb=cb: nc.sync.dma_start(
                out=v_out[:, cb * 256:(cb + 1) * 256].rearrange("(t p) f -> p t f", p=128), in_=stv)), reads=[r_st], dma=True)
            outs.append(o)
    o = k.op("sp", lambda: nc.sync.dma_start(out=h_out.rearrange("(kc p) t -> p kc t", p=128), in_=hT[:]), reads=[r_h], dma=True)
    outs.append(o)
    assert k.widx == len(k.wlist), (k.widx, len(k.wlist))
    cnt = k.p.emit()
    print("launch1: ops", len(k.p.ops), "wtiles", len(k.wlist), "signals", cnt)
    k.p.final_wait("sp", outs)
    return nc


def fm(v, n):
    return np.ascontiguousarray(np.asarray(v, np.float32).reshape(n, 128).T)


def rep(v):
    return np.ascontiguousarray(np.tile(np.asarray(v, np.float32)[None, :], (128, 1)))


def host_inputs_l1(inp, core):
    b, half = core // 2, core % 2
    x = np.asarray(inp["x"], np.float32)
    m = dict(host_consts())
    own = x[b, half * NT:(half + 1) * NT]
    prev = x[b, 0:NT] if half == 1 else np.zeros((NT, D), np.float32)
    m["xT_own"] = np.ascontiguousarray(own.T)
    m["xT_prev"] = np.ascontiguousarray(prev.T)
    m["flag"] = np.full((128, 1), float(half), np.float32)
    m["mix_g0"] = fm(inp["mix_norm"][0], 16)
    m["ffn_g0"] = fm(inp["ffn_norm"][0], 16)
    m["kv_g"] = fm(inp["kv_norm"], 16)
    m["w_in"] = np.asarray(inp["ssm_w_in"][0], np.float32)
    cw = np.asarray(inp["ssm_conv_w"][0], np.float32)
    m["conv_w"] = np.ascontiguousarray(cw.T.reshape(48, 128, 4).transpose(1, 0, 2))
    m["conv_b"] = fm(inp["ssm_conv_b"][0], 48)
    m["dt_bias"] = rep(inp["ssm_dt_bias"][0])
    m["a_log"] = rep(inp["ssm_a_log"][0])
    m["d_skip"] = rep(inp["ssm_d"][0])
    m["norm_w"] = fm(inp["ssm_norm_w"][0], 32)
    m["w_out"] = np.asarray(inp["ssm_w_out"][0], np.float32)
    m["w_k"] = np.asarray(inp["w_k"], np.float32)
    m["w_v"] = np.asarray(inp["w_v"], np.float32)
    m["w_router0"] = np.ascontiguousarray(np.concatenate(
        [np.asarray(inp["moe_w_coarse"][0], np.float32), np.asarray(inp["moe_w_fine"][0], np.float32).reshape(D, 16)], axis=1))
    m["b_router0"] = rep(np.concatenate([np.asarray(inp["moe_b_coarse"][0], np.float32),
                                         np.asarray(inp["moe_b_fine"][0], np.float32).reshape(16)]))
    m["w_gate0"] = np.asarray(inp["moe_w_gate"][0], np.float32)
    m["w_up0"] = np.asarray(inp["moe_w_up"][0], np.float32)
    m["w_down0"] = np.asarray(inp["moe_w_down"][0], np.float32)
    return m


L2_INPUTS = [("hT_in", [2048, NT]), ("kT_all", [2048, 2048]), ("v_all", [2048, 2048]),
             ("mix_g1", [128, 16]), ("ffn_g1", [128, 16]), ("final_g", [128, 16]),
             ("w_q", [2048, 2048]), ("w_o", [2048, 2048]),
             ("w_router1", [2048, 20]), ("b_router1", [128, 20]),
             ("w_gate1", [16, 2048, 512]), ("w_up1", [16, 2048, 512]), ("w_down1", [16, 512, 2048]),
             ("mask_rev", [128, 128])]


def wseq_sq(w):
    return [[(w[:, cb * 256:(cb + 1) * 256], NKC, 256)] for cb in range(8)]


def build_launch2(stop="full"):
    nc = bass.Bass("TRN2", target_bir_lowering=False)
    dr = {n: dram_in(nc, n, s) for n, s in CONST_SHAPES.items()}
    I = {n: dram_in(nc, n, s) for n, s in L2_INPUTS}
    out_d = nc.dram_tensor("outT", [2048, NT], F32, kind="ExternalOutput").ap()
    k = K(nc)
    k.setup()
    load_consts(k, dr, ["ident_bf", "ident_f", "ones_f", "inv128"])
    hn_off = k.bump
    hnT, r_hn = k.sb("hnT", [128, 16, NT], BF16)
    hn_end = k.bump
    prm = {}
    mixg = load_small(k, "mix_g1", I["mix_g1"], [128, 16])
    prm["ffn_g"] = [load_small(k, "ffn_g1", I["ffn_g1"], [128, 16])]
    fing = load_small(k, "final_g", I["final_g"], [128, 16])
    prm["b_router"] = [load_small(k, "b_router1", I["b_router1"], [128, 20])]
    prm["w_router"] = [I["w_router1"]]
    prm["w_gate"] = [[I["w_gate1"][e] for e in range(16)]]
    prm["w_up"] = [[I["w_up1"][e] for e in range(16)]]
    prm["w_down"] = [[I["w_down1"][e] for e in range(16)]]
    mrev, r_mrev = load_small(k, "mask_rev", I["mask_rev"], [128, 128])
    BIG = (k.bump + 63) // 64 * 64
    assert SB_TOP - BIG >= 128 * KB
    LV = ["q", "attn", "wo", "moe", "full"]
    lv = LV.index(stop)
    k.wdeclare(wseq_sq(I["w_q"]))
    if lv >= 2:
        k.wdeclare(wseq_sq(I["w_o"]))
    if lv >= 3:
        k.wdeclare(wseq_moe(prm, 0))

    k.begin_phase("q", BIG + 64 * KB)
    hT, r_h = k.sb("hT", [128, 16, NT], F32)
    k.op("sp", lambda: nc.sync.dma_start(out=hT[:], in_=I["hT_in"].rearrange("(kc p) t -> p kc t", p=128)), writes=[r_h], dma=True)
    k.bump = BIG
    k.limit = BIG + 64 * KB
    qT, r_q = k.sb("qT", [128, 16, NT], BF16)
    rmsnorm(k, src_resident(hT, r_h), mixg[0], mixg[1], hnT, r_hn)
    for cb, pieces in enumerate(wseq_sq(I["w_q"])):
        (wq, r_wq), = k.wget(pieces)
        for c2 in range(2):
            hd = cb * 2 + c2
            for th in range(2):
                ts = slice(th * 512, (th + 1) * 512)
                b = k.bank(); bt, rb = k.banks[b]
                for kc in range(NKC):
                    k.op("pe", (lambda kc=kc, ts=ts, bt=bt, wq=wq, c2=c2: nc.tensor.matmul(
                        bt[:], lhsT=wq[:, kc, c2 * 128:(c2 + 1) * 128], rhs=hnT[:, kc, ts], start=(kc == 0), stop=(kc == NKC - 1))),
                        reads=[r_wq, r_hn], writes=[rb])
                k.op("act", (lambda bt=bt, hd=hd, ts=ts: nc.scalar.activation(out=qT[:, hd, ts], in_=bt[:], func=AF.Copy, scale=128 ** -0.5)),
                     reads=[rb], writes=[r_q])
                k.free(b)

    def finish(src_t, r_src, bf):
        if bf:
            o = k.op("pool", lambda: nc.gpsimd.dma_start(out=out_d.rearrange("(kc p) t -> p kc t", p=128), in_=src_t[:]), reads=[r_src], dma=True)
            eng = "pool"
        else:
            o = k.op("sp", lambda: nc.sync.dma_start(out=out_d.rearrange("(kc p) t -> p kc t", p=128), in_=src_t[:]), reads=[r_src], dma=True)
            eng = "sp"
        assert k.widx == len(k.wlist), (k.widx, len(k.wlist))
        cnt = k.p.emit()
        print("launch2: ops", len(k.p.ops), "wtiles", len(k.wlist), "signals", cnt)
        k.p.final_wait(eng, [o])
        nc._knames = k.names
        return nc
    if lv == 0:
        return finish(qT, r_q, True)

    k.begin_phase("attn", BIG + 32 * KB, BIG + 64 * KB)
    es, r_es = k.sb("at_e", [128, 2048], F32)
    cs, r_cs = k.sb("at_cs", [128, 2048], F32)
    at, r_at = k.sb("at_attn", [128, 2048], BF16)
    atT, r_atT = k.sb("at_attnT", [128, 16, 128], BF16)
    kbuf = [k.sb(f"at_k{i}", [128, 2048], BF16) for i in range(2)]
    k.bump = hn_off
    k.limit = hn_end
    vbuf = [k.sb(f"at_v{i}", [128, 16, 128], BF16) for i in range(2)]
    ones_row, r_onr = k.sb("at_ones", [128, 2048], F32)
    k.op("sp", lambda: nc.sync.dma_start(out=ones_row[:], in_=dr["ones_row"]), writes=[r_onr], dma=True)
    onec, r_onec = k.c["one_col"]
    identb, r_identb = k.c["ident_bf"]
    for hd in range(16):
        kt, r_kt = kbuf[hd % 2]
        vt, r_vt = vbuf[hd % 2]
        k.op("pool", (lambda kt=kt, hd=hd: nc.gpsimd.dma_start(out=kt[:], in_=I["kT_all"][hd * 128:(hd + 1) * 128, :])), writes=[r_kt], dma=True)
        k.op("pool", (lambda vt=vt, hd=hd: nc.gpsimd.dma_start(
            out=vt[:], in_=I["v_all"][:, hd * 128:(hd + 1) * 128].rearrange("(blk p) d -> p blk d", p=128))), writes=[r_vt], dma=True)
        for i in range(8):
            qs = slice(i * 128, (i + 1) * 128)
            c0 = (7 - i) * 128
            ncol = 2048 - c0
            nblk = ncol // 128
            nbank = (ncol + 511) // 512
            zb = []
            for j in range(nbank):
                b = k.bank(); bt, rb = k.banks[b]
                w = min(512, ncol - j * 512)
                k.op("pe", (lambda bt=bt, hd=hd, qs=qs, kt=kt, j=j, w=w, c0=c0: nc.tensor.matmul(
                    bt[:, 0:w], lhsT=qT[:, hd, qs], rhs=kt[:, c0 + j * 512: c0 + j * 512 + w], start=True, stop=True)),
                    reads=[r_q, r_kt], writes=[rb])
                zb.append((b, bt, rb, w))
            for j, (b, bt, rb, w) in enumerate(zb):
                k.op("act", (lambda bt=bt, j=j, w=w: nc.scalar.activation(out=es[:, j * 512: j * 512 + w], in_=bt[:, 0:w], func=AF.Exp)),
                     reads=[rb], writes=[r_es])
            k.op("act", (lambda ncol=ncol: nc.scalar.activation(out=es[:, 0:ncol], in_=es[:, 0:ncol], func=AF.Ln, bias=onec[:], scale=1.0)),
                 reads=[r_es, r_onec], writes=[r_es])
            k.op("dve", lambda: nc.vector.tensor_tensor(out=es[:, 0:128], in0=es[:, 0:128], in1=mrev[:], op=ALU.mult),
                 reads=[r_es, r_mrev], writes=[r_es])
            k.op("dve", (lambda ncol=ncol: nc.vector.tensor_tensor_scan(out=cs[:, 0:ncol], data0=ones_row[:, 0:ncol], data1=es[:, 0:ncol],
                                                                       initial=0.0, op0=ALU.mult, op1=ALU.add)),
                 reads=[r_es, r_onr], writes=[r_cs])
            for j, (b, bt, rb, w) in enumerate(zb):
                k.op("dve", (lambda bt=bt, j=j, w=w: nc.vector.tensor_tensor(out=cs[:, j * 512: j * 512 + w], in0=cs[:, j * 512: j * 512 + w],
                                                                            in1=bt[:, 0:w], op=ALU.subtract)),
                     reads=[r_cs, rb], writes=[r_cs])
                k.free(b)
            k.op("act", (lambda ncol=ncol: nc.scalar.activation(out=at[:, 0:ncol], in_=cs[:, 0:ncol], func=AF.Exp, scale=-1.0)),
                 reads=[r_cs], writes=[r_at])
            k.op("dve", lambda: nc.vector.tensor_tensor(out=at[:, 0:128], in0=at[:, 0:128], in1=mrev[:], op=ALU.mult),
                 reads=[r_at, r_mrev], writes=[r_at])
            for j4 in range((nblk + 3) // 4):
                b = k.bank(); bt, rb = k.banks[b]
                nb = min(4, nblk - j4 * 4)
                for jj in range(nb):
                    blk = j4 * 4 + jj
                    k.op("pe", (lambda bt=bt, jj=jj, blk=blk: nc.tensor.matmul(bt[:, jj * 128:(jj + 1) * 128], lhsT=at[:, blk * 128:(blk + 1) * 128],
                                                                               rhs=identb[:], start=True, stop=True)),
                         reads=[r_at, r_identb], writes=[rb])
                k.op("act", (lambda bt=bt, j4=j4, nb=nb: nc.scalar.copy(out=atT[:, j4 * 4:j4 * 4 + nb, :],
                                                                       in_=bt[:, 0:nb * 128].rearrange("p (b q) -> p b q", b=nb))),
                     reads=[rb], writes=[r_atT])
                k.free(b)
            b = k.bank(); bt, rb = k.banks[b]
            for blk in range(nblk):
                k.op("pe", (lambda bt=bt, blk=blk, vt=vt, i=i, nblk=nblk: nc.tensor.matmul(
                    bt[:, 0:128], lhsT=vt[:, (7 - i) + blk, :], rhs=atT[:, blk, :], start=(blk == 0), stop=(blk == nblk - 1))),
                    reads=[r_vt, r_atT], writes=[rb])
            k.op("act", (lambda bt=bt, hd=hd, qs=qs: nc.scalar.copy(out=qT[:, hd, qs], in_=bt[:, 0:128])), reads=[rb], writes=[r_q])
            k.free(b)
    if lv == 1:
        return finish(qT, r_q, True)

    k.begin_phase("wo", BIG + 32 * KB, BIG + 64 * KB)
    for cb, pieces in enumerate(wseq_sq(I["w_o"])):
        (wo, r_wo), = k.wget(pieces)
        for c2 in range(2):
            dc = cb * 2 + c2
            for th in range(2):
                ts = slice(th * 512, (th + 1) * 512)
                b = k.bank(); bt, rb = k.banks[b]
                for kc in range(NKC):
                    k.op("pe", (lambda kc=kc, ts=ts, bt=bt, wo=wo, c2=c2: nc.tensor.matmul(
                        bt[:], lhsT=wo[:, kc, c2 * 128:(c2 + 1) * 128], rhs=qT[:, kc, ts], start=(kc == 0), stop=(kc == NKC - 1))),
                        reads=[r_wo, r_q], writes=[rb])
                k.op("dve", (lambda dc=dc, ts=ts, bt=bt: nc.vector.tensor_tensor(out=hT[:, dc, ts], in0=hT[:, dc, ts], in1=bt[:], op=ALU.add)),
                     reads=[rb, r_h], writes=[r_h])
                k.free(b)
    if lv == 2:
        return finish(hT, r_h, False)
    k.begin_phase("moe1", BIG, BIG + 64 * KB)
    load_consts(k, dr, ["sel16"])
    moe(k, hT, r_h, hnT, r_hn, prm, 0)
    if lv == 3:
        return finish(hT, r_h, False)
    k.begin_phase("fin", BIG, BIG + 64 * KB)
    outT, r_o = k.sb("outT", [128, 16, NT // 2], F32)
    oo = []
    nc_ = nc
    ones, r_ones = k.c["ones_f"]
    epsc, r_eps = k.c["eps_col"]
    for th in range(2):
        ts = slice(th * 512, (th + 1) * 512)
        b = k.bank(); bt, rb = k.banks[b]
        for kc in range(NKC):
            sq, rsq = k.sb(f"rn_sq{kc % 2}", [128, 512], F32)
            k.op("act", (lambda sq=sq, kc=kc, ts=ts: nc.scalar.activation(out=sq[:], in_=hT[:, kc, ts], func=AF.Square)), reads=[r_h], writes=[rsq])
            k.op("pe", (lambda sq=sq, kc=kc, bt=bt: nc.tensor.matmul(bt[:], lhsT=ones[:], rhs=sq[:], start=(kc == 0), stop=(kc == NKC - 1))),
                 reads=[rsq, r_ones], writes=[rb])
        rt, rr = k.sb("rn_rstd", [128, 512], F32)
        k.op("act", (lambda bt=bt, rt=rt: nc.scalar.activation(out=rt[:], in_=bt[:], func=AF.Ln, bias=epsc[:], scale=1.0 / D)), reads=[rb, r_eps], writes=[rr])
        k.free(b)
        k.op("act", (lambda rt=rt: nc.scalar.activation(out=rt[:], in_=rt[:], func=AF.Exp, scale=-0.5)), reads=[rr], writes=[rr])
        for kc in range(NKC):
            k.op("dve", (lambda kc=kc, ts=ts, rt=rt: nc.vector.scalar_tensor_tensor(
                out=outT[:, kc, :], in0=hT[:, kc, ts], scalar=fing[0][:, kc:kc + 1], in1=rt[:], op0=ALU.mult, op1=ALU.mult)),
                reads=[r_h, fing[1], rr], writes=[r_o])
        oo.append(k.op("sp", (lambda ts=ts: nc.sync.dma_start(out=out_d[:, ts].rearrange("(kc p) t -> p kc t", p=128), in_=outT[:])),
                       reads=[r_o], dma=True))
    assert k.widx == len(k.wlist), (k.widx, len(k.wlist))
    cnt = k.p.emit()
    print("launch2: ops", len(k.p.ops), "wtiles", len(k.wlist), "signals", cnt)
    k.p.final_wait("sp", oo)
    return nc


def host_inputs_l2(inp, core, h_out, kT_outs, v_outs):
    b, half = core // 2, core % 2
    m = dict(host_consts())
    m["hT_in"] = np.ascontiguousarray(h_out)
    kown = kT_outs[core][:, ::-1]
    vown = v_outs[core][::-1, :]
    if half == 1:
        kprev = kT_outs[core - 1][:, ::-1]
        vprev = v_outs[core - 1][::-1, :]
    else:
        kprev = np.zeros_like(kown)
        vprev = np.zeros_like(vown)
    m["kT_all"] = np.ascontiguousarray(np.concatenate([kown, kprev], axis=1))
    m["v_all"] = np.ascontiguousarray(np.concatenate([vown, vprev], axis=0))
    m["mix_g1"] = fm(inp["mix_norm"][1], 16)
    m["ffn_g1"] = fm(inp["ffn_norm"][1], 16)
    m["final_g"] = fm(inp["final_norm"], 16)
    m["w_q"] = np.asarray(inp["sb_w_q"][0], np.float32)
    m["w_o"] = np.asarray(inp["sb_w_out"][0], np.float32)
    m["w_router1"] = np.ascontiguousarray(np.concatenate(
        [np.asarray(inp["moe_w_coarse"][1], np.float32), np.asarray(inp["moe_w_fine"][1], np.float32).reshape(D, 16)], axis=1))
    m["b_router1"] = rep(np.concatenate([np.asarray(inp["moe_b_coarse"][1], np.float32),
                                         np.asarray(inp["moe_b_fine"][1], np.float32).reshape(16)]))
    m["w_gate1"] = np.asarray(inp["moe_w_gate"][1], np.float32)
    m["w_up1"] = np.asarray(inp["moe_w_up"][1], np.float32)
    m["w_down1"] = np.asarray(inp["moe_w_down"][1], np.float32)
    i = np.arange(128)
    m["mask_rev"] = (i[None, :] > 127 - i[:, None]).astype(np.float32)
    return m


F_INPUTS = L1_INPUTS + [("mix_g1", [128, 16]), ("ffn_g1", [128, 16]), ("final_g", [128, 16]),
                        ("w_q", [2048, 2048]), ("w_o", [2048, 2048]),
                        ("w_router1", [2048, 20]), ("b_router1", [128, 20]),
                        ("w_gate1", [16, 2048, 512]), ("w_up1", [16, 2048, 512]), ("w_down1", [16, 512, 2048])]


def build_fused():
    nc = bass.Bass("TRN2", target_bir_lowering=False)
    dr = {n: dram_in(nc, n, s) for n, s in CONST_SHAPES.items()}
    I = {n: dram_in(nc, n, s) for n, s in F_INPUTS}
    out_d = nc.dram_tensor("outT", [2048, NT], F32, kind="ExternalOutput").ap()
    kT_loc = nc.dram_tensor("kT_loc", [2048, NT], BF16, kind="Internal").ap()
    v_loc = nc.dram_tensor("v_loc", [NT, 2048], BF16, kind="Internal").ap()
    kT_prev = nc.dram_tensor("kT_prev", [2048, NT], BF16, kind="Internal").ap()
    v_prev = nc.dram_tensor("v_prev", [NT, 2048], BF16, kind="Internal").ap()
    xloc = nc.dram_tensor("xloc", [2048, 512], BF16, kind="Internal").ap()
    xg = nc.dram_tensor("xg", [4096, 512], BF16, kind="Internal").ap()
    k = K(nc)
    k.setup()

    load_consts(k, dr, ["ident_bf", "ident_f", "ones_f", "tri_incl", "ustrict", "mask_le"])
    hn_off = k.bump
    hnT, r_hn = k.sb("hnT", [128, 16, NT], BF16)
    hn_end = k.bump
    prm = {}
    mixg = [load_small(k, "mix_g0", I["mix_g0"], [128, 16]), load_small(k, "mix_g1", I["mix_g1"], [128, 16])]
    prm["ffn_g"] = [load_small(k, "ffn_g0", I["ffn_g0"], [128, 16]), load_small(k, "ffn_g1", I["ffn_g1"], [128, 16])]
    kvg = load_small(k, "kv_g", I["kv_g"], [128, 16])
    fing = load_small(k, "final_g", I["final_g"], [128, 16])
    prm["b_router"] = [load_small(k, "b_router0", I["b_router0"], [128, 20]), load_small(k, "b_router1", I["b_router1"], [128, 20])]
    prm["w_router"] = [I["w_router0"], I["w_router1"]]
    prm["w_gate"] = [[I["w_gate0"][e] for e in range(16)], [I["w_gate1"][e] for e in range(16)]]
    prm["w_up"] = [[I["w_up0"][e] for e in range(16)], [I["w_up1"][e] for e in range(16)]]
    prm["w_down"] = [[I["w_down0"][e] for e in range(16)], [I["w_down1"][e] for e in range(16)]]
    prm["w_in"] = I["w_in"]
    prm["w_out"] = I["w_out"]
    prm["flag"] = load_small(k, "flag", I["flag"], [128, 1])
    flag, r_flag = prm["flag"]
    prm["dtb"] = load_small(k, "dt_bias", I["dt_bias"], [128, 64])
    prm["D_bc"] = load_small(k, "d_skip", I["d_skip"], [128, 64])
    prm["nw"] = load_small(k, "norm_w", I["norm_w"], [128, 32])
    A_bc, r_A = load_small(k, "a_log", I["a_log"], [128, 64])
    k.op("act", lambda: nc.scalar.activation(out=A_bc[:], in_=A_bc[:], func=AF.Exp), reads=[r_A], writes=[r_A])
    k.op("dve", lambda: nc.vector.tensor_scalar(out=A_bc[:], in0=A_bc[:], scalar1=-1.0, scalar2=None, op0=ALU.mult), reads=[r_A], writes=[r_A])
    prm["A_bc"] = (A_bc, r_A)
    cwh, r_cwh = k.sb("p_cwh", [128, 48, 4], F32)
    k.op("sp", lambda: nc.sync.dma_start(out=cwh[:], in_=I["conv_w"]), writes=[r_cwh], dma=True)
    k.op("dve", lambda: nc.vector.tensor_scalar(out=cwh[:], in0=cwh[:], scalar1=0.5, scalar2=None, op0=ALU.mult), reads=[r_cwh], writes=[r_cwh])
    prm["cwh"] = (cwh, r_cwh)
    cbh, r_cbh = load_small(k, "conv_b", I["conv_b"], [128, 48])
    k.op("dve", lambda: nc.vector.tensor_scalar(out=cbh[:], in0=cbh[:], scalar1=0.5, scalar2=None, op0=ALU.mult), reads=[r_cbh], writes=[r_cbh])
    prm["cbh"] = (cbh, r_cbh)
    wdt, r_wdt = k.sb("p_wdt", [128, 16, 64], BF16)
    k.op("pool", lambda: nc.gpsimd.dma_start(out=wdt[:], in_=I["w_in"][:, 10240:10304].rearrange("(kc p) f -> p kc f", p=128)),
         writes=[r_wdt], dma=True)
    prm["wdt"] = (wdt, r_wdt)
    prm["hal"] = k.sb("p_hal", [128, 48, 3], F32)
    sin_off = k.bump
    prm["S_in"] = k.sb("p_Sin", [128, 8, 512], BF16)
    sin_end = k.bump
    BIG = (k.bump + 63) // 64 * 64
    print("fused: perm end", BIG - SB_BASE, "big bytes", SB_TOP - BIG)
    assert SB_TOP - BIG >= 128 * KB

    for g in range(8):
        k.wdeclare(wseq_ssd_group(prm, g, False))
    for g in range(8):
        k.wdeclare(wseq_ssd_group(prm, g, True))
    k.wdeclare(wseq_outproj(prm))
    k.wdeclare(wseq_moe(prm, 0))
    k.wdeclare(wseq_kv(I))
    k.wdeclare(wseq_sq(I["w_q"]))
    k.wdeclare(wseq_sq(I["w_o"]))
    k.wdeclare(wseq_moe(prm, 1))

    k.begin_phase("nA", BIG)
    rmsnorm(k, src_staged(k, I["xT_prev"]), mixg[0][0], mixg[0][1], hnT, r_hn)
    k.begin_phase("sA", BIG)
    P = ssd_prep(k, hnT, r_hn, prm)
    for g in range(8):
        ssd_group(k, g, "A", hnT, r_hn, P, prm)
    k.begin_phase("nB", BIG)
    rmsnorm(k, src_staged(k, I["xT_own"]), mixg[0][0], mixg[0][1], hnT, r_hn)
    k.begin_phase("sB", BIG)
    ynT_all, r_yn = k.sb("ynT_all", [128, 32, NT], BF16)
    P = ssd_prep(k, hnT, r_hn, prm)
    for g in range(8):
        ssd_group(k, g, "B", hnT, r_hn, P, prm, ynT_all, r_yn)
    k.begin_phase("op", BIG + 64 * KB)
    hT, r_h = k.sb("hT", [128, 16, NT], F32)
    k.op("sp", lambda: nc.sync.dma_start(out=hT[:], in_=I["xT_own"].rearrange("(kc p) t -> p kc t", p=128)), writes=[r_h], dma=True)
    out_proj(k, hT, r_h, ynT_all, r_yn, prm)
    k.begin_phase("moe0", BIG, BIG + 64 * KB)
    load_consts(k, dr, ["sel16", "inv128"])
    moe(k, hT, r_h, hnT, r_hn, prm, 0)

    k.begin_phase("kv", BIG, BIG + 64 * KB)
    r_kloc = k.p.reg("kT_loc"); r_vloc = k.p.reg("v_loc"); r_kp = k.p.reg("kT_prev"); r_vp = k.p.reg("v_prev")
    r_xloc = k.p.reg("xloc"); r_xg = k.p.reg("xg")
    rmsnorm(k, src_resident(hT, r_h), kvg[0], kvg[1], hnT, r_hn)
    seq = wseq_kv(I)
    for cb in range(8):
        (wk, r_wk), = k.wget(seq[cb])
        st, r_st = k.sb(f"kv_st{cb % 2}", [128, 2, NT], BF16)
        for c2 in range(2):
            for th in range(2):
                ts = slice(th * 512, (th + 1) * 512)
                b = k.bank(); bt, rb = k.banks[b]
                for kc in range(NKC):
                    k.op("pe", (lambda kc=kc, ts=ts, bt=bt, wk=wk, c2=c2: nc.tensor.matmul(
                        bt[:], lhsT=wk[:, kc, c2 * 128:(c2 + 1) * 128], rhs=hnT[:, kc, ts],
                        start=(kc == 0), stop=(kc == NKC - 1))), reads=[r_wk, r_hn], writes=[rb])
                k.op("act", (lambda bt=bt, st=st, c2=c2, ts=ts: nc.scalar.copy(out=st[:, c2, ts], in_=bt[:])), reads=[rb], writes=[r_st])
                k.free(b)
        k.op("sp", (lambda st=st, cb=cb: nc.sync.dma_start(
            out=kT_loc[cb * 256:(cb + 1) * 256, :].rearrange("(c p) t -> p c t", p=128), in_=st[:])), reads=[r_st], writes=[r_kloc], dma=True)
    for cb in range(8):
        (wv, r_wv), = k.wget(seq[8 + cb])
        st, r_st = k.sb(f"kv_st{cb % 2}", [128, 2, NT], BF16)
        stv = st[:].rearrange("p c t -> p (c t)").rearrange("p (t f) -> p t f", f=256)
        for t2 in range(4):
            b = k.bank(); bt, rb = k.banks[b]
            for ti in range(2):
                tb = t2 * 2 + ti
                for kc in range(NKC):
                    k.op("pe", (lambda kc=kc, tb=tb, ti=ti, bt=bt, wv=wv: nc.tensor.matmul(
                        bt[:, ti * 256:(ti + 1) * 256], lhsT=hnT[:, kc, tb * 128:(tb + 1) * 128], rhs=wv[:, kc, :],
                        start=(kc == 0), stop=(kc == NKC - 1))), reads=[r_wv, r_hn], writes=[rb])
            k.op("act", (lambda bt=bt, stv=stv, t2=t2: nc.scalar.copy(
                out=stv[:, 2 * t2:2 * t2 + 2, :], in_=bt[:].rearrange("p (t f) -> p t f", f=256))), reads=[rb], writes=[r_st])
            k.free(b)
        k.op("sp", (lambda stv=stv, cb=cb: nc.sync.dma_start(
            out=v_loc[:, cb * 256:(cb + 1) * 256].rearrange("(t p) f -> p t f", p=128), in_=stv)), reads=[r_st], writes=[r_vloc], dma=True)

    k.begin_phase("q", BIG, BIG + 64 * KB)
    rg = [[0, 1], [2, 3], [4, 5], [6, 7]]
    xl_v = xloc.rearrange("(t a) c -> t (a c)", a=4)
    xg_v = xg[0:2048, :].rearrange("(t a) c -> t (a c)", a=4)
    rounds = [(kT_loc[:, 0:512], xloc, xg[0:2048, :], kT_prev[:, 0:512], r_kloc, r_kp),
              (kT_loc[:, 512:1024], xloc, xg[0:2048, :], kT_prev[:, 512:1024], r_kloc, r_kp),
              (v_loc[0:512, :], xl_v, xg_v, v_prev[0:512, :], r_vloc, r_vp),
              (v_loc[512:1024, :], xl_v, xg_v, v_prev[512:1024, :], r_vloc, r_vp)]
    for (src, xin, xout, dst, r_src, r_dst) in rounds:
        k.op("sp", (lambda src=src, xin=xin: nc.sync.dma_start(out=xin, in_=src)), reads=[r_src], writes=[r_xloc], dma=True)
        k.op("pool", lambda: nc.gpsimd.collective_compute("AllGather", op=ALU.bypass, replica_groups=rg, ins=[xloc], outs=[xg]),
             reads=[r_xloc], writes=[r_xg])
        k.op("sp", (lambda xout=xout, dst=dst: nc.sync.dma_start(out=dst, in_=xout)), reads=[r_xg], writes=[r_dst], dma=True)
    qT, r_q = k.sb("qT", [128, 16, NT], BF16)
    rmsnorm(k, src_resident(hT, r_h), mixg[1][0], mixg[1][1], hnT, r_hn)
    for cb, pieces in enumerate(wseq_sq(I["w_q"])):
        (wq, r_wq), = k.wget(pieces)
        for c2 in range(2):
            hd = cb * 2 + c2
            for th in range(2):
                ts = slice(th * 512, (th + 1) * 512)
                b = k.bank(); bt, rb = k.banks[b]
                for kc in range(NKC):
                    k.op("pe", (lambda kc=kc, ts=ts, bt=bt, wq=wq, c2=c2: nc.tensor.matmul(
                        bt[:], lhsT=wq[:, kc, c2 * 128:(c2 + 1) * 128], rhs=hnT[:, kc, ts], start=(kc == 0), stop=(kc == NKC - 1))),
                        reads=[r_wq, r_hn], writes=[rb])
                k.op("act", (lambda bt=bt, hd=hd, ts=ts: nc.scalar.activation(out=qT[:, hd, ts], in_=bt[:], func=AF.Copy, scale=128 ** -0.5)),
                     reads=[rb], writes=[r_q])
                k.free(b)

    k.begin_phase("attn", BIG + 32 * KB, BIG + 64 * KB)
    sets = [dict(), dict()]
    sets[0]["es"] = k.sb("at_e0", [128, 2048], F32)
    sets[0]["cs"] = k.sb("at_cs0", [128, 2048], F32)
    sets[0]["at"] = k.sb("at_attn0", [128, 2048], BF16)
    sets[0]["atT"] = k.sb("at_attnT0", [128, 16, 128], BF16)
    kbuf = [k.sb(f"at_k{i}", [128, 2048], BF16) for i in range(2)]
    k.bump = hn_off
    k.limit = hn_end
    sets[1]["es"] = k.sb("at_e1", [128, 2048], F32)
    sets[1]["cs"] = k.sb("at_cs1", [128, 2048], F32)
    sets[1]["at"] = k.sb("at_attn1", [128, 2048], BF16)
    sets[1]["atT"] = k.sb("at_attnT1", [128, 16, 128], BF16)
    vbuf = [k.sb(f"at_v{i}", [128, 16, 128], BF16) for i in range(2)]
    k.bump = sin_off
    k.limit = sin_end
    load_consts(k, dr, ["mask_lt"])
    sets[0]["negT"] = k.sb("at_negT0", [128, 1], F32)
    sets[1]["negT"] = k.sb("at_negT1", [128, 1], F32)
    onec, r_onec = k.c["one_col"]
    identb, r_identb = k.c["ident_bf"]
    mlt, r_mlt = k.c["mask_lt"]

    rq = {(hd_, i_): k.p.reg(f"q{hd_}_{i_}") for hd_ in range(16) for i_ in range(8)}

    def load_kv(hd):
        kt, r_kt = kbuf[hd % 2]
        vt, r_vt = vbuf[hd % 2]
        k.op("sp", (lambda: nc.sync.dma_start(out=kt[:, 0:NT], in_=kT_prev[hd * 128:(hd + 1) * 128, :])), writes=[r_kt], dma=True)
        k.op("sp", (lambda: nc.sync.dma_start(out=kt[:, NT:2 * NT], in_=kT_loc[hd * 128:(hd + 1) * 128, :])), writes=[r_kt], dma=True)
        k.op("sp", (lambda: nc.sync.dma_start(
            out=vt[:, 0:8, :], in_=v_prev[:, hd * 128:(hd + 1) * 128].rearrange("(blk p) d -> p blk d", p=128))), writes=[r_vt], dma=True)
        k.op("sp", (lambda: nc.sync.dma_start(
            out=vt[:, 8:16, :], in_=v_loc[:, hd * 128:(hd + 1) * 128].rearrange("(blk p) d -> p blk d", p=128))), writes=[r_vt], dma=True)
        k.op("pool", (lambda: nc.gpsimd.tensor_scalar(out=vt[:, 0:8, :], in0=vt[:, 0:8, :], scalar1=flag[:], scalar2=None, op0=ALU.mult)),
             reads=[r_vt, r_flag], writes=[r_vt])

    zstate = {}

    def s1_pe(n, hd, i):
        kt, r_kt = kbuf[hd % 2]
        qs = slice(i * 128, (i + 1) * 128)
        ncol = (9 + i) * 128
        zb = []
        for j in range((ncol + 511) // 512):
            b = k.bank(); bt, rb = k.banks[b]
            w = min(512, ncol - j * 512)
            k.op("pe", (lambda bt=bt, j=j, w=w: nc.tensor.matmul(
                bt[:, 0:w], lhsT=qT[:, hd, qs], rhs=kt[:, j * 512: j * 512 + w], start=True, stop=True)),
                reads=[rq[(hd, i)], r_kt], writes=[rb])
            zb.append((b, bt, rb, w))
        zstate[n] = zb

    def s1_act(n, hd, i):
        S = sets[n % 2]
        es, r_es = S["es"]; cs, r_cs = S["cs"]
        ncol = (9 + i) * 128
        for j, (b, bt, rb, w) in enumerate(zstate[n]):
            k.op("act", (lambda bt=bt, j=j, w=w: nc.scalar.activation(out=cs[:, j * 512: j * 512 + w], in_=bt[:, 0:w], func=AF.Exp)),
                 reads=[rb], writes=[r_cs])
        k.op("act", (lambda: nc.scalar.activation(out=es[:, 0:ncol], in_=cs[:, 0:ncol], func=AF.Ln, bias=onec[:], scale=1.0)),
             reads=[r_cs, r_onec], writes=[r_es])

    def s1_dve(n, hd, i):
        S = sets[n % 2]
        es, r_es = S["es"]; cs, r_cs = S["cs"]; ngT, r_ngT = S["negT"]
        ncol = (9 + i) * 128
        k.op("dve", (lambda: nc.vector.tensor_tensor(out=es[:, ncol - 128:ncol], in0=es[:, ncol - 128:ncol], in1=mlt[:], op=ALU.mult)),
             reads=[r_es, r_mlt], writes=[r_es])
        k.op("dve", (lambda: nc.vector.tensor_tensor_scan(out=cs[:, 0:ncol], data0=onec[:].to_broadcast([128, ncol]), data1=es[:, 0:ncol],
                                                          initial=0.0, op0=ALU.mult, op1=ALU.add)),
             reads=[r_es, r_onec], writes=[r_cs])
        k.op("dve", (lambda: nc.vector.tensor_scalar(out=ngT[:], in0=cs[:, ncol - 1:ncol], scalar1=-1.0, scalar2=None, op0=ALU.mult)),
             reads=[r_cs], writes=[r_ngT])
        for j, (b, bt, rb, w) in enumerate(zstate.pop(n)):
            lo = j * 512
            if j == 0:
                k.op("dve", (lambda bt=bt: nc.vector.tensor_copy(out=es[:, 0:1], in_=bt[:, 0:1])), reads=[rb], writes=[r_es])
                k.op("dve", (lambda bt=bt, w=w: nc.vector.tensor_tensor(out=es[:, 1:w], in0=cs[:, 0:w - 1], in1=bt[:, 1:w], op=ALU.add)),
                     reads=[r_cs, rb], writes=[r_es])
            else:
                k.op("dve", (lambda bt=bt, w=w, lo=lo: nc.vector.tensor_tensor(out=es[:, lo:lo + w], in0=cs[:, lo - 1:lo + w - 1], in1=bt[:, 0:w], op=ALU.add)),
                     reads=[r_cs, rb], writes=[r_es])
            k.free(b)

    def s2_exp(n, hd, i):
        S = sets[n % 2]
        es, r_es = S["es"]; at, r_at = S["at"]; ngT, r_ngT = S["negT"]
        ncol = (9 + i) * 128
        k.op("act", (lambda: nc.scalar.activation(out=at[:, 0:ncol], in_=es[:, 0:ncol], func=AF.Exp, bias=ngT[:], scale=1.0)),
             reads=[r_es, r_ngT], writes=[r_at])
        k.op("dve", (lambda: nc.vector.tensor_tensor(out=at[:, ncol - 128:ncol], in0=at[:, ncol - 128:ncol], in1=mlt[:], op=ALU.mult)),
             reads=[r_at, r_mlt], writes=[r_at])

    def s2_tr(n, hd, i):
        S = sets[n % 2]
        at, r_at = S["at"]; atT, r_atT = S["atT"]
        nblk = 9 + i
        for j4 in range((nblk + 3) // 4):
            b = k.bank(); bt, rb = k.banks[b]
            nb = min(4, nblk - j4 * 4)
            for jj in range(nb):
                blk = j4 * 4 + jj
                k.op("pe", (lambda bt=bt, jj=jj, blk=blk: nc.tensor.matmul(bt[:, jj * 128:(jj + 1) * 128], lhsT=at[:, blk * 128:(blk + 1) * 128],
                                                                           rhs=identb[:], start=True, stop=True)),
                     reads=[r_at, r_identb], writes=[rb])
            if j4 % 2 == 0:
                k.op("act", (lambda bt=bt, j4=j4, nb=nb: nc.scalar.copy(out=atT[:, j4 * 4:j4 * 4 + nb, :],
                                                                       in_=bt[:, 0:nb * 128].rearrange("p (b q) -> p b q", b=nb))),
                     reads=[rb], writes=[r_atT])
            else:
                k.op("dve", (lambda bt=bt, j4=j4, nb=nb: nc.vector.tensor_copy(out=atT[:, j4 * 4:j4 * 4 + nb, :],
                                                                              in_=bt[:, 0:nb * 128].rearrange("p (b q) -> p b q", b=nb))),
                     reads=[rb], writes=[r_atT])
            k.free(b)

    def s3(n, hd, i):
        S = sets[n % 2]
        atT, r_atT = S["atT"]
        vt, r_vt = vbuf[hd % 2]
        qs = slice(i * 128, (i + 1) * 128)
        nblk = 9 + i
        b = k.bank(); bt, rb = k.banks[b]
        for blk in range(nblk):
            k.op("pe", (lambda bt=bt, blk=blk: nc.tensor.matmul(
                bt[:, 0:128], lhsT=vt[:, blk, :], rhs=atT[:, blk, :], start=(blk == 0), stop=(blk == nblk - 1))),
                reads=[r_vt, r_atT], writes=[rb])
        k.op("act", (lambda bt=bt: nc.scalar.copy(out=qT[:, hd, qs], in_=bt[:, 0:128])), reads=[rb], writes=[rq[(hd, i)]])
        k.free(b)

    its = [(hd, i) for hd in range(16) for i in range(8)]
    load_kv(0)
    for n0 in (0, 1):
        s1_pe(n0, *its[n0]); s1_act(n0, *its[n0]); s1_dve(n0, *its[n0])
    for n, (hd, i) in enumerate(its):
        nxt = its[n + 2] if n + 2 < len(its) else None
        if i == 2 and hd + 1 < 16:
            load_kv(hd + 1)
        if nxt:
            s1_pe(n + 2, *nxt)
        s2_exp(n, hd, i)
        if nxt:
            s1_act(n + 2, *nxt)
        s2_tr(n, hd, i)
        if nxt:
            s1_dve(n + 2, *nxt)
        s3(n, hd, i)

    k.begin_phase("wo", BIG + 32 * KB, BIG + 64 * KB)
    for cb, pieces in enumerate(wseq_sq(I["w_o"])):
        (wo, r_wo), = k.wget(pieces)
        for c2 in range(2):
            dc = cb * 2 + c2
            for th in range(2):
                ts = slice(th * 512, (th + 1) * 512)
                b = k.bank(); bt, rb = k.banks[b]
                for kc in range(NKC):
                    k.op("pe", (lambda kc=kc, ts=ts, bt=bt, wo=wo, c2=c2: nc.tensor.matmul(
                        bt[:], lhsT=wo[:, kc, c2 * 128:(c2 + 1) * 128], rhs=qT[:, kc, ts], start=(kc == 0), stop=(kc == NKC - 1))),
                        reads=[r_wo, r_q], writes=[rb])
                k.op("dve", (lambda dc=dc, ts=ts, bt=bt: nc.vector.tensor_tensor(out=hT[:, dc, ts], in0=hT[:, dc, ts], in1=bt[:], op=ALU.add)),
                     reads=[rb, r_h], writes=[r_h])
                k.free(b)
    k.begin_phase("moe1", BIG, BIG + 64 * KB)
    load_consts(k, dr, ["sel16", "inv128"])
    moe(k, hT, r_h, hnT, r_hn, prm, 1)
    k.begin_phase("fin", BIG, BIG + 64 * KB)
    outT, r_o = k.sb("outT", [128, 16, NT // 2], F32)
    oo = []
    ones, r_ones = k.c["ones_f"]
    epsc, r_eps = k.c["eps_col"]
    for th in range(2):
        ts = slice(th * 512, (th + 1) * 512)
        b = k.bank(); bt, rb = k.banks[b]
        for kc in range(NKC):
            sq, rsq = k.sb(f"rn_sq{kc % 2}", [128, 512], F32)
            k.op("act", (lambda sq=sq, kc=kc, ts=ts: nc.scalar.activation(out=sq[:], in_=hT[:, kc, ts], func=AF.Square)), reads=[r_h], writes=[rsq])
            k.op("pe", (lambda sq=sq, kc=kc, bt=bt: nc.tensor.matmul(bt[:], lhsT=ones[:], rhs=sq[:], start=(kc == 0), stop=(kc == NKC - 1))),
                 reads=[rsq, r_ones], writes=[rb])
        rt, rr = k.sb("rn_rstd", [128, 512], F32)
        k.op("act", (lambda bt=bt, rt=rt: nc.scalar.activation(out=rt[:], in_=bt[:], func=AF.Ln, bias=epsc[:], scale=1.0 / D)), reads=[rb, r_eps], writes=[rr])
        k.free(b)
        k.op("act", (lambda rt=rt: nc.scalar.activation(out=rt[:], in_=rt[:], func=AF.Exp, scale=-0.5)), reads=[rr], writes=[rr])
        for kc in range(NKC):
            k.op("dve", (lambda kc=kc, ts=ts, rt=rt: nc.vector.scalar_tensor_tensor(
                out=outT[:, kc, :], in0=hT[:, kc, ts], scalar=fing[0][:, kc:kc + 1], in1=rt[:], op0=ALU.mult, op1=ALU.mult)),
                reads=[r_h, fing[1], rr], writes=[r_o])
        oo.append(k.op("sp", (lambda ts=ts: nc.sync.dma_start(out=out_d[:, ts].rearrange("(kc p) t -> p kc t", p=128), in_=outT[:])),
                       reads=[r_o], dma=True))
    assert k.widx == len(k.wlist), (k.widx, len(k.wlist))
    cnt = k.p.emit()
    print("fused: ops", len(k.p.ops), "wtiles", len(k.wlist), "signals", cnt)
    k.p.final_wait("sp", oo)
    return nc


def host_inputs_fused(inp, core):
    m = host_inputs_l1(inp, core)
    m["mix_g1"] = fm(inp["mix_norm"][1], 16)
    m["ffn_g1"] = fm(inp["ffn_norm"][1], 16)
    m["final_g"] = fm(inp["final_norm"], 16)
    m["w_q"] = np.asarray(inp["sb_w_q"][0], np.float32)
    m["w_o"] = np.asarray(inp["sb_w_out"][0], np.float32)
    m["w_router1"] = np.ascontiguousarray(np.concatenate(
        [np.asarray(inp["moe_w_coarse"][1], np.float32), np.asarray(inp["moe_w_fine"][1], np.float32).reshape(D, 16)], axis=1))
    m["b_router1"] = rep(np.concatenate([np.asarray(inp["moe_b_coarse"][1], np.float32),
                                         np.asarray(inp["moe_b_fine"][1], np.float32).reshape(16)]))
    m["w_gate1"] = np.asarray(inp["moe_w_gate"][1], np.float32)
    m["w_up1"] = np.asarray(inp["moe_w_up"][1], np.float32)
    m["w_down1"] = np.asarray(inp["moe_w_down"][1], np.float32)
    return m


_PROGS = {}


def kernel(**inputs):
    inp = {k_: np.asarray(v) for k_, v in inputs.items()}
    ncores = 8
    names = set(CONST_SHAPES) | {n for n, _ in F_INPUTS}
    if "fused" not in _PROGS:
        _PROGS["fused"] = build_fused()
    nc = _PROGS["fused"]
    ims = []
    for c in range(ncores):
        m = host_inputs_fused(inp, c)
        ims.append({n: v for n, v in m.items() if n in names})
    res = run_bass_kernel_spmd(nc, ims, core_ids=list(range(ncores)))
    out = np.zeros((4, 2048, 2048), np.float32)
    for c in range(ncores):
        b, half = c // 2, c % 2
        out[b, half * NT:(half + 1) * NT, :] = np.asarray(res.results[c]["outT"], np.float32).T
    return out
```
